# Optimizing a Trainium2 kernel written in Bass

```python
import math
import jax, jax.numpy as jnp
from jax import lax
import numpy as np


D_MODEL = 1024
BATCH = 4
SEQ = 8192
DEPTH = 2

GRID_W = 64
CTX_LEN = 256
CHUNK = 128
Q_BLOCK = 128
NORM_EPS = 1e-6
ROPE_THETA = 10000.0

SSD_HEADS = 16
SSD_HEAD_DIM = 64
SSD_INNER = SSD_HEADS * SSD_HEAD_DIM
SSD_GROUPS = 2
SSD_STATE = 128
SSD_CONV = 3
SSD_CONV_CH = SSD_INNER + 2 * SSD_GROUPS * SSD_STATE
RET_HEADS = 4
RET_QK_DIM = 128
RET_V_DIM = 256
RET_QK = RET_HEADS * RET_QK_DIM
RET_INNER = RET_HEADS * RET_V_DIM
EVEN_IN = SSD_INNER + SSD_CONV_CH + 2 * SSD_HEADS + 2 * RET_QK + 2 * RET_INNER
EVEN_MIX = SSD_INNER + RET_INNER
DIFF_HEADS = 8
DIFF_HEAD_DIM = 64
DIFF_V_DIM = 2 * DIFF_HEAD_DIM
DIFF_INNER = DIFF_HEADS * DIFF_V_DIM
ODD_IN = 3 * DIFF_INNER
FFN_DENSE = 2816
N_EXPERTS = 8
TOP_K = 2
FFN_EXPERT = 3584

kernel_name = "hybrid_ssd_retention_diffattn_moe_dit"


def rms_norm(x, gain=None):
    xf = x.astype(jnp.float32)
    y = xf * lax.rsqrt(jnp.mean(xf * xf, axis=-1, keepdims=True) + NORM_EPS)
    if gain is not None:
        y = y * gain.astype(jnp.float32)
    return y.astype(x.dtype)


def adaln(cond, w, b):
    m = jax.nn.silu(cond) @ w + b
    return jnp.split(m[..., None, :], 6, axis=-1)


def modulate(x, gain, shift, scale):
    return rms_norm(x, gain) * (1 + scale) + shift


def rope_angles(pos, dim, theta):
    inv = theta ** (-jnp.arange(dim // 2, dtype=jnp.float32) / (dim // 2))
    return pos.astype(jnp.float32)[:, None] * inv[None, :]


def apply_rope(x, ang):
    half = x.shape[-1] // 2
    shape = (1, ang.shape[0]) + (1,) * (x.ndim - 3) + (half,)
    cos = jnp.cos(ang).reshape(shape).astype(x.dtype)
    sin = jnp.sin(ang).reshape(shape).astype(x.dtype)
    x1, x2 = x[..., :half], x[..., half:]
    return jnp.concatenate([x1 * cos - x2 * sin, x1 * sin + x2 * cos], axis=-1)


def rope_2d(x, ang_row, ang_col):
    half = x.shape[-1] // 2
    return jnp.concatenate([apply_rope(x[..., :half], ang_row), apply_rope(x[..., half:], ang_col)], axis=-1)


def dwconv_centred(x, w, b):
    K, C = w.shape
    y = lax.conv_general_dilated(x, w[:, None, :].astype(x.dtype), window_strides=(1,),
                                 padding=[(K // 2, K // 2)], dimension_numbers=('NWC', 'WIO', 'NWC'),
                                 feature_group_count=C)
    return y + b


def chunked_scan(q, k, v, log_a, h0):
    Bsz, L, G, N = q.shape
    R, P = v.shape[-2:]
    nc = L // CHUNK
    qc = q.reshape(Bsz, nc, CHUNK, G, N)
    kc = k.reshape(Bsz, nc, CHUNK, G, N)
    vc = v.reshape(Bsz, nc, CHUNK, G, R, P)
    a_cs = jnp.cumsum(log_a.astype(jnp.float32).reshape(Bsz, nc, CHUNK, G, R), axis=2)
    idx = jnp.arange(CHUNK)
    causal = (idx[:, None] >= idx[None, :])[:, :, None, None]
    diff = a_cs[:, :, :, None] - a_cs[:, :, None, :]
    decay = jnp.exp(jnp.where(causal, diff, -jnp.inf))
    scores = jnp.einsum('bclgn,bcsgn->bclsg', qc, kc)
    y_diag = jnp.einsum('bclsgr,bcsgrp->bclgrp', scores[..., None] * decay, vc)
    to_end = jnp.exp(a_cs[:, :, -1:] - a_cs)
    states = jnp.einsum('bcsgn,bcsgrp->bcgrpn', kc, vc * to_end[..., None])
    a_tot = jnp.exp(a_cs[:, :, -1])

    def step(h, inp):
        s_c, g_c = inp
        return h * g_c[..., None, None] + s_c, h

    h_fin, h_in = lax.scan(step, h0.astype(jnp.float32), (jnp.moveaxis(states, 1, 0), jnp.moveaxis(a_tot, 1, 0)))
    h_in = jnp.moveaxis(h_in, 0, 1)
    y_off = jnp.einsum('bclgn,bcgrpn->bclgrp', qc, h_in) * jnp.exp(a_cs)[..., None]
    y = (y_diag + y_off).reshape(Bsz, L, G, R, P)
    return y.astype(v.dtype), h_fin


def bidir_scan(q, k, v_f, v_b, la_f, la_b, h0_f, h0_b):
    y_f, h_f = chunked_scan(q, k, v_f, la_f, h0_f)
    flip = lambda t: jnp.flip(t, axis=1)
    y_b, h_b = chunked_scan(flip(q), flip(k), flip(v_b), flip(la_b), h0_b)
    return y_f + flip(y_b), h_f, h_b


def even_prepare(h, ang, w_in, conv_w, conv_b, dt_bias, a_log, ret_decay):
    Bsz, L, _ = h.shape
    G, R = SSD_GROUPS, SSD_HEADS // SSD_GROUPS
    cuts = np.cumsum([SSD_INNER, SSD_CONV_CH, 2 * SSD_HEADS, RET_QK, RET_QK, RET_INNER]).tolist()
    z, xbc, dt, rq, rk, rv, rg = jnp.split(h @ w_in, cuts, axis=-1)
    xbc = jax.nn.silu(dwconv_centred(xbc, conv_w, conv_b))
    xs, bm, cm = jnp.split(xbc, [SSD_INNER, SSD_INNER + SSD_GROUPS * SSD_STATE], axis=-1)
    xs = xs.reshape(Bsz, L, G, R, SSD_HEAD_DIM)
    bm = bm.reshape(Bsz, L, G, SSD_STATE)
    cm = cm.reshape(Bsz, L, G, SSD_STATE)
    dt = jax.nn.softplus((dt.reshape(Bsz, L, 2, SSD_HEADS) + dt_bias).astype(jnp.float32))
    log_a = (dt * -jnp.exp(a_log.astype(jnp.float32))).reshape(Bsz, L, 2, G, R)
    xdt = xs[:, :, None] * dt.reshape(Bsz, L, 2, G, R, 1).astype(xs.dtype)
    ssd = (cm, bm, xdt[:, :, 0], xdt[:, :, 1], log_a[:, :, 0], log_a[:, :, 1])
    rq = apply_rope(rq.reshape(Bsz, L, RET_HEADS, RET_QK_DIM), ang)
    rk = apply_rope(rk.reshape(Bsz, L, RET_HEADS, RET_QK_DIM), ang) * (RET_QK_DIM ** -0.5)
    rv = rv.reshape(Bsz, L, RET_HEADS, 1, RET_V_DIM)
    rla = jnp.broadcast_to(-jnp.exp(ret_decay.astype(jnp.float32))[:, None, None, :, None],
                           (2, Bsz, L, RET_HEADS, 1))
    ret = (rq, rk, rv, rv, rla[0], rla[1])
    return ssd, ret, xs, z, rg


def even_finish(ssd_y, ret_y, xs, z, rg, d_skip, ssd_norm, w_out):
    Bsz, L = z.shape[:2]
    y = ssd_y + xs * d_skip.reshape(SSD_GROUPS, -1, 1)
    y = rms_norm(y.reshape(Bsz, L, SSD_INNER) * jax.nn.silu(z), ssd_norm)
    r = rms_norm(ret_y.reshape(Bsz, L, RET_HEADS, RET_V_DIM)).reshape(Bsz, L, RET_INNER) * jax.nn.silu(rg)
    return jnp.concatenate([y, r], axis=-1) @ w_out


def odd_qkv(h, w_in, q_norm, k_norm, ang_row=None, ang_col=None):
    Bsz, L, _ = h.shape
    q, k, v = jnp.split(h @ w_in, 3, axis=-1)
    q = rms_norm(q.reshape(Bsz, L, DIFF_HEADS, 2, DIFF_HEAD_DIM), q_norm)
    k = rms_norm(k.reshape(Bsz, L, DIFF_HEADS, 2, DIFF_HEAD_DIM), k_norm)
    if ang_row is not None:
        q = rope_2d(q, ang_row, ang_col)
        k = rope_2d(k, ang_row, ang_col)
    return q * (DIFF_HEAD_DIM ** -0.5), k, v.reshape(Bsz, L, DIFF_HEADS, DIFF_V_DIM)


def diff_attend(q, k, v, lam):
    s = jnp.einsum('bqhmd,bkhmd->bhmqk', q, k).astype(jnp.float32)
    p = jax.nn.softmax(s, axis=-1)
    a = p[:, :, 0] - lam.astype(jnp.float32) * p[:, :, 1]
    return jnp.einsum('bhqk,bkhe->bqhe', a.astype(v.dtype), v)


def diff_out(o, lam_init, subln, w_out):
    Bsz, L = o.shape[:2]
    o = rms_norm(o, subln) * (1.0 - lam_init)
    return o.reshape(Bsz, L, DIFF_INNER) @ w_out


def swiglu(h, w_gate, w_up, w_down):
    return (jax.nn.silu(h @ w_gate) * (h @ w_up)) @ w_down


def moe(h, w_router, w_gate, w_up, w_down):
    logits = (h @ w_router).astype(jnp.float32)
    top_val, top_idx = lax.top_k(logits, TOP_K)
    top_w = jax.nn.softmax(top_val, axis=-1)
    combine = jnp.sum(jax.nn.one_hot(top_idx, N_EXPERTS, dtype=jnp.float32) * top_w[..., None], axis=-2)
    out = jnp.zeros_like(h)
    for e in range(N_EXPERTS):
        out = out + combine[..., e:e + 1].astype(h.dtype) * swiglu(h, w_gate[e], w_up[e], w_down[e])
    return out


def setup_inputs(seed: int = 0) -> dict:
    key = jax.random.key(seed)
    ks = iter(jax.random.split(key, 64))
    f32 = jnp.float32
    D = D_MODEL
    ne, no = (DEPTH + 1) // 2, DEPTH // 2

    def nrm(shape, scale):
        return jax.random.normal(next(ks), shape, f32) * scale

    def gain(shape):
        return 1.0 + nrm(shape, 0.01)

    x = nrm((BATCH, SEQ, D), 1.0)
    c = nrm((BATCH, D), 1.0)
    ctx = nrm((BATCH, CTX_LEN, D), 1.0)
    c_ctx = nrm((D,), 1.0)
    u = jax.random.uniform(next(ks), (ne, 2, SSD_HEADS), f32)
    dt0 = jnp.exp(u * (math.log(0.1) - math.log(0.001)) + math.log(0.001))
    gam = 1.0 - 2.0 ** (-5.0 - jnp.arange(RET_HEADS, dtype=f32))
    return {
        "x": x, "c": c, "ctx": ctx, "c_ctx": c_ctx,
        "even_w_mod": nrm((ne, D, 6 * D), 0.5 * D ** -0.5),
        "even_b_mod": nrm((ne, 6 * D), 0.01),
        "even_norm1": gain((ne, D)),
        "even_norm2": gain((ne, D)),
        "even_w_in": nrm((ne, D, EVEN_IN), D ** -0.5),
        "even_conv_w": nrm((ne, SSD_CONV, SSD_CONV_CH), SSD_CONV ** -0.5),
        "even_conv_b": nrm((ne, SSD_CONV_CH), 0.01),
        "even_dt_bias": dt0 + jnp.log(-jnp.expm1(-dt0)),
        "even_a_log": jnp.log(jax.random.uniform(next(ks), (ne, 2, SSD_HEADS), f32, 1.0, 16.0)),
        "even_d": gain((ne, SSD_HEADS)),
        "even_ssd_norm": gain((ne, SSD_INNER)),
        "even_ret_decay": jnp.log(-jnp.log(gam)) + nrm((ne, 2, RET_HEADS), 0.05),
        "even_w_out": nrm((ne, EVEN_MIX, D), EVEN_MIX ** -0.5),
        "even_ffn_gate": nrm((ne, D, FFN_DENSE), D ** -0.5),
        "even_ffn_up": nrm((ne, D, FFN_DENSE), D ** -0.5),
        "even_ffn_down": nrm((ne, FFN_DENSE, D), FFN_DENSE ** -0.5),
        "odd_w_mod": nrm((no, D, 6 * D), 0.5 * D ** -0.5),
        "odd_b_mod": nrm((no, 6 * D), 0.01),
        "odd_norm1": gain((no, D)),
        "odd_norm2": gain((no, D)),
        "odd_w_in": nrm((no, D, ODD_IN), D ** -0.5),
        "odd_q_norm": gain((no, DIFF_HEAD_DIM)),
        "odd_k_norm": gain((no, DIFF_HEAD_DIM)),
        "odd_lambda": nrm((no, 4, DIFF_HEAD_DIM), 0.1),
        "odd_subln": gain((no, DIFF_V_DIM)),
        "odd_w_out": nrm((no, DIFF_INNER, D), DIFF_INNER ** -0.5),
        "odd_router": nrm((no, D, N_EXPERTS), D ** -0.5),
        "odd_exp_gate": nrm((no, N_EXPERTS, D, FFN_EXPERT), D ** -0.5),
        "odd_exp_up": nrm((no, N_EXPERTS, D, FFN_EXPERT), D ** -0.5),
        "odd_exp_down": nrm((no, N_EXPERTS, FFN_EXPERT, D), FFN_EXPERT ** -0.5),
    }


def reference(x, c, ctx, c_ctx, even_w_mod, even_b_mod, even_norm1, even_norm2, even_w_in, even_conv_w,
              even_conv_b, even_dt_bias, even_a_log, even_d, even_ssd_norm, even_ret_decay, even_w_out,
              even_ffn_gate, even_ffn_up, even_ffn_down, odd_w_mod, odd_b_mod, odd_norm1, odd_norm2, odd_w_in,
              odd_q_norm, odd_k_norm, odd_lambda, odd_subln, odd_w_out, odd_router, odd_exp_gate, odd_exp_up,
              odd_exp_down):
    Bsz, L, _ = x.shape
    Lc = ctx.shape[1]
    n_rows = L // GRID_W
    row = jnp.broadcast_to(jnp.arange(n_rows)[:, None], (n_rows, GRID_W)).reshape(-1)
    col = jnp.broadcast_to(jnp.arange(GRID_W)[None, :], (n_rows, GRID_W)).reshape(-1)
    ang_row = rope_angles(row, DIFF_HEAD_DIM // 2, ROPE_THETA)
    ang_col = rope_angles(col, DIFF_HEAD_DIM // 2, ROPE_THETA)
    ang_ret_ctx = rope_angles(jnp.arange(Lc), RET_QK_DIM, ROPE_THETA)
    ang_ret_lat = rope_angles(Lc + jnp.arange(L), RET_QK_DIM, ROPE_THETA)
    zeros_ssd = jnp.zeros((Bsz, SSD_GROUPS, SSD_HEADS // SSD_GROUPS, SSD_HEAD_DIM, SSD_STATE), jnp.float32)
    zeros_ret = jnp.zeros((Bsz, RET_HEADS, 1, RET_V_DIM, RET_QK_DIM), jnp.float32)

    xl, xc = x, ctx
    for i in range(DEPTH):
        j = i // 2
        last = i == DEPTH - 1
        if i % 2 == 0:
            sh1, sc1, g1, sh2, sc2, g2 = adaln(c, even_w_mod[j], even_b_mod[j])
            csh1, csc1, cg1, csh2, csc2, cg2 = adaln(c_ctx, even_w_mod[j], even_b_mod[j])
            hl = modulate(xl, even_norm1[j], sh1, sc1)
            hc = modulate(xc, even_norm1[j], csh1, csc1)
            p_args = (even_w_in[j], even_conv_w[j], even_conv_b[j], even_dt_bias[j], even_a_log[j], even_ret_decay[j])
            ssd_c, ret_c, xs_c, z_c, rg_c = even_prepare(hc, ang_ret_ctx, *p_args)
            ssd_l, ret_l, xs_l, z_l, rg_l = even_prepare(hl, ang_ret_lat, *p_args)
            ys_c, hs_f, hs_b = bidir_scan(*ssd_c, zeros_ssd, zeros_ssd)
            yr_c, hr_f, hr_b = bidir_scan(*ret_c, zeros_ret, zeros_ret)
            ys_l, _, _ = bidir_scan(*ssd_l, hs_f, hs_b)
            yr_l, _, _ = bidir_scan(*ret_l, hr_f, hr_b)
            xl = xl + g1 * even_finish(ys_l, yr_l, xs_l, z_l, rg_l, even_d[j], even_ssd_norm[j], even_w_out[j])
            hl = modulate(xl, even_norm2[j], sh2, sc2)
            xl = xl + g2 * swiglu(hl, even_ffn_gate[j], even_ffn_up[j], even_ffn_down[j])
            if not last:
                xc = xc + cg1 * even_finish(ys_c, yr_c, xs_c, z_c, rg_c, even_d[j], even_ssd_norm[j], even_w_out[j])
                hc = modulate(xc, even_norm2[j], csh2, csc2)
                xc = xc + cg2 * swiglu(hc, even_ffn_gate[j], even_ffn_up[j], even_ffn_down[j])
        else:
            sh1, sc1, g1, sh2, sc2, g2 = adaln(c, odd_w_mod[j], odd_b_mod[j])
            csh1, csc1, cg1, csh2, csc2, cg2 = adaln(c_ctx, odd_w_mod[j], odd_b_mod[j])
            hl = modulate(xl, odd_norm1[j], sh1, sc1)
            hc = modulate(xc, odd_norm1[j], csh1, csc1)
            lam_init = 0.8 - 0.6 * math.exp(-0.3 * i)
            lq1, lk1, lq2, lk2 = odd_lambda[j]
            lam = jnp.exp(jnp.sum(lq1 * lk1)) - jnp.exp(jnp.sum(lq2 * lk2)) + lam_init
            q_c, k_c, v_c = odd_qkv(hc, odd_w_in[j], odd_q_norm[j], odd_k_norm[j])
            q_l, k_l, v_l = odd_qkv(hl, odd_w_in[j], odd_q_norm[j], odd_k_norm[j], ang_row, ang_col)
            k_all = jnp.concatenate([k_c, k_l], axis=1)
            v_all = jnp.concatenate([v_c, v_l], axis=1)
            nb = L // Q_BLOCK
            qb = jnp.moveaxis(q_l.reshape(Bsz, nb, Q_BLOCK, DIFF_HEADS, 2, DIFF_HEAD_DIM), 1, 0)
            ob = lax.map(lambda q_blk: diff_attend(q_blk, k_all, v_all, lam), qb)
            o_l = jnp.moveaxis(ob, 0, 1).reshape(Bsz, L, DIFF_HEADS, DIFF_V_DIM)
            xl = xl + g1 * diff_out(o_l, lam_init, odd_subln[j], odd_w_out[j])
            hl = modulate(xl, odd_norm2[j], sh2, sc2)
            xl = xl + g2 * moe(hl, odd_router[j], odd_exp_gate[j], odd_exp_up[j], odd_exp_down[j])
            if not last:
                o_c = diff_attend(q_c, k_c, v_c, lam)
                xc = xc + cg1 * diff_out(o_c, lam_init, odd_subln[j], odd_w_out[j])
                hc = modulate(xc, odd_norm2[j], csh2, csc2)
                xc = xc + cg2 * moe(hc, odd_router[j], odd_exp_gate[j], odd_exp_up[j], odd_exp_down[j])
    return xl
```

```python
import math
import numpy as np
import ml_dtypes
import concourse.bass as bass
import concourse.mybir as mybir
from concourse.bass_utils import run_bass_kernel_spmd

F32 = mybir.dt.float32
BF16 = mybir.dt.bfloat16
AF = mybir.ActivationFunctionType
ALU = mybir.AluOpType
AX = mybir.AxisListType

SAME_ENGINE_SYNC = True
NSLOT = 10


class Tok:
    __slots__ = ("lw", "rd", "name")

    def __init__(self, name=""):
        self.lw = None
        self.rd = []
        self.name = name


class TT:
    __slots__ = ("ap", "tok")

    def __init__(self, ap, tok):
        self.ap = ap
        self.tok = tok

    def __getitem__(self, k):
        return TT(self.ap[k], self.tok)

    def re(self, s, **kw):
        return TT(self.ap.rearrange(s, **kw), self.tok)

    @property
    def shape(self):
        return self.ap.shape


def bc(tt, dims):
    a = tt.ap
    base = list(a.ap)
    return TT(bass.AP(a.tensor, a.offset, [list(base[0])] + [list(d) for d in dims]), tt.tok)


class Op:
    __slots__ = ("stream", "fn", "deps", "dma", "ms", "slot", "val", "didx")

    def __init__(self, stream, fn, dma):
        self.stream = stream
        self.fn = fn
        self.deps = []
        self.dma = dma
        self.ms = False
        self.slot = None
        self.val = None
        self.didx = None


class Prog:
    STREAMS = ("pe", "act", "dve", "pool", "sp")

    def __init__(self, nc):
        self.nc = nc
        self.ops = {s: [] for s in self.STREAMS}
        self.ndma = {s: 0 for s in self.STREAMS}
        self.dmaops = {s: [] for s in self.STREAMS}
        self._ctx = []
        self._scopes = []
        self.banks = []
        self.bank_i = 0

    def sb(self, name, shape, dt=F32):
        g = self.nc.sbuf_tensor(name, list(shape), dt)
        h = g.__enter__()
        self._ctx.append(g)
        return TT(h[:], Tok(name))

    def ps(self, name, shape, dt=F32):
        g = self.nc.psum_tensor(name, list(shape), dt)
        h = g.__enter__()
        self._ctx.append(g)
        return TT(h[:], Tok(name))

    def dram(self, name, shape, dt=F32, kind="Internal"):
        h = self.nc.dram_tensor(name, list(shape), dt, kind=kind)
        return TT(h.ap(), Tok(name))

    def open_scope(self):
        self._scopes.append(len(self._ctx))

    def close_scope(self):
        n = self._scopes.pop()
        self.barrier()
        while len(self._ctx) > n:
            self._ctx.pop().__exit__(None, None, None)

    def close(self):
        while self._ctx:
            self._ctx.pop().__exit__(None, None, None)

    def bank(self):
        b = self.banks[self.bank_i % len(self.banks)]
        self.bank_i += 1
        return b

    def _rec(self, stream, fn, reads, writes, dma=False, extra=()):
        op = Op(stream, fn, dma)
        deps = list(extra)
        for t in reads:
            if t.tok.lw is not None:
                deps.append(t.tok.lw)
        for t in writes:
            if t.tok.lw is not None:
                deps.append(t.tok.lw)
            deps.extend(t.tok.rd)
        if dma:
            op.didx = self.ndma[stream]
            self.ndma[stream] += 1
            self.dmaops[stream].append(op)
            if op.didx >= NSLOT:
                deps.append(self.dmaops[stream][op.didx - NSLOT])
        seen = set()
        for d in deps:
            if d is op or id(d) in seen:
                continue
            if (not d.dma) and d.stream == stream and (stream == "pe" or not SAME_ENGINE_SYNC):
                continue
            seen.add(id(d))
            op.deps.append(d)
            d.ms = True
        for t in reads:
            t.tok.rd.append(op)
        for t in writes:
            t.tok.lw = op
            t.tok.rd = []
        self.ops[stream].append(op)
        return op

    def barrier(self):
        last = []
        for s in self.STREAMS:
            if self.ops[s]:
                for o in reversed(self.ops[s]):
                    if not o.dma and o.fn is not None:
                        last.append(o)
                        break
            last.extend(self.dmaops[s][-NSLOT:])
        for s in self.STREAMS:
            self._rec(s, None, [], [], extra=last)

    def mm(self, out, lhsT, rhs, start=True, stop=True):
        self._rec("pe", lambda e: e.matmul(out.ap, lhsT.ap, rhs.ap, start=start, stop=stop), [lhsT, rhs], [out])

    def mmgroup(self, out, pairs):
        rd = []
        for l, r in pairs:
            rd += [l, r]
        n = len(pairs)

        def fn(e):
            ins = None
            for i, (l, r) in enumerate(pairs):
                ins = e.matmul(out.ap, l.ap, r.ap, start=(i == 0), stop=(i == n - 1))
            return ins
        self._rec("pe", fn, rd, [out])

    def transpose(self, out, in_, ident):
        self._rec("pe", lambda e: e.transpose(out.ap, in_.ap, ident.ap), [in_, ident], [out])

    def act(self, out, in_, func, bias=None, scale=1.0, accum=None):
        rd = [in_]
        kw = {}
        if bias is not None:
            if isinstance(bias, TT):
                rd.append(bias)
                kw["bias"] = bias.ap
            else:
                kw["bias"] = bias
        if isinstance(scale, TT):
            rd.append(scale)
            kw["scale"] = scale.ap
        else:
            kw["scale"] = scale
        wr = [out]
        if accum is not None:
            wr.append(accum)
            kw["accum_out"] = accum.ap
        self._rec("act", lambda e: e.activation(out.ap, in_.ap, func, **kw), rd, wr)

    def tt(self, eng, out, a, b, op):
        self._rec(eng, lambda e: e.tensor_tensor(out.ap, a.ap, b.ap, op), [a, b], [out])

    def ts(self, eng, out, a, s1, s2=None, op0=ALU.mult, op1=None):
        rd = [a]
        s1a = s1.ap if isinstance(s1, TT) else s1
        s2a = s2.ap if isinstance(s2, TT) else s2
        if isinstance(s1, TT):
            rd.append(s1)
        if isinstance(s2, TT):
            rd.append(s2)
        kw = {}
        if op1 is not None:
            kw["op1"] = op1
        self._rec(eng, lambda e: e.tensor_scalar(out.ap, a.ap, s1a, s2a, op0, **kw), rd, [out])

    def stt(self, eng, out, a, s, b, op0, op1):
        rd = [a, b]
        sa = s.ap if isinstance(s, TT) else s
        if isinstance(s, TT):
            rd.append(s)
        self._rec("dve", lambda e: e.scalar_tensor_tensor(out.ap, a.ap, sa, b.ap, op0, op1), rd, [out])

    def copy(self, eng, out, a):
        if eng == "act":
            self._rec(eng, lambda e: e.copy(out.ap, a.ap), [a], [out])
        else:
            self._rec(eng, lambda e: e.tensor_copy(out.ap, a.ap), [a], [out])

    def memset(self, eng, out, val):
        self._rec(eng, lambda e: e.memset(out.ap, val), [], [out])

    def reduce(self, eng, out, a, op, axis=AX.X):
        self._rec(eng, lambda e: e.tensor_reduce(out.ap, a.ap, axis, op), [a], [out])

    def rsqrt(self, x):
        self._rec("act", lambda e: e.activation(x.ap, x.ap, AF.Sqrt), [x], [x])
        self._rec("dve", lambda e: e.reciprocal(x.ap, x.ap), [x], [x])

    def recip(self, out, a):
        self._rec("dve", lambda e: e.reciprocal(out.ap, a.ap), [a], [out])

    def dma(self, q, out, in_):
        self._rec(q, lambda e: e.dma_start(out.ap, in_.ap), [in_], [out], dma=True)

    def fence(self, stream, tts):
        self._rec(stream, None, list(tts), [])

    def emit(self):
        nc = self.nc
        for s in self.STREAMS:
            k = 0
            for op in self.ops[s]:
                if op.dma:
                    op.slot = op.didx % NSLOT
                    op.val = 16 * (op.didx // NSLOT + 1)
                elif op.ms:
                    k += 1
                    op.val = k
        sem_ctx = []
        csem = {}
        dsem = {}
        for s in self.STREAMS:
            g = nc.semaphore("c_" + s)
            csem[s] = g.__enter__()
            sem_ctx.append(g)
            if self.ndma[s] > 0:
                for i in range(NSLOT):
                    g = nc.semaphore("d_%s_%d" % (s, i))
                    dsem[(s, i)] = g.__enter__()
                    sem_ctx.append(g)
        ops = self.ops

        def run(stream, e):
            waited = {}
            for op in ops[stream]:
                for d in op.deps:
                    if d.dma:
                        key = ("d", d.stream, d.slot)
                        sem = dsem[(d.stream, d.slot)]
                    else:
                        key = ("c", d.stream)
                        sem = csem[d.stream]
                    if waited.get(key, 0) < d.val:
                        e.wait_ge(sem, d.val)
                        waited[key] = d.val
                if op.fn is None:
                    continue
                ins = op.fn(e)
                if op.dma:
                    ins.then_inc(dsem[(stream, op.slot)], 16)
                elif op.ms:
                    ins.then_inc(csem[stream], 1)

        with nc.Block() as block:
            @block.sync
            def _(e):
                run("sp", e)

            @block.tensor
            def _(e):
                run("pe", e)

            @block.scalar
            def _(e):
                run("act", e)

            @block.vector
            def _(e):
                run("dve", e)

            @block.gpsimd
            def _(e):
                run("pool", e)
        for g in reversed(sem_ctx):
            g.__exit__(None, None, None)


D = 1024
KC = 8
EPS = 1e-6
EVEN_IN = 5664
FFN_DENSE = 2816
NEXP = 8
FEXP = 3584
CTX = 256
GRID_W = 64
LAM_INIT = 0.8 - 0.6 * math.exp(-0.3 * 1)
C_Z, C_XBC, C_DT, C_RQ, C_RK, C_RV, C_RG = 0, 1024, 2560, 2592, 3104, 3616, 4640


def host_consts():
    k = np.arange(128)[:, None]
    l = np.arange(128)[None, :]
    c = {}
    c["ident"] = (k == l).astype(np.float32)
    c["le"] = (k <= l).astype(np.float32)
    c["gt"] = (k > l).astype(np.float32)
    c["ge"] = (k >= l).astype(np.float32)
    c["lt"] = (k < l).astype(np.float32)
    c["ones"] = np.ones((128, 128), np.float32)
    cm = np.concatenate([c[n] for n in ("ident", "le", "gt", "ge", "lt", "ones")], axis=1)
    sel = np.zeros((8, 8, 128), np.float32)
    for e in range(8):
        sel[e, e, :] = 1.0
    return cm, sel.reshape(8, 1024)


def rope_tables(L):
    f32 = np.float32
    inv = (np.float32(10000.0) ** (-np.arange(64, dtype=f32) / f32(64))).astype(f32)
    pos = np.arange(CTX + L, dtype=f32)
    ang = (pos[:, None] * inv[None, :]).astype(f32)
    rcs = np.concatenate([np.cos(ang), np.sin(ang)], axis=1).astype(f32)
    inv16 = (np.float32(10000.0) ** (-np.arange(16, dtype=f32) / f32(16))).astype(f32)
    t = np.arange(L)
    row = (t // GRID_W).astype(f32)
    col = (t % GRID_W).astype(f32)
    ar = (row[:, None] * inv16[None, :]).astype(f32)
    ac = (col[:, None] * inv16[None, :]).astype(f32)
    cos = np.concatenate([np.cos(ar), np.cos(ar), np.cos(ac), np.cos(ac)], axis=1)
    sins = np.concatenate([-np.sin(ar), np.sin(ar), -np.sin(ac), np.sin(ac)], axis=1)
    acs = np.concatenate([cos, sins], axis=1).astype(f32)
    return rcs, acs


def build(L=8192, OWN=4096, stop_after=None, dbg=()):
    nc = bass.Bass("TRN2", target_bir_lowering=False)
    P = Prog(nc)
    T = CTX + L
    NCH = T // 128
    NLB = L // 256
    dbg = set(dbg)

    def din(name, shape, dt=F32):
        return P.dram(name, shape, dt, kind="ExternalInput")

    xT = din("xT", [D, L + 2])
    cT = din("ctxT", [D, CTX + 2])
    cvec = din("cvec", [128, 16])
    sel = din("sel", [128, 2])
    consts = din("consts", [128, 768])
    selmat = din("selmat", [8, 1024])
    rcs_d = din("rcs", [T, 128])
    acs_d = din("acs", [L, 128])
    w_mod = [din("w_mod0", [D, 6 * D]), din("w_mod1", [D, 6 * D])]
    b_mod = [din("b_mod0", [128, 48]), din("b_mod1", [128, 48])]
    norms = din("norms", [128, 32])
    w_in0 = din("w_in0", [D, EVEN_IN])
    convw = din("convw", [128, 12, 4])
    rowp = din("rowp", [1, 2560])
    w_out0 = din("w_out0", [2048, D])
    ffg = din("ffg", [D, FFN_DENSE])
    ffu = din("ffu", [D, FFN_DENSE])
    ffd = din("ffd", [FFN_DENSE, D])
    w_in1 = din("w_in1", [D, 3072])
    w_out1 = din("w_out1", [D, D])
    wr = din("router", [D, NEXP])
    eg = din("eg", [NEXP, D, FEXP])
    eu = din("eu", [NEXP, D, FEXP])
    ed = din("ed", [NEXP, FEXP, D])
    outT = P.dram("outT", [D, OWN], F32, kind="ExternalOutput")

    def scratch(name, shape, dt=F32):
        return P.dram(name, shape, dt, kind=("ExternalOutput" if name in dbg else "Internal"))

    r_zr = scratch("r_zr", [NCH, 128, 2048], BF16)
    r_xv = scratch("r_xv", [NCH, 128, 2048], BF16)
    r_kt = scratch("r_kt", [NCH, 128, 768], BF16)
    r_fm = scratch("r_fm", [NCH, 128, 12, 128], BF16)
    r_dt = scratch("r_dt", [NCH, 128, 64], F32)
    r_yf = scratch("r_yf", [NCH, 128, 2048], F32)
    x_mid = scratch("x_mid", [NCH, 128, 8, 128], F32)
    x_l1 = scratch("x_l1", [NCH, 128, 8, 128], F32)
    Kd = scratch("Kd", [8, 128, T], BF16)
    Vd = scratch("Vd", [8, 128, NCH, 130], BF16)
    Qd = scratch("Qd", [8, 128, L], BF16)
    Od = scratch("Od", [128, 8, OWN], BF16)

    cst = P.sb("cst", [128, 768], F32)
    P.dma("sp", cst, consts)
    ident_f = cst[:, 0:128]
    m_le, m_gt, m_ge, m_lt, ones_f = (cst[:, 128 * i:128 * (i + 1)] for i in range(1, 6))
    cstb = P.sb("cstb", [128, 768], BF16)
    P.copy("dve", cstb, cst)
    ident_b = cstb[:, 0:128]
    sel_sb = P.sb("sel_sb", [128, 2], F32)
    P.dma("sp", sel_sb, sel)
    rows = P.sb("rows", [128, 2560], F32)
    P.dma("sp", rows, TT(rowp.ap.rearrange("a b -> (a b)").partition_broadcast(128), rowp.tok))
    norm_sb = P.sb("norm_sb", [128, 32], F32)
    P.dma("sp", norm_sb, norms)
    modfm = [P.sb("modfm0", [128, 48, 2], F32), P.sb("modfm1", [128, 48, 2], F32)]
    P.banks = [P.ps("bank%d" % i, [128, 512], F32) for i in range(8)]

    def bank_bf(b):
        a = b.ap
        return TT(a.bitcast(BF16), b.tok)

    AB = [P.sb("AB%d" % i, [128, 8, 2, 2, 2]) for i in range(2)]
    G = [P.sb("G%d" % i, [128, 8, 2, 2]) for i in range(2)]
    P.open_scope()
    cv = P.sb("cv", [128, 16], F32)
    P.dma("sp", cv, cvec)
    scv = P.sb("scv", [128, 8, 2], F32)
    P.act(scv[:, :, 0], cv[:, 0:8], AF.Silu)
    P.act(scv[:, :, 1], cv[:, 8:16], AF.Silu)
    wm = [P.sb("wm%d" % i, [128, 8, 512], F32) for i in range(2)]
    bm_sb = P.sb("bm_sb", [128, 2, 48], F32)
    P.dma("sp", bm_sb[:, 0, :], b_mod[0])
    P.dma("sp", bm_sb[:, 1, :], b_mod[1])
    it = 0
    for lyr in range(2):
        for cg in range(12):
            w = wm[it % 2]
            it += 1
            P.dma("sp", w, w_mod[lyr][:, cg * 512:(cg + 1) * 512].re("(k p) f -> p k f", p=128))
            pb = P.bank()
            for j in range(4):
                P.mmgroup(pb[:, 2 * j:2 * j + 2], [(w[:, k, j * 128:(j + 1) * 128], scv[:, k, :]) for k in range(8)])
            for j in range(4):
                ch = cg * 4 + j
                P.ts("dve", modfm[lyr][:, ch, :], pb[:, 2 * j:2 * j + 2], bm_sb[:, lyr, ch:ch + 1], None, op0=ALU.add)
    for lyr in range(2):
        for w in range(2):
            for n in range(2):
                shift = modfm[lyr][:, (3 * n) * 8:(3 * n) * 8 + 8, w]
                scale = modfm[lyr][:, (3 * n + 1) * 8:(3 * n + 1) * 8 + 8, w]
                gate = modfm[lyr][:, (3 * n + 2) * 8:(3 * n + 2) * 8 + 8, w]
                gain = norm_sb[:, (2 * lyr + n) * 8:(2 * lyr + n) * 8 + 8]
                P.stt("dve", AB[lyr][:, :, w, n, 0], scale, 1.0, gain, ALU.add, ALU.mult)
                P.copy("dve", AB[lyr][:, :, w, n, 1], shift)
                P.copy("dve", G[lyr][:, :, w, n], gate)
    P.close_scope()

    def rms_modulate(xin, ncol, Atab, out_bf, tmp, out_f32=None, eng="pool"):
        sq = tmp["sq"]
        P.act(sq[:, :, 0:ncol], xin, AF.Square)
        pb = P.bank()
        P.mmgroup(pb[:, 0:ncol], [(ones_f, sq[:, k, 0:ncol]) for k in range(8)])
        rstd = tmp["rstd"]
        P.ts("dve", rstd[:, 0:ncol], pb[:, 0:ncol], 1.0 / D, EPS, op0=ALU.mult, op1=ALU.add)
        P.rsqrt(rstd[:, 0:ncol])
        P.tt("dve", sq[:, :, 0:ncol], xin, bc(rstd[:, 0:ncol], [(0, 8), (1, ncol)]), ALU.mult)
        for k in range(8):
            if eng == "act" or (eng == "mix" and k % 2 == 0):
                P.act(out_bf[:, k, :], sq[:, k, 0:ncol], AF.Identity, bias=Atab[:, k, 1:2], scale=Atab[:, k, 0:1])
            else:
                P.ts("pool", out_bf[:, k, :], sq[:, k, 0:ncol], Atab[:, k, 0:1], Atab[:, k, 1:2], op0=ALU.mult, op1=ALU.add)
            if out_f32 is not None:
                P.ts("pool", out_f32[:, k, :], sq[:, k, 0:ncol], Atab[:, k, 0:1], Atab[:, k, 1:2], op0=ALU.mult, op1=ALU.add)

    r_dtb = rows[:, 0:32]
    r_alog = rows[:, 32:64]
    r_retd = rows[:, 64:72]
    r_dsk = rows[:, 72:88]
    r_ssdn = rows[:, 88:1112]
    r_qn = rows[:, 1112:1176]
    r_kn = rows[:, 1176:1240]
    r_lam = rows[:, 1240:1496]
    r_subln = rows[:, 1496:1624]
    ea = P.sb("ea", [128, 32], F32)
    P.act(ea, r_alog, AF.Exp)
    nla_ret = P.sb("nla_ret", [128, 8], F32)
    P.act(nla_ret, r_retd, AF.Exp)
    P.ts("dve", nla_ret, nla_ret, -1.0, None, op0=ALU.mult)

    P.open_scope()
    w0 = P.sb("w0", [128, 8, EVEN_IN], BF16)
    for k in range(8):
        for c0 in range(0, EVEN_IN, 1888):
            P.dma("pool", w0[:, k, c0:c0 + 1888], w_in0[k * 128:(k + 1) * 128, c0:c0 + 1888])
    cw = P.sb("cw", [128, 12, 4], F32)
    P.dma("sp", cw, convw)
    xin = [P.sb("xin%d" % i, [128, 8, 258], F32) for i in range(2)]
    xbr = P.sb("xbr", [128, 12, 258], F32)
    tmp1 = {"sq": xbr[:, 0:8, :], "rstd": P.sb("rstd1", [128, 258], F32)}
    hbs = [P.sb("hb%d" % i, [128, 8, 258], BF16) for i in range(2)]
    xbcs = [P.sb("xbc%d" % i, [128, 12, 256], BF16) for i in range(2)]
    cvts = [P.sb("cvt%d" % i, [128, 256], F32) for i in range(2)]
    o_zr = P.sb("o_zr", [128, 2, 2048], BF16)
    o_xv = P.sb("o_xv", [128, 2, 2048], BF16)
    o_kt = P.sb("o_kt", [128, 2, 768], BF16)
    o_fm = P.sb("o_fm", [128, 2, 12, 128], BF16)
    o_dt = P.sb("o_dt", [128, 2, 64], F32)
    rtabs = [P.sb("rtab%d" % i, [128, 2, 128], F32) for i in range(3)]
    rt1 = P.sb("rt1", [128, 4, 128], F32)
    rt2 = P.sb("rt2", [128, 4, 128], F32)
    rqk = P.sb("rqk", [128, 2, 512], BF16)
    sp1 = P.sb("sp1", [128, 32], F32)
    sp2 = P.sb("sp2", [128, 32], F32)

    blocks = [("c", 0)] + [("l", i) for i in range(NLB)]

    def binfo(bj):
        kind_, i_ = blocks[bj]
        if kind_ == "c":
            return 0, True, True, 1
        return CTX + i_ * 256, (i_ == 0), (i_ == NLB - 1), 0

    def load1(bj):
        kind_, i_ = blocks[bj]
        if kind_ == "c":
            P.dma("sp", xin[bj % 2], cT.re("(k p) t -> p k t", p=128))
        else:
            P.dma("sp", xin[bj % 2], xT[:, i_ * 256:i_ * 256 + 258].re("(k p) t -> p k t", p=128))
        t0_ = binfo(bj)[0]
        P.dma("sp", rtabs[bj % 3], rcs_d[t0_:t0_ + 256, :].re("(t p) c -> p t c", p=128))

    def stageA(bj):
        tok0, first, last, which = binfo(bj)
        xi, hb, xbc = xin[bj % 2], hbs[bj % 2], xbcs[bj % 2]
        rms_modulate(xi, 258, AB[0][:, :, which, 0, :], hb, tmp1, eng="act")
        for c in range(12):
            pb = P.bank()
            P.mmgroup(pb[:, 0:258], [(w0[:, k, C_XBC + c * 128:C_XBC + (c + 1) * 128], hb[:, k, :]) for k in range(8)])
            P.copy("act", xbr[:, c, :], pb[:, 0:258])
        if first:
            P.memset("pool", xbr[:, :, 0:1], 0.0)
        if last:
            P.memset("pool", xbr[:, :, 257:258], 0.0)
        for c in range(12):
            cvt = cvts[c % 2]
            P.ts("pool", cvt, xbr[:, c, 0:256], cw[:, c, 0:1], None, op0=ALU.mult)
            P.stt("dve", cvt, xbr[:, c, 1:257], cw[:, c, 1:2], cvt, ALU.mult, ALU.add)
            P.stt("dve", cvt, xbr[:, c, 2:258], cw[:, c, 2:3], cvt, ALU.mult, ALU.add)
            P.act(xbc[:, c, :], cvt, AF.Silu, bias=cw[:, c, 3:4])

    def stageB(bj):
        tok0, first, last, which = binfo(bj)
        hb, xbc, rtab = hbs[bj % 2], xbcs[bj % 2], rtabs[bj % 3]
        ch0 = tok0 // 128
        for t in range(2):
            P.copy("pool", o_fm[:, t, 0:4, :], xbc[:, 8:12, t * 128:(t + 1) * 128])
        for t in range(2):
            lt = [hb[:, k, 1 + t * 128:1 + (t + 1) * 128] for k in range(8)]

            def proj(c0, n):
                pb = P.bank()
                P.mmgroup(pb[:, 0:n], [(lt[k], w0[:, k, c0:c0 + n]) for k in range(8)])
                return pb
            for j in range(2):
                pb = proj(C_Z + j * 512, 512)
                P.copy("act", o_zr[:, t, j * 512:(j + 1) * 512], pb)
            for j in range(2):
                pb = proj(C_RG + j * 512, 512)
                P.copy("act", o_zr[:, t, 1024 + j * 512:1024 + (j + 1) * 512], pb)
            for j in range(2):
                pb = proj(C_RV + j * 512, 512)
                P.copy("act", o_xv[:, t, 1024 + j * 512:1024 + (j + 1) * 512], pb)
            pb = proj(C_DT, 32)
            P.tt("dve", sp1, pb[:, 0:32], r_dtb, ALU.add)
            P.act(sp2, sp1, AF.Abs)
            P.act(sp2, sp2, AF.Exp, scale=-1.0)
            P.act(sp2, sp2, AF.Ln, bias=1.0)
            P.stt("dve", o_dt[:, t, 0:32], sp1, 0.0, sp2, ALU.max, ALU.add)
            P.tt("dve", sp1, o_dt[:, t, 0:32], ea, ALU.mult)
            P.ts("dve", o_dt[:, t, 32:64], sp1, -1.0, None, op0=ALU.mult)
            for qi, c0 in enumerate((C_RQ, C_RK)):
                pb = proj(c0, 512)
                pv = pb.re("p (h d) -> p h d", h=4)
                cos2 = bc(rtab[:, t, 0:64], [(0, 4), (0, 2), (1, 64)])
                P.tt("dve", rt1.re("p h (a d) -> p h a d", a=2), pv.re("p h (a d) -> p h a d", a=2), cos2, ALU.mult)
                sin1 = bc(rtab[:, t, 64:128], [(0, 4), (1, 64)])
                P.tt("dve", rt2[:, :, 0:64], pv[:, :, 64:128], sin1, ALU.mult)
                P.tt("dve", rt2[:, :, 64:128], pv[:, :, 0:64], sin1, ALU.mult)
                rv = rqk[:, qi, :].re("p (h d) -> p h d", h=4)
                P.tt("pool", rt1[:, :, 0:64], rt1[:, :, 0:64], rt2[:, :, 0:64], ALU.subtract)
                P.tt("pool", rt1[:, :, 64:128], rt1[:, :, 64:128], rt2[:, :, 64:128], ALU.add)
                P.act(rv, rt1, AF.Copy, scale=(1.0 if qi == 0 else 128.0 ** -0.5))
            P.copy("pool", o_kt[:, t, 256:768], rqk[:, 1, :])
            pb = P.bank()
            pbb = bank_bf(pb)
            for qi in range(2):
                for h in range(4):
                    P.transpose(pbb[:, (qi * 4 + h) * 128:(qi * 4 + h + 1) * 128], rqk[:, qi, h * 128:(h + 1) * 128], ident_b)
            P.copy("dve", o_fm[:, t, 4:12, :], pbb.re("p (n t) -> p n t", t=128))
            pb = P.bank()
            pbb = bank_bf(pb)
            for c in range(8):
                P.transpose(pbb[:, c * 128:(c + 1) * 128], xbc[:, c, t * 128:(t + 1) * 128], ident_b)
            P.copy("dve", o_xv[:, t, 0:1024], pbb)
            pb = P.bank()
            pbb = bank_bf(pb)
            for c in range(2):
                P.transpose(pbb[:, c * 128:(c + 1) * 128], xbc[:, 8 + c, t * 128:(t + 1) * 128], ident_b)
            P.copy("dve", o_kt[:, t, 0:256], pbb[:, 0:256])
        for t in range(2):
            P.dma("sp", r_zr[ch0 + t], o_zr[:, t, :])
            P.dma("sp", r_xv[ch0 + t], o_xv[:, t, :])
            P.dma("sp", r_kt[ch0 + t], o_kt[:, t, :])
            P.dma("sp", r_fm[ch0 + t], o_fm[:, t])
            P.dma("sp", r_dt[ch0 + t], o_dt[:, t, :])

    nblk = len(blocks)
    load1(0)
    if nblk > 1:
        load1(1)
    stageA(0)
    for bi in range(nblk):
        if bi + 2 < nblk:
            load1(bi + 2)
        if bi + 1 < nblk:
            stageA(bi + 1)
        stageB(bi)
    P.close_scope()
    if stop_after == "P1":
        return _finish(P, nc, [r_zr, r_xv, r_kt, r_fm, r_dt])

    P.open_scope()
    wo0 = P.sb("wo0", [128, 16, D], BF16)
    for k in range(16):
        P.dma("pool", wo0[:, k, :], w_out0[k * 128:(k + 1) * 128, :])
    Er = P.sb("Er", [128, 2, 3, 4], F32)
    Dret = P.sb("Dret", [128, 2, 4, 128], F32)
    lmr = P.sb("lmr", [128, 4, 128], F32)
    for d in range(2):
        la = nla_ret[:, d * 4:(d + 1) * 4]
        pb = P.bank()
        mA, mT = (m_le, m_gt) if d == 0 else (m_ge, m_lt)
        P.mm(pb[:, 0:4], mA, la)
        P.mm(pb[:, 4:8], mT, la)
        P.mm(pb[:, 8:12], ones_f, la)
        P.act(Er[:, d].re("p a h -> p (a h)"), pb[:, 0:12], AF.Exp)
        mS, mR, mM = (m_gt, m_le, m_le) if d == 0 else (m_lt, m_ge, m_ge)
        P.tt("dve", lmr, bc(mS, [(0, 4), (1, 128)]), bc(la, [(1, 4), (0, 128)]), ALU.mult)
        pb = P.bank()
        for h in range(4):
            P.mm(pb[:, h * 128:(h + 1) * 128], lmr[:, h, :], mR)
        P.act(Dret[:, d].re("p h l -> p (h l)"), pb, AF.Exp)
        P.tt("dve", Dret[:, d], Dret[:, d], bc(mM, [(0, 4), (1, 128)]), ALU.mult)

    Hs = P.sb("Hs", [128, 1024], F32)
    Hr = P.sb("Hr", [128, 1024], F32)
    Hsb = P.sb("Hsb", [128, 1024], BF16)
    Hrb = P.sb("Hrb", [128, 1024], BF16)
    i_xv = [P.sb("i_xv%d" % i, [128, 2048], BF16) for i in range(2)]
    i_kt = [P.sb("i_kt%d" % i, [128, 768], BF16) for i in range(2)]
    i_fm = [P.sb("i_fm%d" % i, [128, 12, 128], BF16) for i in range(2)]
    i_dt = [P.sb("i_dt%d" % i, [128, 64], F32) for i in range(2)]
    i_zr = [P.sb("i_zr%d" % i, [128, 2048], BF16) for i in range(2)]
    i_yf = [P.sb("i_yf%d" % i, [128, 2048], F32) for i in range(2)]
    i_x = [P.sb("i_x%d" % i, [128, 8, 128], F32) for i in range(2)]
    E = P.sb("E", [128, 3, 16], F32)
    scm = P.sb("scm", [128, 2, 128], F32)
    Lm = P.sb("Lm", [128, 16, 128], F32)
    expD = P.sb("expD", [128, 16, 128], F32)
    MT = P.sb("MT", [128, 16, 128], BF16)
    MTr = P.sb("MTr", [128, 4, 128], BF16)
    xdt = P.sb("xdt", [128, 1024], BF16)
    xw = P.sb("xw", [128, 1024], BF16)
    rvw = P.sb("rvw", [128, 1024], BF16)
    wv = P.sb("wv", [128, 16], F32)
    ytmp = P.sb("ytmp", [128, 1024], F32)
    yo = [P.sb("yo%d" % i, [128, 2048], F32) for i in range(2)]
    sz = P.sb("sz", [128, 1024], F32)
    junk = P.sb("junk", [128, 1024], F32)
    ss = P.sb("ss", [128, 8], F32)
    ycat = P.sb("ycat", [128, 2048], BF16)
    ycT = P.sb("ycT", [128, 16, 128], BF16)
    xo = P.sb("xo", [128, 8, 128], F32)

    fwd_order = list(range(NCH))
    bwd_order = [1, 0] + list(range(NCH - 1, 1, -1))

    for d in range(2):
        order = fwd_order if d == 0 else bwd_order
        P.memset("dve", Hs, 0.0)
        P.memset("dve", Hr, 0.0)
        P.memset("pool", Hsb, 0.0)
        P.memset("pool", Hrb, 0.0)
        mA, mT = (m_le, m_gt) if d == 0 else (m_ge, m_lt)
        mS, mR, mM = (m_gt, m_le, m_le) if d == 0 else (m_lt, m_ge, m_ge)
        def load_sw(cj):
            ch_ = order[cj]
            b_ = cj % 2
            P.dma("sp", i_xv[b_], r_xv[ch_])
            P.dma("sp", i_kt[b_], r_kt[ch_])
            P.dma("sp", i_fm[b_], r_fm[ch_])
            P.dma("sp", i_dt[b_], r_dt[ch_])
            if d == 1:
                P.dma("sp", i_zr[b_], r_zr[ch_])
                P.dma("sp", i_yf[b_], r_yf[ch_])
                if ch_ < 2:
                    P.dma("sp", i_x[b_], cT[:, 1 + ch_ * 128:1 + (ch_ + 1) * 128].re("(k p) t -> p k t", p=128))
                else:
                    P.dma("sp", i_x[b_], xT[:, 1 + (ch_ - 2) * 128:1 + (ch_ - 1) * 128].re("(k p) t -> p k t", p=128))
        load_sw(0)
        for ci, ch in enumerate(order):
            b = ci % 2
            xv, kt, fm, dtt = i_xv[b], i_kt[b], i_fm[b], i_dt[b]
            if ci + 1 < len(order):
                load_sw(ci + 1)
            la = dtt[:, 32 + d * 16:32 + (d + 1) * 16]
            dtd = dtt[:, d * 16:(d + 1) * 16]
            xs = xv[:, 0:1024]
            rvv = xv[:, 1024:2048]
            yout = yo[ci % 2]
            pb = P.bank()
            P.mm(pb[:, 0:16], mA, la)
            P.mm(pb[:, 16:32], mT, la)
            P.mm(pb[:, 32:48], ones_f, la)
            P.act(E.re("p a h -> p (a h)"), pb[:, 0:48], AF.Exp)
            pb = P.bank()
            for g in range(2):
                P.mm(pb[:, g * 128:(g + 1) * 128], fm[:, g, :], fm[:, 2 + g, :])
            P.tt("dve", scm, pb[:, 0:256].re("p (g l) -> p g l", g=2), bc(mM, [(0, 2), (1, 128)]), ALU.mult)
            P.tt("pool", Lm, bc(mS, [(0, 16), (1, 128)]), bc(la, [(1, 16), (0, 128)]), ALU.mult)
            for q in range(4):
                pb = P.bank()
                for j in range(4):
                    P.mm(pb[:, j * 128:(j + 1) * 128], Lm[:, q * 4 + j, :], mR)
                P.act(expD[:, q * 4:(q + 1) * 4, :].re("p h l -> p (h l)"), pb, AF.Exp)
            for g in range(2):
                P.tt("pool" if g == 1 else "dve", MT[:, g * 8:(g + 1) * 8, :], expD[:, g * 8:(g + 1) * 8, :], bc(scm[:, g, :], [(0, 8), (1, 128)]), ALU.mult)
            P.tt("pool", xdt.re("p (h d) -> p h d", h=16), xs.re("p (h d) -> p h d", h=16), bc(dtd, [(1, 16), (0, 64)]), ALU.mult)
            pd = [P.bank(), P.bank()]
            for h in range(16):
                P.mm(pd[h // 8][:, (h % 8) * 64:(h % 8 + 1) * 64], MT[:, h, :], xdt[:, h * 64:(h + 1) * 64])
            for g in range(2):
                po = P.bank()
                P.mm(po, fm[:, 2 + g, :], Hsb[:, g * 512:(g + 1) * 512])
                P.tt("dve", ytmp[:, g * 512:(g + 1) * 512].re("p (h d) -> p h d", h=8), po.re("p (h d) -> p h d", h=8),
                     bc(E[:, 0, g * 8:(g + 1) * 8], [(1, 8), (0, 64)]), ALU.mult)
                P.tt("dve", yout[:, g * 512:(g + 1) * 512], ytmp[:, g * 512:(g + 1) * 512], pd[g], ALU.add)
            P.tt("dve", wv, dtd, E[:, 1, :], ALU.mult)
            P.tt("pool", xw.re("p (h d) -> p h d", h=16), xs.re("p (h d) -> p h d", h=16), bc(wv, [(1, 16), (0, 64)]), ALU.mult)
            P.tt("dve", Hs.re("p (h d) -> p h d", h=16), Hs.re("p (h d) -> p h d", h=16), bc(E[:, 2, :], [(1, 16), (0, 64)]), ALU.mult)
            for g in range(2):
                pS = P.bank()
                P.mm(pS, kt[:, g * 128:(g + 1) * 128], xw[:, g * 512:(g + 1) * 512])
                P.tt("dve", Hs[:, g * 512:(g + 1) * 512], Hs[:, g * 512:(g + 1) * 512], pS, ALU.add)
            P.copy("act", Hsb, Hs)
            pb = P.bank()
            for h in range(4):
                P.mm(pb[:, h * 128:(h + 1) * 128], fm[:, 8 + h, :], fm[:, 4 + h, :])
            P.tt("dve", MTr, pb.re("p (h l) -> p h l", h=4), Dret[:, d], ALU.mult)
            pd = [P.bank(), P.bank()]
            for h in range(4):
                P.mm(pd[h // 2][:, (h % 2) * 256:(h % 2 + 1) * 256], MTr[:, h, :], rvv[:, h * 256:(h + 1) * 256])
            for g in range(2):
                po = P.bank()
                for hh in range(2):
                    h = g * 2 + hh
                    P.mm(po[:, hh * 256:(hh + 1) * 256], fm[:, 4 + h, :], Hrb[:, h * 256:(h + 1) * 256])
                P.tt("dve", ytmp[:, g * 512:(g + 1) * 512].re("p (h d) -> p h d", h=2), po.re("p (h d) -> p h d", h=2),
                     bc(Er[:, d, 0, g * 2:(g + 1) * 2], [(1, 2), (0, 256)]), ALU.mult)
                P.tt("dve", yout[:, 1024 + g * 512:1024 + (g + 1) * 512], ytmp[:, g * 512:(g + 1) * 512], pd[g], ALU.add)
            P.tt("pool", rvw.re("p (h d) -> p h d", h=4), rvv.re("p (h d) -> p h d", h=4), bc(Er[:, d, 1, :], [(1, 4), (0, 256)]), ALU.mult)
            P.tt("dve", Hr.re("p (h d) -> p h d", h=4), Hr.re("p (h d) -> p h d", h=4), bc(Er[:, d, 2, :], [(1, 4), (0, 256)]), ALU.mult)
            for g in range(2):
                pS = P.bank()
                for hh in range(2):
                    h = g * 2 + hh
                    P.mm(pS[:, hh * 256:(hh + 1) * 256], kt[:, 256 + h * 128:256 + (h + 1) * 128], rvw[:, h * 256:(h + 1) * 256])
                P.tt("dve", Hr[:, g * 512:(g + 1) * 512], Hr[:, g * 512:(g + 1) * 512], pS, ALU.add)
            P.copy("act", Hrb, Hr)
            if d == 0:
                P.dma("sp", r_yf[ch], yout)
                continue
            zr = i_zr[b]
            which = 1 if ch < 2 else 0
            P.tt("dve", yout, yout, i_yf[b], ALU.add)
            ys = yout[:, 0:1024]
            yr = yout[:, 1024:2048]
            P.tt("pool", ytmp.re("p (h d) -> p h d", h=16), xs.re("p (h d) -> p h d", h=16), bc(r_dsk, [(1, 16), (0, 64)]), ALU.mult)
            P.tt("dve", ys, ys, ytmp, ALU.add)
            P.act(sz, zr[:, 0:1024], AF.Silu)
            P.tt("dve", ys, ys, sz, ALU.mult)
            P.act(junk, ys, AF.Square)
            P.reduce("dve", ss[:, 0:1], junk, ALU.add)
            P.ts("dve", ss[:, 1:2], ss[:, 0:1], 1.0 / 1024, EPS, op0=ALU.mult, op1=ALU.add)
            P.rsqrt(ss[:, 1:2])
            P.stt("dve", ycat[:, 0:1024], ys, ss[:, 1:2], r_ssdn, ALU.mult, ALU.mult)
            P.act(junk, yr, AF.Square)
            P.reduce("dve", ss[:, 2:6], junk.re("p (h d) -> p h d", h=4), ALU.add)
            P.ts("dve", ss[:, 2:6], ss[:, 2:6], 1.0 / 256, EPS, op0=ALU.mult, op1=ALU.add)
            P.rsqrt(ss[:, 2:6])
            P.act(sz, zr[:, 1024:2048], AF.Silu)
            P.tt("dve", yr.re("p (h d) -> p h d", h=4), yr.re("p (h d) -> p h d", h=4), bc(ss[:, 2:6], [(1, 4), (0, 256)]), ALU.mult)
            P.tt("dve", ycat[:, 1024:2048], yr, sz, ALU.mult)
            for q in range(2):
                pb = P.bank()
                pbb = bank_bf(pb)
                for j in range(8):
                    P.transpose(pbb[:, j * 128:(j + 1) * 128], ycat[:, (q * 8 + j) * 128:(q * 8 + j + 1) * 128], ident_b)
                P.copy("act", ycT[:, q * 8:(q + 1) * 8, :].re("p n t -> p (n t)"), pbb)
            for q in range(2):
                pb = P.bank()
                for j in range(4):
                    dc = q * 4 + j
                    P.mmgroup(pb[:, j * 128:(j + 1) * 128], [(wo0[:, k, dc * 128:(dc + 1) * 128], ycT[:, k, :]) for k in range(16)])
                for j in range(4):
                    dc = q * 4 + j
                    P.stt("dve", xo[:, dc, :], pb[:, j * 128:(j + 1) * 128], G[0][:, dc, which, 0:1], i_x[b][:, dc, :], ALU.mult, ALU.add)
            P.dma("sp", x_mid[ch], xo)
    P.close_scope()
    if stop_after == "P3":
        return _finish(P, nc, [x_mid, r_yf])

    P.open_scope()
    NF = FFN_DENSE // 128
    wg = P.sb("wg", [128, 8, FFN_DENSE], BF16)
    wu = P.sb("wu", [128, 8, FFN_DENSE], BF16)
    wd = P.sb("wd", [128, NF, D], BF16)
    for k in range(8):
        P.dma("pool", wg[:, k, :], ffg[k * 128:(k + 1) * 128, :])
        P.dma("pool", wu[:, k, :], ffu[k * 128:(k + 1) * 128, :])
    for f in range(NF):
        P.dma("pool", wd[:, f, :], ffd[f * 128:(f + 1) * 128, :])
    xb4 = [P.sb("xb4_%d" % i, [128, 8, 256], F32) for i in range(2)]
    tmp4 = {"sq": P.sb("sq4", [128, 8, 256], F32), "rstd": P.sb("rstd4", [128, 256], F32)}
    h4 = P.sb("h4", [128, 8, 256], BF16)
    a4 = P.sb("a4", [128, NF, 256], BF16)
    sg4 = [P.sb("sg4_%d" % i, [128, 256], F32) for i in range(2)]
    xo4 = [P.sb("xo4_%d" % i, [128, 8, 256], F32) for i in range(1)]
    def load4(bj):
        for t in range(2):
            P.dma("sp", xb4[bj % 2][:, :, t * 128:(t + 1) * 128], x_mid[bj * 2 + t])
    load4(0)
    for bi in range(NCH // 2):
        x4 = xb4[bi % 2]
        which = 1 if bi == 0 else 0
        if bi + 1 < NCH // 2:
            load4(bi + 1)
        rms_modulate(x4, 256, AB[0][:, :, which, 1, :], h4, tmp4)
        for f in range(NF):
            pb = P.bank()
            P.mmgroup(pb[:, 0:256], [(wg[:, k, f * 128:(f + 1) * 128], h4[:, k, :]) for k in range(8)])
            P.mmgroup(pb[:, 256:512], [(wu[:, k, f * 128:(f + 1) * 128], h4[:, k, :]) for k in range(8)])
            sg = sg4[f % 2]
            P.act(sg, pb[:, 0:256], AF.Silu)
            P.tt("dve", a4[:, f, :], sg, pb[:, 256:512], ALU.mult)
        xo_ = xo4[0]
        for q in range(4):
            pb = P.bank()
            for j in range(2):
                dc = q * 2 + j
                P.mmgroup(pb[:, j * 256:(j + 1) * 256], [(wd[:, f, dc * 128:(dc + 1) * 128], a4[:, f, :]) for f in range(NF)])
            for j in range(2):
                dc = q * 2 + j
                P.stt("dve", xo_[:, dc, :], pb[:, j * 256:(j + 1) * 256], G[0][:, dc, which, 1:2], x4[:, dc, :], ALU.mult, ALU.add)
        for t in range(2):
            P.dma("sp", x_l1[bi * 2 + t], xo_[:, :, t * 128:(t + 1) * 128])
    P.close_scope()
    if stop_after == "P4":
        return _finish(P, nc, [x_l1])

    P.open_scope()
    w1 = P.sb("w1", [128, 8, 3072], BF16)
    for k in range(8):
        P.dma("pool", w1[:, k, :], w_in1[k * 128:(k + 1) * 128, :])
    xb5 = [P.sb("xb5_%d" % i, [128, 8, 256], F32) for i in range(2)]
    tmp5 = {"sq": P.sb("sq5", [128, 8, 256], F32), "rstd": P.sb("rstd5", [128, 256], F32)}
    h5 = P.sb("h5", [128, 8, 256], BF16)
    atab = P.sb("atab", [128, 2, 128], F32)
    qsq = P.sb("qsq", [128, 1024], F32)
    qn = P.sb("qn", [128, 1024], F32)
    q1 = P.sb("q1", [128, 1024], F32)
    q2 = P.sb("q2", [128, 1024], F32)
    ss5 = P.sb("ss5", [128, 16], F32)
    qkb = P.sb("qkb", [128, 2, 1024], BF16)
    qkT = P.sb("qkT", [128, 2, 8, 256], BF16)
    v5 = [P.sb("v5_%d" % i, [128, 2, 8, 130], BF16) for i in range(2)]
    for i in range(2):
        P.memset("dve", v5[i], 1.0)
    qg = P.sb("qg", [128, 64], F32)
    P.ts("dve", qg, r_qn, 64.0 ** -0.5, None, op0=ALU.mult)
    atabs = [atab, P.sb("atab2", [128, 2, 128], F32), P.sb("atab3", [128, 2, 128], F32)]
    h5s = [h5, P.sb("h5b", [128, 8, 256], BF16)]
    qsqs = [qsq, P.sb("qsq_b", [128, 1024], F32)]
    qns = [qn, P.sb("qn_b", [128, 1024], F32)]
    q1s = [q1, P.sb("q1_b", [128, 1024], F32)]
    q2s = [q2, P.sb("q2_b", [128, 1024], F32)]
    ss5s = [ss5, P.sb("ss5_b", [128, 16], F32)]
    NB5 = NCH // 2

    def load5(bj):
        for t in range(2):
            P.dma("sp", xb5[bj % 2][:, :, t * 128:(t + 1) * 128], x_l1[bj * 2 + t])
        if bj > 0:
            l0 = (bj - 1) * 256
            P.dma("sp", atabs[bj % 3], acs_d[l0:l0 + 256, :].re("(t p) c -> p t c", p=128))

    def stage5A(bj):
        which = 1 if bj == 0 else 0
        rms_modulate(xb5[bj % 2], 256, AB[1][:, :, which, 0, :], h5s[bj % 2], tmp5, eng="act")

    def stage5B(bi):
        h5 = h5s[bi % 2]
        atab = atabs[bi % 3]
        which = 1 if bi == 0 else 0
        vv = v5[bi % 2]
        for t in range(2):
            lt = [h5[:, k, t * 128:(t + 1) * 128] for k in range(8)]
            for qi in range(2):
                if qi == 0 and which == 1:
                    continue
                qsq, qn, q1, q2, ss5 = qsqs[qi], qns[qi], q1s[qi], q2s[qi], ss5s[qi]
                pbs = []
                for j in range(2):
                    pb = P.bank()
                    c0 = qi * 1024 + j * 512
                    P.mmgroup(pb, [(lt[k], w1[:, k, c0:c0 + 512]) for k in range(8)])
                    P.act(qsq[:, j * 512:(j + 1) * 512], pb, AF.Square)
                    pbs.append(pb)
                P.reduce("dve", ss5, qsq.re("p (g d) -> p g d", d=64), ALU.add)
                P.ts("dve", ss5, ss5, 1.0 / 64, EPS, op0=ALU.mult, op1=ALU.add)
                P.rsqrt(ss5)
                for j in range(2):
                    P.tt("dve", qn[:, j * 512:(j + 1) * 512].re("p (g d) -> p g d", d=64), pbs[j].re("p (g d) -> p g d", d=64),
                         bc(ss5[:, j * 8:(j + 1) * 8], [(1, 8), (0, 64)]), ALU.mult)
                gn = qg if qi == 0 else r_kn
                dst = qkb[:, qi, :]
                if which == 1:
                    P.tt("pool", dst.re("p (g d) -> p g d", d=64), qn.re("p (g d) -> p g d", d=64), bc(gn, [(0, 16), (1, 64)]), ALU.mult)
                else:
                    P.tt("pool", qn.re("p (g d) -> p g d", d=64), qn.re("p (g d) -> p g d", d=64), bc(gn, [(0, 16), (1, 64)]), ALU.mult)
                    P.tt("dve", q1.re("p (g d) -> p g d", d=64), qn.re("p (g d) -> p g d", d=64), bc(atab[:, t, 0:64], [(0, 16), (1, 64)]), ALU.mult)
                    qv = qn.re("p (g a u d) -> p g a u d", a=2, u=2, d=16)
                    q2v = q2.re("p (g a u d) -> p g a u d", a=2, u=2, d=16)
                    for s_ in range(2):
                        sn = bass.AP(atab.ap.tensor, atab[:, t, 64 + s_ * 16:64 + s_ * 16 + 16].ap.offset,
                                     [list(atab.ap.ap[0]), [0, 16], [32, 2], [1, 16]])
                        P.tt("pool", q2v[:, :, :, s_, :], qv[:, :, :, 1 - s_, :], TT(sn, atab.tok), ALU.mult)
                    P.tt("dve", dst, q1, q2, ALU.add)
                pb = P.bank()
                pbb = bank_bf(pb)
                for h in range(8):
                    P.transpose(pbb[:, h * 128:(h + 1) * 128], qkb[:, qi, h * 128:(h + 1) * 128], ident_b)
                P.copy("act", qkT[:, qi, :, t * 128:(t + 1) * 128], pbb.re("p (h t) -> p h t", h=8))
            for j in range(2):
                pb = P.bank()
                c0 = 2048 + j * 512
                P.mmgroup(pb, [(lt[k], w1[:, k, c0:c0 + 512]) for k in range(8)])
                P.copy("act", vv[:, t, j * 4:(j + 1) * 4, 0:128], pb.re("p (h e) -> p h e", h=4))
        tok0 = bi * 256
        P.dma("sp", Kd[:, :, tok0:tok0 + 256].re("h p t -> p h t"), qkT[:, 1])
        if which == 0:
            P.dma("sp", Qd[:, :, tok0 - CTX:tok0 - CTX + 256].re("h p t -> p h t"), qkT[:, 0])
        for t in range(2):
            P.dma("sp", Vd[:, :, bi * 2 + t, :].re("h p e -> p h e"), vv[:, t])

    load5(0)
    if NB5 > 1:
        load5(1)
    stage5A(0)
    for bi in range(NB5):
        if bi + 2 < NB5:
            load5(bi + 2)
        if bi + 1 < NB5:
            stage5A(bi + 1)
        stage5B(bi)
    P.close_scope()
    if stop_after == "P5":
        return _finish(P, nc, [Kd, Vd, Qd])

    P.open_scope()
    NKT = NCH
    NQB = OWN // 512
    lt_ = P.sb("lt_", [128, 128], F32)
    lam2 = P.sb("lam2", [128, 4], F32)
    P.tt("dve", lt_[:, 0:64], r_lam[:, 0:64], r_lam[:, 64:128], ALU.mult)
    P.tt("dve", lt_[:, 64:128], r_lam[:, 128:192], r_lam[:, 192:256], ALU.mult)
    P.reduce("dve", lam2[:, 0:2], lt_.re("p (a d) -> p a d", a=2), ALU.add)
    P.act(lam2[:, 0:2], lam2[:, 0:2], AF.Exp)
    P.tt("dve", lam2[:, 2:3], lam2[:, 1:2], lam2[:, 0:1], ALU.subtract)
    P.ts("dve", lam2[:, 3:4], lam2[:, 2:3], -LAM_INIT, None, op0=ALU.add)
    neglam = lam2[:, 3:4]
    sub_g = P.sb("sub_g", [128, 128], F32)
    P.ts("dve", sub_g, r_subln, 1.0 - LAM_INIT, None, op0=ALU.mult)
    Kh = [P.sb("Kh%d" % i, [128, T], BF16) for i in range(2)]
    Vh = [P.sb("Vh%d" % i, [128, NKT, 130], BF16) for i in range(2)]
    qa = [P.sb("qa%d" % i, [128, 512], BF16) for i in range(2)]
    qb_ = [P.sb("qb%d" % i, [128, 512], BF16) for i in range(2)]
    qs = [P.sb("qs%d" % i, [128, 512], BF16) for i in range(2)]
    pT = [P.sb("pT%d" % i, [128, 512], BF16) for i in range(4)]
    accs = [[P.sb("accs%d_%d" % (i, j), [128, 512], F32) for j in range(2)] for i in range(2)]
    o0 = P.sb("o0", [128, 512], F32)
    o1 = P.sb("o1", [128, 512], F32)
    rcp = P.sb("rcp", [128, 512], F32)
    osq = P.sb("osq", [128, 512], F32)
    oT = [P.sb("oT%d" % i, [128, 512], BF16) for i in range(2)]
    subg_fm = P.sb("subg_fm", [128, 1], F32)
    pbt = P.banks[7]
    P.transpose(pbt[:, 0:128], sub_g, ident_f)
    P.copy("dve", subg_fm, pbt[:, 0:1])
    spb = [P.banks[0], P.banks[1], P.banks[2], P.banks[3]]
    obk = [P.banks[4], P.banks[5]]
    aux = [P.banks[6], P.banks[7]]
    groups = [(h, qb) for h in range(8) for qb in range(NQB)]
    steps = [(m, kt) for m in range(2) for kt in range(NKT)]

    def load_kv(h):
        P.dma("sp", Kh[h % 2], Kd[h])
        P.dma("sp", Vh[h % 2], Vd[h])

    def load_q(gi):
        h, qb = groups[gi]
        A, B_, Q_ = qa[gi % 2], qb_[gi % 2], qs[gi % 2]
        P.dma("sp", A, Qd[h, :, qb * 512:(qb + 1) * 512])
        if L > OWN:
            P.dma("sp", B_, Qd[h, :, OWN + qb * 512:OWN + (qb + 1) * 512])
            P.ts("pool", Q_, A, sel_sb[:, 0:1], None, op0=ALU.mult)
            P.stt("dve", Q_, B_, sel_sb[:, 1:2], Q_, ALU.mult, ALU.add)
            return Q_
        return A

    load_kv(0)
    Qn = load_q(0)
    pi = 0
    for gi, (h, qb) in enumerate(groups):
        K_, V_ = Kh[h % 2], Vh[h % 2]
        Q_ = Qn
        if qb == 0 and h + 1 < 8:
            load_kv(h + 1)
        if gi + 1 < len(groups):
            Qn = load_q(gi + 1)

        def emit_s(i):
            m, kt = steps[i]
            P.mm(spb[(pi + i) % 4], K_[m * 64:(m + 1) * 64, kt * 128:(kt + 1) * 128], Q_[m * 64:(m + 1) * 64, :])
        emit_s(0)
        emit_s(1)
        emit_s(2)
        for i, (m, kt) in enumerate(steps):
            if i + 3 < len(steps):
                emit_s(i + 3)
            sp_ = spb[(pi + i) % 4]
            p_ = pT[(pi + i) % 4]
            ei = kt % 2
            eng = "dve" if ei == 0 else "pool"
            acc = accs[m][ei]
            P.act(p_, sp_, AF.Exp)
            P.mm(obk[m], V_[:, kt, 0:128], p_, start=(kt == 0), stop=(kt == NKT - 1))
            if kt < 2:
                P.copy(eng, acc, p_)
            else:
                P.tt(eng, acc, acc, p_, ALU.add)
            if kt == NKT - 1:
                ax = aux[m]
                P.mmgroup(ax, [(ones_f, accs[m][0]), (ones_f, accs[m][1])])
                P.recip(rcp, ax)
                if m == 0:
                    P.tt("dve", o0, obk[m], rcp, ALU.mult)
                else:
                    P.tt("dve", o1, obk[m], rcp, ALU.mult)
                    P.stt("dve", o0, o1, neglam, o0, ALU.mult, ALU.add)
        pi += len(steps)
        P.act(osq, o0, AF.Square)
        ax = aux[0]
        P.mm(ax, ones_f, osq)
        P.ts("dve", rcp, ax, 1.0 / 128, EPS, op0=ALU.mult, op1=ALU.add)
        P.rsqrt(rcp)
        o_ = oT[gi % 2]
        P.stt("dve", o_, o0, subg_fm[:, 0:1], rcp, ALU.mult, ALU.mult)
        P.dma("sp", Od[:, h, qb * 512:(qb + 1) * 512], o_)
    P.close_scope()
    if stop_after == "P6":
        return _finish(P, nc, [Od])

    P.open_scope()
    BLK = min(1024, OWN)
    NB = OWN // BLK
    NH = BLK // 512
    NT7 = BLK // 128
    wo1 = P.sb("wo1", [128, 8, D], BF16)
    for k in range(8):
        P.dma("pool", wo1[:, k, :], w_out1[k * 128:(k + 1) * 128, :])
    wr_sb = P.sb("wr_sb", [128, 8, 8], F32)
    P.dma("sp", wr_sb, wr.re("(k p) e -> p k e", p=128))
    selm = P.sb("selm", [8, 1024], F32)
    P.dma("sp", selm, selmat)
    o7 = P.sb("o7", [128, 8, BLK], BF16)
    xa = P.sb("xa", [128, 8, BLK], F32)
    xb7 = P.sb("xb7", [128, 8, 128], F32)
    sq7 = P.sb("sq7", [128, 8, 512], F32)
    rstd7 = P.sb("rstd7", [128, 512], F32)
    h7 = P.sb("h7", [128, 8, BLK], BF16)
    h7f = sq7
    yacc = P.sb("yacc", [128, 8, BLK], F32)
    lg = P.sb("lg", [128, 8], F32)
    lg2 = P.sb("lg2", [128, 8], F32)
    eq1 = P.sb("eq1", [128, 8], F32)
    eq2 = P.sb("eq2", [128, 8], F32)
    mx = P.sb("mx", [128, 8], F32)
    comb = P.sb("comb", [128, 8], F32)
    combT = P.sb("combT", [8, BLK], F32)
    cbc = [P.sb("cbc%d" % i, [128, BLK], BF16) for i in range(2)]
    FG = 2
    NFG = FEXP // (128 * FG)
    FW = 128 * FG
    wge = [P.sb("wge%d" % i, [128, 8, FW], BF16) for i in range(2)]
    wue = [P.sb("wue%d" % i, [128, 8, FW], BF16) for i in range(2)]
    wde = [P.sb("wde%d" % i, [128, FG, D], BF16) for i in range(2)]
    a7 = [P.sb("a7_%d" % i, [128, FG, BLK], BF16) for i in range(2)]
    sg7 = [P.sb("sg7_%d" % i, [128, 512], F32) for i in range(2)]
    t7 = [P.sb("t7_%d" % i, [128, 512], F32) for i in range(2)]
    its = [(nb, e, fg) for nb in range(NB) for e in range(NEXP) for fg in range(NFG)]

    def issue_w(ii):
        nb_, e_, fg_ = its[ii]
        b_ = ii % 2
        f0_ = fg_ * FW
        P.dma("pool", wge[b_], eg[e_, :, f0_:f0_ + FW].re("(k p) f -> p k f", p=128))
        P.dma("pool", wue[b_], eu[e_, :, f0_:f0_ + FW].re("(k p) f -> p k f", p=128))
        P.dma("pool", wde[b_], ed[e_, f0_:f0_ + FW, :].re("(f p) d -> p f d", p=128))
    issue_w(0)
    wi = 0
    for nb in range(NB):
        P.dma("sp", o7, Od[:, :, nb * BLK:(nb + 1) * BLK])
        for t in range(NT7):
            chA = 2 + (nb * BLK) // 128 + t
            P.dma("sp", xa[:, :, t * 128:(t + 1) * 128], x_l1[chA])
            if L > OWN:
                P.dma("sp", xb7, x_l1[chA + OWN // 128])
                P.ts("pool", xa[:, :, t * 128:(t + 1) * 128], xa[:, :, t * 128:(t + 1) * 128], sel_sb[:, 0:1], None, op0=ALU.mult)
                P.stt("pool", xa[:, :, t * 128:(t + 1) * 128], xb7, sel_sb[:, 1:2], xa[:, :, t * 128:(t + 1) * 128], ALU.mult, ALU.add)
        for hf in range(NH):
            cs = slice(hf * 512, (hf + 1) * 512)
            for dc in range(8):
                pb = P.bank()
                P.mmgroup(pb, [(wo1[:, hh, dc * 128:(dc + 1) * 128], o7[:, hh, cs]) for hh in range(8)])
                P.stt("dve", xa[:, dc, cs], pb, G[1][:, dc, 0, 0:1], xa[:, dc, cs], ALU.mult, ALU.add)
            P.act(sq7, xa[:, :, cs], AF.Square)
            pb = P.bank()
            P.mmgroup(pb, [(ones_f, sq7[:, k, :]) for k in range(8)])
            P.ts("dve", rstd7, pb, 1.0 / D, EPS, op0=ALU.mult, op1=ALU.add)
            P.rsqrt(rstd7)
            P.tt("dve", sq7, xa[:, :, cs], bc(rstd7, [(0, 8), (1, 512)]), ALU.mult)
            A2 = AB[1][:, :, 0, 1, :]
            for k in range(8):
                P.ts("pool", h7f[:, k, :], sq7[:, k, :], A2[:, k, 0:1], A2[:, k, 1:2], op0=ALU.mult, op1=ALU.add)
                P.copy("act", h7[:, k, cs], h7f[:, k, :])
            for t4 in range(4):
                pb = P.bank()
                P.mmgroup(pb[:, 0:8], [(h7f[:, k, t4 * 128:(t4 + 1) * 128], wr_sb[:, k, :]) for k in range(8)])
                P.copy("dve", lg, pb[:, 0:8])
                P.reduce("dve", mx[:, 0:1], lg, ALU.max)
                P.ts("dve", eq1, lg, mx[:, 0:1], None, op0=ALU.is_equal)
                P.stt("dve", lg2, eq1, -1e30, lg, ALU.mult, ALU.add)
                P.reduce("dve", mx[:, 1:2], lg2, ALU.max)
                P.ts("dve", eq2, lg2, mx[:, 1:2], None, op0=ALU.is_equal)
                P.tt("dve", mx[:, 2:3], mx[:, 1:2], mx[:, 0:1], ALU.subtract)
                P.act(mx[:, 3:4], mx[:, 2:3], AF.Exp)
                P.ts("dve", mx[:, 4:5], mx[:, 3:4], 1.0, None, op0=ALU.add)
                P.recip(mx[:, 5:6], mx[:, 4:5])
                P.tt("dve", mx[:, 6:7], mx[:, 3:4], mx[:, 5:6], ALU.mult)
                P.ts("dve", comb, eq1, mx[:, 5:6], None, op0=ALU.mult)
                P.stt("dve", comb, eq2, mx[:, 6:7], comb, ALU.mult, ALU.add)
                pb = P.bank()
                P.transpose(pb[0:8, 0:128], comb, ident_f)
                P.copy("dve", combT[:, hf * 512 + t4 * 128:hf * 512 + (t4 + 1) * 128], pb[0:8, 0:128])
        P.memset("pool", yacc, 0.0)
        for e in range(NEXP):
            cb = cbc[e % 2]
            for hf in range(NH):
                cs = slice(hf * 512, (hf + 1) * 512)
                pb = P.bank()
                P.mm(pb, selm[:, e * 128:(e + 1) * 128], combT[:, cs])
                P.copy("act", cb[:, cs], pb)
            for fg in range(NFG):
                b = wi % 2
                wi += 1
                if wi < len(its):
                    issue_w(wi)
                aa = a7[b]
                ii = 0
                for f in range(FG):
                    for hf in range(NH):
                        cs = slice(hf * 512, (hf + 1) * 512)
                        pg = P.bank()
                        pu = P.bank()
                        P.mmgroup(pg, [(wge[b][:, k, f * 128:(f + 1) * 128], h7[:, k, cs]) for k in range(8)])
                        P.mmgroup(pu, [(wue[b][:, k, f * 128:(f + 1) * 128], h7[:, k, cs]) for k in range(8)])
                        sg = sg7[ii % 2]
                        tt_ = t7[ii % 2]
                        ii += 1
                        P.act(sg, pg, AF.Silu)
                        P.tt("dve", tt_, sg, pu, ALU.mult)
                        P.tt("pool", aa[:, f, cs], tt_, cb[:, cs], ALU.mult)
                for hf in range(NH):
                    cs = slice(hf * 512, (hf + 1) * 512)
                    for dc in range(8):
                        pb = P.bank()
                        P.mmgroup(pb, [(wde[b][:, f, dc * 128:(dc + 1) * 128], aa[:, f, cs]) for f in range(FG)])
                        P.tt("dve", yacc[:, dc, cs], yacc[:, dc, cs], pb, ALU.add)
        for dc in range(8):
            P.stt("dve", yacc[:, dc, :], yacc[:, dc, :], G[1][:, dc, 0, 1:2], xa[:, dc, :], ALU.mult, ALU.add)
        P.dma("sp", outT[:, nb * BLK:(nb + 1) * BLK].re("(k p) t -> p k t", p=128), yacc)
    P.close_scope()
    return _finish(P, nc, [outT])


def P_sb_keep(P, name, shape):
    g = P.nc.sbuf_tensor(name, list(shape), F32)
    h = g.__enter__()
    P._ctx.insert(0, g)
    for i in range(len(P._scopes)):
        P._scopes[i] += 1
    return TT(h[:], Tok(name))


def _finish(P, nc, outs):
    P.fence("sp", outs)
    P.emit()
    P.close()
    return nc


def fm_vec(v):
    v = np.asarray(v, np.float32).reshape(-1, 128)
    return np.ascontiguousarray(v.T)


def prep_inputs(inp, L, OWN, ncores_per_batch, nbatch):
    cm, selm = host_consts()
    rcs, acs = rope_tables(L)
    shared = {
        "consts": cm, "selmat": selm, "rcs": rcs, "acs": acs,
        "w_mod0": np.ascontiguousarray(inp["even_w_mod"][0]), "w_mod1": np.ascontiguousarray(inp["odd_w_mod"][0]),
        "b_mod0": fm_vec(inp["even_b_mod"][0]), "b_mod1": fm_vec(inp["odd_b_mod"][0]),
        "norms": np.concatenate([fm_vec(inp["even_norm1"][0]), fm_vec(inp["even_norm2"][0]),
                                 fm_vec(inp["odd_norm1"][0]), fm_vec(inp["odd_norm2"][0])], axis=1),
        "w_in0": np.ascontiguousarray(inp["even_w_in"][0]),
        "w_out0": np.ascontiguousarray(inp["even_w_out"][0]),
        "ffg": np.ascontiguousarray(inp["even_ffn_gate"][0]), "ffu": np.ascontiguousarray(inp["even_ffn_up"][0]),
        "ffd": np.ascontiguousarray(inp["even_ffn_down"][0]),
        "w_in1": np.ascontiguousarray(inp["odd_w_in"][0]), "w_out1": np.ascontiguousarray(inp["odd_w_out"][0]),
        "router": np.ascontiguousarray(inp["odd_router"][0]),
        "eg": np.ascontiguousarray(inp["odd_exp_gate"][0]), "eu": np.ascontiguousarray(inp["odd_exp_up"][0]),
        "ed": np.ascontiguousarray(inp["odd_exp_down"][0]),
    }
    cw = np.concatenate([inp["even_conv_w"][0], inp["even_conv_b"][0][None, :]], axis=0)
    shared["convw"] = np.ascontiguousarray(cw.reshape(4, 12, 128).transpose(2, 1, 0))
    rowp = np.concatenate([
        inp["even_dt_bias"][0].reshape(-1), inp["even_a_log"][0].reshape(-1), inp["even_ret_decay"][0].reshape(-1),
        inp["even_d"][0].reshape(-1), inp["even_ssd_norm"][0].reshape(-1), inp["odd_q_norm"][0].reshape(-1),
        inp["odd_k_norm"][0].reshape(-1), inp["odd_lambda"][0].reshape(-1), inp["odd_subln"][0].reshape(-1)]).astype(np.float32)
    rp = np.zeros((1, 2560), np.float32)
    rp[0, :rowp.size] = rowp
    shared["rowp"] = rp
    maps = []
    for b in range(nbatch):
        xT = np.zeros((D, L + 2), np.float32)
        xT[:, 1:L + 1] = inp["x"][b].T
        cT = np.zeros((D, CTX + 2), np.float32)
        cT[:, 1:CTX + 1] = inp["ctx"][b].T
        cvec = np.concatenate([fm_vec(inp["c"][b]), fm_vec(inp["c_ctx"])], axis=1)
        for hf in range(ncores_per_batch):
            s = np.zeros((128, 2), np.float32)
            s[:, hf] = 1.0
            m = dict(shared)
            m.update({"xT": xT, "ctxT": cT, "cvec": cvec, "sel": s})
            maps.append(m)
    return maps


_NC_CACHE = {}


def kernel(**inputs):
    inp = {k: np.asarray(v) for k, v in inputs.items()}
    B, L, _ = inp["x"].shape
    OWN = L // 2
    key = (L, OWN)
    if key not in _NC_CACHE:
        _NC_CACHE[key] = build(L, OWN)
    nc = _NC_CACHE[key]
    maps = prep_inputs(inp, L, OWN, 2, B)
    res = run_bass_kernel_spmd(nc, maps, core_ids=list(range(len(maps))))
    out = np.empty((B, L, D), np.float32)
    for b in range(B):
        for hf in range(2):
            out[b, hf * OWN:(hf + 1) * OWN, :] = res.results[b * 2 + hf]["outT"].T
    return out
```

```python
import math
import numpy as np
import ml_dtypes
import concourse.bass as bass
import concourse.mybir as mybir
from concourse.bass_utils import run_bass_kernel_spmd

F32 = mybir.dt.float32
BF16 = mybir.dt.bfloat16
AF = mybir.ActivationFunctionType
ALU = mybir.AluOpType
AX = mybir.AxisListType

SAME_ENGINE_SYNC = True
NSLOT = 10


class Tok:
    __slots__ = ("lw", "rd", "name")

    def __init__(self, name=""):
        self.lw = None
        self.rd = []
        self.name = name


class TT:
    __slots__ = ("ap", "tok")

    def __init__(self, ap, tok):
        self.ap = ap
        self.tok = tok

    def __getitem__(self, k):
        return TT(self.ap[k], self.tok)

    def re(self, s, **kw):
        return TT(self.ap.rearrange(s, **kw), self.tok)

    @property
    def shape(self):
        return self.ap.shape


def bc(tt, dims):
    a = tt.ap
    base = list(a.ap)
    return TT(bass.AP(a.tensor, a.offset, [list(base[0])] + [list(d) for d in dims]), tt.tok)


class Op:
    __slots__ = ("stream", "fn", "deps", "dma", "ms", "slot", "val", "didx")

    def __init__(self, stream, fn, dma):
        self.stream = stream
        self.fn = fn
        self.deps = []
        self.dma = dma
        self.ms = False
        self.slot = None
        self.val = None
        self.didx = None


class Prog:
    STREAMS = ("pe", "act", "dve", "pool", "sp")

    def __init__(self, nc):
        self.nc = nc
        self.ops = {s: [] for s in self.STREAMS}
        self.ndma = {s: 0 for s in self.STREAMS}
        self.dmaops = {s: [] for s in self.STREAMS}
        self._ctx = []
        self._scopes = []
        self.banks = []
        self.bank_i = 0

    def sb(self, name, shape, dt=F32):
        g = self.nc.sbuf_tensor(name, list(shape), dt)
        h = g.__enter__()
        self._ctx.append(g)
        return TT(h[:], Tok(name))

    def ps(self, name, shape, dt=F32):
        g = self.nc.psum_tensor(name, list(shape), dt)
        h = g.__enter__()
        self._ctx.append(g)
        return TT(h[:], Tok(name))

    def dram(self, name, shape, dt=F32, kind="Internal"):
        h = self.nc.dram_tensor(name, list(shape), dt, kind=kind)
        return TT(h.ap(), Tok(name))

    def open_scope(self):
        self._scopes.append(len(self._ctx))

    def close_scope(self):
        n = self._scopes.pop()
        self.barrier()
        while len(self._ctx) > n:
            self._ctx.pop().__exit__(None, None, None)

    def close(self):
        while self._ctx:
            self._ctx.pop().__exit__(None, None, None)

    def bank(self):
        b = self.banks[self.bank_i % len(self.banks)]
        self.bank_i += 1
        return b

    def _rec(self, stream, fn, reads, writes, dma=False, extra=()):
        op = Op(stream, fn, dma)
        deps = list(extra)
        for t in reads:
            if t.tok.lw is not None:
                deps.append(t.tok.lw)
        for t in writes:
            if t.tok.lw is not None:
                deps.append(t.tok.lw)
            deps.extend(t.tok.rd)
        if dma:
            op.didx = self.ndma[stream]
            self.ndma[stream] += 1
            self.dmaops[stream].append(op)
            if op.didx >= NSLOT:
                deps.append(self.dmaops[stream][op.didx - NSLOT])
        seen = set()
        for d in deps:
            if d is op or id(d) in seen:
                continue
            if (not d.dma) and d.stream == stream and (stream == "pe" or not SAME_ENGINE_SYNC):
                continue
            seen.add(id(d))
            op.deps.append(d)
            d.ms = True
        for t in reads:
            t.tok.rd.append(op)
        for t in writes:
            t.tok.lw = op
            t.tok.rd = []
        self.ops[stream].append(op)
        return op

    def barrier(self):
        last = []
        for s in self.STREAMS:
            if self.ops[s]:
                for o in reversed(self.ops[s]):
                    if not o.dma and o.fn is not None:
                        last.append(o)
                        break
            last.extend(self.dmaops[s][-NSLOT:])
        for s in self.STREAMS:
            self._rec(s, None, [], [], extra=last)

    def mm(self, out, lhsT, rhs, start=True, stop=True):
        self._rec("pe", lambda e: e.matmul(out.ap, lhsT.ap, rhs.ap, start=start, stop=stop), [lhsT, rhs], [out])

    def mmgroup(self, out, pairs):
        rd = []
        for l, r in pairs:
            rd += [l, r]
        n = len(pairs)

        def fn(e):
            ins = None
            for i, (l, r) in enumerate(pairs):
                ins = e.matmul(out.ap, l.ap, r.ap, start=(i == 0), stop=(i == n - 1))
            return ins
        self._rec("pe", fn, rd, [out])

    def transpose(self, out, in_, ident):
        self._rec("pe", lambda e: e.transpose(out.ap, in_.ap, ident.ap), [in_, ident], [out])

    def act(self, out, in_, func, bias=None, scale=1.0, accum=None):
        rd = [in_]
        kw = {}
        if bias is not None:
            if isinstance(bias, TT):
                rd.append(bias)
                kw["bias"] = bias.ap
            else:
                kw["bias"] = bias
        if isinstance(scale, TT):
            rd.append(scale)
            kw["scale"] = scale.ap
        else:
            kw["scale"] = scale
        wr = [out]
        if accum is not None:
            wr.append(accum)
            kw["accum_out"] = accum.ap
        self._rec("act", lambda e: e.activation(out.ap, in_.ap, func, **kw), rd, wr)

    def tt(self, eng, out, a, b, op):
        self._rec(eng, lambda e: e.tensor_tensor(out.ap, a.ap, b.ap, op), [a, b], [out])

    def ts(self, eng, out, a, s1, s2=None, op0=ALU.mult, op1=None):
        rd = [a]
        s1a = s1.ap if isinstance(s1, TT) else s1
        s2a = s2.ap if isinstance(s2, TT) else s2
        if isinstance(s1, TT):
            rd.append(s1)
        if isinstance(s2, TT):
            rd.append(s2)
        kw = {}
        if op1 is not None:
            kw["op1"] = op1
        self._rec(eng, lambda e: e.tensor_scalar(out.ap, a.ap, s1a, s2a, op0, **kw), rd, [out])

    def stt(self, eng, out, a, s, b, op0, op1):
        rd = [a, b]
        sa = s.ap if isinstance(s, TT) else s
        if isinstance(s, TT):
            rd.append(s)
        self._rec("dve", lambda e: e.scalar_tensor_tensor(out.ap, a.ap, sa, b.ap, op0, op1), rd, [out])

    def copy(self, eng, out, a):
        if eng == "act":
            self._rec(eng, lambda e: e.copy(out.ap, a.ap), [a], [out])
        else:
            self._rec(eng, lambda e: e.tensor_copy(out.ap, a.ap), [a], [out])

    def memset(self, eng, out, val):
        self._rec(eng, lambda e: e.memset(out.ap, val), [], [out])

    def reduce(self, eng, out, a, op, axis=AX.X):
        self._rec(eng, lambda e: e.tensor_reduce(out.ap, a.ap, axis, op), [a], [out])

    def rsqrt(self, x):
        self._rec("act", lambda e: e.activation(x.ap, x.ap, AF.Sqrt), [x], [x])
        self._rec("dve", lambda e: e.reciprocal(x.ap, x.ap), [x], [x])

    def recip(self, out, a):
        self._rec("dve", lambda e: e.reciprocal(out.ap, a.ap), [a], [out])

    def dma(self, q, out, in_):
        self._rec(q, lambda e: e.dma_start(out.ap, in_.ap), [in_], [out], dma=True)

    def fence(self, stream, tts):
        self._rec(stream, None, list(tts), [])

    def emit(self):
        nc = self.nc
        for s in self.STREAMS:
            k = 0
            for op in self.ops[s]:
                if op.dma:
                    op.slot = op.didx % NSLOT
                    op.val = 16 * (op.didx // NSLOT + 1)
                elif op.ms:
                    k += 1
                    op.val = k
        sem_ctx = []
        csem = {}
        dsem = {}
        for s in self.STREAMS:
            g = nc.semaphore("c_" + s)
            csem[s] = g.__enter__()
            sem_ctx.append(g)
            if self.ndma[s] > 0:
                for i in range(NSLOT):
                    g = nc.semaphore("d_%s_%d" % (s, i))
                    dsem[(s, i)] = g.__enter__()
                    sem_ctx.append(g)
        ops = self.ops

        def run(stream, e):
            waited = {}
            for op in ops[stream]:
                for d in op.deps:
                    if d.dma:
                        key = ("d", d.stream, d.slot)
                        sem = dsem[(d.stream, d.slot)]
                    else:
                        key = ("c", d.stream)
                        sem = csem[d.stream]
                    if waited.get(key, 0) < d.val:
                        e.wait_ge(sem, d.val)
                        waited[key] = d.val
                if op.fn is None:
                    continue
                ins = op.fn(e)
                if op.dma:
                    ins.then_inc(dsem[(stream, op.slot)], 16)
                elif op.ms:
                    ins.then_inc(csem[stream], 1)

        with nc.Block() as block:
            @block.sync
            def _(e):
                run("sp", e)

            @block.tensor
            def _(e):
                run("pe", e)

            @block.scalar
            def _(e):
                run("act", e)

            @block.vector
            def _(e):
                run("dve", e)

            @block.gpsimd
            def _(e):
                run("pool", e)
        for g in reversed(sem_ctx):
            g.__exit__(None, None, None)


D = 1024
KC = 8
EPS = 1e-6
EVEN_IN = 5664
FFN_DENSE = 2816
NEXP = 8
FEXP = 3584
CTX = 256
GRID_W = 64
LAM_INIT = 0.8 - 0.6 * math.exp(-0.3 * 1)
C_Z, C_XBC, C_DT, C_RQ, C_RK, C_RV, C_RG = 0, 1024, 2560, 2592, 3104, 3616, 4640


def host_consts():
    k = np.arange(128)[:, None]
    l = np.arange(128)[None, :]
    c = {}
    c["ident"] = (k == l).astype(np.float32)
    c["le"] = (k <= l).astype(np.float32)
    c["gt"] = (k > l).astype(np.float32)
    c["ge"] = (k >= l).astype(np.float32)
    c["lt"] = (k < l).astype(np.float32)
    c["ones"] = np.ones((128, 128), np.float32)
    cm = np.concatenate([c[n] for n in ("ident", "le", "gt", "ge", "lt", "ones")], axis=1)
    sel = np.zeros((8, 8, 128), np.float32)
    for e in range(8):
        sel[e, e, :] = 1.0
    return cm, sel.reshape(8, 1024)


def rope_tables(L):
    f32 = np.float32
    inv = (np.float32(10000.0) ** (-np.arange(64, dtype=f32) / f32(64))).astype(f32)
    pos = np.arange(CTX + L, dtype=f32)
    ang = (pos[:, None] * inv[None, :]).astype(f32)
    rcs = np.concatenate([np.cos(ang), np.sin(ang)], axis=1).astype(f32)
    inv16 = (np.float32(10000.0) ** (-np.arange(16, dtype=f32) / f32(16))).astype(f32)
    t = np.arange(L)
    row = (t // GRID_W).astype(f32)
    col = (t % GRID_W).astype(f32)
    ar = (row[:, None] * inv16[None, :]).astype(f32)
    ac = (col[:, None] * inv16[None, :]).astype(f32)
    cos = np.concatenate([np.cos(ar), np.cos(ar), np.cos(ac), np.cos(ac)], axis=1)
    sins = np.concatenate([-np.sin(ar), np.sin(ar), -np.sin(ac), np.sin(ac)], axis=1)
    acs = np.concatenate([cos, sins], axis=1).astype(f32)
    return rcs, acs


def build(L=8192, OWN=4096, stop_after=None, dbg=()):
    nc = bass.Bass("TRN2", target_bir_lowering=False)
    P = Prog(nc)
    T = CTX + L
    NCH = T // 128
    NLB = L // 256
    dbg = set(dbg)

    def din(name, shape, dt=F32):
        return P.dram(name, shape, dt, kind="ExternalInput")

    xT = din("xT", [D, L + 2])
    cT = din("ctxT", [D, CTX + 2])
    cvec = din("cvec", [128, 16])
    sel = din("sel", [128, 2])
    consts = din("consts", [128, 768])
    selmat = din("selmat", [8, 1024])
    rcs_d = din("rcs", [T, 128])
    acs_d = din("acs", [L, 128])
    w_mod = [din("w_mod0", [D, 6 * D]), din("w_mod1", [D, 6 * D])]
    b_mod = [din("b_mod0", [128, 48]), din("b_mod1", [128, 48])]
    norms = din("norms", [128, 32])
    w_in0 = din("w_in0", [D, EVEN_IN])
    convw = din("convw", [128, 12, 4])
    rowp = din("rowp", [1, 2560])
    w_out0 = din("w_out0", [2048, D])
    ffg = din("ffg", [D, FFN_DENSE])
    ffu = din("ffu", [D, FFN_DENSE])
    ffd = din("ffd", [FFN_DENSE, D])
    w_in1 = din("w_in1", [D, 3072])
    w_out1 = din("w_out1", [D, D])
    wr = din("router", [D, NEXP])
    eg = din("eg", [NEXP, D, FEXP])
    eu = din("eu", [NEXP, D, FEXP])
    ed = din("ed", [NEXP, FEXP, D])
    outT = P.dram("outT", [D, OWN], F32, kind="ExternalOutput")

    def scratch(name, shape, dt=F32):
        return P.dram(name, shape, dt, kind=("ExternalOutput" if name in dbg else "Internal"))

    r_zr = scratch("r_zr", [NCH, 128, 2048], BF16)
    r_xv = scratch("r_xv", [NCH, 128, 2048], BF16)
    r_kt = scratch("r_kt", [NCH, 128, 768], BF16)
    r_fm = scratch("r_fm", [NCH, 128, 12, 128], BF16)
    r_dt = scratch("r_dt", [NCH, 128, 64], F32)
    r_yf = scratch("r_yf", [NCH, 128, 2048], F32)
    x_mid = scratch("x_mid", [NCH, 128, 8, 128], F32)
    x_l1 = scratch("x_l1", [NCH, 128, 8, 128], F32)
    Kd = scratch("Kd", [8, 128, T], BF16)
    Vd = scratch("Vd", [8, 128, NCH, 130], BF16)
    Qd = scratch("Qd", [8, 128, L], BF16)
    Od = scratch("Od", [128, 8, OWN], BF16)

    cst = P.sb("cst", [128, 768], F32)
    P.dma("sp", cst, consts)
    ident_f = cst[:, 0:128]
    m_le, m_gt, m_ge, m_lt, ones_f = (cst[:, 128 * i:128 * (i + 1)] for i in range(1, 6))
    cstb = P.sb("cstb", [128, 768], BF16)
    P.copy("dve", cstb, cst)
    ident_b = cstb[:, 0:128]
    sel_sb = P.sb("sel_sb", [128, 2], F32)
    P.dma("sp", sel_sb, sel)
    rows = P.sb("rows", [128, 2560], F32)
    P.dma("sp", rows, TT(rowp.ap.rearrange("a b -> (a b)").partition_broadcast(128), rowp.tok))
    norm_sb = P.sb("norm_sb", [128, 32], F32)
    P.dma("sp", norm_sb, norms)
    modfm = [P.sb("modfm0", [128, 48, 2], F32), P.sb("modfm1", [128, 48, 2], F32)]
    P.banks = [P.ps("bank%d" % i, [128, 512], F32) for i in range(8)]

    def bank_bf(b):
        a = b.ap
        return TT(a.bitcast(BF16), b.tok)

    AB = [P.sb("AB%d" % i, [128, 8, 2, 2, 2]) for i in range(2)]
    G = [P.sb("G%d" % i, [128, 8, 2, 2]) for i in range(2)]
    P.open_scope()
    cv = P.sb("cv", [128, 16], F32)
    P.dma("sp", cv, cvec)
    scv = P.sb("scv", [128, 8, 2], F32)
    P.act(scv[:, :, 0], cv[:, 0:8], AF.Silu)
    P.act(scv[:, :, 1], cv[:, 8:16], AF.Silu)
    wm = [P.sb("wm%d" % i, [128, 8, 512], F32) for i in range(2)]
    bm_sb = P.sb("bm_sb", [128, 2, 48], F32)
    P.dma("sp", bm_sb[:, 0, :], b_mod[0])
    P.dma("sp", bm_sb[:, 1, :], b_mod[1])
    it = 0
    for lyr in range(2):
        for cg in range(12):
            w = wm[it % 2]
            it += 1
            P.dma("sp", w, w_mod[lyr][:, cg * 512:(cg + 1) * 512].re("(k p) f -> p k f", p=128))
            pb = P.bank()
            for j in range(4):
                P.mmgroup(pb[:, 2 * j:2 * j + 2], [(w[:, k, j * 128:(j + 1) * 128], scv[:, k, :]) for k in range(8)])
            for j in range(4):
                ch = cg * 4 + j
                P.ts("dve", modfm[lyr][:, ch, :], pb[:, 2 * j:2 * j + 2], bm_sb[:, lyr, ch:ch + 1], None, op0=ALU.add)
    for lyr in range(2):
        for w in range(2):
            for n in range(2):
                shift = modfm[lyr][:, (3 * n) * 8:(3 * n) * 8 + 8, w]
                scale = modfm[lyr][:, (3 * n + 1) * 8:(3 * n + 1) * 8 + 8, w]
                gate = modfm[lyr][:, (3 * n + 2) * 8:(3 * n + 2) * 8 + 8, w]
                gain = norm_sb[:, (2 * lyr + n) * 8:(2 * lyr + n) * 8 + 8]
                P.stt("dve", AB[lyr][:, :, w, n, 0], scale, 1.0, gain, ALU.add, ALU.mult)
                P.copy("dve", AB[lyr][:, :, w, n, 1], shift)
                P.copy("dve", G[lyr][:, :, w, n], gate)
    P.close_scope()

    def rms_modulate(xin, ncol, Atab, out_bf, tmp, out_f32=None, eng="pool"):
        sq = tmp["sq"]
        P.act(sq[:, :, 0:ncol], xin, AF.Square)
        pb = P.bank()
        P.mmgroup(pb[:, 0:ncol], [(ones_f, sq[:, k, 0:ncol]) for k in range(8)])
        rstd = tmp["rstd"]
        P.ts("dve", rstd[:, 0:ncol], pb[:, 0:ncol], 1.0 / D, EPS, op0=ALU.mult, op1=ALU.add)
        P.rsqrt(rstd[:, 0:ncol])
        P.tt("dve", sq[:, :, 0:ncol], xin, bc(rstd[:, 0:ncol], [(0, 8), (1, ncol)]), ALU.mult)
        for k in range(8):
            if eng == "act" or (eng == "mix" and k % 2 == 0):
                P.act(out_bf[:, k, :], sq[:, k, 0:ncol], AF.Identity, bias=Atab[:, k, 1:2], scale=Atab[:, k, 0:1])
            else:
                P.ts("pool", out_bf[:, k, :], sq[:, k, 0:ncol], Atab[:, k, 0:1], Atab[:, k, 1:2], op0=ALU.mult, op1=ALU.add)
            if out_f32 is not None:
                P.ts("pool", out_f32[:, k, :], sq[:, k, 0:ncol], Atab[:, k, 0:1], Atab[:, k, 1:2], op0=ALU.mult, op1=ALU.add)

    r_dtb = rows[:, 0:32]
    r_alog = rows[:, 32:64]
    r_retd = rows[:, 64:72]
    r_dsk = rows[:, 72:88]
    r_ssdn = rows[:, 88:1112]
    r_qn = rows[:, 1112:1176]
    r_kn = rows[:, 1176:1240]
    r_lam = rows[:, 1240:1496]
    r_subln = rows[:, 1496:1624]
    ea = P.sb("ea", [128, 32], F32)
    P.act(ea, r_alog, AF.Exp)
    nla_ret = P.sb("nla_ret", [128, 8], F32)
    P.act(nla_ret, r_retd, AF.Exp)
    P.ts("dve", nla_ret, nla_ret, -1.0, None, op0=ALU.mult)

    P.open_scope()
    w0 = P.sb("w0", [128, 8, EVEN_IN], BF16)
    for k in range(8):
        for c0 in range(0, EVEN_IN, 1888):
            P.dma("pool", w0[:, k, c0:c0 + 1888], w_in0[k * 128:(k + 1) * 128, c0:c0 + 1888])
    cw = P.sb("cw", [128, 12, 4], F32)
    P.dma("sp", cw, convw)
    xin = [P.sb("xin%d" % i, [128, 8, 258], F32) for i in range(2)]
    xbr = P.sb("xbr", [128, 12, 258], F32)
    tmp1 = {"sq": xbr[:, 0:8, :], "rstd": P.sb("rstd1", [128, 258], F32)}
    hbs = [P.sb("hb%d" % i, [128, 8, 258], BF16) for i in range(2)]
    xbcs = [P.sb("xbc%d" % i, [128, 12, 256], BF16) for i in range(2)]
    cvts = [P.sb("cvt%d" % i, [128, 256], F32) for i in range(2)]
    o_zr = P.sb("o_zr", [128, 2, 2048], BF16)
    o_xv = P.sb("o_xv", [128, 2, 2048], BF16)
    o_kt = P.sb("o_kt", [128, 2, 768], BF16)
    o_fm = P.sb("o_fm", [128, 2, 12, 128], BF16)
    o_dt = P.sb("o_dt", [128, 2, 64], F32)
    rtabs = [P.sb("rtab%d" % i, [128, 2, 128], F32) for i in range(3)]
    rt1 = P.sb("rt1", [128, 4, 128], F32)
    rt2 = P.sb("rt2", [128, 4, 128], F32)
    rqk = P.sb("rqk", [128, 2, 512], BF16)
    sp1 = P.sb("sp1", [128, 32], F32)
    sp2 = P.sb("sp2", [128, 32], F32)

    blocks = [("c", 0)] + [("l", i) for i in range(NLB)]

    def binfo(bj):
        kind_, i_ = blocks[bj]
        if kind_ == "c":
            return 0, True, True, 1
        return CTX + i_ * 256, (i_ == 0), (i_ == NLB - 1), 0

    def load1(bj):
        kind_, i_ = blocks[bj]
        if kind_ == "c":
            P.dma("sp", xin[bj % 2], cT.re("(k p) t -> p k t", p=128))
        else:
            P.dma("sp", xin[bj % 2], xT[:, i_ * 256:i_ * 256 + 258].re("(k p) t -> p k t", p=128))
        t0_ = binfo(bj)[0]
        P.dma("sp", rtabs[bj % 3], rcs_d[t0_:t0_ + 256, :].re("(t p) c -> p t c", p=128))

    def stageA(bj):
        tok0, first, last, which = binfo(bj)
        xi, hb, xbc = xin[bj % 2], hbs[bj % 2], xbcs[bj % 2]
        rms_modulate(xi, 258, AB[0][:, :, which, 0, :], hb, tmp1, eng="act")
        for c in range(12):
            pb = P.bank()
            P.mmgroup(pb[:, 0:258], [(w0[:, k, C_XBC + c * 128:C_XBC + (c + 1) * 128], hb[:, k, :]) for k in range(8)])
            P.copy("act", xbr[:, c, :], pb[:, 0:258])
        if first:
            P.memset("pool", xbr[:, :, 0:1], 0.0)
        if last:
            P.memset("pool", xbr[:, :, 257:258], 0.0)
        for c in range(12):
            cvt = cvts[c % 2]
            P.ts("pool", cvt, xbr[:, c, 0:256], cw[:, c, 0:1], None, op0=ALU.mult)
            P.stt("dve", cvt, xbr[:, c, 1:257], cw[:, c, 1:2], cvt, ALU.mult, ALU.add)
            P.stt("dve", cvt, xbr[:, c, 2:258], cw[:, c, 2:3], cvt, ALU.mult, ALU.add)
            P.act(xbc[:, c, :], cvt, AF.Silu, bias=cw[:, c, 3:4])

    def stageB(bj):
        tok0, first, last, which = binfo(bj)
        hb, xbc, rtab = hbs[bj % 2], xbcs[bj % 2], rtabs[bj % 3]
        ch0 = tok0 // 128
        for t in range(2):
            P.copy("pool", o_fm[:, t, 0:4, :], xbc[:, 8:12, t * 128:(t + 1) * 128])
        for t in range(2):
            lt = [hb[:, k, 1 + t * 128:1 + (t + 1) * 128] for k in range(8)]

            def proj(c0, n):
                pb = P.bank()
                P.mmgroup(pb[:, 0:n], [(lt[k], w0[:, k, c0:c0 + n]) for k in range(8)])
                return pb
            for j in range(2):
                pb = proj(C_Z + j * 512, 512)
                P.copy("act", o_zr[:, t, j * 512:(j + 1) * 512], pb)
            for j in range(2):
                pb = proj(C_RG + j * 512, 512)
                P.copy("act", o_zr[:, t, 1024 + j * 512:1024 + (j + 1) * 512], pb)
            for j in range(2):
                pb = proj(C_RV + j * 512, 512)
                P.copy("act", o_xv[:, t, 1024 + j * 512:1024 + (j + 1) * 512], pb)
            pb = proj(C_DT, 32)
            P.tt("dve", sp1, pb[:, 0:32], r_dtb, ALU.add)
            P.act(sp2, sp1, AF.Abs)
            P.act(sp2, sp2, AF.Exp, scale=-1.0)
            P.act(sp2, sp2, AF.Ln, bias=1.0)
            P.stt("dve", o_dt[:, t, 0:32], sp1, 0.0, sp2, ALU.max, ALU.add)
            P.tt("dve", sp1, o_dt[:, t, 0:32], ea, ALU.mult)
            P.ts("dve", o_dt[:, t, 32:64], sp1, -1.0, None, op0=ALU.mult)
            for qi, c0 in enumerate((C_RQ, C_RK)):
                pb = proj(c0, 512)
                pv = pb.re("p (h d) -> p h d", h=4)
                cos2 = bc(rtab[:, t, 0:64], [(0, 4), (0, 2), (1, 64)])
                P.tt("dve", rt1.re("p h (a d) -> p h a d", a=2), pv.re("p h (a d) -> p h a d", a=2), cos2, ALU.mult)
                sin1 = bc(rtab[:, t, 64:128], [(0, 4), (1, 64)])
                P.tt("dve", rt2[:, :, 0:64], pv[:, :, 64:128], sin1, ALU.mult)
                P.tt("dve", rt2[:, :, 64:128], pv[:, :, 0:64], sin1, ALU.mult)
                rv = rqk[:, qi, :].re("p (h d) -> p h d", h=4)
                P.tt("pool", rt1[:, :, 0:64], rt1[:, :, 0:64], rt2[:, :, 0:64], ALU.subtract)
                P.tt("pool", rt1[:, :, 64:128], rt1[:, :, 64:128], rt2[:, :, 64:128], ALU.add)
                P.act(rv, rt1, AF.Copy, scale=(1.0 if qi == 0 else 128.0 ** -0.5))
            P.copy("pool", o_kt[:, t, 256:768], rqk[:, 1, :])
            pb = P.bank()
            pbb = bank_bf(pb)
            for qi in range(2):
                for h in range(4):
                    P.transpose(pbb[:, (qi * 4 + h) * 128:(qi * 4 + h + 1) * 128], rqk[:, qi, h * 128:(h + 1) * 128], ident_b)
            P.copy("dve", o_fm[:, t, 4:12, :], pbb.re("p (n t) -> p n t", t=128))
            pb = P.bank()
            pbb = bank_bf(pb)
            for c in range(8):
                P.transpose(pbb[:, c * 128:(c + 1) * 128], xbc[:, c, t * 128:(t + 1) * 128], ident_b)
            P.copy("dve", o_xv[:, t, 0:1024], pbb)
            pb = P.bank()
            pbb = bank_bf(pb)
            for c in range(2):
                P.transpose(pbb[:, c * 128:(c + 1) * 128], xbc[:, 8 + c, t * 128:(t + 1) * 128], ident_b)
            P.copy("dve", o_kt[:, t, 0:256], pbb[:, 0:256])
        for t in range(2):
            P.dma("sp", r_zr[ch0 + t], o_zr[:, t, :])
            P.dma("sp", r_xv[ch0 + t], o_xv[:, t, :])
            P.dma("sp", r_kt[ch0 + t], o_kt[:, t, :])
            P.dma("sp", r_fm[ch0 + t], o_fm[:, t])
            P.dma("sp", r_dt[ch0 + t], o_dt[:, t, :])

    nblk = len(blocks)
    load1(0)
    if nblk > 1:
        load1(1)
    stageA(0)
    for bi in range(nblk):
        if bi + 2 < nblk:
            load1(bi + 2)
        if bi + 1 < nblk:
            stageA(bi + 1)
        stageB(bi)
    P.close_scope()
    if stop_after == "P1":
        return _finish(P, nc, [r_zr, r_xv, r_kt, r_fm, r_dt])

    P.open_scope()
    wo0 = P.sb("wo0", [128, 16, D], BF16)
    for k in range(16):
        P.dma("pool", wo0[:, k, :], w_out0[k * 128:(k + 1) * 128, :])
    Er = P.sb("Er", [128, 2, 3, 4], F32)
    Dret = P.sb("Dret", [128, 2, 4, 128], F32)
    lmr = P.sb("lmr", [128, 4, 128], F32)
    for d in range(2):
        la = nla_ret[:, d * 4:(d + 1) * 4]
        pb = P.bank()
        mA, mT = (m_le, m_gt) if d == 0 else (m_ge, m_lt)
        P.mm(pb[:, 0:4], mA, la)
        P.mm(pb[:, 4:8], mT, la)
        P.mm(pb[:, 8:12], ones_f, la)
        P.act(Er[:, d].re("p a h -> p (a h)"), pb[:, 0:12], AF.Exp)
        mS, mR, mM = (m_gt, m_le, m_le) if d == 0 else (m_lt, m_ge, m_ge)
        P.tt("dve", lmr, bc(mS, [(0, 4), (1, 128)]), bc(la, [(1, 4), (0, 128)]), ALU.mult)
        pb = P.bank()
        for h in range(4):
            P.mm(pb[:, h * 128:(h + 1) * 128], lmr[:, h, :], mR)
        P.act(Dret[:, d].re("p h l -> p (h l)"), pb, AF.Exp)
        P.tt("dve", Dret[:, d], Dret[:, d], bc(mM, [(0, 4), (1, 128)]), ALU.mult)

    Hs = P.sb("Hs", [128, 1024], F32)
    Hr = P.sb("Hr", [128, 1024], F32)
    Hsb = P.sb("Hsb", [128, 1024], BF16)
    Hrb = P.sb("Hrb", [128, 1024], BF16)
    i_xv = [P.sb("i_xv%d" % i, [128, 2048], BF16) for i in range(2)]
    i_kt = [P.sb("i_kt%d" % i, [128, 768], BF16) for i in range(2)]
    i_fm = [P.sb("i_fm%d" % i, [128, 12, 128], BF16) for i in range(2)]
    i_dt = [P.sb("i_dt%d" % i, [128, 64], F32) for i in range(2)]
    i_zr = [P.sb("i_zr%d" % i, [128, 2048], BF16) for i in range(2)]
    i_yf = [P.sb("i_yf%d" % i, [128, 2048], F32) for i in range(2)]
    i_x = [P.sb("i_x%d" % i, [128, 8, 128], F32) for i in range(2)]
    E = P.sb("E", [128, 3, 16], F32)
    scm = P.sb("scm", [128, 2, 128], F32)
    Lm = P.sb("Lm", [128, 16, 128], F32)
    expD = P.sb("expD", [128, 16, 128], F32)
    MT = P.sb("MT", [128, 16, 128], BF16)
    MTr = P.sb("MTr", [128, 4, 128], BF16)
    xdt = P.sb("xdt", [128, 1024], BF16)
    xw = P.sb("xw", [128, 1024], BF16)
    rvw = P.sb("rvw", [128, 1024], BF16)
    wv = P.sb("wv", [128, 16], F32)
    ytmp = P.sb("ytmp", [128, 1024], F32)
    yo = [P.sb("yo%d" % i, [128, 2048], F32) for i in range(2)]
    sz = P.sb("sz", [128, 1024], F32)
    junk = P.sb("junk", [128, 1024], F32)
    ss = P.sb("ss", [128, 8], F32)
    ycat = P.sb("ycat", [128, 2048], BF16)
    ycT = P.sb("ycT", [128, 16, 128], BF16)
    xo = P.sb("xo", [128, 8, 128], F32)

    fwd_order = list(range(NCH))
    bwd_order = [1, 0] + list(range(NCH - 1, 1, -1))

    for d in range(2):
        order = fwd_order if d == 0 else bwd_order
        P.memset("dve", Hs, 0.0)
        P.memset("dve", Hr, 0.0)
        P.memset("pool", Hsb, 0.0)
        P.memset("pool", Hrb, 0.0)
        mA, mT = (m_le, m_gt) if d == 0 else (m_ge, m_lt)
        mS, mR, mM = (m_gt, m_le, m_le) if d == 0 else (m_lt, m_ge, m_ge)
        def load_sw(cj):
            ch_ = order[cj]
            b_ = cj % 2
            P.dma("sp", i_xv[b_], r_xv[ch_])
            P.dma("sp", i_kt[b_], r_kt[ch_])
            P.dma("sp", i_fm[b_], r_fm[ch_])
            P.dma("sp", i_dt[b_], r_dt[ch_])
            if d == 1:
                P.dma("sp", i_zr[b_], r_zr[ch_])
                P.dma("sp", i_yf[b_], r_yf[ch_])
                if ch_ < 2:
                    P.dma("sp", i_x[b_], cT[:, 1 + ch_ * 128:1 + (ch_ + 1) * 128].re("(k p) t -> p k t", p=128))
                else:
                    P.dma("sp", i_x[b_], xT[:, 1 + (ch_ - 2) * 128:1 + (ch_ - 1) * 128].re("(k p) t -> p k t", p=128))
        load_sw(0)
        for ci, ch in enumerate(order):
            b = ci % 2
            xv, kt, fm, dtt = i_xv[b], i_kt[b], i_fm[b], i_dt[b]
            if ci + 1 < len(order):
                load_sw(ci + 1)
            la = dtt[:, 32 + d * 16:32 + (d + 1) * 16]
            dtd = dtt[:, d * 16:(d + 1) * 16]
            xs = xv[:, 0:1024]
            rvv = xv[:, 1024:2048]
            yout = yo[ci % 2]
            pb = P.bank()
            P.mm(pb[:, 0:16], mA, la)
            P.mm(pb[:, 16:32], mT, la)
            P.mm(pb[:, 32:48], ones_f, la)
            P.act(E.re("p a h -> p (a h)"), pb[:, 0:48], AF.Exp)
            pb = P.bank()
            for g in range(2):
                P.mm(pb[:, g * 128:(g + 1) * 128], fm[:, g, :], fm[:, 2 + g, :])
            P.tt("dve", scm, pb[:, 0:256].re("p (g l) -> p g l", g=2), bc(mM, [(0, 2), (1, 128)]), ALU.mult)
            P.tt("pool", Lm, bc(mS, [(0, 16), (1, 128)]), bc(la, [(1, 16), (0, 128)]), ALU.mult)
            for q in range(4):
                pb = P.bank()
                for j in range(4):
                    P.mm(pb[:, j * 128:(j + 1) * 128], Lm[:, q * 4 + j, :], mR)
                P.act(expD[:, q * 4:(q + 1) * 4, :].re("p h l -> p (h l)"), pb, AF.Exp)
            for g in range(2):
                P.tt("pool" if g == 1 else "dve", MT[:, g * 8:(g + 1) * 8, :], expD[:, g * 8:(g + 1) * 8, :], bc(scm[:, g, :], [(0, 8), (1, 128)]), ALU.mult)
            P.tt("pool", xdt.re("p (h d) -> p h d", h=16), xs.re("p (h d) -> p h d", h=16), bc(dtd, [(1, 16), (0, 64)]), ALU.mult)
            pd = [P.bank(), P.bank()]
            for h in range(16):
                P.mm(pd[h // 8][:, (h % 8) * 64:(h % 8 + 1) * 64], MT[:, h, :], xdt[:, h * 64:(h + 1) * 64])
            for g in range(2):
                po = P.bank()
                P.mm(po, fm[:, 2 + g, :], Hsb[:, g * 512:(g + 1) * 512])
                P.tt("dve", ytmp[:, g * 512:(g + 1) * 512].re("p (h d) -> p h d", h=8), po.re("p (h d) -> p h d", h=8),
                     bc(E[:, 0, g * 8:(g + 1) * 8], [(1, 8), (0, 64)]), ALU.mult)
                P.tt("dve", yout[:, g * 512:(g + 1) * 512], ytmp[:, g * 512:(g + 1) * 512], pd[g], ALU.add)
            P.tt("dve", wv, dtd, E[:, 1, :], ALU.mult)
            P.tt("pool", xw.re("p (h d) -> p h d", h=16), xs.re("p (h d) -> p h d", h=16), bc(wv, [(1, 16), (0, 64)]), ALU.mult)
            P.tt("dve", Hs.re("p (h d) -> p h d", h=16), Hs.re("p (h d) -> p h d", h=16), bc(E[:, 2, :], [(1, 16), (0, 64)]), ALU.mult)
            for g in range(2):
                pS = P.bank()
                P.mm(pS, kt[:, g * 128:(g + 1) * 128], xw[:, g * 512:(g + 1) * 512])
                P.tt("dve", Hs[:, g * 512:(g + 1) * 512], Hs[:, g * 512:(g + 1) * 512], pS, ALU.add)
            P.copy("act", Hsb, Hs)
            pb = P.bank()
            for h in range(4):
                P.mm(pb[:, h * 128:(h + 1) * 128], fm[:, 8 + h, :], fm[:, 4 + h, :])
            P.tt("dve", MTr, pb.re("p (h l) -> p h l", h=4), Dret[:, d], ALU.mult)
            pd = [P.bank(), P.bank()]
            for h in range(4):
                P.mm(pd[h // 2][:, (h % 2) * 256:(h % 2 + 1) * 256], MTr[:, h, :], rvv[:, h * 256:(h + 1) * 256])
            for g in range(2):
                po = P.bank()
                for hh in range(2):
                    h = g * 2 + hh
                    P.mm(po[:, hh * 256:(hh + 1) * 256], fm[:, 4 + h, :], Hrb[:, h * 256:(h + 1) * 256])
                P.tt("dve", ytmp[:, g * 512:(g + 1) * 512].re("p (h d) -> p h d", h=2), po.re("p (h d) -> p h d", h=2),
                     bc(Er[:, d, 0, g * 2:(g + 1) * 2], [(1, 2), (0, 256)]), ALU.mult)
                P.tt("dve", yout[:, 1024 + g * 512:1024 + (g + 1) * 512], ytmp[:, g * 512:(g + 1) * 512], pd[g], ALU.add)
            P.tt("pool", rvw.re("p (h d) -> p h d", h=4), rvv.re("p (h d) -> p h d", h=4), bc(Er[:, d, 1, :], [(1, 4), (0, 256)]), ALU.mult)
            P.tt("dve", Hr.re("p (h d) -> p h d", h=4), Hr.re("p (h d) -> p h d", h=4), bc(Er[:, d, 2, :], [(1, 4), (0, 256)]), ALU.mult)
            for g in range(2):
                pS = P.bank()
                for hh in range(2):
                    h = g * 2 + hh
                    P.mm(pS[:, hh * 256:(hh + 1) * 256], kt[:, 256 + h * 128:256 + (h + 1) * 128], rvw[:, h * 256:(h + 1) * 256])
                P.tt("dve", Hr[:, g * 512:(g + 1) * 512], Hr[:, g * 512:(g + 1) * 512], pS, ALU.add)
            P.copy("act", Hrb, Hr)
            if d == 0:
                P.dma("sp", r_yf[ch], yout)
                continue
            zr = i_zr[b]
            which = 1 if ch < 2 else 0
            P.tt("dve", yout, yout, i_yf[b], ALU.add)
            ys = yout[:, 0:1024]
            yr = yout[:, 1024:2048]
            P.tt("pool", ytmp.re("p (h d) -> p h d", h=16), xs.re("p (h d) -> p h d", h=16), bc(r_dsk, [(1, 16), (0, 64)]), ALU.mult)
            P.tt("dve", ys, ys, ytmp, ALU.add)
            P.act(sz, zr[:, 0:1024], AF.Silu)
            P.tt("dve", ys, ys, sz, ALU.mult)
            P.act(junk, ys, AF.Square)
            P.reduce("dve", ss[:, 0:1], junk, ALU.add)
            P.ts("dve", ss[:, 1:2], ss[:, 0:1], 1.0 / 1024, EPS, op0=ALU.mult, op1=ALU.add)
            P.rsqrt(ss[:, 1:2])
            P.stt("dve", ycat[:, 0:1024], ys, ss[:, 1:2], r_ssdn, ALU.mult, ALU.mult)
            P.act(junk, yr, AF.Square)
            P.reduce("dve", ss[:, 2:6], junk.re("p (h d) -> p h d", h=4), ALU.add)
            P.ts("dve", ss[:, 2:6], ss[:, 2:6], 1.0 / 256, EPS, op0=ALU.mult, op1=ALU.add)
            P.rsqrt(ss[:, 2:6])
            P.act(sz, zr[:, 1024:2048], AF.Silu)
            P.tt("dve", yr.re("p (h d) -> p h d", h=4), yr.re("p (h d) -> p h d", h=4), bc(ss[:, 2:6], [(1, 4), (0, 256)]), ALU.mult)
            P.tt("dve", ycat[:, 1024:2048], yr, sz, ALU.mult)
            for q in range(2):
                pb = P.bank()
                pbb = bank_bf(pb)
                for j in range(8):
                    P.transpose(pbb[:, j * 128:(j + 1) * 128], ycat[:, (q * 8 + j) * 128:(q * 8 + j + 1) * 128], ident_b)
                P.copy("act", ycT[:, q * 8:(q + 1) * 8, :].re("p n t -> p (n t)"), pbb)
            for q in range(2):
                pb = P.bank()
                for j in range(4):
                    dc = q * 4 + j
                    P.mmgroup(pb[:, j * 128:(j + 1) * 128], [(wo0[:, k, dc * 128:(dc + 1) * 128], ycT[:, k, :]) for k in range(16)])
                for j in range(4):
                    dc = q * 4 + j
                    P.stt("dve", xo[:, dc, :], pb[:, j * 128:(j + 1) * 128], G[0][:, dc, which, 0:1], i_x[b][:, dc, :], ALU.mult, ALU.add)
            P.dma("sp", x_mid[ch], xo)
    P.close_scope()
    if stop_after == "P3":
        return _finish(P, nc, [x_mid, r_yf])

    P.open_scope()
    NF = FFN_DENSE // 128
    wg = P.sb("wg", [128, 8, FFN_DENSE], BF16)
    wu = P.sb("wu", [128, 8, FFN_DENSE], BF16)
    wd = P.sb("wd", [128, NF, D], BF16)
    for k in range(8):
        P.dma("pool", wg[:, k, :], ffg[k * 128:(k + 1) * 128, :])
        P.dma("pool", wu[:, k, :], ffu[k * 128:(k + 1) * 128, :])
    for f in range(NF):
        P.dma("pool", wd[:, f, :], ffd[f * 128:(f + 1) * 128, :])
    xb4 = [P.sb("xb4_%d" % i, [128, 8, 256], F32) for i in range(2)]
    tmp4 = {"sq": P.sb("sq4", [128, 8, 256], F32), "rstd": P.sb("rstd4", [128, 256], F32)}
    h4 = P.sb("h4", [128, 8, 256], BF16)
    a4 = P.sb("a4", [128, NF, 256], BF16)
    sg4 = [P.sb("sg4_%d" % i, [128, 256], F32) for i in range(2)]
    xo4 = [P.sb("xo4_%d" % i, [128, 8, 256], F32) for i in range(1)]
    def load4(bj):
        for t in range(2):
            P.dma("sp", xb4[bj % 2][:, :, t * 128:(t + 1) * 128], x_mid[bj * 2 + t])
    load4(0)
    for bi in range(NCH // 2):
        x4 = xb4[bi % 2]
        which = 1 if bi == 0 else 0
        if bi + 1 < NCH // 2:
            load4(bi + 1)
        rms_modulate(x4, 256, AB[0][:, :, which, 1, :], h4, tmp4)
        for f in range(NF):
            pb = P.bank()
            P.mmgroup(pb[:, 0:256], [(wg[:, k, f * 128:(f + 1) * 128], h4[:, k, :]) for k in range(8)])
            P.mmgroup(pb[:, 256:512], [(wu[:, k, f * 128:(f + 1) * 128], h4[:, k, :]) for k in range(8)])
            sg = sg4[f % 2]
            P.act(sg, pb[:, 0:256], AF.Silu)
            P.tt("dve", a4[:, f, :], sg, pb[:, 256:512], ALU.mult)
        xo_ = xo4[0]
        for q in range(4):
            pb = P.bank()
            for j in range(2):
                dc = q * 2 + j
                P.mmgroup(pb[:, j * 256:(j + 1) * 256], [(wd[:, f, dc * 128:(dc + 1) * 128], a4[:, f, :]) for f in range(NF)])
            for j in range(2):
                dc = q * 2 + j
                P.stt("dve", xo_[:, dc, :], pb[:, j * 256:(j + 1) * 256], G[0][:, dc, which, 1:2], x4[:, dc, :], ALU.mult, ALU.add)
        for t in range(2):
            P.dma("sp", x_l1[bi * 2 + t], xo_[:, :, t * 128:(t + 1) * 128])
    P.close_scope()
    if stop_after == "P4":
        return _finish(P, nc, [x_l1])

    P.open_scope()
    w1 = P.sb("w1", [128, 8, 3072], BF16)
    for k in range(8):
        P.dma("pool", w1[:, k, :], w_in1[k * 128:(k + 1) * 128, :])
    xb5 = [P.sb("xb5_%d" % i, [128, 8, 256], F32) for i in range(2)]
    tmp5 = {"sq": P.sb("sq5", [128, 8, 256], F32), "rstd": P.sb("rstd5", [128, 256], F32)}
    h5 = P.sb("h5", [128, 8, 256], BF16)
    atab = P.sb("atab", [128, 2, 128], F32)
    qsq = P.sb("qsq", [128, 1024], F32)
    qn = P.sb("qn", [128, 1024], F32)
    q1 = P.sb("q1", [128, 1024], F32)
    q2 = P.sb("q2", [128, 1024], F32)
    ss5 = P.sb("ss5", [128, 16], F32)
    qkb = P.sb("qkb", [128, 2, 1024], BF16)
    qkT = P.sb("qkT", [128, 2, 8, 256], BF16)
    v5 = [P.sb("v5_%d" % i, [128, 2, 8, 130], BF16) for i in range(2)]
    for i in range(2):
        P.memset("dve", v5[i], 1.0)
    qg = P.sb("qg", [128, 64], F32)
    P.ts("dve", qg, r_qn, 64.0 ** -0.5, None, op0=ALU.mult)
    atabs = [atab, P.sb("atab2", [128, 2, 128], F32), P.sb("atab3", [128, 2, 128], F32)]
    h5s = [h5, P.sb("h5b", [128, 8, 256], BF16)]
    qsqs = [qsq, P.sb("qsq_b", [128, 1024], F32)]
    qns = [qn, P.sb("qn_b", [128, 1024], F32)]
    q1s = [q1, P.sb("q1_b", [128, 1024], F32)]
    q2s = [q2, P.sb("q2_b", [128, 1024], F32)]
    ss5s = [ss5, P.sb("ss5_b", [128, 16], F32)]
    NB5 = NCH // 2

    def load5(bj):
        for t in range(2):
            P.dma("sp", xb5[bj % 2][:, :, t * 128:(t + 1) * 128], x_l1[bj * 2 + t])
        if bj > 0:
            l0 = (bj - 1) * 256
            P.dma("sp", atabs[bj % 3], acs_d[l0:l0 + 256, :].re("(t p) c -> p t c", p=128))

    def stage5A(bj):
        which = 1 if bj == 0 else 0
        rms_modulate(xb5[bj % 2], 256, AB[1][:, :, which, 0, :], h5s[bj % 2], tmp5, eng="act")

    def stage5B(bi):
        h5 = h5s[bi % 2]
        atab = atabs[bi % 3]
        which = 1 if bi == 0 else 0
        vv = v5[bi % 2]
        for t in range(2):
            lt = [h5[:, k, t * 128:(t + 1) * 128] for k in range(8)]
            for qi in range(2):
                if qi == 0 and which == 1:
                    continue
                qsq, qn, q1, q2, ss5 = qsqs[qi], qns[qi], q1s[qi], q2s[qi], ss5s[qi]
                pbs = []
                for j in range(2):
                    pb = P.bank()
                    c0 = qi * 1024 + j * 512
                    P.mmgroup(pb, [(lt[k], w1[:, k, c0:c0 + 512]) for k in range(8)])
                    P.act(qsq[:, j * 512:(j + 1) * 512], pb, AF.Square)
                    pbs.append(pb)
                P.reduce("dve", ss5, qsq.re("p (g d) -> p g d", d=64), ALU.add)
                P.ts("dve", ss5, ss5, 1.0 / 64, EPS, op0=ALU.mult, op1=ALU.add)
                P.rsqrt(ss5)
                for j in range(2):
                    P.tt("dve", qn[:, j * 512:(j + 1) * 512].re("p (g d) -> p g d", d=64), pbs[j].re("p (g d) -> p g d", d=64),
                         bc(ss5[:, j * 8:(j + 1) * 8], [(1, 8), (0, 64)]), ALU.mult)
                gn = qg if qi == 0 else r_kn
                dst = qkb[:, qi, :]
                if which == 1:
                    P.tt("pool", dst.re("p (g d) -> p g d", d=64), qn.re("p (g d) -> p g d", d=64), bc(gn, [(0, 16), (1, 64)]), ALU.mult)
                else:
                    P.tt("pool", qn.re("p (g d) -> p g d", d=64), qn.re("p (g d) -> p g d", d=64), bc(gn, [(0, 16), (1, 64)]), ALU.mult)
                    P.tt("dve", q1.re("p (g d) -> p g d", d=64), qn.re("p (g d) -> p g d", d=64), bc(atab[:, t, 0:64], [(0, 16), (1, 64)]), ALU.mult)
                    qv = qn.re("p (g a u d) -> p g a u d", a=2, u=2, d=16)
                    q2v = q2.re("p (g a u d) -> p g a u d", a=2, u=2, d=16)
                    for s_ in range(2):
                        sn = bass.AP(atab.ap.tensor, atab[:, t, 64 + s_ * 16:64 + s_ * 16 + 16].ap.offset,
                                     [list(atab.ap.ap[0]), [0, 16], [32, 2], [1, 16]])
                        P.tt("pool", q2v[:, :, :, s_, :], qv[:, :, :, 1 - s_, :], TT(sn, atab.tok), ALU.mult)
                    P.tt("dve", dst, q1, q2, ALU.add)
                pb = P.bank()
                pbb = bank_bf(pb)
                for h in range(8):
                    P.transpose(pbb[:, h * 128:(h + 1) * 128], qkb[:, qi, h * 128:(h + 1) * 128], ident_b)
                P.copy("act", qkT[:, qi, :, t * 128:(t + 1) * 128], pbb.re("p (h t) -> p h t", h=8))
            for j in range(2):
                pb = P.bank()
                c0 = 2048 + j * 512
                P.mmgroup(pb, [(lt[k], w1[:, k, c0:c0 + 512]) for k in range(8)])
                P.copy("act", vv[:, t, j * 4:(j + 1) * 4, 0:128], pb.re("p (h e) -> p h e", h=4))
        tok0 = bi * 256
        P.dma("sp", Kd[:, :, tok0:tok0 + 256].re("h p t -> p h t"), qkT[:, 1])
        if which == 0:
            P.dma("sp", Qd[:, :, tok0 - CTX:tok0 - CTX + 256].re("h p t -> p h t"), qkT[:, 0])
        for t in range(2):
            P.dma("sp", Vd[:, :, bi * 2 + t, :].re("h p e -> p h e"), vv[:, t])

    load5(0)
    if NB5 > 1:
        load5(1)
    stage5A(0)
    for bi in range(NB5):
        if bi + 2 < NB5:
            load5(bi + 2)
        if bi + 1 < NB5:
            stage5A(bi + 1)
        stage5B(bi)
    P.close_scope()
    if stop_after == "P5":
        return _finish(P, nc, [Kd, Vd, Qd])

    P.open_scope()
    NKT = NCH
    NQB = OWN // 512
    lt_ = P.sb("lt_", [128, 128], F32)
    lam2 = P.sb("lam2", [128, 4], F32)
    P.tt("dve", lt_[:, 0:64], r_lam[:, 0:64], r_lam[:, 64:128], ALU.mult)
    P.tt("dve", lt_[:, 64:128], r_lam[:, 128:192], r_lam[:, 192:256], ALU.mult)
    P.reduce("dve", lam2[:, 0:2], lt_.re("p (a d) -> p a d", a=2), ALU.add)
    P.act(lam2[:, 0:2], lam2[:, 0:2], AF.Exp)
    P.tt("dve", lam2[:, 2:3], lam2[:, 1:2], lam2[:, 0:1], ALU.subtract)
    P.ts("dve", lam2[:, 3:4], lam2[:, 2:3], -LAM_INIT, None, op0=ALU.add)
    neglam = lam2[:, 3:4]
    sub_g = P.sb("sub_g", [128, 128], F32)
    P.ts("dve", sub_g, r_subln, 1.0 - LAM_INIT, None, op0=ALU.mult)
    Kh = [P.sb("Kh%d" % i, [128, T], BF16) for i in range(2)]
    Vh = [P.sb("Vh%d" % i, [128, NKT, 130], BF16) for i in range(2)]
    qa = [P.sb("qa%d" % i, [128, 512], BF16) for i in range(2)]
    qb_ = [P.sb("qb%d" % i, [128, 512], BF16) for i in range(2)]
    qs = [P.sb("qs%d" % i, [128, 512], BF16) for i in range(2)]
    pT = [P.sb("pT%d" % i, [128, 512], BF16) for i in range(3)]
    o0 = P.sb("o0", [128, 4, 128], F32)
    o1 = P.sb("o1", [128, 128], F32)
    osq = P.sb("osq", [128, 4, 128], F32)
    rs6 = P.sb("rs6", [128, 4], F32)
    on = P.sb("on", [128, 4, 128], BF16)
    oT = [P.sb("oT%d" % i, [128, 512], BF16) for i in range(2)]
    spb = [P.banks[0], P.banks[1], P.banks[2]]
    ob = [P.banks[3], P.banks[4], P.banks[5], P.banks[6]]
    tb = P.banks[7]
    groups = [(h, qb) for h in range(8) for qb in range(NQB)]
    steps = [(m, kt) for m in range(2) for kt in range(NKT)]

    def load_kv(h):
        P.dma("sp", Kh[h % 2], Kd[h])
        P.dma("sp", Vh[h % 2], Vd[h])

    def load_q(gi):
        h, qb = groups[gi]
        A, B_, Q_ = qa[gi % 2], qb_[gi % 2], qs[gi % 2]
        P.dma("sp", A, Qd[h, :, qb * 512:(qb + 1) * 512])
        if L > OWN:
            P.dma("sp", B_, Qd[h, :, OWN + qb * 512:OWN + (qb + 1) * 512])
            P.ts("pool", Q_, A, sel_sb[:, 0:1], None, op0=ALU.mult)
            P.stt("dve", Q_, B_, sel_sb[:, 1:2], Q_, ALU.mult, ALU.add)
            return Q_
        return A

    load_kv(0)
    Qn = load_q(0)
    pi = 0
    for gi, (h, qb) in enumerate(groups):
        K_, V_ = Kh[h % 2], Vh[h % 2]
        Q_ = Qn
        if qb == 0 and h + 1 < 8:
            load_kv(h + 1)
        if gi + 1 < len(groups):
            Qn = load_q(gi + 1)

        def emit_s(i):
            m, kt = steps[i]
            P.mm(spb[(pi + i) % 3], K_[m * 64:(m + 1) * 64, kt * 128:(kt + 1) * 128], Q_[m * 64:(m + 1) * 64, :])
        emit_s(0)
        emit_s(1)
        for i, (m, kt) in enumerate(steps):
            if i + 2 < len(steps):
                emit_s(i + 2)
            sp_ = spb[(pi + i) % 3]
            p_ = pT[(pi + i) % 3]
            P.act(p_, sp_, AF.Exp)
            for s_ in range(4):
                P.mm(ob[s_][:, 0:129], p_[:, s_ * 128:(s_ + 1) * 128], V_[:, kt, 0:129], start=(kt == 0), stop=(kt == NKT - 1))
            if kt == NKT - 1:
                for s_ in range(4):
                    P.recip(rs6[:, s_:s_ + 1], ob[s_][:, 128:129])
                    if m == 0:
                        P.ts("dve", o0[:, s_, :], ob[s_][:, 0:128], rs6[:, s_:s_ + 1], None, op0=ALU.mult)
                    else:
                        P.ts("dve", o1, ob[s_][:, 0:128], rs6[:, s_:s_ + 1], neglam, op0=ALU.mult, op1=ALU.mult)
                        P.tt("dve", o0[:, s_, :], o0[:, s_, :], o1, ALU.add)
        pi += len(steps)
        P.act(osq, o0, AF.Square)
        P.reduce("dve", rs6, osq, ALU.add)
        P.ts("dve", rs6, rs6, 1.0 / 128, EPS, op0=ALU.mult, op1=ALU.add)
        P.rsqrt(rs6)
        tbb = bank_bf(tb)
        for s_ in range(4):
            P.stt("dve", on[:, s_, :], o0[:, s_, :], rs6[:, s_:s_ + 1], sub_g, ALU.mult, ALU.mult)
            P.transpose(tbb[:, s_ * 128:(s_ + 1) * 128], on[:, s_, :], ident_b)
        o_ = oT[gi % 2]
        P.copy("dve", o_, tbb[:, 0:512])
        P.dma("sp", Od[:, h, qb * 512:(qb + 1) * 512], o_)
    P.close_scope()
    if stop_after == "P6":
        return _finish(P, nc, [Od])

    P.open_scope()
    BLK = min(1024, OWN)
    NB = OWN // BLK
    NH = BLK // 512
    NT7 = BLK // 128
    wo1 = P.sb("wo1", [128, 8, D], BF16)
    for k in range(8):
        P.dma("pool", wo1[:, k, :], w_out1[k * 128:(k + 1) * 128, :])
    wr_sb = P.sb("wr_sb", [128, 8, 8], F32)
    P.dma("sp", wr_sb, wr.re("(k p) e -> p k e", p=128))
    selm = P.sb("selm", [8, 1024], F32)
    P.dma("sp", selm, selmat)
    o7 = P.sb("o7", [128, 8, BLK], BF16)
    xa = P.sb("xa", [128, 8, BLK], F32)
    xb7 = P.sb("xb7", [128, 8, 128], F32)
    sq7 = P.sb("sq7", [128, 8, 512], F32)
    rstd7 = P.sb("rstd7", [128, 512], F32)
    h7 = P.sb("h7", [128, 8, BLK], BF16)
    h7f = sq7
    yacc = P.sb("yacc", [128, 8, BLK], F32)
    lg = P.sb("lg", [128, 8], F32)
    lg2 = P.sb("lg2", [128, 8], F32)
    eq1 = P.sb("eq1", [128, 8], F32)
    eq2 = P.sb("eq2", [128, 8], F32)
    mx = P.sb("mx", [128, 8], F32)
    comb = P.sb("comb", [128, 8], F32)
    combT = P.sb("combT", [8, BLK], F32)
    cbc = [P.sb("cbc%d" % i, [128, BLK], BF16) for i in range(2)]
    FG = 2
    NFG = FEXP // (128 * FG)
    FW = 128 * FG
    wge = [P.sb("wge%d" % i, [128, 8, FW], BF16) for i in range(2)]
    wue = [P.sb("wue%d" % i, [128, 8, FW], BF16) for i in range(2)]
    wde = [P.sb("wde%d" % i, [128, FG, D], BF16) for i in range(2)]
    a7 = [P.sb("a7_%d" % i, [128, FG, BLK], BF16) for i in range(2)]
    sg7 = [P.sb("sg7_%d" % i, [128, 512], F32) for i in range(2)]
    t7 = [P.sb("t7_%d" % i, [128, 512], F32) for i in range(2)]
    its = [(nb, e, fg) for nb in range(NB) for e in range(NEXP) for fg in range(NFG)]

    def issue_w(ii):
        nb_, e_, fg_ = its[ii]
        b_ = ii % 2
        f0_ = fg_ * FW
        P.dma("pool", wge[b_], eg[e_, :, f0_:f0_ + FW].re("(k p) f -> p k f", p=128))
        P.dma("pool", wue[b_], eu[e_, :, f0_:f0_ + FW].re("(k p) f -> p k f", p=128))
        P.dma("pool", wde[b_], ed[e_, f0_:f0_ + FW, :].re("(f p) d -> p f d", p=128))
    issue_w(0)
    wi = 0
    for nb in range(NB):
        P.dma("sp", o7, Od[:, :, nb * BLK:(nb + 1) * BLK])
        for t in range(NT7):
            chA = 2 + (nb * BLK) // 128 + t
            P.dma("sp", xa[:, :, t * 128:(t + 1) * 128], x_l1[chA])
            if L > OWN:
                P.dma("sp", xb7, x_l1[chA + OWN // 128])
                P.ts("pool", xa[:, :, t * 128:(t + 1) * 128], xa[:, :, t * 128:(t + 1) * 128], sel_sb[:, 0:1], None, op0=ALU.mult)
                P.stt("pool", xa[:, :, t * 128:(t + 1) * 128], xb7, sel_sb[:, 1:2], xa[:, :, t * 128:(t + 1) * 128], ALU.mult, ALU.add)
        for hf in range(NH):
            cs = slice(hf * 512, (hf + 1) * 512)
            for dc in range(8):
                pb = P.bank()
                P.mmgroup(pb, [(wo1[:, hh, dc * 128:(dc + 1) * 128], o7[:, hh, cs]) for hh in range(8)])
                P.stt("dve", xa[:, dc, cs], pb, G[1][:, dc, 0, 0:1], xa[:, dc, cs], ALU.mult, ALU.add)
            P.act(sq7, xa[:, :, cs], AF.Square)
            pb = P.bank()
            P.mmgroup(pb, [(ones_f, sq7[:, k, :]) for k in range(8)])
            P.ts("dve", rstd7, pb, 1.0 / D, EPS, op0=ALU.mult, op1=ALU.add)
            P.rsqrt(rstd7)
            P.tt("dve", sq7, xa[:, :, cs], bc(rstd7, [(0, 8), (1, 512)]), ALU.mult)
            A2 = AB[1][:, :, 0, 1, :]
            for k in range(8):
                P.ts("pool", h7f[:, k, :], sq7[:, k, :], A2[:, k, 0:1], A2[:, k, 1:2], op0=ALU.mult, op1=ALU.add)
                P.copy("act", h7[:, k, cs], h7f[:, k, :])
            for t4 in range(4):
                pb = P.bank()
                P.mmgroup(pb[:, 0:8], [(h7f[:, k, t4 * 128:(t4 + 1) * 128], wr_sb[:, k, :]) for k in range(8)])
                P.copy("dve", lg, pb[:, 0:8])
                P.reduce("dve", mx[:, 0:1], lg, ALU.max)
                P.ts("dve", eq1, lg, mx[:, 0:1], None, op0=ALU.is_equal)
                P.stt("dve", lg2, eq1, -1e30, lg, ALU.mult, ALU.add)
                P.reduce("dve", mx[:, 1:2], lg2, ALU.max)
                P.ts("dve", eq2, lg2, mx[:, 1:2], None, op0=ALU.is_equal)
                P.tt("dve", mx[:, 2:3], mx[:, 1:2], mx[:, 0:1], ALU.subtract)
                P.act(mx[:, 3:4], mx[:, 2:3], AF.Exp)
                P.ts("dve", mx[:, 4:5], mx[:, 3:4], 1.0, None, op0=ALU.add)
                P.recip(mx[:, 5:6], mx[:, 4:5])
                P.tt("dve", mx[:, 6:7], mx[:, 3:4], mx[:, 5:6], ALU.mult)
                P.ts("dve", comb, eq1, mx[:, 5:6], None, op0=ALU.mult)
                P.stt("dve", comb, eq2, mx[:, 6:7], comb, ALU.mult, ALU.add)
                pb = P.bank()
                P.transpose(pb[0:8, 0:128], comb, ident_f)
                P.copy("dve", combT[:, hf * 512 + t4 * 128:hf * 512 + (t4 + 1) * 128], pb[0:8, 0:128])
        P.memset("pool", yacc, 0.0)
        for e in range(NEXP):
            cb = cbc[e % 2]
            for hf in range(NH):
                cs = slice(hf * 512, (hf + 1) * 512)
                pb = P.bank()
                P.mm(pb, selm[:, e * 128:(e + 1) * 128], combT[:, cs])
                P.copy("act", cb[:, cs], pb)
            for fg in range(NFG):
                b = wi % 2
                wi += 1
                if wi < len(its):
                    issue_w(wi)
                aa = a7[b]
                ii = 0
                for f in range(FG):
                    for hf in range(NH):
                        cs = slice(hf * 512, (hf + 1) * 512)
                        pg = P.bank()
                        pu = P.bank()
                        P.mmgroup(pg, [(wge[b][:, k, f * 128:(f + 1) * 128], h7[:, k, cs]) for k in range(8)])
                        P.mmgroup(pu, [(wue[b][:, k, f * 128:(f + 1) * 128], h7[:, k, cs]) for k in range(8)])
                        sg = sg7[ii % 2]
                        tt_ = t7[ii % 2]
                        ii += 1
                        P.act(sg, pg, AF.Silu)
                        P.tt("dve", tt_, sg, pu, ALU.mult)
                        P.tt("pool", aa[:, f, cs], tt_, cb[:, cs], ALU.mult)
                for hf in range(NH):
                    cs = slice(hf * 512, (hf + 1) * 512)
                    for dc in range(8):
                        pb = P.bank()
                        P.mmgroup(pb, [(wde[b][:, f, dc * 128:(dc + 1) * 128], aa[:, f, cs]) for f in range(FG)])
                        P.tt("dve", yacc[:, dc, cs], yacc[:, dc, cs], pb, ALU.add)
        for dc in range(8):
            P.stt("dve", yacc[:, dc, :], yacc[:, dc, :], G[1][:, dc, 0, 1:2], xa[:, dc, :], ALU.mult, ALU.add)
        P.dma("sp", outT[:, nb * BLK:(nb + 1) * BLK].re("(k p) t -> p k t", p=128), yacc)
    P.close_scope()
    return _finish(P, nc, [outT])


def P_sb_keep(P, name, shape):
    g = P.nc.sbuf_tensor(name, list(shape), F32)
    h = g.__enter__()
    P._ctx.insert(0, g)
    for i in range(len(P._scopes)):
        P._scopes[i] += 1
    return TT(h[:], Tok(name))


def _finish(P, nc, outs):
    P.fence("sp", outs)
    P.emit()
    P.close()
    return nc


def fm_vec(v):
    v = np.asarray(v, np.float32).reshape(-1, 128)
    return np.ascontiguousarray(v.T)


def prep_inputs(inp, L, OWN, ncores_per_batch, nbatch):
    cm, selm = host_consts()
    rcs, acs = rope_tables(L)
    shared = {
        "consts": cm, "selmat": selm, "rcs": rcs, "acs": acs,
        "w_mod0": np.ascontiguousarray(inp["even_w_mod"][0]), "w_mod1": np.ascontiguousarray(inp["odd_w_mod"][0]),
        "b_mod0": fm_vec(inp["even_b_mod"][0]), "b_mod1": fm_vec(inp["odd_b_mod"][0]),
        "norms": np.concatenate([fm_vec(inp["even_norm1"][0]), fm_vec(inp["even_norm2"][0]),
                                 fm_vec(inp["odd_norm1"][0]), fm_vec(inp["odd_norm2"][0])], axis=1),
        "w_in0": np.ascontiguousarray(inp["even_w_in"][0]),
        "w_out0": np.ascontiguousarray(inp["even_w_out"][0]),
        "ffg": np.ascontiguousarray(inp["even_ffn_gate"][0]), "ffu": np.ascontiguousarray(inp["even_ffn_up"][0]),
        "ffd": np.ascontiguousarray(inp["even_ffn_down"][0]),
        "w_in1": np.ascontiguousarray(inp["odd_w_in"][0]), "w_out1": np.ascontiguousarray(inp["odd_w_out"][0]),
        "router": np.ascontiguousarray(inp["odd_router"][0]),
        "eg": np.ascontiguousarray(inp["odd_exp_gate"][0]), "eu": np.ascontiguousarray(inp["odd_exp_up"][0]),
        "ed": np.ascontiguousarray(inp["odd_exp_down"][0]),
    }
    cw = np.concatenate([inp["even_conv_w"][0], inp["even_conv_b"][0][None, :]], axis=0)
    shared["convw"] = np.ascontiguousarray(cw.reshape(4, 12, 128).transpose(2, 1, 0))
    rowp = np.concatenate([
        inp["even_dt_bias"][0].reshape(-1), inp["even_a_log"][0].reshape(-1), inp["even_ret_decay"][0].reshape(-1),
        inp["even_d"][0].reshape(-1), inp["even_ssd_norm"][0].reshape(-1), inp["odd_q_norm"][0].reshape(-1),
        inp["odd_k_norm"][0].reshape(-1), inp["odd_lambda"][0].reshape(-1), inp["odd_subln"][0].reshape(-1)]).astype(np.float32)
    rp = np.zeros((1, 2560), np.float32)
    rp[0, :rowp.size] = rowp
    shared["rowp"] = rp
    maps = []
    for b in range(nbatch):
        xT = np.zeros((D, L + 2), np.float32)
        xT[:, 1:L + 1] = inp["x"][b].T
        cT = np.zeros((D, CTX + 2), np.float32)
        cT[:, 1:CTX + 1] = inp["ctx"][b].T
        cvec = np.concatenate([fm_vec(inp["c"][b]), fm_vec(inp["c_ctx"])], axis=1)
        for hf in range(ncores_per_batch):
            s = np.zeros((128, 2), np.float32)
            s[:, hf] = 1.0
            m = dict(shared)
            m.update({"xT": xT, "ctxT": cT, "cvec": cvec, "sel": s})
            maps.append(m)
    return maps


_NC_CACHE = {}


def kernel(**inputs):
    inp = {k: np.asarray(v) for k, v in inputs.items()}
    B, L, _ = inp["x"].shape
    OWN = L // 2
    key = (L, OWN)
    if key not in _NC_CACHE:
        _NC_CACHE[key] = build(L, OWN)
    nc = _NC_CACHE[key]
    maps = prep_inputs(inp, L, OWN, 2, B)
    res = run_bass_kernel_spmd(nc, maps, core_ids=list(range(len(maps))))
    out = np.empty((B, L, D), np.float32)
    for b in range(B):
        for hf in range(2):
            out[b, hf * OWN:(hf + 1) * OWN, :] = res.results[b * 2 + hf]["outT"].T
    return out
```

```python
import math
import numpy as np
import ml_dtypes
import concourse.bass as bass
import concourse.mybir as mybir
from concourse.bass_utils import run_bass_kernel_spmd

F32 = mybir.dt.float32
BF16 = mybir.dt.bfloat16
AF = mybir.ActivationFunctionType
ALU = mybir.AluOpType
AX = mybir.AxisListType

SAME_ENGINE_SYNC = True
NSLOT = 10


class Tok:
    __slots__ = ("lw", "rd", "name")

    def __init__(self, name=""):
        self.lw = None
        self.rd = []
        self.name = name


class TT:
    __slots__ = ("ap", "tok")

    def __init__(self, ap, tok):
        self.ap = ap
        self.tok = tok

    def __getitem__(self, k):
        return TT(self.ap[k], self.tok)

    def re(self, s, **kw):
        return TT(self.ap.rearrange(s, **kw), self.tok)

    @property
    def shape(self):
        return self.ap.shape


def bc(tt, dims):
    a = tt.ap
    base = list(a.ap)
    return TT(bass.AP(a.tensor, a.offset, [list(base[0])] + [list(d) for d in dims]), tt.tok)


class Op:
    __slots__ = ("stream", "fn", "deps", "dma", "ms", "slot", "val", "didx")

    def __init__(self, stream, fn, dma):
        self.stream = stream
        self.fn = fn
        self.deps = []
        self.dma = dma
        self.ms = False
        self.slot = None
        self.val = None
        self.didx = None


class Prog:
    STREAMS = ("pe", "act", "dve", "pool", "sp")

    def __init__(self, nc):
        self.nc = nc
        self.ops = {s: [] for s in self.STREAMS}
        self.ndma = {s: 0 for s in self.STREAMS}
        self.dmaops = {s: [] for s in self.STREAMS}
        self._ctx = []
        self._scopes = []
        self.banks = []
        self.bank_i = 0

    def sb(self, name, shape, dt=F32):
        g = self.nc.sbuf_tensor(name, list(shape), dt)
        h = g.__enter__()
        self._ctx.append(g)
        return TT(h[:], Tok(name))

    def ps(self, name, shape, dt=F32):
        g = self.nc.psum_tensor(name, list(shape), dt)
        h = g.__enter__()
        self._ctx.append(g)
        return TT(h[:], Tok(name))

    def dram(self, name, shape, dt=F32, kind="Internal"):
        h = self.nc.dram_tensor(name, list(shape), dt, kind=kind)
        return TT(h.ap(), Tok(name))

    def open_scope(self):
        self._scopes.append(len(self._ctx))

    def close_scope(self):
        n = self._scopes.pop()
        self.barrier()
        while len(self._ctx) > n:
            self._ctx.pop().__exit__(None, None, None)

    def close(self):
        while self._ctx:
            self._ctx.pop().__exit__(None, None, None)

    def bank(self):
        b = self.banks[self.bank_i % len(self.banks)]
        self.bank_i += 1
        return b

    def _rec(self, stream, fn, reads, writes, dma=False, extra=()):
        op = Op(stream, fn, dma)
        deps = list(extra)
        for t in reads:
            if t.tok.lw is not None:
                deps.append(t.tok.lw)
        for t in writes:
            if t.tok.lw is not None:
                deps.append(t.tok.lw)
            deps.extend(t.tok.rd)
        if dma:
            op.didx = self.ndma[stream]
            self.ndma[stream] += 1
            self.dmaops[stream].append(op)
            if op.didx >= NSLOT:
                deps.append(self.dmaops[stream][op.didx - NSLOT])
        seen = set()
        for d in deps:
            if d is op or id(d) in seen:
                continue
            if (not d.dma) and d.stream == stream and (stream == "pe" or not SAME_ENGINE_SYNC):
                continue
            seen.add(id(d))
            op.deps.append(d)
            d.ms = True
        for t in reads:
            t.tok.rd.append(op)
        for t in writes:
            t.tok.lw = op
            t.tok.rd = []
        self.ops[stream].append(op)
        return op

    def barrier(self):
        last = []
        for s in self.STREAMS:
            if self.ops[s]:
                for o in reversed(self.ops[s]):
                    if not o.dma and o.fn is not None:
                        last.append(o)
                        break
            last.extend(self.dmaops[s][-NSLOT:])
        for s in self.STREAMS:
            self._rec(s, None, [], [], extra=last)

    def mm(self, out, lhsT, rhs, start=True, stop=True):
        self._rec("pe", lambda e: e.matmul(out.ap, lhsT.ap, rhs.ap, start=start, stop=stop), [lhsT, rhs], [out])

    def mmgroup(self, out, pairs):
        rd = []
        for l, r in pairs:
            rd += [l, r]
        n = len(pairs)

        def fn(e):
            ins = None
            for i, (l, r) in enumerate(pairs):
                ins = e.matmul(out.ap, l.ap, r.ap, start=(i == 0), stop=(i == n - 1))
            return ins
        self._rec("pe", fn, rd, [out])

    def transpose(self, out, in_, ident):
        self._rec("pe", lambda e: e.transpose(out.ap, in_.ap, ident.ap), [in_, ident], [out])

    def act(self, out, in_, func, bias=None, scale=1.0, accum=None):
        rd = [in_]
        kw = {}
        if bias is not None:
            if isinstance(bias, TT):
                rd.append(bias)
                kw["bias"] = bias.ap
            else:
                kw["bias"] = bias
        if isinstance(scale, TT):
            rd.append(scale)
            kw["scale"] = scale.ap
        else:
            kw["scale"] = scale
        wr = [out]
        if accum is not None:
            wr.append(accum)
            kw["accum_out"] = accum.ap
        self._rec("act", lambda e: e.activation(out.ap, in_.ap, func, **kw), rd, wr)

    def tt(self, eng, out, a, b, op):
        self._rec(eng, lambda e: e.tensor_tensor(out.ap, a.ap, b.ap, op), [a, b], [out])

    def ts(self, eng, out, a, s1, s2=None, op0=ALU.mult, op1=None):
        rd = [a]
        s1a = s1.ap if isinstance(s1, TT) else s1
        s2a = s2.ap if isinstance(s2, TT) else s2
        if isinstance(s1, TT):
            rd.append(s1)
        if isinstance(s2, TT):
            rd.append(s2)
        kw = {}
        if op1 is not None:
            kw["op1"] = op1
        self._rec(eng, lambda e: e.tensor_scalar(out.ap, a.ap, s1a, s2a, op0, **kw), rd, [out])

    def stt(self, eng, out, a, s, b, op0, op1):
        rd = [a, b]
        sa = s.ap if isinstance(s, TT) else s
        if isinstance(s, TT):
            rd.append(s)
        self._rec("dve", lambda e: e.scalar_tensor_tensor(out.ap, a.ap, sa, b.ap, op0, op1), rd, [out])

    def copy(self, eng, out, a):
        if eng == "act":
            self._rec(eng, lambda e: e.copy(out.ap, a.ap), [a], [out])
        else:
            self._rec(eng, lambda e: e.tensor_copy(out.ap, a.ap), [a], [out])

    def memset(self, eng, out, val):
        self._rec(eng, lambda e: e.memset(out.ap, val), [], [out])

    def reduce(self, eng, out, a, op, axis=AX.X):
        self._rec(eng, lambda e: e.tensor_reduce(out.ap, a.ap, axis, op), [a], [out])

    def rsqrt(self, x):
        self._rec("act", lambda e: e.activation(x.ap, x.ap, AF.Sqrt), [x], [x])
        self._rec("dve", lambda e: e.reciprocal(x.ap, x.ap), [x], [x])

    def recip(self, out, a):
        self._rec("dve", lambda e: e.reciprocal(out.ap, a.ap), [a], [out])

    def dma(self, q, out, in_):
        self._rec(q, lambda e: e.dma_start(out.ap, in_.ap), [in_], [out], dma=True)

    def fence(self, stream, tts):
        self._rec(stream, None, list(tts), [])

    def emit(self):
        nc = self.nc
        for s in self.STREAMS:
            k = 0
            for op in self.ops[s]:
                if op.dma:
                    op.slot = op.didx % NSLOT
                    op.val = 16 * (op.didx // NSLOT + 1)
                elif op.ms:
                    k += 1
                    op.val = k
        sem_ctx = []
        csem = {}
        dsem = {}
        for s in self.STREAMS:
            g = nc.semaphore("c_" + s)
            csem[s] = g.__enter__()
            sem_ctx.append(g)
            if self.ndma[s] > 0:
                for i in range(NSLOT):
                    g = nc.semaphore("d_%s_%d" % (s, i))
                    dsem[(s, i)] = g.__enter__()
                    sem_ctx.append(g)
        ops = self.ops

        def run(stream, e):
            waited = {}
            for op in ops[stream]:
                for d in op.deps:
                    if d.dma:
                        key = ("d", d.stream, d.slot)
                        sem = dsem[(d.stream, d.slot)]
                    else:
                        key = ("c", d.stream)
                        sem = csem[d.stream]
                    if waited.get(key, 0) < d.val:
                        e.wait_ge(sem, d.val)
                        waited[key] = d.val
                if op.fn is None:
                    continue
                ins = op.fn(e)
                if op.dma:
                    ins.then_inc(dsem[(stream, op.slot)], 16)
                elif op.ms:
                    ins.then_inc(csem[stream], 1)

        with nc.Block() as block:
            @block.sync
            def _(e):
                run("sp", e)

            @block.tensor
            def _(e):
                run("pe", e)

            @block.scalar
            def _(e):
                run("act", e)

            @block.vector
            def _(e):
                run("dve", e)

            @block.gpsimd
            def _(e):
                run("pool", e)
        for g in reversed(sem_ctx):
            g.__exit__(None, None, None)


D = 1024
KC = 8
EPS = 1e-6
EVEN_IN = 5664
FFN_DENSE = 2816
NEXP = 8
FEXP = 3584
CTX = 256
GRID_W = 64
LAM_INIT = 0.8 - 0.6 * math.exp(-0.3 * 1)
C_Z, C_XBC, C_DT, C_RQ, C_RK, C_RV, C_RG = 0, 1024, 2560, 2592, 3104, 3616, 4640


def host_consts():
    k = np.arange(128)[:, None]
    l = np.arange(128)[None, :]
    c = {}
    c["ident"] = (k == l).astype(np.float32)
    c["le"] = (k <= l).astype(np.float32)
    c["gt"] = (k > l).astype(np.float32)
    c["ge"] = (k >= l).astype(np.float32)
    c["lt"] = (k < l).astype(np.float32)
    c["ones"] = np.ones((128, 128), np.float32)
    cm = np.concatenate([c[n] for n in ("ident", "le", "gt", "ge", "lt", "ones")], axis=1)
    sel = np.zeros((8, 8, 128), np.float32)
    for e in range(8):
        sel[e, e, :] = 1.0
    return cm, sel.reshape(8, 1024)


def rope_tables(L):
    f32 = np.float32
    inv = (np.float32(10000.0) ** (-np.arange(64, dtype=f32) / f32(64))).astype(f32)
    pos = np.arange(CTX + L, dtype=f32)
    ang = (pos[:, None] * inv[None, :]).astype(f32)
    rcs = np.concatenate([np.cos(ang), np.sin(ang)], axis=1).astype(f32)
    inv16 = (np.float32(10000.0) ** (-np.arange(16, dtype=f32) / f32(16))).astype(f32)
    t = np.arange(L)
    row = (t // GRID_W).astype(f32)
    col = (t % GRID_W).astype(f32)
    ar = (row[:, None] * inv16[None, :]).astype(f32)
    ac = (col[:, None] * inv16[None, :]).astype(f32)
    cos = np.concatenate([np.cos(ar), np.cos(ar), np.cos(ac), np.cos(ac)], axis=1)
    sins = np.concatenate([-np.sin(ar), np.sin(ar), -np.sin(ac), np.sin(ac)], axis=1)
    acs = np.concatenate([cos, sins], axis=1).astype(f32)
    return rcs, acs


def build(L=8192, OWN=4096, stop_after=None, dbg=()):
    nc = bass.Bass("TRN2", target_bir_lowering=False)
    P = Prog(nc)
    T = CTX + L
    NCH = T // 128
    NLB = L // 256
    dbg = set(dbg)

    def din(name, shape, dt=F32):
        return P.dram(name, shape, dt, kind="ExternalInput")

    xT = din("xT", [D, L + 2])
    cT = din("ctxT", [D, CTX + 2])
    cvec = din("cvec", [128, 16])
    sel = din("sel", [128, 2])
    consts = din("consts", [128, 768])
    selmat = din("selmat", [8, 1024])
    rcs_d = din("rcs", [T, 128])
    acs_d = din("acs", [L, 128])
    w_mod = [din("w_mod0", [D, 6 * D]), din("w_mod1", [D, 6 * D])]
    b_mod = [din("b_mod0", [128, 48]), din("b_mod1", [128, 48])]
    norms = din("norms", [128, 32])
    w_in0 = din("w_in0", [D, EVEN_IN])
    convw = din("convw", [128, 12, 4])
    rowp = din("rowp", [1, 2560])
    w_out0 = din("w_out0", [2048, D])
    ffg = din("ffg", [D, FFN_DENSE])
    ffu = din("ffu", [D, FFN_DENSE])
    ffd = din("ffd", [FFN_DENSE, D])
    w_in1 = din("w_in1", [D, 3072])
    w_out1 = din("w_out1", [D, D])
    wr = din("router", [D, NEXP])
    eg = din("eg", [NEXP, D, FEXP])
    eu = din("eu", [NEXP, D, FEXP])
    ed = din("ed", [NEXP, FEXP, D])
    outT = P.dram("outT", [D, OWN], F32, kind="ExternalOutput")

    def scratch(name, shape, dt=F32):
        return P.dram(name, shape, dt, kind=("ExternalOutput" if name in dbg else "Internal"))

    r_zr = scratch("r_zr", [NCH, 128, 2048], BF16)
    r_xv = scratch("r_xv", [NCH, 128, 2048], BF16)
    r_kt = scratch("r_kt", [NCH, 128, 768], BF16)
    r_fm = scratch("r_fm", [NCH, 128, 12, 128], BF16)
    r_dt = scratch("r_dt", [NCH, 128, 64], F32)
    r_yf = scratch("r_yf", [NCH, 128, 2048], F32)
    x_mid = scratch("x_mid", [NCH, 128, 8, 128], F32)
    x_l1 = scratch("x_l1", [NCH, 128, 8, 128], F32)
    Kd = scratch("Kd", [8, 128, T], BF16)
    Vd = scratch("Vd", [8, 128, NCH, 130], BF16)
    Qd = scratch("Qd", [8, 128, L], BF16)
    Od = scratch("Od", [128, 8, OWN], BF16)

    cst = P.sb("cst", [128, 768], F32)
    P.dma("sp", cst, consts)
    ident_f = cst[:, 0:128]
    m_le, m_gt, m_ge, m_lt, ones_f = (cst[:, 128 * i:128 * (i + 1)] for i in range(1, 6))
    cstb = P.sb("cstb", [128, 768], BF16)
    P.copy("dve", cstb, cst)
    ident_b = cstb[:, 0:128]
    sel_sb = P.sb("sel_sb", [128, 2], F32)
    P.dma("sp", sel_sb, sel)
    rows = P.sb("rows", [128, 2560], F32)
    P.dma("sp", rows, TT(rowp.ap.rearrange("a b -> (a b)").partition_broadcast(128), rowp.tok))
    norm_sb = P.sb("norm_sb", [128, 32], F32)
    P.dma("sp", norm_sb, norms)
    modfm = [P.sb("modfm0", [128, 48, 2], F32), P.sb("modfm1", [128, 48, 2], F32)]
    P.banks = [P.ps("bank%d" % i, [128, 512], F32) for i in range(8)]

    def bank_bf(b):
        a = b.ap
        return TT(a.bitcast(BF16), b.tok)

    AB = [P.sb("AB%d" % i, [128, 8, 2, 2, 2]) for i in range(2)]
    G = [P.sb("G%d" % i, [128, 8, 2, 2]) for i in range(2)]
    P.open_scope()
    cv = P.sb("cv", [128, 16], F32)
    P.dma("sp", cv, cvec)
    scv = P.sb("scv", [128, 8, 2], F32)
    P.act(scv[:, :, 0], cv[:, 0:8], AF.Silu)
    P.act(scv[:, :, 1], cv[:, 8:16], AF.Silu)
    wm = [P.sb("wm%d" % i, [128, 8, 512], F32) for i in range(2)]
    bm_sb = P.sb("bm_sb", [128, 2, 48], F32)
    P.dma("sp", bm_sb[:, 0, :], b_mod[0])
    P.dma("sp", bm_sb[:, 1, :], b_mod[1])
    it = 0
    for lyr in range(2):
        for cg in range(12):
            w = wm[it % 2]
            it += 1
            P.dma("sp", w, w_mod[lyr][:, cg * 512:(cg + 1) * 512].re("(k p) f -> p k f", p=128))
            pb = P.bank()
            for j in range(4):
                P.mmgroup(pb[:, 2 * j:2 * j + 2], [(w[:, k, j * 128:(j + 1) * 128], scv[:, k, :]) for k in range(8)])
            for j in range(4):
                ch = cg * 4 + j
                P.ts("dve", modfm[lyr][:, ch, :], pb[:, 2 * j:2 * j + 2], bm_sb[:, lyr, ch:ch + 1], None, op0=ALU.add)
    for lyr in range(2):
        for w in range(2):
            for n in range(2):
                shift = modfm[lyr][:, (3 * n) * 8:(3 * n) * 8 + 8, w]
                scale = modfm[lyr][:, (3 * n + 1) * 8:(3 * n + 1) * 8 + 8, w]
                gate = modfm[lyr][:, (3 * n + 2) * 8:(3 * n + 2) * 8 + 8, w]
                gain = norm_sb[:, (2 * lyr + n) * 8:(2 * lyr + n) * 8 + 8]
                P.stt("dve", AB[lyr][:, :, w, n, 0], scale, 1.0, gain, ALU.add, ALU.mult)
                P.copy("dve", AB[lyr][:, :, w, n, 1], shift)
                P.copy("dve", G[lyr][:, :, w, n], gate)
    P.close_scope()

    def rms_modulate(xin, ncol, Atab, out_bf, tmp, out_f32=None, eng="pool"):
        sq = tmp["sq"]
        P.act(sq[:, :, 0:ncol], xin, AF.Square)
        pb = P.bank()
        P.mmgroup(pb[:, 0:ncol], [(ones_f, sq[:, k, 0:ncol]) for k in range(8)])
        rstd = tmp["rstd"]
        P.ts("dve", rstd[:, 0:ncol], pb[:, 0:ncol], 1.0 / D, EPS, op0=ALU.mult, op1=ALU.add)
        P.rsqrt(rstd[:, 0:ncol])
        P.tt("dve", sq[:, :, 0:ncol], xin, bc(rstd[:, 0:ncol], [(0, 8), (1, ncol)]), ALU.mult)
        for k in range(8):
            if eng == "act" or (eng == "mix" and k % 2 == 0):
                P.act(out_bf[:, k, :], sq[:, k, 0:ncol], AF.Identity, bias=Atab[:, k, 1:2], scale=Atab[:, k, 0:1])
            else:
                P.ts("pool", out_bf[:, k, :], sq[:, k, 0:ncol], Atab[:, k, 0:1], Atab[:, k, 1:2], op0=ALU.mult, op1=ALU.add)
            if out_f32 is not None:
                P.ts("pool", out_f32[:, k, :], sq[:, k, 0:ncol], Atab[:, k, 0:1], Atab[:, k, 1:2], op0=ALU.mult, op1=ALU.add)

    r_dtb = rows[:, 0:32]
    r_alog = rows[:, 32:64]
    r_retd = rows[:, 64:72]
    r_dsk = rows[:, 72:88]
    r_ssdn = rows[:, 88:1112]
    r_qn = rows[:, 1112:1176]
    r_kn = rows[:, 1176:1240]
    r_lam = rows[:, 1240:1496]
    r_subln = rows[:, 1496:1624]
    ea = P.sb("ea", [128, 32], F32)
    P.act(ea, r_alog, AF.Exp)
    nla_ret = P.sb("nla_ret", [128, 8], F32)
    P.act(nla_ret, r_retd, AF.Exp)
    P.ts("dve", nla_ret, nla_ret, -1.0, None, op0=ALU.mult)

    P.open_scope()
    w0 = P.sb("w0", [128, 8, EVEN_IN], BF16)
    for k in range(8):
        for c0 in range(0, EVEN_IN, 1888):
            P.dma("pool", w0[:, k, c0:c0 + 1888], w_in0[k * 128:(k + 1) * 128, c0:c0 + 1888])
    cw = P.sb("cw", [128, 12, 4], F32)
    P.dma("sp", cw, convw)
    xin = [P.sb("xin%d" % i, [128, 8, 258], F32) for i in range(2)]
    xbr = P.sb("xbr", [128, 12, 258], F32)
    tmp1 = {"sq": xbr[:, 0:8, :], "rstd": P.sb("rstd1", [128, 258], F32)}
    hbs = [P.sb("hb%d" % i, [128, 8, 258], BF16) for i in range(2)]
    xbcs = [P.sb("xbc%d" % i, [128, 12, 256], BF16) for i in range(2)]
    cvts = [P.sb("cvt%d" % i, [128, 256], F32) for i in range(2)]
    o_zr = P.sb("o_zr", [128, 2, 2048], BF16)
    o_xv = P.sb("o_xv", [128, 2, 2048], BF16)
    o_kt = P.sb("o_kt", [128, 2, 768], BF16)
    o_fm = P.sb("o_fm", [128, 2, 12, 128], BF16)
    o_dt = P.sb("o_dt", [128, 2, 64], F32)
    rtabs = [P.sb("rtab%d" % i, [128, 2, 128], F32) for i in range(3)]
    rt1 = P.sb("rt1", [128, 4, 128], F32)
    rt2 = P.sb("rt2", [128, 4, 128], F32)
    rqk = P.sb("rqk", [128, 2, 512], BF16)
    sp1 = P.sb("sp1", [128, 32], F32)
    sp2 = P.sb("sp2", [128, 32], F32)

    blocks = [("c", 0)] + [("l", i) for i in range(NLB)]

    def binfo(bj):
        kind_, i_ = blocks[bj]
        if kind_ == "c":
            return 0, True, True, 1
        return CTX + i_ * 256, (i_ == 0), (i_ == NLB - 1), 0

    def load1(bj):
        kind_, i_ = blocks[bj]
        if kind_ == "c":
            P.dma("sp", xin[bj % 2], cT.re("(k p) t -> p k t", p=128))
        else:
            P.dma("sp", xin[bj % 2], xT[:, i_ * 256:i_ * 256 + 258].re("(k p) t -> p k t", p=128))
        t0_ = binfo(bj)[0]
        P.dma("sp", rtabs[bj % 3], rcs_d[t0_:t0_ + 256, :].re("(t p) c -> p t c", p=128))

    def stageA(bj):
        tok0, first, last, which = binfo(bj)
        xi, hb, xbc = xin[bj % 2], hbs[bj % 2], xbcs[bj % 2]
        rms_modulate(xi, 258, AB[0][:, :, which, 0, :], hb, tmp1, eng="act")
        for c in range(12):
            pb = P.bank()
            P.mmgroup(pb[:, 0:258], [(w0[:, k, C_XBC + c * 128:C_XBC + (c + 1) * 128], hb[:, k, :]) for k in range(8)])
            P.copy("act", xbr[:, c, :], pb[:, 0:258])
        if first:
            P.memset("pool", xbr[:, :, 0:1], 0.0)
        if last:
            P.memset("pool", xbr[:, :, 257:258], 0.0)
        for c in range(12):
            cvt = cvts[c % 2]
            P.ts("pool", cvt, xbr[:, c, 0:256], cw[:, c, 0:1], None, op0=ALU.mult)
            P.stt("dve", cvt, xbr[:, c, 1:257], cw[:, c, 1:2], cvt, ALU.mult, ALU.add)
            P.stt("dve", cvt, xbr[:, c, 2:258], cw[:, c, 2:3], cvt, ALU.mult, ALU.add)
            P.act(xbc[:, c, :], cvt, AF.Silu, bias=cw[:, c, 3:4])

    def stageB(bj):
        tok0, first, last, which = binfo(bj)
        hb, xbc, rtab = hbs[bj % 2], xbcs[bj % 2], rtabs[bj % 3]
        ch0 = tok0 // 128
        for t in range(2):
            P.copy("pool", o_fm[:, t, 0:4, :], xbc[:, 8:12, t * 128:(t + 1) * 128])
        for t in range(2):
            lt = [hb[:, k, 1 + t * 128:1 + (t + 1) * 128] for k in range(8)]

            def proj(c0, n):
                pb = P.bank()
                P.mmgroup(pb[:, 0:n], [(lt[k], w0[:, k, c0:c0 + n]) for k in range(8)])
                return pb
            for j in range(2):
                pb = proj(C_Z + j * 512, 512)
                P.copy("act", o_zr[:, t, j * 512:(j + 1) * 512], pb)
            for j in range(2):
                pb = proj(C_RG + j * 512, 512)
                P.copy("act", o_zr[:, t, 1024 + j * 512:1024 + (j + 1) * 512], pb)
            for j in range(2):
                pb = proj(C_RV + j * 512, 512)
                P.copy("act", o_xv[:, t, 1024 + j * 512:1024 + (j + 1) * 512], pb)
            pb = proj(C_DT, 32)
            P.tt("dve", sp1, pb[:, 0:32], r_dtb, ALU.add)
            P.act(sp2, sp1, AF.Abs)
            P.act(sp2, sp2, AF.Exp, scale=-1.0)
            P.act(sp2, sp2, AF.Ln, bias=1.0)
            P.stt("dve", o_dt[:, t, 0:32], sp1, 0.0, sp2, ALU.max, ALU.add)
            P.tt("dve", sp1, o_dt[:, t, 0:32], ea, ALU.mult)
            P.ts("dve", o_dt[:, t, 32:64], sp1, -1.0, None, op0=ALU.mult)
            for qi, c0 in enumerate((C_RQ, C_RK)):
                pb = proj(c0, 512)
                pv = pb.re("p (h d) -> p h d", h=4)
                cos2 = bc(rtab[:, t, 0:64], [(0, 4), (0, 2), (1, 64)])
                P.tt("dve", rt1.re("p h (a d) -> p h a d", a=2), pv.re("p h (a d) -> p h a d", a=2), cos2, ALU.mult)
                sin1 = bc(rtab[:, t, 64:128], [(0, 4), (1, 64)])
                P.tt("dve", rt2[:, :, 0:64], pv[:, :, 64:128], sin1, ALU.mult)
                P.tt("dve", rt2[:, :, 64:128], pv[:, :, 0:64], sin1, ALU.mult)
                rv = rqk[:, qi, :].re("p (h d) -> p h d", h=4)
                P.tt("pool", rt1[:, :, 0:64], rt1[:, :, 0:64], rt2[:, :, 0:64], ALU.subtract)
                P.tt("pool", rt1[:, :, 64:128], rt1[:, :, 64:128], rt2[:, :, 64:128], ALU.add)
                P.act(rv, rt1, AF.Copy, scale=(1.0 if qi == 0 else 128.0 ** -0.5))
            P.copy("pool", o_kt[:, t, 256:768], rqk[:, 1, :])
            pb = P.bank()
            pbb = bank_bf(pb)
            for qi in range(2):
                for h in range(4):
                    P.transpose(pbb[:, (qi * 4 + h) * 128:(qi * 4 + h + 1) * 128], rqk[:, qi, h * 128:(h + 1) * 128], ident_b)
            P.copy("dve", o_fm[:, t, 4:12, :], pbb.re("p (n t) -> p n t", t=128))
            pb = P.bank()
            pbb = bank_bf(pb)
            for c in range(8):
                P.transpose(pbb[:, c * 128:(c + 1) * 128], xbc[:, c, t * 128:(t + 1) * 128], ident_b)
            P.copy("dve", o_xv[:, t, 0:1024], pbb)
            pb = P.bank()
            pbb = bank_bf(pb)
            for c in range(2):
                P.transpose(pbb[:, c * 128:(c + 1) * 128], xbc[:, 8 + c, t * 128:(t + 1) * 128], ident_b)
            P.copy("dve", o_kt[:, t, 0:256], pbb[:, 0:256])
        for t in range(2):
            P.dma("sp", r_zr[ch0 + t], o_zr[:, t, :])
            P.dma("sp", r_xv[ch0 + t], o_xv[:, t, :])
            P.dma("sp", r_kt[ch0 + t], o_kt[:, t, :])
            P.dma("sp", r_fm[ch0 + t], o_fm[:, t])
            P.dma("sp", r_dt[ch0 + t], o_dt[:, t, :])

    nblk = len(blocks)
    load1(0)
    if nblk > 1:
        load1(1)
    stageA(0)
    for bi in range(nblk):
        if bi + 2 < nblk:
            load1(bi + 2)
        if bi + 1 < nblk:
            stageA(bi + 1)
        stageB(bi)
    P.close_scope()
    if stop_after == "P1":
        return _finish(P, nc, [r_zr, r_xv, r_kt, r_fm, r_dt])

    P.open_scope()
    wo0 = P.sb("wo0", [128, 16, D], BF16)
    for k in range(16):
        P.dma("pool", wo0[:, k, :], w_out0[k * 128:(k + 1) * 128, :])
    Er = P.sb("Er", [128, 2, 3, 4], F32)
    Dret = P.sb("Dret", [128, 2, 4, 128], F32)
    lmr = P.sb("lmr", [128, 4, 128], F32)
    for d in range(2):
        la = nla_ret[:, d * 4:(d + 1) * 4]
        pb = P.bank()
        mA, mT = (m_le, m_gt) if d == 0 else (m_ge, m_lt)
        P.mm(pb[:, 0:4], mA, la)
        P.mm(pb[:, 4:8], mT, la)
        P.mm(pb[:, 8:12], ones_f, la)
        P.act(Er[:, d].re("p a h -> p (a h)"), pb[:, 0:12], AF.Exp)
        mS, mR, mM = (m_gt, m_le, m_le) if d == 0 else (m_lt, m_ge, m_ge)
        P.tt("dve", lmr, bc(mS, [(0, 4), (1, 128)]), bc(la, [(1, 4), (0, 128)]), ALU.mult)
        pb = P.bank()
        for h in range(4):
            P.mm(pb[:, h * 128:(h + 1) * 128], lmr[:, h, :], mR)
        P.act(Dret[:, d].re("p h l -> p (h l)"), pb, AF.Exp)
        P.tt("dve", Dret[:, d], Dret[:, d], bc(mM, [(0, 4), (1, 128)]), ALU.mult)

    Hs = P.sb("Hs", [128, 1024], F32)
    Hr = P.sb("Hr", [128, 1024], F32)
    Hsb = P.sb("Hsb", [128, 1024], BF16)
    Hrb = P.sb("Hrb", [128, 1024], BF16)
    i_xv = [P.sb("i_xv%d" % i, [128, 2048], BF16) for i in range(2)]
    i_kt = [P.sb("i_kt%d" % i, [128, 768], BF16) for i in range(2)]
    i_fm = [P.sb("i_fm%d" % i, [128, 12, 128], BF16) for i in range(2)]
    i_dt = [P.sb("i_dt%d" % i, [128, 64], F32) for i in range(2)]
    i_zr = [P.sb("i_zr%d" % i, [128, 2048], BF16) for i in range(2)]
    i_yf = [P.sb("i_yf%d" % i, [128, 2048], F32) for i in range(2)]
    i_x = [P.sb("i_x%d" % i, [128, 8, 128], F32) for i in range(2)]
    E = P.sb("E", [128, 3, 16], F32)
    scm = P.sb("scm", [128, 2, 128], F32)
    Lm = P.sb("Lm", [128, 16, 128], F32)
    expD = P.sb("expD", [128, 16, 128], F32)
    MT = P.sb("MT", [128, 16, 128], BF16)
    MTr = P.sb("MTr", [128, 4, 128], BF16)
    xdt = P.sb("xdt", [128, 1024], BF16)
    xw = P.sb("xw", [128, 1024], BF16)
    rvw = P.sb("rvw", [128, 1024], BF16)
    wv = P.sb("wv", [128, 16], F32)
    ytmp = P.sb("ytmp", [128, 1024], F32)
    yo = [P.sb("yo%d" % i, [128, 2048], F32) for i in range(2)]
    sz = P.sb("sz", [128, 1024], F32)
    junk = P.sb("junk", [128, 1024], F32)
    ss = P.sb("ss", [128, 8], F32)
    ycat = P.sb("ycat", [128, 2048], BF16)
    ycT = P.sb("ycT", [128, 16, 128], BF16)
    xo = P.sb("xo", [128, 8, 128], F32)

    fwd_order = list(range(NCH))
    bwd_order = [1, 0] + list(range(NCH - 1, 1, -1))

    for d in range(2):
        order = fwd_order if d == 0 else bwd_order
        P.memset("dve", Hs, 0.0)
        P.memset("dve", Hr, 0.0)
        P.memset("pool", Hsb, 0.0)
        P.memset("pool", Hrb, 0.0)
        mA, mT = (m_le, m_gt) if d == 0 else (m_ge, m_lt)
        mS, mR, mM = (m_gt, m_le, m_le) if d == 0 else (m_lt, m_ge, m_ge)
        def load_sw(cj):
            ch_ = order[cj]
            b_ = cj % 2
            P.dma("sp", i_xv[b_], r_xv[ch_])
            P.dma("sp", i_kt[b_], r_kt[ch_])
            P.dma("sp", i_fm[b_], r_fm[ch_])
            P.dma("sp", i_dt[b_], r_dt[ch_])
            if d == 1:
                P.dma("sp", i_zr[b_], r_zr[ch_])
                P.dma("sp", i_yf[b_], r_yf[ch_])
                if ch_ < 2:
                    P.dma("sp", i_x[b_], cT[:, 1 + ch_ * 128:1 + (ch_ + 1) * 128].re("(k p) t -> p k t", p=128))
                else:
                    P.dma("sp", i_x[b_], xT[:, 1 + (ch_ - 2) * 128:1 + (ch_ - 1) * 128].re("(k p) t -> p k t", p=128))
        load_sw(0)
        for ci, ch in enumerate(order):
            b = ci % 2
            xv, kt, fm, dtt = i_xv[b], i_kt[b], i_fm[b], i_dt[b]
            if ci + 1 < len(order):
                load_sw(ci + 1)
            la = dtt[:, 32 + d * 16:32 + (d + 1) * 16]
            dtd = dtt[:, d * 16:(d + 1) * 16]
            xs = xv[:, 0:1024]
            rvv = xv[:, 1024:2048]
            yout = yo[ci % 2]
            pb = P.bank()
            P.mm(pb[:, 0:16], mA, la)
            P.mm(pb[:, 16:32], mT, la)
            P.mm(pb[:, 32:48], ones_f, la)
            P.act(E.re("p a h -> p (a h)"), pb[:, 0:48], AF.Exp)
            pb = P.bank()
            for g in range(2):
                P.mm(pb[:, g * 128:(g + 1) * 128], fm[:, g, :], fm[:, 2 + g, :])
            P.tt("dve", scm, pb[:, 0:256].re("p (g l) -> p g l", g=2), bc(mM, [(0, 2), (1, 128)]), ALU.mult)
            P.tt("pool", Lm, bc(mS, [(0, 16), (1, 128)]), bc(la, [(1, 16), (0, 128)]), ALU.mult)
            for q in range(4):
                pb = P.bank()
                for j in range(4):
                    P.mm(pb[:, j * 128:(j + 1) * 128], Lm[:, q * 4 + j, :], mR)
                P.act(expD[:, q * 4:(q + 1) * 4, :].re("p h l -> p (h l)"), pb, AF.Exp)
            for g in range(2):
                P.tt("dve", MT[:, g * 8:(g + 1) * 8, :], expD[:, g * 8:(g + 1) * 8, :], bc(scm[:, g, :], [(0, 8), (1, 128)]), ALU.mult)
            P.tt("pool", xdt.re("p (h d) -> p h d", h=16), xs.re("p (h d) -> p h d", h=16), bc(dtd, [(1, 16), (0, 64)]), ALU.mult)
            pd = [P.bank(), P.bank()]
            for h in range(16):
                P.mm(pd[h // 8][:, (h % 8) * 64:(h % 8 + 1) * 64], MT[:, h, :], xdt[:, h * 64:(h + 1) * 64])
            for g in range(2):
                po = P.bank()
                P.mm(po, fm[:, 2 + g, :], Hsb[:, g * 512:(g + 1) * 512])
                P.tt("dve", ytmp[:, g * 512:(g + 1) * 512].re("p (h d) -> p h d", h=8), po.re("p (h d) -> p h d", h=8),
                     bc(E[:, 0, g * 8:(g + 1) * 8], [(1, 8), (0, 64)]), ALU.mult)
                P.tt("dve", yout[:, g * 512:(g + 1) * 512], ytmp[:, g * 512:(g + 1) * 512], pd[g], ALU.add)
            P.tt("dve", wv, dtd, E[:, 1, :], ALU.mult)
            P.tt("pool", xw.re("p (h d) -> p h d", h=16), xs.re("p (h d) -> p h d", h=16), bc(wv, [(1, 16), (0, 64)]), ALU.mult)
            P.tt("dve", Hs.re("p (h d) -> p h d", h=16), Hs.re("p (h d) -> p h d", h=16), bc(E[:, 2, :], [(1, 16), (0, 64)]), ALU.mult)
            for g in range(2):
                pS = P.bank()
                P.mm(pS, kt[:, g * 128:(g + 1) * 128], xw[:, g * 512:(g + 1) * 512])
                P.tt("dve", Hs[:, g * 512:(g + 1) * 512], Hs[:, g * 512:(g + 1) * 512], pS, ALU.add)
            P.copy("act", Hsb, Hs)
            pb = P.bank()
            for h in range(4):
                P.mm(pb[:, h * 128:(h + 1) * 128], fm[:, 8 + h, :], fm[:, 4 + h, :])
            P.tt("dve", MTr, pb.re("p (h l) -> p h l", h=4), Dret[:, d], ALU.mult)
            pd = [P.bank(), P.bank()]
            for h in range(4):
                P.mm(pd[h // 2][:, (h % 2) * 256:(h % 2 + 1) * 256], MTr[:, h, :], rvv[:, h * 256:(h + 1) * 256])
            for g in range(2):
                po = P.bank()
                for hh in range(2):
                    h = g * 2 + hh
                    P.mm(po[:, hh * 256:(hh + 1) * 256], fm[:, 4 + h, :], Hrb[:, h * 256:(h + 1) * 256])
                P.tt("dve", ytmp[:, g * 512:(g + 1) * 512].re("p (h d) -> p h d", h=2), po.re("p (h d) -> p h d", h=2),
                     bc(Er[:, d, 0, g * 2:(g + 1) * 2], [(1, 2), (0, 256)]), ALU.mult)
                P.tt("dve", yout[:, 1024 + g * 512:1024 + (g + 1) * 512], ytmp[:, g * 512:(g + 1) * 512], pd[g], ALU.add)
            P.tt("pool", rvw.re("p (h d) -> p h d", h=4), rvv.re("p (h d) -> p h d", h=4), bc(Er[:, d, 1, :], [(1, 4), (0, 256)]), ALU.mult)
            P.tt("dve", Hr.re("p (h d) -> p h d", h=4), Hr.re("p (h d) -> p h d", h=4), bc(Er[:, d, 2, :], [(1, 4), (0, 256)]), ALU.mult)
            for g in range(2):
                pS = P.bank()
                for hh in range(2):
                    h = g * 2 + hh
                    P.mm(pS[:, hh * 256:(hh + 1) * 256], kt[:, 256 + h * 128:256 + (h + 1) * 128], rvw[:, h * 256:(h + 1) * 256])
                P.tt("dve", Hr[:, g * 512:(g + 1) * 512], Hr[:, g * 512:(g + 1) * 512], pS, ALU.add)
            P.copy("act", Hrb, Hr)
            if d == 0:
                P.dma("sp", r_yf[ch], yout)
                continue
            zr = i_zr[b]
            which = 1 if ch < 2 else 0
            P.tt("dve", yout, yout, i_yf[b], ALU.add)
            ys = yout[:, 0:1024]
            yr = yout[:, 1024:2048]
            P.tt("pool", ytmp.re("p (h d) -> p h d", h=16), xs.re("p (h d) -> p h d", h=16), bc(r_dsk, [(1, 16), (0, 64)]), ALU.mult)
            P.tt("dve", ys, ys, ytmp, ALU.add)
            P.act(sz, zr[:, 0:1024], AF.Silu)
            P.tt("dve", ys, ys, sz, ALU.mult)
            P.act(junk, ys, AF.Square)
            P.reduce("dve", ss[:, 0:1], junk, ALU.add)
            P.ts("dve", ss[:, 1:2], ss[:, 0:1], 1.0 / 1024, EPS, op0=ALU.mult, op1=ALU.add)
            P.rsqrt(ss[:, 1:2])
            P.stt("dve", ycat[:, 0:1024], ys, ss[:, 1:2], r_ssdn, ALU.mult, ALU.mult)
            P.act(junk, yr, AF.Square)
            P.reduce("dve", ss[:, 2:6], junk.re("p (h d) -> p h d", h=4), ALU.add)
            P.ts("dve", ss[:, 2:6], ss[:, 2:6], 1.0 / 256, EPS, op0=ALU.mult, op1=ALU.add)
            P.rsqrt(ss[:, 2:6])
            P.act(sz, zr[:, 1024:2048], AF.Silu)
            P.tt("dve", yr.re("p (h d) -> p h d", h=4), yr.re("p (h d) -> p h d", h=4), bc(ss[:, 2:6], [(1, 4), (0, 256)]), ALU.mult)
            P.tt("dve", ycat[:, 1024:2048], yr, sz, ALU.mult)
            for q in range(2):
                pb = P.bank()
                pbb = bank_bf(pb)
                for j in range(8):
                    P.transpose(pbb[:, j * 128:(j + 1) * 128], ycat[:, (q * 8 + j) * 128:(q * 8 + j + 1) * 128], ident_b)
                P.copy("act", ycT[:, q * 8:(q + 1) * 8, :].re("p n t -> p (n t)"), pbb)
            for q in range(2):
                pb = P.bank()
                for j in range(4):
                    dc = q * 4 + j
                    P.mmgroup(pb[:, j * 128:(j + 1) * 128], [(wo0[:, k, dc * 128:(dc + 1) * 128], ycT[:, k, :]) for k in range(16)])
                for j in range(4):
                    dc = q * 4 + j
                    P.stt("dve", xo[:, dc, :], pb[:, j * 128:(j + 1) * 128], G[0][:, dc, which, 0:1], i_x[b][:, dc, :], ALU.mult, ALU.add)
            P.dma("sp", x_mid[ch], xo)
    P.close_scope()
    if stop_after == "P3":
        return _finish(P, nc, [x_mid, r_yf])

    P.open_scope()
    NF = FFN_DENSE // 128
    wg = P.sb("wg", [128, 8, FFN_DENSE], BF16)
    wu = P.sb("wu", [128, 8, FFN_DENSE], BF16)
    wd = P.sb("wd", [128, NF, D], BF16)
    for k in range(8):
        P.dma("pool", wg[:, k, :], ffg[k * 128:(k + 1) * 128, :])
        P.dma("pool", wu[:, k, :], ffu[k * 128:(k + 1) * 128, :])
    for f in range(NF):
        P.dma("pool", wd[:, f, :], ffd[f * 128:(f + 1) * 128, :])
    xb4 = [P.sb("xb4_%d" % i, [128, 8, 256], F32) for i in range(2)]
    tmp4 = {"sq": P.sb("sq4", [128, 8, 256], F32), "rstd": P.sb("rstd4", [128, 256], F32)}
    h4 = P.sb("h4", [128, 8, 256], BF16)
    a4 = P.sb("a4", [128, NF, 256], BF16)
    sg4 = [P.sb("sg4_%d" % i, [128, 256], F32) for i in range(2)]
    xo4 = [P.sb("xo4_%d" % i, [128, 8, 256], F32) for i in range(1)]
    def load4(bj):
        for t in range(2):
            P.dma("sp", xb4[bj % 2][:, :, t * 128:(t + 1) * 128], x_mid[bj * 2 + t])
    load4(0)
    for bi in range(NCH // 2):
        x4 = xb4[bi % 2]
        which = 1 if bi == 0 else 0
        if bi + 1 < NCH // 2:
            load4(bi + 1)
        rms_modulate(x4, 256, AB[0][:, :, which, 1, :], h4, tmp4)
        for f in range(NF):
            pb = P.bank()
            P.mmgroup(pb[:, 0:256], [(wg[:, k, f * 128:(f + 1) * 128], h4[:, k, :]) for k in range(8)])
            P.mmgroup(pb[:, 256:512], [(wu[:, k, f * 128:(f + 1) * 128], h4[:, k, :]) for k in range(8)])
            sg = sg4[f % 2]
            P.act(sg, pb[:, 0:256], AF.Silu)
            P.tt("dve", a4[:, f, :], sg, pb[:, 256:512], ALU.mult)
        xo_ = xo4[0]
        for q in range(4):
            pb = P.bank()
            for j in range(2):
                dc = q * 2 + j
                P.mmgroup(pb[:, j * 256:(j + 1) * 256], [(wd[:, f, dc * 128:(dc + 1) * 128], a4[:, f, :]) for f in range(NF)])
            for j in range(2):
                dc = q * 2 + j
                P.stt("dve", xo_[:, dc, :], pb[:, j * 256:(j + 1) * 256], G[0][:, dc, which, 1:2], x4[:, dc, :], ALU.mult, ALU.add)
        for t in range(2):
            P.dma("sp", x_l1[bi * 2 + t], xo_[:, :, t * 128:(t + 1) * 128])
    P.close_scope()
    if stop_after == "P4":
        return _finish(P, nc, [x_l1])

    P.open_scope()
    w1 = P.sb("w1", [128, 8, 3072], BF16)
    for k in range(8):
        P.dma("pool", w1[:, k, :], w_in1[k * 128:(k + 1) * 128, :])
    xb5 = [P.sb("xb5_%d" % i, [128, 8, 256], F32) for i in range(2)]
    tmp5 = {"sq": P.sb("sq5", [128, 8, 256], F32), "rstd": P.sb("rstd5", [128, 256], F32)}
    h5 = P.sb("h5", [128, 8, 256], BF16)
    atab = P.sb("atab", [128, 2, 128], F32)
    qsq = P.sb("qsq", [128, 1024], F32)
    qn = P.sb("qn", [128, 1024], F32)
    q1 = P.sb("q1", [128, 1024], F32)
    q2 = P.sb("q2", [128, 1024], F32)
    ss5 = P.sb("ss5", [128, 16], F32)
    qkb = P.sb("qkb", [128, 2, 1024], BF16)
    qkT = P.sb("qkT", [128, 2, 8, 256], BF16)
    v5 = [P.sb("v5_%d" % i, [128, 2, 8, 130], BF16) for i in range(2)]
    for i in range(2):
        P.memset("dve", v5[i], 1.0)
    qg = P.sb("qg", [128, 64], F32)
    P.ts("dve", qg, r_qn, 64.0 ** -0.5, None, op0=ALU.mult)
    atabs = [atab, P.sb("atab2", [128, 2, 128], F32), P.sb("atab3", [128, 2, 128], F32)]
    h5s = [h5, P.sb("h5b", [128, 8, 256], BF16)]
    qsqs = [qsq, P.sb("qsq_b", [128, 1024], F32)]
    qns = [qn, P.sb("qn_b", [128, 1024], F32)]
    q1s = [q1, P.sb("q1_b", [128, 1024], F32)]
    q2s = [q2, P.sb("q2_b", [128, 1024], F32)]
    ss5s = [ss5, P.sb("ss5_b", [128, 16], F32)]
    NB5 = NCH // 2

    def load5(bj):
        for t in range(2):
            P.dma("sp", xb5[bj % 2][:, :, t * 128:(t + 1) * 128], x_l1[bj * 2 + t])
        if bj > 0:
            l0 = (bj - 1) * 256
            P.dma("sp", atabs[bj % 3], acs_d[l0:l0 + 256, :].re("(t p) c -> p t c", p=128))

    def stage5A(bj):
        which = 1 if bj == 0 else 0
        rms_modulate(xb5[bj % 2], 256, AB[1][:, :, which, 0, :], h5s[bj % 2], tmp5, eng="act")

    def stage5B(bi):
        h5 = h5s[bi % 2]
        atab = atabs[bi % 3]
        which = 1 if bi == 0 else 0
        vv = v5[bi % 2]
        qis = [1] if which == 1 else [0, 1]
        for t in range(2):
            lt = [h5[:, k, t * 128:(t + 1) * 128] for k in range(8)]
            pbs = {}
            for qi in qis:
                pbs[qi] = []
                for j in range(2):
                    pb = P.bank()
                    c0 = qi * 1024 + j * 512
                    P.mmgroup(pb, [(lt[k], w1[:, k, c0:c0 + 512]) for k in range(8)])
                    pbs[qi].append(pb)
            for qi in qis:
                for j in range(2):
                    P.act(qsqs[qi][:, j * 512:(j + 1) * 512], pbs[qi][j], AF.Square)
            for qi in qis:
                P.reduce("dve", ss5s[qi], qsqs[qi].re("p (g d) -> p g d", d=64), ALU.add)
                P.ts("dve", ss5s[qi], ss5s[qi], 1.0 / 64, EPS, op0=ALU.mult, op1=ALU.add)
            for qi in qis:
                P.rsqrt(ss5s[qi])
            for qi in qis:
                for j in range(2):
                    P.tt("dve", qns[qi][:, j * 512:(j + 1) * 512].re("p (g d) -> p g d", d=64), pbs[qi][j].re("p (g d) -> p g d", d=64),
                         bc(ss5s[qi][:, j * 8:(j + 1) * 8], [(1, 8), (0, 64)]), ALU.mult)
            pvs = []
            for j in range(2):
                pb = P.bank()
                c0 = 2048 + j * 512
                P.mmgroup(pb, [(lt[k], w1[:, k, c0:c0 + 512]) for k in range(8)])
                pvs.append(pb)
            for qi in qis:
                qn = qns[qi]
                gn = qg if qi == 0 else r_kn
                dst = qkb[:, qi, :]
                if which == 1:
                    P.tt("dve", dst.re("p (g d) -> p g d", d=64), qn.re("p (g d) -> p g d", d=64), bc(gn, [(0, 16), (1, 64)]), ALU.mult)
                else:
                    P.tt("dve", qn.re("p (g d) -> p g d", d=64), qn.re("p (g d) -> p g d", d=64), bc(gn, [(0, 16), (1, 64)]), ALU.mult)
            for j in range(2):
                P.copy("act", vv[:, t, j * 4:(j + 1) * 4, 0:128], pvs[j].re("p (h e) -> p h e", h=4))
            if which == 0:
                for qi in qis:
                    qn, q1, q2 = qns[qi], q1s[qi], q2s[qi]
                    P.tt("dve", q1.re("p (g d) -> p g d", d=64), qn.re("p (g d) -> p g d", d=64), bc(atab[:, t, 0:64], [(0, 16), (1, 64)]), ALU.mult)
                    qv = qn.re("p (g a u d) -> p g a u d", a=2, u=2, d=16)
                    q2v = q2.re("p (g a u d) -> p g a u d", a=2, u=2, d=16)
                    for s_ in range(2):
                        sn = bass.AP(atab.ap.tensor, atab[:, t, 64 + s_ * 16:64 + s_ * 16 + 16].ap.offset,
                                     [list(atab.ap.ap[0]), [0, 16], [32, 2], [1, 16]])
                        P.tt("pool", q2v[:, :, :, s_, :], qv[:, :, :, 1 - s_, :], TT(sn, atab.tok), ALU.mult)
                for qi in qis:
                    P.tt("dve", qkb[:, qi, :], q1s[qi], q2s[qi], ALU.add)
            for qi in qis:
                pb = P.bank()
                pbb = bank_bf(pb)
                for h in range(8):
                    P.transpose(pbb[:, h * 128:(h + 1) * 128], qkb[:, qi, h * 128:(h + 1) * 128], ident_b)
                P.copy("act", qkT[:, qi, :, t * 128:(t + 1) * 128], pbb.re("p (h t) -> p h t", h=8))
        tok0 = bi * 256
        P.dma("sp", Kd[:, :, tok0:tok0 + 256].re("h p t -> p h t"), qkT[:, 1])
        if which == 0:
            P.dma("sp", Qd[:, :, tok0 - CTX:tok0 - CTX + 256].re("h p t -> p h t"), qkT[:, 0])
        for t in range(2):
            P.dma("sp", Vd[:, :, bi * 2 + t, :].re("h p e -> p h e"), vv[:, t])

    load5(0)
    if NB5 > 1:
        load5(1)
    stage5A(0)
    for bi in range(NB5):
        if bi + 2 < NB5:
            load5(bi + 2)
        if bi + 1 < NB5:
            stage5A(bi + 1)
        stage5B(bi)
    P.close_scope()
    if stop_after == "P5":
        return _finish(P, nc, [Kd, Vd, Qd])

    P.open_scope()
    NKT = NCH
    NQB = OWN // 512
    lt_ = P.sb("lt_", [128, 128], F32)
    lam2 = P.sb("lam2", [128, 4], F32)
    P.tt("dve", lt_[:, 0:64], r_lam[:, 0:64], r_lam[:, 64:128], ALU.mult)
    P.tt("dve", lt_[:, 64:128], r_lam[:, 128:192], r_lam[:, 192:256], ALU.mult)
    P.reduce("dve", lam2[:, 0:2], lt_.re("p (a d) -> p a d", a=2), ALU.add)
    P.act(lam2[:, 0:2], lam2[:, 0:2], AF.Exp)
    P.tt("dve", lam2[:, 2:3], lam2[:, 1:2], lam2[:, 0:1], ALU.subtract)
    P.ts("dve", lam2[:, 3:4], lam2[:, 2:3], -LAM_INIT, None, op0=ALU.add)
    neglam = lam2[:, 3:4]
    sub_g = P.sb("sub_g", [128, 128], F32)
    P.ts("dve", sub_g, r_subln, 1.0 - LAM_INIT, None, op0=ALU.mult)
    Kh = [P.sb("Kh%d" % i, [128, T], BF16) for i in range(2)]
    Vh = [P.sb("Vh%d" % i, [128, NKT, 130], BF16) for i in range(2)]
    qa = [P.sb("qa%d" % i, [128, 512], BF16) for i in range(2)]
    qb_ = [P.sb("qb%d" % i, [128, 512], BF16) for i in range(2)]
    qs = [P.sb("qs%d" % i, [128, 512], BF16) for i in range(2)]
    pT = [P.sb("pT%d" % i, [128, 512], BF16) for i in range(4)]
    accs = [[P.sb("accs%d_%d" % (i, j), [128, 512], F32) for j in range(2)] for i in range(2)]
    o0 = P.sb("o0", [128, 512], F32)
    o1 = P.sb("o1", [128, 512], F32)
    rcp = P.sb("rcp", [128, 512], F32)
    osq = P.sb("osq", [128, 512], F32)
    oT = [P.sb("oT%d" % i, [128, 512], BF16) for i in range(2)]
    subg_fm = P.sb("subg_fm", [128, 1], F32)
    pbt = P.banks[7]
    P.transpose(pbt[:, 0:128], sub_g, ident_f)
    P.copy("dve", subg_fm, pbt[:, 0:1])
    spb = [P.banks[0], P.banks[1], P.banks[2], P.banks[3]]
    obk = [P.banks[4], P.banks[5]]
    aux = [P.banks[6], P.banks[7]]
    groups = [(h, qb) for h in range(8) for qb in range(NQB)]
    steps = [(m, kt) for m in range(2) for kt in range(NKT)]

    def load_kv(h):
        P.dma("sp", Kh[h % 2], Kd[h])
        P.dma("sp", Vh[h % 2], Vd[h])

    def load_q(gi):
        h, qb = groups[gi]
        A, B_, Q_ = qa[gi % 2], qb_[gi % 2], qs[gi % 2]
        P.dma("sp", A, Qd[h, :, qb * 512:(qb + 1) * 512])
        if L > OWN:
            P.dma("sp", B_, Qd[h, :, OWN + qb * 512:OWN + (qb + 1) * 512])
            P.ts("pool", Q_, A, sel_sb[:, 0:1], None, op0=ALU.mult)
            P.stt("dve", Q_, B_, sel_sb[:, 1:2], Q_, ALU.mult, ALU.add)
            return Q_
        return A

    load_kv(0)
    Qn = load_q(0)
    pi = 0
    for gi, (h, qb) in enumerate(groups):
        K_, V_ = Kh[h % 2], Vh[h % 2]
        Q_ = Qn
        if qb == 0 and h + 1 < 8:
            load_kv(h + 1)
        if gi + 1 < len(groups):
            Qn = load_q(gi + 1)

        def emit_s(i):
            m, kt = steps[i]
            P.mm(spb[(pi + i) % 4], K_[m * 64:(m + 1) * 64, kt * 128:(kt + 1) * 128], Q_[m * 64:(m + 1) * 64, :])
        emit_s(0)
        emit_s(1)
        emit_s(2)
        for i, (m, kt) in enumerate(steps):
            if i + 3 < len(steps):
                emit_s(i + 3)
            sp_ = spb[(pi + i) % 4]
            p_ = pT[(pi + i) % 4]
            ei = kt % 2
            eng = "dve"
            acc = accs[m][ei]
            P.act(p_, sp_, AF.Exp)
            P.mm(obk[m], V_[:, kt, 0:128], p_, start=(kt == 0), stop=(kt == NKT - 1))
            if kt < 2:
                P.copy(eng, acc, p_)
            else:
                P.tt(eng, acc, acc, p_, ALU.add)
            if kt == NKT - 1:
                ax = aux[m]
                P.mmgroup(ax, [(ones_f, accs[m][0]), (ones_f, accs[m][1])])
                P.recip(rcp, ax)
                if m == 0:
                    P.tt("dve", o0, obk[m], rcp, ALU.mult)
                else:
                    P.tt("dve", o1, obk[m], rcp, ALU.mult)
                    P.stt("dve", o0, o1, neglam, o0, ALU.mult, ALU.add)
        pi += len(steps)
        P.act(osq, o0, AF.Square)
        ax = aux[0]
        P.mm(ax, ones_f, osq)
        P.ts("dve", rcp, ax, 1.0 / 128, EPS, op0=ALU.mult, op1=ALU.add)
        P.rsqrt(rcp)
        o_ = oT[gi % 2]
        P.stt("dve", o_, o0, subg_fm[:, 0:1], rcp, ALU.mult, ALU.mult)
        P.dma("sp", Od[:, h, qb * 512:(qb + 1) * 512], o_)
    P.close_scope()
    if stop_after == "P6":
        return _finish(P, nc, [Od])

    P.open_scope()
    BLK = min(1024, OWN)
    NB = OWN // BLK
    NH = BLK // 512
    NT7 = BLK // 128
    wo1 = P.sb("wo1", [128, 8, D], BF16)
    for k in range(8):
        P.dma("pool", wo1[:, k, :], w_out1[k * 128:(k + 1) * 128, :])
    wr_sb = P.sb("wr_sb", [128, 8, 8], F32)
    P.dma("sp", wr_sb, wr.re("(k p) e -> p k e", p=128))
    selm = P.sb("selm", [8, 1024], F32)
    P.dma("sp", selm, selmat)
    o7 = P.sb("o7", [128, 8, BLK], BF16)
    xa = P.sb("xa", [128, 8, BLK], F32)
    xb7 = P.sb("xb7", [128, 8, 128], F32)
    sq7 = P.sb("sq7", [128, 8, 512], F32)
    rstd7 = P.sb("rstd7", [128, 512], F32)
    h7 = P.sb("h7", [128, 8, BLK], BF16)
    h7f = sq7
    yacc = P.sb("yacc", [128, 8, BLK], F32)
    lg = P.sb("lg", [128, 8], F32)
    lg2 = P.sb("lg2", [128, 8], F32)
    eq1 = P.sb("eq1", [128, 8], F32)
    eq2 = P.sb("eq2", [128, 8], F32)
    mx = P.sb("mx", [128, 8], F32)
    comb = P.sb("comb", [128, 8], F32)
    combT = P.sb("combT", [8, BLK], F32)
    cbc = [P.sb("cbc%d" % i, [128, BLK], BF16) for i in range(2)]
    FG = 2
    NFG = FEXP // (128 * FG)
    FW = 128 * FG
    wge = [P.sb("wge%d" % i, [128, 8, FW], BF16) for i in range(2)]
    wue = [P.sb("wue%d" % i, [128, 8, FW], BF16) for i in range(2)]
    wde = [P.sb("wde%d" % i, [128, FG, D], BF16) for i in range(2)]
    a7 = [P.sb("a7_%d" % i, [128, FG, BLK], BF16) for i in range(2)]
    sg7 = [P.sb("sg7_%d" % i, [128, 512], F32) for i in range(2)]
    t7 = [P.sb("t7_%d" % i, [128, 512], F32) for i in range(2)]
    its = [(nb, e, fg) for nb in range(NB) for e in range(NEXP) for fg in range(NFG)]

    def issue_w(ii):
        nb_, e_, fg_ = its[ii]
        b_ = ii % 2
        f0_ = fg_ * FW
        P.dma("pool", wge[b_], eg[e_, :, f0_:f0_ + FW].re("(k p) f -> p k f", p=128))
        P.dma("pool", wue[b_], eu[e_, :, f0_:f0_ + FW].re("(k p) f -> p k f", p=128))
        P.dma("pool", wde[b_], ed[e_, f0_:f0_ + FW, :].re("(f p) d -> p f d", p=128))
    issue_w(0)
    wi = 0
    for nb in range(NB):
        P.dma("sp", o7, Od[:, :, nb * BLK:(nb + 1) * BLK])
        for t in range(NT7):
            chA = 2 + (nb * BLK) // 128 + t
            P.dma("sp", xa[:, :, t * 128:(t + 1) * 128], x_l1[chA])
            if L > OWN:
                P.dma("sp", xb7, x_l1[chA + OWN // 128])
                P.ts("pool", xa[:, :, t * 128:(t + 1) * 128], xa[:, :, t * 128:(t + 1) * 128], sel_sb[:, 0:1], None, op0=ALU.mult)
                P.stt("pool", xa[:, :, t * 128:(t + 1) * 128], xb7, sel_sb[:, 1:2], xa[:, :, t * 128:(t + 1) * 128], ALU.mult, ALU.add)
        for hf in range(NH):
            cs = slice(hf * 512, (hf + 1) * 512)
            for dc in range(8):
                pb = P.bank()
                P.mmgroup(pb, [(wo1[:, hh, dc * 128:(dc + 1) * 128], o7[:, hh, cs]) for hh in range(8)])
                P.stt("dve", xa[:, dc, cs], pb, G[1][:, dc, 0, 0:1], xa[:, dc, cs], ALU.mult, ALU.add)
            P.act(sq7, xa[:, :, cs], AF.Square)
            pb = P.bank()
            P.mmgroup(pb, [(ones_f, sq7[:, k, :]) for k in range(8)])
            P.ts("dve", rstd7, pb, 1.0 / D, EPS, op0=ALU.mult, op1=ALU.add)
            P.rsqrt(rstd7)
            P.tt("dve", sq7, xa[:, :, cs], bc(rstd7, [(0, 8), (1, 512)]), ALU.mult)
            A2 = AB[1][:, :, 0, 1, :]
            for k in range(8):
                P.ts("pool", h7f[:, k, :], sq7[:, k, :], A2[:, k, 0:1], A2[:, k, 1:2], op0=ALU.mult, op1=ALU.add)
                P.copy("act", h7[:, k, cs], h7f[:, k, :])
            for t4 in range(4):
                pb = P.bank()
                P.mmgroup(pb[:, 0:8], [(h7f[:, k, t4 * 128:(t4 + 1) * 128], wr_sb[:, k, :]) for k in range(8)])
                P.copy("dve", lg, pb[:, 0:8])
                P.reduce("dve", mx[:, 0:1], lg, ALU.max)
                P.ts("dve", eq1, lg, mx[:, 0:1], None, op0=ALU.is_equal)
                P.stt("dve", lg2, eq1, -1e30, lg, ALU.mult, ALU.add)
                P.reduce("dve", mx[:, 1:2], lg2, ALU.max)
                P.ts("dve", eq2, lg2, mx[:, 1:2], None, op0=ALU.is_equal)
                P.tt("dve", mx[:, 2:3], mx[:, 1:2], mx[:, 0:1], ALU.subtract)
                P.act(mx[:, 3:4], mx[:, 2:3], AF.Exp)
                P.ts("dve", mx[:, 4:5], mx[:, 3:4], 1.0, None, op0=ALU.add)
                P.recip(mx[:, 5:6], mx[:, 4:5])
                P.tt("dve", mx[:, 6:7], mx[:, 3:4], mx[:, 5:6], ALU.mult)
                P.ts("dve", comb, eq1, mx[:, 5:6], None, op0=ALU.mult)
                P.stt("dve", comb, eq2, mx[:, 6:7], comb, ALU.mult, ALU.add)
                pb = P.bank()
                P.transpose(pb[0:8, 0:128], comb, ident_f)
                P.copy("dve", combT[:, hf * 512 + t4 * 128:hf * 512 + (t4 + 1) * 128], pb[0:8, 0:128])
        P.memset("pool", yacc, 0.0)
        for e in range(NEXP):
            cb = cbc[e % 2]
            for hf in range(NH):
                cs = slice(hf * 512, (hf + 1) * 512)
                pb = P.bank()
                P.mm(pb, selm[:, e * 128:(e + 1) * 128], combT[:, cs])
                P.copy("act", cb[:, cs], pb)
            for fg in range(NFG):
                b = wi % 2
                wi += 1
                if wi < len(its):
                    issue_w(wi)
                aa = a7[b]
                ii = 0
                for f in range(FG):
                    for hf in range(NH):
                        cs = slice(hf * 512, (hf + 1) * 512)
                        pg = P.bank()
                        pu = P.bank()
                        P.mmgroup(pg, [(wge[b][:, k, f * 128:(f + 1) * 128], h7[:, k, cs]) for k in range(8)])
                        P.mmgroup(pu, [(wue[b][:, k, f * 128:(f + 1) * 128], h7[:, k, cs]) for k in range(8)])
                        sg = sg7[ii % 2]
                        tt_ = t7[ii % 2]
                        ii += 1
                        P.act(sg, pg, AF.Silu)
                        P.tt("dve", tt_, sg, pu, ALU.mult)
                        P.tt("pool", aa[:, f, cs], tt_, cb[:, cs], ALU.mult)
                for hf in range(NH):
                    cs = slice(hf * 512, (hf + 1) * 512)
                    for dc in range(8):
                        pb = P.bank()
                        P.mmgroup(pb, [(wde[b][:, f, dc * 128:(dc + 1) * 128], aa[:, f, cs]) for f in range(FG)])
                        P.tt("dve", yacc[:, dc, cs], yacc[:, dc, cs], pb, ALU.add)
        for dc in range(8):
            P.stt("dve", yacc[:, dc, :], yacc[:, dc, :], G[1][:, dc, 0, 1:2], xa[:, dc, :], ALU.mult, ALU.add)
        P.dma("sp", outT[:, nb * BLK:(nb + 1) * BLK].re("(k p) t -> p k t", p=128), yacc)
    P.close_scope()
    return _finish(P, nc, [outT])


def P_sb_keep(P, name, shape):
    g = P.nc.sbuf_tensor(name, list(shape), F32)
    h = g.__enter__()
    P._ctx.insert(0, g)
    for i in range(len(P._scopes)):
        P._scopes[i] += 1
    return TT(h[:], Tok(name))


def _finish(P, nc, outs):
    P.fence("sp", outs)
    P.emit()
    P.close()
    return nc


def fm_vec(v):
    v = np.asarray(v, np.float32).reshape(-1, 128)
    return np.ascontiguousarray(v.T)


def prep_inputs(inp, L, OWN, ncores_per_batch, nbatch):
    cm, selm = host_consts()
    rcs, acs = rope_tables(L)
    shared = {
        "consts": cm, "selmat": selm, "rcs": rcs, "acs": acs,
        "w_mod0": np.ascontiguousarray(inp["even_w_mod"][0]), "w_mod1": np.ascontiguousarray(inp["odd_w_mod"][0]),
        "b_mod0": fm_vec(inp["even_b_mod"][0]), "b_mod1": fm_vec(inp["odd_b_mod"][0]),
        "norms": np.concatenate([fm_vec(inp["even_norm1"][0]), fm_vec(inp["even_norm2"][0]),
                                 fm_vec(inp["odd_norm1"][0]), fm_vec(inp["odd_norm2"][0])], axis=1),
        "w_in0": np.ascontiguousarray(inp["even_w_in"][0]),
        "w_out0": np.ascontiguousarray(inp["even_w_out"][0]),
        "ffg": np.ascontiguousarray(inp["even_ffn_gate"][0]), "ffu": np.ascontiguousarray(inp["even_ffn_up"][0]),
        "ffd": np.ascontiguousarray(inp["even_ffn_down"][0]),
        "w_in1": np.ascontiguousarray(inp["odd_w_in"][0]), "w_out1": np.ascontiguousarray(inp["odd_w_out"][0]),
        "router": np.ascontiguousarray(inp["odd_router"][0]),
        "eg": np.ascontiguousarray(inp["odd_exp_gate"][0]), "eu": np.ascontiguousarray(inp["odd_exp_up"][0]),
        "ed": np.ascontiguousarray(inp["odd_exp_down"][0]),
    }
    cw = np.concatenate([inp["even_conv_w"][0], inp["even_conv_b"][0][None, :]], axis=0)
    shared["convw"] = np.ascontiguousarray(cw.reshape(4, 12, 128).transpose(2, 1, 0))
    rowp = np.concatenate([
        inp["even_dt_bias"][0].reshape(-1), inp["even_a_log"][0].reshape(-1), inp["even_ret_decay"][0].reshape(-1),
        inp["even_d"][0].reshape(-1), inp["even_ssd_norm"][0].reshape(-1), inp["odd_q_norm"][0].reshape(-1),
        inp["odd_k_norm"][0].reshape(-1), inp["odd_lambda"][0].reshape(-1), inp["odd_subln"][0].reshape(-1)]).astype(np.float32)
    rp = np.zeros((1, 2560), np.float32)
    rp[0, :rowp.size] = rowp
    shared["rowp"] = rp
    maps = []
    for b in range(nbatch):
        xT = np.zeros((D, L + 2), np.float32)
        xT[:, 1:L + 1] = inp["x"][b].T
        cT = np.zeros((D, CTX + 2), np.float32)
        cT[:, 1:CTX + 1] = inp["ctx"][b].T
        cvec = np.concatenate([fm_vec(inp["c"][b]), fm_vec(inp["c_ctx"])], axis=1)
        for hf in range(ncores_per_batch):
            s = np.zeros((128, 2), np.float32)
            s[:, hf] = 1.0
            m = dict(shared)
            m.update({"xT": xT, "ctxT": cT, "cvec": cvec, "sel": s})
            maps.append(m)
    return maps


_NC_CACHE = {}


def kernel(**inputs):
    inp = {k: np.asarray(v) for k, v in inputs.items()}
    B, L, _ = inp["x"].shape
    OWN = L // 2
    key = (L, OWN)
    if key not in _NC_CACHE:
        _NC_CACHE[key] = build(L, OWN)
    nc = _NC_CACHE[key]
    maps = prep_inputs(inp, L, OWN, 2, B)
    res = run_bass_kernel_spmd(nc, maps, core_ids=list(range(len(maps))))
    out = np.empty((B, L, D), np.float32)
    for b in range(B):
        for hf in range(2):
            out[b, hf * OWN:(hf + 1) * OWN, :] = res.results[b * 2 + hf]["outT"].T
    return out
```

```python
import math
import numpy as np
import ml_dtypes
import concourse.bass as bass
import concourse.mybir as mybir
from concourse.bass_utils import run_bass_kernel_spmd

F32 = mybir.dt.float32
BF16 = mybir.dt.bfloat16
AF = mybir.ActivationFunctionType
ALU = mybir.AluOpType
AX = mybir.AxisListType

SAME_ENGINE_SYNC = True
NSLOT = 10


class Tok:
    __slots__ = ("lw", "rd", "name")

    def __init__(self, name=""):
        self.lw = None
        self.rd = []
        self.name = name


class TT:
    __slots__ = ("ap", "tok")

    def __init__(self, ap, tok):
        self.ap = ap
        self.tok = tok

    def __getitem__(self, k):
        return TT(self.ap[k], self.tok)

    def re(self, s, **kw):
        return TT(self.ap.rearrange(s, **kw), self.tok)

    @property
    def shape(self):
        return self.ap.shape


def bc(tt, dims):
    a = tt.ap
    base = list(a.ap)
    return TT(bass.AP(a.tensor, a.offset, [list(base[0])] + [list(d) for d in dims]), tt.tok)


class Op:
    __slots__ = ("stream", "fn", "deps", "dma", "ms", "slot", "val", "didx")

    def __init__(self, stream, fn, dma):
        self.stream = stream
        self.fn = fn
        self.deps = []
        self.dma = dma
        self.ms = False
        self.slot = None
        self.val = None
        self.didx = None


class Prog:
    STREAMS = ("pe", "act", "dve", "pool", "sp")

    def __init__(self, nc):
        self.nc = nc
        self.ops = {s: [] for s in self.STREAMS}
        self.ndma = {s: 0 for s in self.STREAMS}
        self.dmaops = {s: [] for s in self.STREAMS}
        self._ctx = []
        self._scopes = []
        self.banks = []
        self.bank_i = 0

    def sb(self, name, shape, dt=F32):
        g = self.nc.sbuf_tensor(name, list(shape), dt)
        h = g.__enter__()
        self._ctx.append(g)
        return TT(h[:], Tok(name))

    def ps(self, name, shape, dt=F32):
        g = self.nc.psum_tensor(name, list(shape), dt)
        h = g.__enter__()
        self._ctx.append(g)
        return TT(h[:], Tok(name))

    def dram(self, name, shape, dt=F32, kind="Internal"):
        h = self.nc.dram_tensor(name, list(shape), dt, kind=kind)
        return TT(h.ap(), Tok(name))

    def open_scope(self):
        self._scopes.append(len(self._ctx))

    def close_scope(self):
        n = self._scopes.pop()
        self.barrier()
        while len(self._ctx) > n:
            self._ctx.pop().__exit__(None, None, None)

    def close(self):
        while self._ctx:
            self._ctx.pop().__exit__(None, None, None)

    def bank(self):
        b = self.banks[self.bank_i % len(self.banks)]
        self.bank_i += 1
        return b

    def _rec(self, stream, fn, reads, writes, dma=False, extra=()):
        op = Op(stream, fn, dma)
        deps = list(extra)
        for t in reads:
            if t.tok.lw is not None:
                deps.append(t.tok.lw)
        for t in writes:
            if t.tok.lw is not None:
                deps.append(t.tok.lw)
            deps.extend(t.tok.rd)
        if dma:
            op.didx = self.ndma[stream]
            self.ndma[stream] += 1
            self.dmaops[stream].append(op)
            if op.didx >= NSLOT:
                deps.append(self.dmaops[stream][op.didx - NSLOT])
        seen = set()
        for d in deps:
            if d is op or id(d) in seen:
                continue
            if (not d.dma) and d.stream == stream and (stream == "pe" or not SAME_ENGINE_SYNC):
                continue
            seen.add(id(d))
            op.deps.append(d)
            d.ms = True
        for t in reads:
            t.tok.rd.append(op)
        for t in writes:
            t.tok.lw = op
            t.tok.rd = []
        self.ops[stream].append(op)
        return op

    def barrier(self):
        last = []
        for s in self.STREAMS:
            if self.ops[s]:
                for o in reversed(self.ops[s]):
                    if not o.dma and o.fn is not None:
                        last.append(o)
                        break
            last.extend(self.dmaops[s][-NSLOT:])
        for s in self.STREAMS:
            self._rec(s, None, [], [], extra=last)

    def mm(self, out, lhsT, rhs, start=True, stop=True):
        self._rec("pe", lambda e: e.matmul(out.ap, lhsT.ap, rhs.ap, start=start, stop=stop), [lhsT, rhs], [out])

    def mmgroup(self, out, pairs):
        rd = []
        for l, r in pairs:
            rd += [l, r]
        n = len(pairs)

        def fn(e):
            ins = None
            for i, (l, r) in enumerate(pairs):
                ins = e.matmul(out.ap, l.ap, r.ap, start=(i == 0), stop=(i == n - 1))
            return ins
        self._rec("pe", fn, rd, [out])

    def transpose(self, out, in_, ident):
        self._rec("pe", lambda e: e.transpose(out.ap, in_.ap, ident.ap), [in_, ident], [out])

    def act(self, out, in_, func, bias=None, scale=1.0, accum=None):
        rd = [in_]
        kw = {}
        if bias is not None:
            if isinstance(bias, TT):
                rd.append(bias)
                kw["bias"] = bias.ap
            else:
                kw["bias"] = bias
        if isinstance(scale, TT):
            rd.append(scale)
            kw["scale"] = scale.ap
        else:
            kw["scale"] = scale
        wr = [out]
        if accum is not None:
            wr.append(accum)
            kw["accum_out"] = accum.ap
        self._rec("act", lambda e: e.activation(out.ap, in_.ap, func, **kw), rd, wr)

    def tt(self, eng, out, a, b, op):
        self._rec(eng, lambda e: e.tensor_tensor(out.ap, a.ap, b.ap, op), [a, b], [out])

    def ts(self, eng, out, a, s1, s2=None, op0=ALU.mult, op1=None):
        rd = [a]
        s1a = s1.ap if isinstance(s1, TT) else s1
        s2a = s2.ap if isinstance(s2, TT) else s2
        if isinstance(s1, TT):
            rd.append(s1)
        if isinstance(s2, TT):
            rd.append(s2)
        kw = {}
        if op1 is not None:
            kw["op1"] = op1
        self._rec(eng, lambda e: e.tensor_scalar(out.ap, a.ap, s1a, s2a, op0, **kw), rd, [out])

    def stt(self, eng, out, a, s, b, op0, op1):
        rd = [a, b]
        sa = s.ap if isinstance(s, TT) else s
        if isinstance(s, TT):
            rd.append(s)
        self._rec("dve", lambda e: e.scalar_tensor_tensor(out.ap, a.ap, sa, b.ap, op0, op1), rd, [out])

    def copy(self, eng, out, a):
        if eng == "act":
            self._rec(eng, lambda e: e.copy(out.ap, a.ap), [a], [out])
        else:
            self._rec(eng, lambda e: e.tensor_copy(out.ap, a.ap), [a], [out])

    def memset(self, eng, out, val):
        self._rec(eng, lambda e: e.memset(out.ap, val), [], [out])

    def reduce(self, eng, out, a, op, axis=AX.X):
        self._rec(eng, lambda e: e.tensor_reduce(out.ap, a.ap, axis, op), [a], [out])

    def rsqrt(self, x):
        self._rec("act", lambda e: e.activation(x.ap, x.ap, AF.Sqrt), [x], [x])
        self._rec("dve", lambda e: e.reciprocal(x.ap, x.ap), [x], [x])

    def recip(self, out, a):
        self._rec("dve", lambda e: e.reciprocal(out.ap, a.ap), [a], [out])

    def dma(self, q, out, in_):
        self._rec(q, lambda e: e.dma_start(out.ap, in_.ap), [in_], [out], dma=True)

    def fence(self, stream, tts):
        self._rec(stream, None, list(tts), [])

    def emit(self):
        nc = self.nc
        for s in self.STREAMS:
            k = 0
            for op in self.ops[s]:
                if op.dma:
                    op.slot = op.didx % NSLOT
                    op.val = 16 * (op.didx // NSLOT + 1)
                elif op.ms:
                    k += 1
                    op.val = k
        sem_ctx = []
        csem = {}
        dsem = {}
        for s in self.STREAMS:
            g = nc.semaphore("c_" + s)
            csem[s] = g.__enter__()
            sem_ctx.append(g)
            if self.ndma[s] > 0:
                for i in range(NSLOT):
                    g = nc.semaphore("d_%s_%d" % (s, i))
                    dsem[(s, i)] = g.__enter__()
                    sem_ctx.append(g)
        ops = self.ops

        def run(stream, e):
            waited = {}
            for op in ops[stream]:
                for d in op.deps:
                    if d.dma:
                        key = ("d", d.stream, d.slot)
                        sem = dsem[(d.stream, d.slot)]
                    else:
                        key = ("c", d.stream)
                        sem = csem[d.stream]
                    if waited.get(key, 0) < d.val:
                        e.wait_ge(sem, d.val)
                        waited[key] = d.val
                if op.fn is None:
                    continue
                ins = op.fn(e)
                if op.dma:
                    ins.then_inc(dsem[(stream, op.slot)], 16)
                elif op.ms:
                    ins.then_inc(csem[stream], 1)

        with nc.Block() as block:
            @block.sync
            def _(e):
                run("sp", e)

            @block.tensor
            def _(e):
                run("pe", e)

            @block.scalar
            def _(e):
                run("act", e)

            @block.vector
            def _(e):
                run("dve", e)

            @block.gpsimd
            def _(e):
                run("pool", e)
        for g in reversed(sem_ctx):
            g.__exit__(None, None, None)


D = 1024
KC = 8
EPS = 1e-6
EVEN_IN = 5664
FFN_DENSE = 2816
NEXP = 8
FEXP = 3584
CTX = 256
GRID_W = 64
LAM_INIT = 0.8 - 0.6 * math.exp(-0.3 * 1)
C_Z, C_XBC, C_DT, C_RQ, C_RK, C_RV, C_RG = 0, 1024, 2560, 2592, 3104, 3616, 4640


def host_consts():
    k = np.arange(128)[:, None]
    l = np.arange(128)[None, :]
    c = {}
    c["ident"] = (k == l).astype(np.float32)
    c["le"] = (k <= l).astype(np.float32)
    c["gt"] = (k > l).astype(np.float32)
    c["ge"] = (k >= l).astype(np.float32)
    c["lt"] = (k < l).astype(np.float32)
    c["ones"] = np.ones((128, 128), np.float32)
    cm = np.concatenate([c[n] for n in ("ident", "le", "gt", "ge", "lt", "ones")], axis=1)
    sel = np.zeros((8, 8, 128), np.float32)
    for e in range(8):
        sel[e, e, :] = 1.0
    return cm, sel.reshape(8, 1024)


def rope_tables(L):
    f32 = np.float32
    inv = (np.float32(10000.0) ** (-np.arange(64, dtype=f32) / f32(64))).astype(f32)
    pos = np.arange(CTX + L, dtype=f32)
    ang = (pos[:, None] * inv[None, :]).astype(f32)
    rcs = np.concatenate([np.cos(ang), np.sin(ang)], axis=1).astype(f32)
    inv16 = (np.float32(10000.0) ** (-np.arange(16, dtype=f32) / f32(16))).astype(f32)
    t = np.arange(L)
    row = (t // GRID_W).astype(f32)
    col = (t % GRID_W).astype(f32)
    ar = (row[:, None] * inv16[None, :]).astype(f32)
    ac = (col[:, None] * inv16[None, :]).astype(f32)
    cos = np.concatenate([np.cos(ar), np.cos(ar), np.cos(ac), np.cos(ac)], axis=1)
    sins = np.concatenate([-np.sin(ar), np.sin(ar), -np.sin(ac), np.sin(ac)], axis=1)
    acs = np.concatenate([cos, sins], axis=1).astype(f32)
    return rcs, acs


def build(L=8192, OWN=4096, stop_after=None, dbg=()):
    nc = bass.Bass("TRN2", target_bir_lowering=False)
    P = Prog(nc)
    T = CTX + L
    NCH = T // 128
    NLB = L // 256
    dbg = set(dbg)

    def din(name, shape, dt=F32):
        return P.dram(name, shape, dt, kind="ExternalInput")

    xT = din("xT", [D, L + 2])
    cT = din("ctxT", [D, CTX + 2])
    cvec = din("cvec", [128, 16])
    sel = din("sel", [128, 2])
    consts = din("consts", [128, 768])
    selmat = din("selmat", [8, 1024])
    rcs_d = din("rcs", [T, 128])
    acs_d = din("acs", [L, 128])
    w_mod = [din("w_mod0", [D, 6 * D]), din("w_mod1", [D, 6 * D])]
    b_mod = [din("b_mod0", [128, 48]), din("b_mod1", [128, 48])]
    norms = din("norms", [128, 32])
    w_in0 = din("w_in0", [D, EVEN_IN])
    convw = din("convw", [128, 12, 4])
    rowp = din("rowp", [1, 2560])
    w_out0 = din("w_out0", [2048, D])
    ffg = din("ffg", [D, FFN_DENSE])
    ffu = din("ffu", [D, FFN_DENSE])
    ffd = din("ffd", [FFN_DENSE, D])
    w_in1 = din("w_in1", [D, 3072])
    w_out1 = din("w_out1", [D, D])
    wr = din("router", [D, NEXP])
    eg = din("eg", [NEXP, D, FEXP])
    eu = din("eu", [NEXP, D, FEXP])
    ed = din("ed", [NEXP, FEXP, D])
    outT = P.dram("outT", [D, OWN], F32, kind="ExternalOutput")

    def scratch(name, shape, dt=F32):
        return P.dram(name, shape, dt, kind=("ExternalOutput" if name in dbg else "Internal"))

    r_zr = scratch("r_zr", [NCH, 128, 2048], BF16)
    r_xv = scratch("r_xv", [NCH, 128, 2048], BF16)
    r_kt = scratch("r_kt", [NCH, 128, 768], BF16)
    r_fm = scratch("r_fm", [NCH, 128, 12, 128], BF16)
    r_dt = scratch("r_dt", [NCH, 128, 64], F32)
    r_yf = scratch("r_yf", [NCH, 128, 2048], F32)
    x_mid = scratch("x_mid", [NCH, 128, 8, 128], F32)
    x_l1 = scratch("x_l1", [NCH, 128, 8, 128], F32)
    Kd = scratch("Kd", [8, 128, T], BF16)
    Vd = scratch("Vd", [8, 128, NCH, 130], BF16)
    Qd = scratch("Qd", [8, 128, L], BF16)
    Od = scratch("Od", [128, 8, OWN], BF16)

    cst = P.sb("cst", [128, 768], F32)
    P.dma("sp", cst, consts)
    ident_f = cst[:, 0:128]
    m_le, m_gt, m_ge, m_lt, ones_f = (cst[:, 128 * i:128 * (i + 1)] for i in range(1, 6))
    cstb = P.sb("cstb", [128, 768], BF16)
    P.copy("dve", cstb, cst)
    ident_b = cstb[:, 0:128]
    sel_sb = P.sb("sel_sb", [128, 2], F32)
    P.dma("sp", sel_sb, sel)
    rows = P.sb("rows", [128, 2560], F32)
    P.dma("sp", rows, TT(rowp.ap.rearrange("a b -> (a b)").partition_broadcast(128), rowp.tok))
    norm_sb = P.sb("norm_sb", [128, 32], F32)
    P.dma("sp", norm_sb, norms)
    modfm = [P.sb("modfm0", [128, 48, 2], F32), P.sb("modfm1", [128, 48, 2], F32)]
    P.banks = [P.ps("bank%d" % i, [128, 512], F32) for i in range(8)]

    def bank_bf(b):
        a = b.ap
        return TT(a.bitcast(BF16), b.tok)

    AB = [P.sb("AB%d" % i, [128, 8, 2, 2, 2]) for i in range(2)]
    G = [P.sb("G%d" % i, [128, 8, 2, 2]) for i in range(2)]
    P.open_scope()
    cv = P.sb("cv", [128, 16], F32)
    P.dma("sp", cv, cvec)
    scv = P.sb("scv", [128, 8, 2], F32)
    P.act(scv[:, :, 0], cv[:, 0:8], AF.Silu)
    P.act(scv[:, :, 1], cv[:, 8:16], AF.Silu)
    wm = [P.sb("wm%d" % i, [128, 8, 512], F32) for i in range(2)]
    bm_sb = P.sb("bm_sb", [128, 2, 48], F32)
    P.dma("sp", bm_sb[:, 0, :], b_mod[0])
    P.dma("sp", bm_sb[:, 1, :], b_mod[1])
    it = 0
    for lyr in range(2):
        for cg in range(12):
            w = wm[it % 2]
            it += 1
            P.dma("sp", w, w_mod[lyr][:, cg * 512:(cg + 1) * 512].re("(k p) f -> p k f", p=128))
            pb = P.bank()
            for j in range(4):
                P.mmgroup(pb[:, 2 * j:2 * j + 2], [(w[:, k, j * 128:(j + 1) * 128], scv[:, k, :]) for k in range(8)])
            for j in range(4):
                ch = cg * 4 + j
                P.ts("dve", modfm[lyr][:, ch, :], pb[:, 2 * j:2 * j + 2], bm_sb[:, lyr, ch:ch + 1], None, op0=ALU.add)
    for lyr in range(2):
        for w in range(2):
            for n in range(2):
                shift = modfm[lyr][:, (3 * n) * 8:(3 * n) * 8 + 8, w]
                scale = modfm[lyr][:, (3 * n + 1) * 8:(3 * n + 1) * 8 + 8, w]
                gate = modfm[lyr][:, (3 * n + 2) * 8:(3 * n + 2) * 8 + 8, w]
                gain = norm_sb[:, (2 * lyr + n) * 8:(2 * lyr + n) * 8 + 8]
                P.stt("dve", AB[lyr][:, :, w, n, 0], scale, 1.0, gain, ALU.add, ALU.mult)
                P.copy("dve", AB[lyr][:, :, w, n, 1], shift)
                P.copy("dve", G[lyr][:, :, w, n], gate)
    P.close_scope()

    def rms_modulate(xin, ncol, Atab, out_bf, tmp, out_f32=None, eng="pool"):
        sq = tmp["sq"]
        P.act(sq[:, :, 0:ncol], xin, AF.Square)
        pb = P.bank()
        P.mmgroup(pb[:, 0:ncol], [(ones_f, sq[:, k, 0:ncol]) for k in range(8)])
        rstd = tmp["rstd"]
        P.ts("dve", rstd[:, 0:ncol], pb[:, 0:ncol], 1.0 / D, EPS, op0=ALU.mult, op1=ALU.add)
        P.rsqrt(rstd[:, 0:ncol])
        P.tt("dve", sq[:, :, 0:ncol], xin, bc(rstd[:, 0:ncol], [(0, 8), (1, ncol)]), ALU.mult)
        for k in range(8):
            if eng == "act" or (eng == "mix" and k % 2 == 0):
                P.act(out_bf[:, k, :], sq[:, k, 0:ncol], AF.Identity, bias=Atab[:, k, 1:2], scale=Atab[:, k, 0:1])
            else:
                P.ts("pool", out_bf[:, k, :], sq[:, k, 0:ncol], Atab[:, k, 0:1], Atab[:, k, 1:2], op0=ALU.mult, op1=ALU.add)
            if out_f32 is not None:
                P.ts("pool", out_f32[:, k, :], sq[:, k, 0:ncol], Atab[:, k, 0:1], Atab[:, k, 1:2], op0=ALU.mult, op1=ALU.add)

    r_dtb = rows[:, 0:32]
    r_alog = rows[:, 32:64]
    r_retd = rows[:, 64:72]
    r_dsk = rows[:, 72:88]
    r_ssdn = rows[:, 88:1112]
    r_qn = rows[:, 1112:1176]
    r_kn = rows[:, 1176:1240]
    r_lam = rows[:, 1240:1496]
    r_subln = rows[:, 1496:1624]
    ea = P.sb("ea", [128, 32], F32)
    P.act(ea, r_alog, AF.Exp)
    nla_ret = P.sb("nla_ret", [128, 8], F32)
    P.act(nla_ret, r_retd, AF.Exp)
    P.ts("dve", nla_ret, nla_ret, -1.0, None, op0=ALU.mult)

    P.open_scope()
    w0 = P.sb("w0", [128, 8, EVEN_IN], BF16)
    for k in range(8):
        for c0 in range(0, EVEN_IN, 1888):
            P.dma("pool", w0[:, k, c0:c0 + 1888], w_in0[k * 128:(k + 1) * 128, c0:c0 + 1888])
    cw = P.sb("cw", [128, 12, 4], F32)
    P.dma("sp", cw, convw)
    xin = [P.sb("xin%d" % i, [128, 8, 258], F32) for i in range(2)]
    xbr = P.sb("xbr", [128, 12, 258], F32)
    tmp1 = {"sq": xbr[:, 0:8, :], "rstd": P.sb("rstd1", [128, 258], F32)}
    hbs = [P.sb("hb%d" % i, [128, 8, 258], BF16) for i in range(2)]
    xbcs = [P.sb("xbc%d" % i, [128, 12, 256], BF16) for i in range(2)]
    cvts = [P.sb("cvt%d" % i, [128, 256], F32) for i in range(2)]
    o_zr = P.sb("o_zr", [128, 2, 2048], BF16)
    o_xv = P.sb("o_xv", [128, 2, 2048], BF16)
    o_kt = P.sb("o_kt", [128, 2, 768], BF16)
    o_fm = P.sb("o_fm", [128, 2, 12, 128], BF16)
    o_dt = P.sb("o_dt", [128, 2, 64], F32)
    rtabs = [P.sb("rtab%d" % i, [128, 2, 128], F32) for i in range(3)]
    rt1 = P.sb("rt1", [128, 4, 128], F32)
    rt2 = P.sb("rt2", [128, 4, 128], F32)
    rqk = P.sb("rqk", [128, 2, 512], BF16)
    sp1 = P.sb("sp1", [128, 32], F32)
    sp2 = P.sb("sp2", [128, 32], F32)

    blocks = [("c", 0)] + [("l", i) for i in range(NLB)]

    def binfo(bj):
        kind_, i_ = blocks[bj]
        if kind_ == "c":
            return 0, True, True, 1
        return CTX + i_ * 256, (i_ == 0), (i_ == NLB - 1), 0

    def load1(bj):
        kind_, i_ = blocks[bj]
        if kind_ == "c":
            P.dma("sp", xin[bj % 2], cT.re("(k p) t -> p k t", p=128))
        else:
            P.dma("sp", xin[bj % 2], xT[:, i_ * 256:i_ * 256 + 258].re("(k p) t -> p k t", p=128))
        t0_ = binfo(bj)[0]
        P.dma("sp", rtabs[bj % 3], rcs_d[t0_:t0_ + 256, :].re("(t p) c -> p t c", p=128))

    def stageA(bj):
        tok0, first, last, which = binfo(bj)
        xi, hb, xbc = xin[bj % 2], hbs[bj % 2], xbcs[bj % 2]
        rms_modulate(xi, 258, AB[0][:, :, which, 0, :], hb, tmp1, eng="act")
        for c in range(12):
            pb = P.bank()
            P.mmgroup(pb[:, 0:258], [(w0[:, k, C_XBC + c * 128:C_XBC + (c + 1) * 128], hb[:, k, :]) for k in range(8)])
            P.copy("act", xbr[:, c, :], pb[:, 0:258])
        if first:
            P.memset("pool", xbr[:, :, 0:1], 0.0)
        if last:
            P.memset("pool", xbr[:, :, 257:258], 0.0)
        for c in range(12):
            cvt = cvts[c % 2]
            P.ts("pool", cvt, xbr[:, c, 0:256], cw[:, c, 0:1], None, op0=ALU.mult)
            P.stt("dve", cvt, xbr[:, c, 1:257], cw[:, c, 1:2], cvt, ALU.mult, ALU.add)
            P.stt("dve", cvt, xbr[:, c, 2:258], cw[:, c, 2:3], cvt, ALU.mult, ALU.add)
            P.act(xbc[:, c, :], cvt, AF.Silu, bias=cw[:, c, 3:4])

    def stageB(bj):
        tok0, first, last, which = binfo(bj)
        hb, xbc, rtab = hbs[bj % 2], xbcs[bj % 2], rtabs[bj % 3]
        ch0 = tok0 // 128
        for t in range(2):
            P.copy("pool", o_fm[:, t, 0:4, :], xbc[:, 8:12, t * 128:(t + 1) * 128])
        for t in range(2):
            lt = [hb[:, k, 1 + t * 128:1 + (t + 1) * 128] for k in range(8)]

            def proj(c0, n):
                pb = P.bank()
                P.mmgroup(pb[:, 0:n], [(lt[k], w0[:, k, c0:c0 + n]) for k in range(8)])
                return pb
            for j in range(2):
                pb = proj(C_Z + j * 512, 512)
                P.copy("act", o_zr[:, t, j * 512:(j + 1) * 512], pb)
            for j in range(2):
                pb = proj(C_RG + j * 512, 512)
                P.copy("act", o_zr[:, t, 1024 + j * 512:1024 + (j + 1) * 512], pb)
            for j in range(2):
                pb = proj(C_RV + j * 512, 512)
                P.copy("act", o_xv[:, t, 1024 + j * 512:1024 + (j + 1) * 512], pb)
            pb = proj(C_DT, 32)
            P.tt("dve", sp1, pb[:, 0:32], r_dtb, ALU.add)
            P.act(sp2, sp1, AF.Abs)
            P.act(sp2, sp2, AF.Exp, scale=-1.0)
            P.act(sp2, sp2, AF.Ln, bias=1.0)
            P.stt("dve", o_dt[:, t, 0:32], sp1, 0.0, sp2, ALU.max, ALU.add)
            P.tt("dve", sp1, o_dt[:, t, 0:32], ea, ALU.mult)
            P.ts("dve", o_dt[:, t, 32:64], sp1, -1.0, None, op0=ALU.mult)
            for qi, c0 in enumerate((C_RQ, C_RK)):
                pb = proj(c0, 512)
                pv = pb.re("p (h d) -> p h d", h=4)
                cos2 = bc(rtab[:, t, 0:64], [(0, 4), (0, 2), (1, 64)])
                P.tt("dve", rt1.re("p h (a d) -> p h a d", a=2), pv.re("p h (a d) -> p h a d", a=2), cos2, ALU.mult)
                sin1 = bc(rtab[:, t, 64:128], [(0, 4), (1, 64)])
                P.tt("dve", rt2[:, :, 0:64], pv[:, :, 64:128], sin1, ALU.mult)
                P.tt("dve", rt2[:, :, 64:128], pv[:, :, 0:64], sin1, ALU.mult)
                rv = rqk[:, qi, :].re("p (h d) -> p h d", h=4)
                P.tt("pool", rt1[:, :, 0:64], rt1[:, :, 0:64], rt2[:, :, 0:64], ALU.subtract)
                P.tt("pool", rt1[:, :, 64:128], rt1[:, :, 64:128], rt2[:, :, 64:128], ALU.add)
                P.act(rv, rt1, AF.Copy, scale=(1.0 if qi == 0 else 128.0 ** -0.5))
            P.copy("pool", o_kt[:, t, 256:768], rqk[:, 1, :])
            pb = P.bank()
            pbb = bank_bf(pb)
            for qi in range(2):
                for h in range(4):
                    P.transpose(pbb[:, (qi * 4 + h) * 128:(qi * 4 + h + 1) * 128], rqk[:, qi, h * 128:(h + 1) * 128], ident_b)
            P.copy("dve", o_fm[:, t, 4:12, :], pbb.re("p (n t) -> p n t", t=128))
            pb = P.bank()
            pbb = bank_bf(pb)
            for c in range(8):
                P.transpose(pbb[:, c * 128:(c + 1) * 128], xbc[:, c, t * 128:(t + 1) * 128], ident_b)
            P.copy("dve", o_xv[:, t, 0:1024], pbb)
            pb = P.bank()
            pbb = bank_bf(pb)
            for c in range(2):
                P.transpose(pbb[:, c * 128:(c + 1) * 128], xbc[:, 8 + c, t * 128:(t + 1) * 128], ident_b)
            P.copy("dve", o_kt[:, t, 0:256], pbb[:, 0:256])
        for t in range(2):
            P.dma("sp", r_zr[ch0 + t], o_zr[:, t, :])
            P.dma("sp", r_xv[ch0 + t], o_xv[:, t, :])
            P.dma("sp", r_kt[ch0 + t], o_kt[:, t, :])
            P.dma("sp", r_fm[ch0 + t], o_fm[:, t])
            P.dma("sp", r_dt[ch0 + t], o_dt[:, t, :])

    nblk = len(blocks)
    load1(0)
    if nblk > 1:
        load1(1)
    stageA(0)
    for bi in range(nblk):
        if bi + 2 < nblk:
            load1(bi + 2)
        if bi + 1 < nblk:
            stageA(bi + 1)
        stageB(bi)
    P.close_scope()
    if stop_after == "P1":
        return _finish(P, nc, [r_zr, r_xv, r_kt, r_fm, r_dt])

    P.open_scope()
    wo0 = P.sb("wo0", [128, 16, D], BF16)
    for k in range(16):
        P.dma("pool", wo0[:, k, :], w_out0[k * 128:(k + 1) * 128, :])
    Er = P.sb("Er", [128, 2, 3, 4], F32)
    Dret = P.sb("Dret", [128, 2, 4, 128], F32)
    lmr = P.sb("lmr", [128, 4, 128], F32)
    for d in range(2):
        la = nla_ret[:, d * 4:(d + 1) * 4]
        pb = P.bank()
        mA, mT = (m_le, m_gt) if d == 0 else (m_ge, m_lt)
        P.mm(pb[:, 0:4], mA, la)
        P.mm(pb[:, 4:8], mT, la)
        P.mm(pb[:, 8:12], ones_f, la)
        P.act(Er[:, d].re("p a h -> p (a h)"), pb[:, 0:12], AF.Exp)
        mS, mR, mM = (m_gt, m_le, m_le) if d == 0 else (m_lt, m_ge, m_ge)
        P.tt("dve", lmr, bc(mS, [(0, 4), (1, 128)]), bc(la, [(1, 4), (0, 128)]), ALU.mult)
        pb = P.bank()
        for h in range(4):
            P.mm(pb[:, h * 128:(h + 1) * 128], lmr[:, h, :], mR)
        P.act(Dret[:, d].re("p h l -> p (h l)"), pb, AF.Exp)
        P.tt("dve", Dret[:, d], Dret[:, d], bc(mM, [(0, 4), (1, 128)]), ALU.mult)

    Hs = P.sb("Hs", [128, 1024], F32)
    Hr = P.sb("Hr", [128, 1024], F32)
    Hsb = P.sb("Hsb", [128, 1024], BF16)
    Hrb = P.sb("Hrb", [128, 1024], BF16)
    i_xv = [P.sb("i_xv%d" % i, [128, 2048], BF16) for i in range(2)]
    i_kt = [P.sb("i_kt%d" % i, [128, 768], BF16) for i in range(2)]
    i_fm = [P.sb("i_fm%d" % i, [128, 12, 128], BF16) for i in range(2)]
    i_dt = [P.sb("i_dt%d" % i, [128, 64], F32) for i in range(2)]
    i_zr = [P.sb("i_zr%d" % i, [128, 2048], BF16) for i in range(2)]
    i_yf = [P.sb("i_yf%d" % i, [128, 2048], F32) for i in range(2)]
    i_x = [P.sb("i_x%d" % i, [128, 8, 128], F32) for i in range(2)]
    E = P.sb("E", [128, 3, 16], F32)
    scm = P.sb("scm", [128, 2, 128], F32)
    Lm = P.sb("Lm", [128, 16, 128], F32)
    expD = P.sb("expD", [128, 16, 128], F32)
    MT = P.sb("MT", [128, 16, 128], BF16)
    MTr = P.sb("MTr", [128, 4, 128], BF16)
    xdt = P.sb("xdt", [128, 1024], BF16)
    xw = P.sb("xw", [128, 1024], BF16)
    rvw = P.sb("rvw", [128, 1024], BF16)
    wv = P.sb("wv", [128, 16], F32)
    ytmp = P.sb("ytmp", [128, 1024], F32)
    yo = [P.sb("yo%d" % i, [128, 2048], F32) for i in range(2)]
    sz = P.sb("sz", [128, 1024], F32)
    junk = P.sb("junk", [128, 1024], F32)
    ss = P.sb("ss", [128, 8], F32)
    ycat = P.sb("ycat", [128, 2048], BF16)
    ycT = P.sb("ycT", [128, 16, 128], BF16)
    xo = P.sb("xo", [128, 8, 128], F32)

    fwd_order = list(range(NCH))
    bwd_order = [1, 0] + list(range(NCH - 1, 1, -1))

    for d in range(2):
        order = fwd_order if d == 0 else bwd_order
        P.memset("dve", Hs, 0.0)
        P.memset("dve", Hr, 0.0)
        P.memset("pool", Hsb, 0.0)
        P.memset("pool", Hrb, 0.0)
        mA, mT = (m_le, m_gt) if d == 0 else (m_ge, m_lt)
        mS, mR, mM = (m_gt, m_le, m_le) if d == 0 else (m_lt, m_ge, m_ge)
        def load_sw(cj):
            ch_ = order[cj]
            b_ = cj % 2
            P.dma("sp", i_xv[b_], r_xv[ch_])
            P.dma("sp", i_kt[b_], r_kt[ch_])
            P.dma("sp", i_fm[b_], r_fm[ch_])
            P.dma("sp", i_dt[b_], r_dt[ch_])

        def load_fin(cj):
            ch_ = order[cj]
            b_ = cj % 2
            P.dma("sp", i_zr[b_], r_zr[ch_])
            P.dma("sp", i_yf[b_], r_yf[ch_])
            if ch_ < 2:
                P.dma("sp", i_x[b_], cT[:, 1 + ch_ * 128:1 + (ch_ + 1) * 128].re("(k p) t -> p k t", p=128))
            else:
                P.dma("sp", i_x[b_], xT[:, 1 + (ch_ - 2) * 128:1 + (ch_ - 1) * 128].re("(k p) t -> p k t", p=128))

        def finish(cj):
            ch = order[cj]
            b = cj % 2
            yout = yo[cj % 2]
            zr = i_zr[b]
            which = 1 if ch < 2 else 0
            P.tt("dve", yout, yout, i_yf[b], ALU.add)
            ys = yout[:, 0:1024]
            yr = yout[:, 1024:2048]
            P.act(sz, zr[:, 0:1024], AF.Silu)
            P.tt("dve", ys, ys, sz, ALU.mult)
            P.act(junk, ys, AF.Square)
            P.reduce("dve", ss[:, 0:1], junk, ALU.add)
            P.ts("dve", ss[:, 1:2], ss[:, 0:1], 1.0 / 1024, EPS, op0=ALU.mult, op1=ALU.add)
            P.rsqrt(ss[:, 1:2])
            P.stt("dve", ycat[:, 0:1024], ys, ss[:, 1:2], r_ssdn, ALU.mult, ALU.mult)
            P.act(junk, yr, AF.Square)
            P.reduce("dve", ss[:, 2:6], junk.re("p (h d) -> p h d", h=4), ALU.add)
            P.ts("dve", ss[:, 2:6], ss[:, 2:6], 1.0 / 256, EPS, op0=ALU.mult, op1=ALU.add)
            P.rsqrt(ss[:, 2:6])
            P.act(sz, zr[:, 1024:2048], AF.Silu)
            P.tt("dve", yr.re("p (h d) -> p h d", h=4), yr.re("p (h d) -> p h d", h=4), bc(ss[:, 2:6], [(1, 4), (0, 256)]), ALU.mult)
            P.tt("dve", ycat[:, 1024:2048], yr, sz, ALU.mult)
            for q in range(2):
                pb = P.bank()
                pbb = bank_bf(pb)
                for j in range(8):
                    P.transpose(pbb[:, j * 128:(j + 1) * 128], ycat[:, (q * 8 + j) * 128:(q * 8 + j + 1) * 128], ident_b)
                P.copy("act", ycT[:, q * 8:(q + 1) * 8, :].re("p n t -> p (n t)"), pbb)
            for q in range(2):
                pb = P.bank()
                for j in range(4):
                    dc = q * 4 + j
                    P.mmgroup(pb[:, j * 128:(j + 1) * 128], [(wo0[:, k, dc * 128:(dc + 1) * 128], ycT[:, k, :]) for k in range(16)])
                for j in range(4):
                    dc = q * 4 + j
                    P.stt("dve", xo[:, dc, :], pb[:, j * 128:(j + 1) * 128], G[0][:, dc, which, 0:1], i_x[b][:, dc, :], ALU.mult, ALU.add)
            P.dma("sp", x_mid[ch], xo)

        load_sw(0)
        for ci, ch in enumerate(order):
            b = ci % 2
            xv, kt, fm, dtt = i_xv[b], i_kt[b], i_fm[b], i_dt[b]
            if ci + 1 < len(order):
                load_sw(ci + 1)
            if d == 1:
                load_fin(ci)
            la = dtt[:, 32 + d * 16:32 + (d + 1) * 16]
            dtd = dtt[:, d * 16:(d + 1) * 16]
            xs = xv[:, 0:1024]
            rvv = xv[:, 1024:2048]
            yout = yo[ci % 2]
            pb = P.bank()
            P.mm(pb[:, 0:16], mA, la)
            P.mm(pb[:, 16:32], mT, la)
            P.mm(pb[:, 32:48], ones_f, la)
            P.act(E.re("p a h -> p (a h)"), pb[:, 0:48], AF.Exp)
            pb = P.bank()
            for g in range(2):
                P.mm(pb[:, g * 128:(g + 1) * 128], fm[:, g, :], fm[:, 2 + g, :])
            P.tt("dve", scm, pb[:, 0:256].re("p (g l) -> p g l", g=2), bc(mM, [(0, 2), (1, 128)]), ALU.mult)
            P.tt("pool", Lm, bc(mS, [(0, 16), (1, 128)]), bc(la, [(1, 16), (0, 128)]), ALU.mult)
            for q in range(4):
                pb = P.bank()
                for j in range(4):
                    P.mm(pb[:, j * 128:(j + 1) * 128], Lm[:, q * 4 + j, :], mR)
                P.act(expD[:, q * 4:(q + 1) * 4, :].re("p h l -> p (h l)"), pb, AF.Exp)
            for g in range(2):
                P.tt("dve", MT[:, g * 8:(g + 1) * 8, :], expD[:, g * 8:(g + 1) * 8, :], bc(scm[:, g, :], [(0, 8), (1, 128)]), ALU.mult)
            P.tt("pool", xdt.re("p (h d) -> p h d", h=16), xs.re("p (h d) -> p h d", h=16), bc(dtd, [(1, 16), (0, 64)]), ALU.mult)
            pd = [P.bank(), P.bank()]
            for h in range(16):
                P.mm(pd[h // 8][:, (h % 8) * 64:(h % 8 + 1) * 64], MT[:, h, :], xdt[:, h * 64:(h + 1) * 64])
            for g in range(2):
                po = P.bank()
                P.mm(po, fm[:, 2 + g, :], Hsb[:, g * 512:(g + 1) * 512])
                P.tt("dve", ytmp[:, g * 512:(g + 1) * 512].re("p (h d) -> p h d", h=8), po.re("p (h d) -> p h d", h=8),
                     bc(E[:, 0, g * 8:(g + 1) * 8], [(1, 8), (0, 64)]), ALU.mult)
                P.tt("dve", yout[:, g * 512:(g + 1) * 512], ytmp[:, g * 512:(g + 1) * 512], pd[g], ALU.add)
            P.tt("dve", wv, dtd, E[:, 1, :], ALU.mult)
            P.tt("pool", xw.re("p (h d) -> p h d", h=16), xs.re("p (h d) -> p h d", h=16), bc(wv, [(1, 16), (0, 64)]), ALU.mult)
            P.tt("dve", Hs.re("p (h d) -> p h d", h=16), Hs.re("p (h d) -> p h d", h=16), bc(E[:, 2, :], [(1, 16), (0, 64)]), ALU.mult)
            for g in range(2):
                pS = P.bank()
                P.mm(pS, kt[:, g * 128:(g + 1) * 128], xw[:, g * 512:(g + 1) * 512])
                P.tt("dve", Hs[:, g * 512:(g + 1) * 512], Hs[:, g * 512:(g + 1) * 512], pS, ALU.add)
            P.copy("act", Hsb, Hs)
            pb = P.bank()
            for h in range(4):
                P.mm(pb[:, h * 128:(h + 1) * 128], fm[:, 8 + h, :], fm[:, 4 + h, :])
            P.tt("dve", MTr, pb.re("p (h l) -> p h l", h=4), Dret[:, d], ALU.mult)
            pd = [P.bank(), P.bank()]
            for h in range(4):
                P.mm(pd[h // 2][:, (h % 2) * 256:(h % 2 + 1) * 256], MTr[:, h, :], rvv[:, h * 256:(h + 1) * 256])
            for g in range(2):
                po = P.bank()
                for hh in range(2):
                    h = g * 2 + hh
                    P.mm(po[:, hh * 256:(hh + 1) * 256], fm[:, 4 + h, :], Hrb[:, h * 256:(h + 1) * 256])
                P.tt("dve", ytmp[:, g * 512:(g + 1) * 512].re("p (h d) -> p h d", h=2), po.re("p (h d) -> p h d", h=2),
                     bc(Er[:, d, 0, g * 2:(g + 1) * 2], [(1, 2), (0, 256)]), ALU.mult)
                P.tt("dve", yout[:, 1024 + g * 512:1024 + (g + 1) * 512], ytmp[:, g * 512:(g + 1) * 512], pd[g], ALU.add)
            P.tt("pool", rvw.re("p (h d) -> p h d", h=4), rvv.re("p (h d) -> p h d", h=4), bc(Er[:, d, 1, :], [(1, 4), (0, 256)]), ALU.mult)
            P.tt("dve", Hr.re("p (h d) -> p h d", h=4), Hr.re("p (h d) -> p h d", h=4), bc(Er[:, d, 2, :], [(1, 4), (0, 256)]), ALU.mult)
            for g in range(2):
                pS = P.bank()
                for hh in range(2):
                    h = g * 2 + hh
                    P.mm(pS[:, hh * 256:(hh + 1) * 256], kt[:, 256 + h * 128:256 + (h + 1) * 128], rvw[:, h * 256:(h + 1) * 256])
                P.tt("dve", Hr[:, g * 512:(g + 1) * 512], Hr[:, g * 512:(g + 1) * 512], pS, ALU.add)
            P.copy("act", Hrb, Hr)
            if d == 0:
                P.dma("sp", r_yf[ch], yout)
                continue
            P.tt("pool", ytmp.re("p (h d) -> p h d", h=16), xs.re("p (h d) -> p h d", h=16), bc(r_dsk, [(1, 16), (0, 64)]), ALU.mult)
            P.tt("dve", yout[:, 0:1024], yout[:, 0:1024], ytmp, ALU.add)
            if ci >= 1:
                finish(ci - 1)
        if d == 1:
            finish(len(order) - 1)
    P.close_scope()
    if stop_after == "P3":
        return _finish(P, nc, [x_mid, r_yf])

    P.open_scope()
    NF = FFN_DENSE // 128
    wg = P.sb("wg", [128, 8, FFN_DENSE], BF16)
    wu = P.sb("wu", [128, 8, FFN_DENSE], BF16)
    wd = P.sb("wd", [128, NF, D], BF16)
    for k in range(8):
        P.dma("pool", wg[:, k, :], ffg[k * 128:(k + 1) * 128, :])
        P.dma("pool", wu[:, k, :], ffu[k * 128:(k + 1) * 128, :])
    for f in range(NF):
        P.dma("pool", wd[:, f, :], ffd[f * 128:(f + 1) * 128, :])
    xb4 = [P.sb("xb4_%d" % i, [128, 8, 256], F32) for i in range(2)]
    tmp4 = {"sq": P.sb("sq4", [128, 8, 256], F32), "rstd": P.sb("rstd4", [128, 256], F32)}
    h4 = P.sb("h4", [128, 8, 256], BF16)
    a4 = P.sb("a4", [128, NF, 256], BF16)
    sg4 = [P.sb("sg4_%d" % i, [128, 256], F32) for i in range(2)]
    xo4 = [P.sb("xo4_%d" % i, [128, 8, 256], F32) for i in range(1)]
    def load4(bj):
        for t in range(2):
            P.dma("sp", xb4[bj % 2][:, :, t * 128:(t + 1) * 128], x_mid[bj * 2 + t])
    load4(0)
    for bi in range(NCH // 2):
        x4 = xb4[bi % 2]
        which = 1 if bi == 0 else 0
        if bi + 1 < NCH // 2:
            load4(bi + 1)
        rms_modulate(x4, 256, AB[0][:, :, which, 1, :], h4, tmp4)
        for f in range(NF):
            pb = P.bank()
            P.mmgroup(pb[:, 0:256], [(wg[:, k, f * 128:(f + 1) * 128], h4[:, k, :]) for k in range(8)])
            P.mmgroup(pb[:, 256:512], [(wu[:, k, f * 128:(f + 1) * 128], h4[:, k, :]) for k in range(8)])
            sg = sg4[f % 2]
            P.act(sg, pb[:, 0:256], AF.Silu)
            P.tt("dve", a4[:, f, :], sg, pb[:, 256:512], ALU.mult)
        xo_ = xo4[0]
        for q in range(4):
            pb = P.bank()
            for j in range(2):
                dc = q * 2 + j
                P.mmgroup(pb[:, j * 256:(j + 1) * 256], [(wd[:, f, dc * 128:(dc + 1) * 128], a4[:, f, :]) for f in range(NF)])
            for j in range(2):
                dc = q * 2 + j
                P.stt("dve", xo_[:, dc, :], pb[:, j * 256:(j + 1) * 256], G[0][:, dc, which, 1:2], x4[:, dc, :], ALU.mult, ALU.add)
        for t in range(2):
            P.dma("sp", x_l1[bi * 2 + t], xo_[:, :, t * 128:(t + 1) * 128])
    P.close_scope()
    if stop_after == "P4":
        return _finish(P, nc, [x_l1])

    P.open_scope()
    w1 = P.sb("w1", [128, 8, 3072], BF16)
    for k in range(8):
        P.dma("pool", w1[:, k, :], w_in1[k * 128:(k + 1) * 128, :])
    xb5 = [P.sb("xb5_%d" % i, [128, 8, 256], F32) for i in range(2)]
    tmp5 = {"sq": P.sb("sq5", [128, 8, 256], F32), "rstd": P.sb("rstd5", [128, 256], F32)}
    h5 = P.sb("h5", [128, 8, 256], BF16)
    atab = P.sb("atab", [128, 2, 128], F32)
    qsq = P.sb("qsq", [128, 1024], F32)
    qn = P.sb("qn", [128, 1024], F32)
    q1 = P.sb("q1", [128, 1024], F32)
    q2 = P.sb("q2", [128, 1024], F32)
    ss5 = P.sb("ss5", [128, 16], F32)
    qkb = P.sb("qkb", [128, 2, 1024], BF16)
    qkT = P.sb("qkT", [128, 2, 8, 256], BF16)
    v5 = [P.sb("v5_%d" % i, [128, 2, 8, 130], BF16) for i in range(2)]
    for i in range(2):
        P.memset("dve", v5[i], 1.0)
    qg = P.sb("qg", [128, 64], F32)
    P.ts("dve", qg, r_qn, 64.0 ** -0.5, None, op0=ALU.mult)
    atabs = [atab, P.sb("atab2", [128, 2, 128], F32), P.sb("atab3", [128, 2, 128], F32)]
    h5s = [h5, P.sb("h5b", [128, 8, 256], BF16)]
    qsqs = [qsq, P.sb("qsq_b", [128, 1024], F32)]
    qns = [qn, P.sb("qn_b", [128, 1024], F32)]
    q1s = [q1, P.sb("q1_b", [128, 1024], F32)]
    q2s = [q2, P.sb("q2_b", [128, 1024], F32)]
    ss5s = [ss5, P.sb("ss5_b", [128, 16], F32)]
    NB5 = NCH // 2

    def load5(bj):
        for t in range(2):
            P.dma("sp", xb5[bj % 2][:, :, t * 128:(t + 1) * 128], x_l1[bj * 2 + t])
        if bj > 0:
            l0 = (bj - 1) * 256
            P.dma("sp", atabs[bj % 3], acs_d[l0:l0 + 256, :].re("(t p) c -> p t c", p=128))

    def stage5A(bj):
        which = 1 if bj == 0 else 0
        rms_modulate(xb5[bj % 2], 256, AB[1][:, :, which, 0, :], h5s[bj % 2], tmp5, eng="act")

    def stage5B(bi):
        h5 = h5s[bi % 2]
        atab = atabs[bi % 3]
        which = 1 if bi == 0 else 0
        vv = v5[bi % 2]
        qis = [1] if which == 1 else [0, 1]
        for t in range(2):
            lt = [h5[:, k, t * 128:(t + 1) * 128] for k in range(8)]
            pbs = {}
            for qi in qis:
                pbs[qi] = []
                for j in range(2):
                    pb = P.bank()
                    c0 = qi * 1024 + j * 512
                    P.mmgroup(pb, [(lt[k], w1[:, k, c0:c0 + 512]) for k in range(8)])
                    pbs[qi].append(pb)
            for qi in qis:
                for j in range(2):
                    P.act(qsqs[qi][:, j * 512:(j + 1) * 512], pbs[qi][j], AF.Square)
            for qi in qis:
                P.reduce("dve", ss5s[qi], qsqs[qi].re("p (g d) -> p g d", d=64), ALU.add)
                P.ts("dve", ss5s[qi], ss5s[qi], 1.0 / 64, EPS, op0=ALU.mult, op1=ALU.add)
            for qi in qis:
                P.rsqrt(ss5s[qi])
            for qi in qis:
                for j in range(2):
                    P.tt("dve", qns[qi][:, j * 512:(j + 1) * 512].re("p (g d) -> p g d", d=64), pbs[qi][j].re("p (g d) -> p g d", d=64),
                         bc(ss5s[qi][:, j * 8:(j + 1) * 8], [(1, 8), (0, 64)]), ALU.mult)
            pvs = []
            for j in range(2):
                pb = P.bank()
                c0 = 2048 + j * 512
                P.mmgroup(pb, [(lt[k], w1[:, k, c0:c0 + 512]) for k in range(8)])
                pvs.append(pb)
            for qi in qis:
                qn = qns[qi]
                gn = qg if qi == 0 else r_kn
                dst = qkb[:, qi, :]
                if which == 1:
                    P.tt("dve", dst.re("p (g d) -> p g d", d=64), qn.re("p (g d) -> p g d", d=64), bc(gn, [(0, 16), (1, 64)]), ALU.mult)
                else:
                    P.tt("dve", qn.re("p (g d) -> p g d", d=64), qn.re("p (g d) -> p g d", d=64), bc(gn, [(0, 16), (1, 64)]), ALU.mult)
            for j in range(2):
                P.copy("act", vv[:, t, j * 4:(j + 1) * 4, 0:128], pvs[j].re("p (h e) -> p h e", h=4))
            if which == 0:
                for qi in qis:
                    qn, q1, q2 = qns[qi], q1s[qi], q2s[qi]
                    P.tt("dve", q1.re("p (g d) -> p g d", d=64), qn.re("p (g d) -> p g d", d=64), bc(atab[:, t, 0:64], [(0, 16), (1, 64)]), ALU.mult)
                    qv = qn.re("p (g a u d) -> p g a u d", a=2, u=2, d=16)
                    q2v = q2.re("p (g a u d) -> p g a u d", a=2, u=2, d=16)
                    for s_ in range(2):
                        sn = bass.AP(atab.ap.tensor, atab[:, t, 64 + s_ * 16:64 + s_ * 16 + 16].ap.offset,
                                     [list(atab.ap.ap[0]), [0, 16], [32, 2], [1, 16]])
                        P.tt("pool", q2v[:, :, :, s_, :], qv[:, :, :, 1 - s_, :], TT(sn, atab.tok), ALU.mult)
                for qi in qis:
                    P.tt("dve", qkb[:, qi, :], q1s[qi], q2s[qi], ALU.add)
            for qi in qis:
                pb = P.bank()
                pbb = bank_bf(pb)
                for h in range(8):
                    P.transpose(pbb[:, h * 128:(h + 1) * 128], qkb[:, qi, h * 128:(h + 1) * 128], ident_b)
                P.copy("act", qkT[:, qi, :, t * 128:(t + 1) * 128], pbb.re("p (h t) -> p h t", h=8))
        tok0 = bi * 256
        P.dma("sp", Kd[:, :, tok0:tok0 + 256].re("h p t -> p h t"), qkT[:, 1])
        if which == 0:
            P.dma("sp", Qd[:, :, tok0 - CTX:tok0 - CTX + 256].re("h p t -> p h t"), qkT[:, 0])
        for t in range(2):
            P.dma("sp", Vd[:, :, bi * 2 + t, :].re("h p e -> p h e"), vv[:, t])

    load5(0)
    if NB5 > 1:
        load5(1)
    stage5A(0)
    for bi in range(NB5):
        if bi + 2 < NB5:
            load5(bi + 2)
        if bi + 1 < NB5:
            stage5A(bi + 1)
        stage5B(bi)
    P.close_scope()
    if stop_after == "P5":
        return _finish(P, nc, [Kd, Vd, Qd])

    P.open_scope()
    NKT = NCH
    NQB = OWN // 512
    lt_ = P.sb("lt_", [128, 128], F32)
    lam2 = P.sb("lam2", [128, 4], F32)
    P.tt("dve", lt_[:, 0:64], r_lam[:, 0:64], r_lam[:, 64:128], ALU.mult)
    P.tt("dve", lt_[:, 64:128], r_lam[:, 128:192], r_lam[:, 192:256], ALU.mult)
    P.reduce("dve", lam2[:, 0:2], lt_.re("p (a d) -> p a d", a=2), ALU.add)
    P.act(lam2[:, 0:2], lam2[:, 0:2], AF.Exp)
    P.tt("dve", lam2[:, 2:3], lam2[:, 1:2], lam2[:, 0:1], ALU.subtract)
    P.ts("dve", lam2[:, 3:4], lam2[:, 2:3], -LAM_INIT, None, op0=ALU.add)
    neglam = lam2[:, 3:4]
    sub_g = P.sb("sub_g", [128, 128], F32)
    P.ts("dve", sub_g, r_subln, 1.0 - LAM_INIT, None, op0=ALU.mult)
    Kh = [P.sb("Kh%d" % i, [128, T], BF16) for i in range(2)]
    Vh = [P.sb("Vh%d" % i, [128, NKT, 130], BF16) for i in range(2)]
    qa = [P.sb("qa%d" % i, [128, 512], BF16) for i in range(2)]
    qb_ = [P.sb("qb%d" % i, [128, 512], BF16) for i in range(2)]
    qs = [P.sb("qs%d" % i, [128, 512], BF16) for i in range(2)]
    pT = [P.sb("pT%d" % i, [128, 512], BF16) for i in range(3)]
    o0 = P.sb("o0", [128, 4, 128], F32)
    o1 = P.sb("o1", [128, 128], F32)
    osq = P.sb("osq", [128, 4, 128], F32)
    rs6 = P.sb("rs6", [128, 4], F32)
    on = P.sb("on", [128, 4, 128], BF16)
    oT = [P.sb("oT%d" % i, [128, 512], BF16) for i in range(2)]
    spb = [P.banks[0], P.banks[1], P.banks[2]]
    ob = [P.banks[3], P.banks[4], P.banks[5], P.banks[6]]
    tb = P.banks[7]
    groups = [(h, qb) for h in range(8) for qb in range(NQB)]
    steps = [(m, kt) for m in range(2) for kt in range(NKT)]

    def load_kv(h):
        P.dma("sp", Kh[h % 2], Kd[h])
        P.dma("sp", Vh[h % 2], Vd[h])

    def load_q(gi):
        h, qb = groups[gi]
        A, B_, Q_ = qa[gi % 2], qb_[gi % 2], qs[gi % 2]
        P.dma("sp", A, Qd[h, :, qb * 512:(qb + 1) * 512])
        if L > OWN:
            P.dma("sp", B_, Qd[h, :, OWN + qb * 512:OWN + (qb + 1) * 512])
            P.ts("pool", Q_, A, sel_sb[:, 0:1], None, op0=ALU.mult)
            P.stt("dve", Q_, B_, sel_sb[:, 1:2], Q_, ALU.mult, ALU.add)
            return Q_
        return A

    load_kv(0)
    Qn = load_q(0)
    pi = 0
    for gi, (h, qb) in enumerate(groups):
        K_, V_ = Kh[h % 2], Vh[h % 2]
        Q_ = Qn
        if qb == 0 and h + 1 < 8:
            load_kv(h + 1)
        if gi + 1 < len(groups):
            Qn = load_q(gi + 1)

        def emit_s(i):
            m, kt = steps[i]
            P.mm(spb[(pi + i) % 3], K_[m * 64:(m + 1) * 64, kt * 128:(kt + 1) * 128], Q_[m * 64:(m + 1) * 64, :])
        emit_s(0)
        emit_s(1)
        for i, (m, kt) in enumerate(steps):
            if i + 2 < len(steps):
                emit_s(i + 2)
            sp_ = spb[(pi + i) % 3]
            p_ = pT[(pi + i) % 3]
            P.act(p_, sp_, AF.Exp)
            for s_ in range(4):
                P.mm(ob[s_][:, 0:129], p_[:, s_ * 128:(s_ + 1) * 128], V_[:, kt, 0:129], start=(kt == 0), stop=(kt == NKT - 1))
            if kt == NKT - 1:
                for s_ in range(4):
                    P.recip(rs6[:, s_:s_ + 1], ob[s_][:, 128:129])
                    if m == 0:
                        P.ts("dve", o0[:, s_, :], ob[s_][:, 0:128], rs6[:, s_:s_ + 1], None, op0=ALU.mult)
                    else:
                        P.ts("dve", o1, ob[s_][:, 0:128], rs6[:, s_:s_ + 1], neglam, op0=ALU.mult, op1=ALU.mult)
                        P.tt("dve", o0[:, s_, :], o0[:, s_, :], o1, ALU.add)
        pi += len(steps)
        P.act(osq, o0, AF.Square)
        P.reduce("dve", rs6, osq, ALU.add)
        P.ts("dve", rs6, rs6, 1.0 / 128, EPS, op0=ALU.mult, op1=ALU.add)
        P.rsqrt(rs6)
        tbb = bank_bf(tb)
        for s_ in range(4):
            P.stt("dve", on[:, s_, :], o0[:, s_, :], rs6[:, s_:s_ + 1], sub_g, ALU.mult, ALU.mult)
            P.transpose(tbb[:, s_ * 128:(s_ + 1) * 128], on[:, s_, :], ident_b)
        o_ = oT[gi % 2]
        P.copy("dve", o_, tbb[:, 0:512])
        P.dma("sp", Od[:, h, qb * 512:(qb + 1) * 512], o_)
    P.close_scope()
    if stop_after == "P6":
        return _finish(P, nc, [Od])

    P.open_scope()
    BLK = min(1024, OWN)
    NB = OWN // BLK
    NH = BLK // 512
    NT7 = BLK // 128
    wo1 = P.sb("wo1", [128, 8, D], BF16)
    for k in range(8):
        P.dma("pool", wo1[:, k, :], w_out1[k * 128:(k + 1) * 128, :])
    wr_sb = P.sb("wr_sb", [128, 8, 8], F32)
    P.dma("sp", wr_sb, wr.re("(k p) e -> p k e", p=128))
    selm = P.sb("selm", [8, 1024], F32)
    P.dma("sp", selm, selmat)
    o7 = P.sb("o7", [128, 8, BLK], BF16)
    xa = P.sb("xa", [128, 8, BLK], F32)
    xb7 = P.sb("xb7", [128, 8, 128], F32)
    sq7 = P.sb("sq7", [128, 8, 512], F32)
    rstd7 = P.sb("rstd7", [128, 512], F32)
    h7 = P.sb("h7", [128, 8, BLK], BF16)
    h7f = sq7
    yacc = P.sb("yacc", [128, 8, BLK], F32)
    lg = P.sb("lg", [128, 8], F32)
    lg2 = P.sb("lg2", [128, 8], F32)
    eq1 = P.sb("eq1", [128, 8], F32)
    eq2 = P.sb("eq2", [128, 8], F32)
    mx = P.sb("mx", [128, 8], F32)
    comb = P.sb("comb", [128, 8], F32)
    combT = P.sb("combT", [8, BLK], F32)
    cbc = [P.sb("cbc%d" % i, [128, BLK], BF16) for i in range(2)]
    FG = 2
    NFG = FEXP // (128 * FG)
    FW = 128 * FG
    wge = [P.sb("wge%d" % i, [128, 8, FW], BF16) for i in range(2)]
    wue = [P.sb("wue%d" % i, [128, 8, FW], BF16) for i in range(2)]
    wde = [P.sb("wde%d" % i, [128, FG, D], BF16) for i in range(2)]
    a7 = [P.sb("a7_%d" % i, [128, FG, BLK], BF16) for i in range(2)]
    sg7 = [P.sb("sg7_%d" % i, [128, 512], F32) for i in range(2)]
    t7 = [P.sb("t7_%d" % i, [128, 512], F32) for i in range(2)]
    its = [(nb, e, fg) for nb in range(NB) for e in range(NEXP) for fg in range(NFG)]

    def issue_w(ii):
        nb_, e_, fg_ = its[ii]
        b_ = ii % 2
        f0_ = fg_ * FW
        P.dma("pool", wge[b_], eg[e_, :, f0_:f0_ + FW].re("(k p) f -> p k f", p=128))
        P.dma("pool", wue[b_], eu[e_, :, f0_:f0_ + FW].re("(k p) f -> p k f", p=128))
        P.dma("pool", wde[b_], ed[e_, f0_:f0_ + FW, :].re("(f p) d -> p f d", p=128))
    issue_w(0)
    wi = 0
    for nb in range(NB):
        P.dma("sp", o7, Od[:, :, nb * BLK:(nb + 1) * BLK])
        for t in range(NT7):
            chA = 2 + (nb * BLK) // 128 + t
            P.dma("sp", xa[:, :, t * 128:(t + 1) * 128], x_l1[chA])
            if L > OWN:
                P.dma("sp", xb7, x_l1[chA + OWN // 128])
                P.ts("pool", xa[:, :, t * 128:(t + 1) * 128], xa[:, :, t * 128:(t + 1) * 128], sel_sb[:, 0:1], None, op0=ALU.mult)
                P.stt("pool", xa[:, :, t * 128:(t + 1) * 128], xb7, sel_sb[:, 1:2], xa[:, :, t * 128:(t + 1) * 128], ALU.mult, ALU.add)
        for hf in range(NH):
            cs = slice(hf * 512, (hf + 1) * 512)
            for dc in range(8):
                pb = P.bank()
                P.mmgroup(pb, [(wo1[:, hh, dc * 128:(dc + 1) * 128], o7[:, hh, cs]) for hh in range(8)])
                P.stt("dve", xa[:, dc, cs], pb, G[1][:, dc, 0, 0:1], xa[:, dc, cs], ALU.mult, ALU.add)
            P.act(sq7, xa[:, :, cs], AF.Square)
            pb = P.bank()
            P.mmgroup(pb, [(ones_f, sq7[:, k, :]) for k in range(8)])
            P.ts("dve", rstd7, pb, 1.0 / D, EPS, op0=ALU.mult, op1=ALU.add)
            P.rsqrt(rstd7)
            P.tt("dve", sq7, xa[:, :, cs], bc(rstd7, [(0, 8), (1, 512)]), ALU.mult)
            A2 = AB[1][:, :, 0, 1, :]
            for k in range(8):
                P.ts("pool", h7f[:, k, :], sq7[:, k, :], A2[:, k, 0:1], A2[:, k, 1:2], op0=ALU.mult, op1=ALU.add)
                P.copy("act", h7[:, k, cs], h7f[:, k, :])
            for t4 in range(4):
                pb = P.bank()
                P.mmgroup(pb[:, 0:8], [(h7f[:, k, t4 * 128:(t4 + 1) * 128], wr_sb[:, k, :]) for k in range(8)])
                P.copy("dve", lg, pb[:, 0:8])
                P.reduce("dve", mx[:, 0:1], lg, ALU.max)
                P.ts("dve", eq1, lg, mx[:, 0:1], None, op0=ALU.is_equal)
                P.stt("dve", lg2, eq1, -1e30, lg, ALU.mult, ALU.add)
                P.reduce("dve", mx[:, 1:2], lg2, ALU.max)
                P.ts("dve", eq2, lg2, mx[:, 1:2], None, op0=ALU.is_equal)
                P.tt("dve", mx[:, 2:3], mx[:, 1:2], mx[:, 0:1], ALU.subtract)
                P.act(mx[:, 3:4], mx[:, 2:3], AF.Exp)
                P.ts("dve", mx[:, 4:5], mx[:, 3:4], 1.0, None, op0=ALU.add)
                P.recip(mx[:, 5:6], mx[:, 4:5])
                P.tt("dve", mx[:, 6:7], mx[:, 3:4], mx[:, 5:6], ALU.mult)
                P.ts("dve", comb, eq1, mx[:, 5:6], None, op0=ALU.mult)
                P.stt("dve", comb, eq2, mx[:, 6:7], comb, ALU.mult, ALU.add)
                pb = P.bank()
                P.transpose(pb[0:8, 0:128], comb, ident_f)
                P.copy("dve", combT[:, hf * 512 + t4 * 128:hf * 512 + (t4 + 1) * 128], pb[0:8, 0:128])
        P.memset("pool", yacc, 0.0)
        for e in range(NEXP):
            cb = cbc[e % 2]
            for hf in range(NH):
                cs = slice(hf * 512, (hf + 1) * 512)
                pb = P.bank()
                P.mm(pb, selm[:, e * 128:(e + 1) * 128], combT[:, cs])
                P.copy("act", cb[:, cs], pb)
            for fg in range(NFG):
                b = wi % 2
                wi += 1
                if wi < len(its):
                    issue_w(wi)
                aa = a7[b]
                ii = 0
                for f in range(FG):
                    for hf in range(NH):
                        cs = slice(hf * 512, (hf + 1) * 512)
                        pg = P.bank()
                        pu = P.bank()
                        P.mmgroup(pg, [(wge[b][:, k, f * 128:(f + 1) * 128], h7[:, k, cs]) for k in range(8)])
                        P.mmgroup(pu, [(wue[b][:, k, f * 128:(f + 1) * 128], h7[:, k, cs]) for k in range(8)])
                        sg = sg7[ii % 2]
                        tt_ = t7[ii % 2]
                        ii += 1
                        P.act(sg, pg, AF.Silu)
                        P.tt("dve", tt_, sg, pu, ALU.mult)
                        P.tt("pool", aa[:, f, cs], tt_, cb[:, cs], ALU.mult)
                for hf in range(NH):
                    cs = slice(hf * 512, (hf + 1) * 512)
                    for dc in range(8):
                        pb = P.bank()
                        P.mmgroup(pb, [(wde[b][:, f, dc * 128:(dc + 1) * 128], aa[:, f, cs]) for f in range(FG)])
                        P.tt("dve", yacc[:, dc, cs], yacc[:, dc, cs], pb, ALU.add)
        for dc in range(8):
            P.stt("dve", yacc[:, dc, :], yacc[:, dc, :], G[1][:, dc, 0, 1:2], xa[:, dc, :], ALU.mult, ALU.add)
        P.dma("sp", outT[:, nb * BLK:(nb + 1) * BLK].re("(k p) t -> p k t", p=128), yacc)
    P.close_scope()
    return _finish(P, nc, [outT])


def P_sb_keep(P, name, shape):
    g = P.nc.sbuf_tensor(name, list(shape), F32)
    h = g.__enter__()
    P._ctx.insert(0, g)
    for i in range(len(P._scopes)):
        P._scopes[i] += 1
    return TT(h[:], Tok(name))


def _finish(P, nc, outs):
    P.fence("sp", outs)
    P.emit()
    P.close()
    return nc


def fm_vec(v):
    v = np.asarray(v, np.float32).reshape(-1, 128)
    return np.ascontiguousarray(v.T)


def prep_inputs(inp, L, OWN, ncores_per_batch, nbatch):
    cm, selm = host_consts()
    rcs, acs = rope_tables(L)
    shared = {
        "consts": cm, "selmat": selm, "rcs": rcs, "acs": acs,
        "w_mod0": np.ascontiguousarray(inp["even_w_mod"][0]), "w_mod1": np.ascontiguousarray(inp["odd_w_mod"][0]),
        "b_mod0": fm_vec(inp["even_b_mod"][0]), "b_mod1": fm_vec(inp["odd_b_mod"][0]),
        "norms": np.concatenate([fm_vec(inp["even_norm1"][0]), fm_vec(inp["even_norm2"][0]),
                                 fm_vec(inp["odd_norm1"][0]), fm_vec(inp["odd_norm2"][0])], axis=1),
        "w_in0": np.ascontiguousarray(inp["even_w_in"][0]),
        "w_out0": np.ascontiguousarray(inp["even_w_out"][0]),
        "ffg": np.ascontiguousarray(inp["even_ffn_gate"][0]), "ffu": np.ascontiguousarray(inp["even_ffn_up"][0]),
        "ffd": np.ascontiguousarray(inp["even_ffn_down"][0]),
        "w_in1": np.ascontiguousarray(inp["odd_w_in"][0]), "w_out1": np.ascontiguousarray(inp["odd_w_out"][0]),
        "router": np.ascontiguousarray(inp["odd_router"][0]),
        "eg": np.ascontiguousarray(inp["odd_exp_gate"][0]), "eu": np.ascontiguousarray(inp["odd_exp_up"][0]),
        "ed": np.ascontiguousarray(inp["odd_exp_down"][0]),
    }
    cw = np.concatenate([inp["even_conv_w"][0], inp["even_conv_b"][0][None, :]], axis=0)
    shared["convw"] = np.ascontiguousarray(cw.reshape(4, 12, 128).transpose(2, 1, 0))
    rowp = np.concatenate([
        inp["even_dt_bias"][0].reshape(-1), inp["even_a_log"][0].reshape(-1), inp["even_ret_decay"][0].reshape(-1),
        inp["even_d"][0].reshape(-1), inp["even_ssd_norm"][0].reshape(-1), inp["odd_q_norm"][0].reshape(-1),
        inp["odd_k_norm"][0].reshape(-1), inp["odd_lambda"][0].reshape(-1), inp["odd_subln"][0].reshape(-1)]).astype(np.float32)
    rp = np.zeros((1, 2560), np.float32)
    rp[0, :rowp.size] = rowp
    shared["rowp"] = rp
    maps = []
    for b in range(nbatch):
        xT = np.zeros((D, L + 2), np.float32)
        xT[:, 1:L + 1] = inp["x"][b].T
        cT = np.zeros((D, CTX + 2), np.float32)
        cT[:, 1:CTX + 1] = inp["ctx"][b].T
        cvec = np.concatenate([fm_vec(inp["c"][b]), fm_vec(inp["c_ctx"])], axis=1)
        for hf in range(ncores_per_batch):
            s = np.zeros((128, 2), np.float32)
            s[:, hf] = 1.0
            m = dict(shared)
            m.update({"xT": xT, "ctxT": cT, "cvec": cvec, "sel": s})
            maps.append(m)
    return maps


_NC_CACHE = {}


def kernel(**inputs):
    inp = {k: np.asarray(v) for k, v in inputs.items()}
    B, L, _ = inp["x"].shape
    OWN = L // 2
    key = (L, OWN)
    if key not in _NC_CACHE:
        _NC_CACHE[key] = build(L, OWN)
    nc = _NC_CACHE[key]
    maps = prep_inputs(inp, L, OWN, 2, B)
    res = run_bass_kernel_spmd(nc, maps, core_ids=list(range(len(maps))))
    out = np.empty((B, L, D), np.float32)
    for b in range(B):
        for hf in range(2):
            out[b, hf * OWN:(hf + 1) * OWN, :] = res.results[b * 2 + hf]["outT"].T
    return out
```

```python
import math
import numpy as np
import ml_dtypes
import concourse.bass as bass
import concourse.mybir as mybir
from concourse.bass_utils import run_bass_kernel_spmd

F32 = mybir.dt.float32
BF16 = mybir.dt.bfloat16
AF = mybir.ActivationFunctionType
ALU = mybir.AluOpType
AX = mybir.AxisListType

SAME_ENGINE_SYNC = True
NSLOT = 10


class Tok:
    __slots__ = ("lw", "rd", "name")

    def __init__(self, name=""):
        self.lw = None
        self.rd = []
        self.name = name


class TT:
    __slots__ = ("ap", "tok")

    def __init__(self, ap, tok):
        self.ap = ap
        self.tok = tok

    def __getitem__(self, k):
        return TT(self.ap[k], self.tok)

    def re(self, s, **kw):
        return TT(self.ap.rearrange(s, **kw), self.tok)

    @property
    def shape(self):
        return self.ap.shape


def bc(tt, dims):
    a = tt.ap
    base = list(a.ap)
    return TT(bass.AP(a.tensor, a.offset, [list(base[0])] + [list(d) for d in dims]), tt.tok)


class Op:
    __slots__ = ("stream", "fn", "deps", "dma", "ms", "slot", "val", "didx")

    def __init__(self, stream, fn, dma):
        self.stream = stream
        self.fn = fn
        self.deps = []
        self.dma = dma
        self.ms = False
        self.slot = None
        self.val = None
        self.didx = None


class Prog:
    STREAMS = ("pe", "act", "dve", "pool", "sp")

    def __init__(self, nc):
        self.nc = nc
        self.ops = {s: [] for s in self.STREAMS}
        self.ndma = {s: 0 for s in self.STREAMS}
        self.dmaops = {s: [] for s in self.STREAMS}
        self._ctx = []
        self._scopes = []
        self.banks = []
        self.bank_i = 0

    def sb(self, name, shape, dt=F32):
        g = self.nc.sbuf_tensor(name, list(shape), dt)
        h = g.__enter__()
        self._ctx.append(g)
        return TT(h[:], Tok(name))

    def ps(self, name, shape, dt=F32):
        g = self.nc.psum_tensor(name, list(shape), dt)
        h = g.__enter__()
        self._ctx.append(g)
        return TT(h[:], Tok(name))

    def dram(self, name, shape, dt=F32, kind="Internal"):
        h = self.nc.dram_tensor(name, list(shape), dt, kind=kind)
        return TT(h.ap(), Tok(name))

    def open_scope(self):
        self._scopes.append(len(self._ctx))

    def close_scope(self):
        n = self._scopes.pop()
        self.barrier()
        while len(self._ctx) > n:
            self._ctx.pop().__exit__(None, None, None)

    def close(self):
        while self._ctx:
            self._ctx.pop().__exit__(None, None, None)

    def bank(self):
        b = self.banks[self.bank_i % len(self.banks)]
        self.bank_i += 1
        return b

    def _rec(self, stream, fn, reads, writes, dma=False, extra=()):
        op = Op(stream, fn, dma)
        deps = list(extra)
        for t in reads:
            if t.tok.lw is not None:
                deps.append(t.tok.lw)
        for t in writes:
            if t.tok.lw is not None:
                deps.append(t.tok.lw)
            deps.extend(t.tok.rd)
        if dma:
            op.didx = self.ndma[stream]
            self.ndma[stream] += 1
            self.dmaops[stream].append(op)
            if op.didx >= NSLOT:
                deps.append(self.dmaops[stream][op.didx - NSLOT])
        seen = set()
        for d in deps:
            if d is op or id(d) in seen:
                continue
            if (not d.dma) and d.stream == stream and (stream == "pe" or not SAME_ENGINE_SYNC):
                continue
            seen.add(id(d))
            op.deps.append(d)
            d.ms = True
        for t in reads:
            t.tok.rd.append(op)
        for t in writes:
            t.tok.lw = op
            t.tok.rd = []
        self.ops[stream].append(op)
        return op

    def barrier(self):
        last = []
        for s in self.STREAMS:
            if self.ops[s]:
                for o in reversed(self.ops[s]):
                    if not o.dma and o.fn is not None:
                        last.append(o)
                        break
            last.extend(self.dmaops[s][-NSLOT:])
        for s in self.STREAMS:
            self._rec(s, None, [], [], extra=last)

    def mm(self, out, lhsT, rhs, start=True, stop=True):
        self._rec("pe", lambda e: e.matmul(out.ap, lhsT.ap, rhs.ap, start=start, stop=stop), [lhsT, rhs], [out])

    def mmgroup(self, out, pairs):
        rd = []
        for l, r in pairs:
            rd += [l, r]
        n = len(pairs)

        def fn(e):
            ins = None
            for i, (l, r) in enumerate(pairs):
                ins = e.matmul(out.ap, l.ap, r.ap, start=(i == 0), stop=(i == n - 1))
            return ins
        self._rec("pe", fn, rd, [out])

    def transpose(self, out, in_, ident):
        self._rec("pe", lambda e: e.transpose(out.ap, in_.ap, ident.ap), [in_, ident], [out])

    def act(self, out, in_, func, bias=None, scale=1.0, accum=None):
        rd = [in_]
        kw = {}
        if bias is not None:
            if isinstance(bias, TT):
                rd.append(bias)
                kw["bias"] = bias.ap
            else:
                kw["bias"] = bias
        if isinstance(scale, TT):
            rd.append(scale)
            kw["scale"] = scale.ap
        else:
            kw["scale"] = scale
        wr = [out]
        if accum is not None:
            wr.append(accum)
            kw["accum_out"] = accum.ap
        self._rec("act", lambda e: e.activation(out.ap, in_.ap, func, **kw), rd, wr)

    def tt(self, eng, out, a, b, op):
        self._rec(eng, lambda e: e.tensor_tensor(out.ap, a.ap, b.ap, op), [a, b], [out])

    def ts(self, eng, out, a, s1, s2=None, op0=ALU.mult, op1=None):
        rd = [a]
        s1a = s1.ap if isinstance(s1, TT) else s1
        s2a = s2.ap if isinstance(s2, TT) else s2
        if isinstance(s1, TT):
            rd.append(s1)
        if isinstance(s2, TT):
            rd.append(s2)
        kw = {}
        if op1 is not None:
            kw["op1"] = op1
        self._rec(eng, lambda e: e.tensor_scalar(out.ap, a.ap, s1a, s2a, op0, **kw), rd, [out])

    def stt(self, eng, out, a, s, b, op0, op1):
        rd = [a, b]
        sa = s.ap if isinstance(s, TT) else s
        if isinstance(s, TT):
            rd.append(s)
        self._rec("dve", lambda e: e.scalar_tensor_tensor(out.ap, a.ap, sa, b.ap, op0, op1), rd, [out])

    def copy(self, eng, out, a):
        if eng == "act":
            self._rec(eng, lambda e: e.copy(out.ap, a.ap), [a], [out])
        else:
            self._rec(eng, lambda e: e.tensor_copy(out.ap, a.ap), [a], [out])

    def memset(self, eng, out, val):
        self._rec(eng, lambda e: e.memset(out.ap, val), [], [out])

    def reduce(self, eng, out, a, op, axis=AX.X):
        self._rec(eng, lambda e: e.tensor_reduce(out.ap, a.ap, axis, op), [a], [out])

    def rsqrt(self, x):
        self._rec("act", lambda e: e.activation(x.ap, x.ap, AF.Sqrt), [x], [x])
        self._rec("dve", lambda e: e.reciprocal(x.ap, x.ap), [x], [x])

    def recip(self, out, a):
        self._rec("dve", lambda e: e.reciprocal(out.ap, a.ap), [a], [out])

    def dma(self, q, out, in_):
        self._rec(q, lambda e: e.dma_start(out.ap, in_.ap), [in_], [out], dma=True)

    def fence(self, stream, tts):
        self._rec(stream, None, list(tts), [])

    def emit(self):
        nc = self.nc
        for s in self.STREAMS:
            k = 0
            for op in self.ops[s]:
                if op.dma:
                    op.slot = op.didx % NSLOT
                    op.val = 16 * (op.didx // NSLOT + 1)
                elif op.ms:
                    k += 1
                    op.val = k
        sem_ctx = []
        csem = {}
        dsem = {}
        for s in self.STREAMS:
            g = nc.semaphore("c_" + s)
            csem[s] = g.__enter__()
            sem_ctx.append(g)
            if self.ndma[s] > 0:
                for i in range(NSLOT):
                    g = nc.semaphore("d_%s_%d" % (s, i))
                    dsem[(s, i)] = g.__enter__()
                    sem_ctx.append(g)
        ops = self.ops

        def run(stream, e):
            waited = {}
            for op in ops[stream]:
                for d in op.deps:
                    if d.dma:
                        key = ("d", d.stream, d.slot)
                        sem = dsem[(d.stream, d.slot)]
                    else:
                        key = ("c", d.stream)
                        sem = csem[d.stream]
                    if waited.get(key, 0) < d.val:
                        e.wait_ge(sem, d.val)
                        waited[key] = d.val
                if op.fn is None:
                    continue
                ins = op.fn(e)
                if op.dma:
                    ins.then_inc(dsem[(stream, op.slot)], 16)
                elif op.ms:
                    ins.then_inc(csem[stream], 1)

        with nc.Block() as block:
            @block.sync
            def _(e):
                run("sp", e)

            @block.tensor
            def _(e):
                run("pe", e)

            @block.scalar
            def _(e):
                run("act", e)

            @block.vector
            def _(e):
                run("dve", e)

            @block.gpsimd
            def _(e):
                run("pool", e)
        for g in reversed(sem_ctx):
            g.__exit__(None, None, None)


D = 1024
KC = 8
EPS = 1e-6
EVEN_IN = 5664
FFN_DENSE = 2816
NEXP = 8
FEXP = 3584
CTX = 256
GRID_W = 64
LAM_INIT = 0.8 - 0.6 * math.exp(-0.3 * 1)
C_Z, C_XBC, C_DT, C_RQ, C_RK, C_RV, C_RG = 0, 1024, 2560, 2592, 3104, 3616, 4640


def host_consts():
    k = np.arange(128)[:, None]
    l = np.arange(128)[None, :]
    c = {}
    c["ident"] = (k == l).astype(np.float32)
    c["le"] = (k <= l).astype(np.float32)
    c["gt"] = (k > l).astype(np.float32)
    c["ge"] = (k >= l).astype(np.float32)
    c["lt"] = (k < l).astype(np.float32)
    c["ones"] = np.ones((128, 128), np.float32)
    cm = np.concatenate([c[n] for n in ("ident", "le", "gt", "ge", "lt", "ones")], axis=1)
    sel = np.zeros((8, 8, 128), np.float32)
    for e in range(8):
        sel[e, e, :] = 1.0
    return cm, sel.reshape(8, 1024)


def rope_tables(L):
    f32 = np.float32
    inv = (np.float32(10000.0) ** (-np.arange(64, dtype=f32) / f32(64))).astype(f32)
    pos = np.arange(CTX + L, dtype=f32)
    ang = (pos[:, None] * inv[None, :]).astype(f32)
    rcs = np.concatenate([np.cos(ang), np.sin(ang)], axis=1).astype(f32)
    inv16 = (np.float32(10000.0) ** (-np.arange(16, dtype=f32) / f32(16))).astype(f32)
    t = np.arange(L)
    row = (t // GRID_W).astype(f32)
    col = (t % GRID_W).astype(f32)
    ar = (row[:, None] * inv16[None, :]).astype(f32)
    ac = (col[:, None] * inv16[None, :]).astype(f32)
    cos = np.concatenate([np.cos(ar), np.cos(ar), np.cos(ac), np.cos(ac)], axis=1)
    sins = np.concatenate([-np.sin(ar), np.sin(ar), -np.sin(ac), np.sin(ac)], axis=1)
    acs = np.concatenate([cos, sins], axis=1).astype(f32)
    return rcs, acs


def build(L=8192, OWN=4096, stop_after=None, dbg=()):
    nc = bass.Bass("TRN2", target_bir_lowering=False)
    P = Prog(nc)
    T = CTX + L
    NCH = T // 128
    NLB = L // 256
    dbg = set(dbg)

    def din(name, shape, dt=F32):
        return P.dram(name, shape, dt, kind="ExternalInput")

    xT = din("xT", [D, L + 2])
    cT = din("ctxT", [D, CTX + 2])
    cvec = din("cvec", [128, 16])
    sel = din("sel", [128, 2])
    consts = din("consts", [128, 768])
    selmat = din("selmat", [8, 1024])
    rcs_d = din("rcs", [T, 128])
    acs_d = din("acs", [L, 128])
    w_mod = [din("w_mod0", [D, 6 * D]), din("w_mod1", [D, 6 * D])]
    b_mod = [din("b_mod0", [128, 48]), din("b_mod1", [128, 48])]
    norms = din("norms", [128, 32])
    w_in0 = din("w_in0", [D, EVEN_IN])
    convw = din("convw", [128, 12, 4])
    rowp = din("rowp", [1, 2560])
    w_out0 = din("w_out0", [2048, D])
    ffg = din("ffg", [D, FFN_DENSE])
    ffu = din("ffu", [D, FFN_DENSE])
    ffd = din("ffd", [FFN_DENSE, D])
    w_in1 = din("w_in1", [D, 3072])
    w_out1 = din("w_out1", [D, D])
    wr = din("router", [D, NEXP])
    eg = din("eg", [NEXP, D, FEXP])
    eu = din("eu", [NEXP, D, FEXP])
    ed = din("ed", [NEXP, FEXP, D])
    outT = P.dram("outT", [D, OWN], F32, kind="ExternalOutput")

    def scratch(name, shape, dt=F32):
        return P.dram(name, shape, dt, kind=("ExternalOutput" if name in dbg else "Internal"))

    r_zr = scratch("r_zr", [NCH, 128, 2048], BF16)
    r_xv = scratch("r_xv", [NCH, 128, 2048], BF16)
    r_kt = scratch("r_kt", [NCH, 128, 768], BF16)
    r_fm = scratch("r_fm", [NCH, 128, 12, 128], BF16)
    r_dt = scratch("r_dt", [NCH, 128, 64], F32)
    r_yf = scratch("r_yf", [NCH, 128, 2048], F32)
    x_mid = scratch("x_mid", [NCH, 128, 8, 128], F32)
    x_l1 = scratch("x_l1", [NCH, 128, 8, 128], F32)
    Kd = scratch("Kd", [8, 128, T], BF16)
    Vd = scratch("Vd", [8, 128, NCH, 130], BF16)
    Qd = scratch("Qd", [8, 128, L], BF16)
    Od = scratch("Od", [128, 8, OWN], BF16)

    cst = P.sb("cst", [128, 768], F32)
    P.dma("sp", cst, consts)
    ident_f = cst[:, 0:128]
    m_le, m_gt, m_ge, m_lt, ones_f = (cst[:, 128 * i:128 * (i + 1)] for i in range(1, 6))
    cstb = P.sb("cstb", [128, 768], BF16)
    P.copy("dve", cstb, cst)
    ident_b = cstb[:, 0:128]
    sel_sb = P.sb("sel_sb", [128, 2], F32)
    P.dma("sp", sel_sb, sel)
    rows = P.sb("rows", [128, 2560], F32)
    P.dma("sp", rows, TT(rowp.ap.rearrange("a b -> (a b)").partition_broadcast(128), rowp.tok))
    norm_sb = P.sb("norm_sb", [128, 32], F32)
    P.dma("sp", norm_sb, norms)
    modfm = [P.sb("modfm0", [128, 48, 2], F32), P.sb("modfm1", [128, 48, 2], F32)]
    P.banks = [P.ps("bank%d" % i, [128, 512], F32) for i in range(8)]

    def bank_bf(b):
        a = b.ap
        return TT(a.bitcast(BF16), b.tok)

    AB = [P.sb("AB%d" % i, [128, 8, 2, 2, 2]) for i in range(2)]
    G = [P.sb("G%d" % i, [128, 8, 2, 2]) for i in range(2)]
    P.open_scope()
    cv = P.sb("cv", [128, 16], F32)
    P.dma("sp", cv, cvec)
    scv = P.sb("scv", [128, 8, 2], F32)
    P.act(scv[:, :, 0], cv[:, 0:8], AF.Silu)
    P.act(scv[:, :, 1], cv[:, 8:16], AF.Silu)
    wm = [P.sb("wm%d" % i, [128, 8, 512], F32) for i in range(2)]
    bm_sb = P.sb("bm_sb", [128, 2, 48], F32)
    P.dma("sp", bm_sb[:, 0, :], b_mod[0])
    P.dma("sp", bm_sb[:, 1, :], b_mod[1])
    it = 0
    for lyr in range(2):
        for cg in range(12):
            w = wm[it % 2]
            it += 1
            P.dma("sp", w, w_mod[lyr][:, cg * 512:(cg + 1) * 512].re("(k p) f -> p k f", p=128))
            pb = P.bank()
            for j in range(4):
                P.mmgroup(pb[:, 2 * j:2 * j + 2], [(w[:, k, j * 128:(j + 1) * 128], scv[:, k, :]) for k in range(8)])
            for j in range(4):
                ch = cg * 4 + j
                P.ts("dve", modfm[lyr][:, ch, :], pb[:, 2 * j:2 * j + 2], bm_sb[:, lyr, ch:ch + 1], None, op0=ALU.add)
    for lyr in range(2):
        for w in range(2):
            for n in range(2):
                shift = modfm[lyr][:, (3 * n) * 8:(3 * n) * 8 + 8, w]
                scale = modfm[lyr][:, (3 * n + 1) * 8:(3 * n + 1) * 8 + 8, w]
                gate = modfm[lyr][:, (3 * n + 2) * 8:(3 * n + 2) * 8 + 8, w]
                gain = norm_sb[:, (2 * lyr + n) * 8:(2 * lyr + n) * 8 + 8]
                P.stt("dve", AB[lyr][:, :, w, n, 0], scale, 1.0, gain, ALU.add, ALU.mult)
                P.copy("dve", AB[lyr][:, :, w, n, 1], shift)
                P.copy("dve", G[lyr][:, :, w, n], gate)
    P.close_scope()

    def rms_modulate(xin, ncol, Atab, out_bf, tmp, out_f32=None, eng="pool"):
        sq = tmp["sq"]
        P.act(sq[:, :, 0:ncol], xin, AF.Square)
        pb = P.bank()
        P.mmgroup(pb[:, 0:ncol], [(ones_f, sq[:, k, 0:ncol]) for k in range(8)])
        rstd = tmp["rstd"]
        P.ts("dve", rstd[:, 0:ncol], pb[:, 0:ncol], 1.0 / D, EPS, op0=ALU.mult, op1=ALU.add)
        P.rsqrt(rstd[:, 0:ncol])
        P.tt("dve", sq[:, :, 0:ncol], xin, bc(rstd[:, 0:ncol], [(0, 8), (1, ncol)]), ALU.mult)
        for k in range(8):
            if eng == "act" or (eng == "mix" and k % 2 == 0):
                P.act(out_bf[:, k, :], sq[:, k, 0:ncol], AF.Identity, bias=Atab[:, k, 1:2], scale=Atab[:, k, 0:1])
            else:
                P.ts("pool", out_bf[:, k, :], sq[:, k, 0:ncol], Atab[:, k, 0:1], Atab[:, k, 1:2], op0=ALU.mult, op1=ALU.add)
            if out_f32 is not None:
                P.ts("pool", out_f32[:, k, :], sq[:, k, 0:ncol], Atab[:, k, 0:1], Atab[:, k, 1:2], op0=ALU.mult, op1=ALU.add)

    r_dtb = rows[:, 0:32]
    r_alog = rows[:, 32:64]
    r_retd = rows[:, 64:72]
    r_dsk = rows[:, 72:88]
    r_ssdn = rows[:, 88:1112]
    r_qn = rows[:, 1112:1176]
    r_kn = rows[:, 1176:1240]
    r_lam = rows[:, 1240:1496]
    r_subln = rows[:, 1496:1624]
    ea = P.sb("ea", [128, 32], F32)
    P.act(ea, r_alog, AF.Exp)
    nla_ret = P.sb("nla_ret", [128, 8], F32)
    P.act(nla_ret, r_retd, AF.Exp)
    P.ts("dve", nla_ret, nla_ret, -1.0, None, op0=ALU.mult)

    P.open_scope()
    w0 = P.sb("w0", [128, 8, EVEN_IN], BF16)
    for k in range(8):
        for c0 in range(0, EVEN_IN, 1888):
            P.dma("pool", w0[:, k, c0:c0 + 1888], w_in0[k * 128:(k + 1) * 128, c0:c0 + 1888])
    cw = P.sb("cw", [128, 12, 4], F32)
    P.dma("sp", cw, convw)
    xin = [P.sb("xin%d" % i, [128, 8, 258], F32) for i in range(2)]
    xbr = P.sb("xbr", [128, 12, 258], F32)
    tmp1 = {"sq": xbr[:, 0:8, :], "rstd": P.sb("rstd1", [128, 258], F32)}
    hbs = [P.sb("hb%d" % i, [128, 8, 258], BF16) for i in range(2)]
    xbcs = [P.sb("xbc%d" % i, [128, 12, 256], BF16) for i in range(2)]
    cvts = [P.sb("cvt%d" % i, [128, 256], F32) for i in range(2)]
    o_zr = P.sb("o_zr", [128, 2, 2048], BF16)
    o_xv = P.sb("o_xv", [128, 2, 2048], BF16)
    o_kt = P.sb("o_kt", [128, 2, 768], BF16)
    o_fm = P.sb("o_fm", [128, 2, 12, 128], BF16)
    o_dt = P.sb("o_dt", [128, 2, 64], F32)
    rtabs = [P.sb("rtab%d" % i, [128, 2, 128], F32) for i in range(3)]
    rt1 = P.sb("rt1", [128, 4, 128], F32)
    rt2 = P.sb("rt2", [128, 4, 128], F32)
    rqk = P.sb("rqk", [128, 2, 512], BF16)
    sp1 = P.sb("sp1", [128, 32], F32)
    sp2 = P.sb("sp2", [128, 32], F32)

    blocks = [("c", 0)] + [("l", i) for i in range(NLB)]

    def binfo(bj):
        kind_, i_ = blocks[bj]
        if kind_ == "c":
            return 0, True, True, 1
        return CTX + i_ * 256, (i_ == 0), (i_ == NLB - 1), 0

    def load1(bj):
        kind_, i_ = blocks[bj]
        if kind_ == "c":
            P.dma("sp", xin[bj % 2], cT.re("(k p) t -> p k t", p=128))
        else:
            P.dma("sp", xin[bj % 2], xT[:, i_ * 256:i_ * 256 + 258].re("(k p) t -> p k t", p=128))
        t0_ = binfo(bj)[0]
        P.dma("sp", rtabs[bj % 3], rcs_d[t0_:t0_ + 256, :].re("(t p) c -> p t c", p=128))

    def stageA(bj):
        tok0, first, last, which = binfo(bj)
        xi, hb, xbc = xin[bj % 2], hbs[bj % 2], xbcs[bj % 2]
        rms_modulate(xi, 258, AB[0][:, :, which, 0, :], hb, tmp1, eng="act")
        for c in range(12):
            pb = P.bank()
            P.mmgroup(pb[:, 0:258], [(w0[:, k, C_XBC + c * 128:C_XBC + (c + 1) * 128], hb[:, k, :]) for k in range(8)])
            P.copy("act", xbr[:, c, :], pb[:, 0:258])
        if first:
            P.memset("pool", xbr[:, :, 0:1], 0.0)
        if last:
            P.memset("pool", xbr[:, :, 257:258], 0.0)
        for c in range(12):
            cvt = cvts[c % 2]
            P.act(cvt, xbr[:, c, 0:256], AF.Identity, scale=cw[:, c, 0:1])
            P.stt("dve", cvt, xbr[:, c, 1:257], cw[:, c, 1:2], cvt, ALU.mult, ALU.add)
            P.stt("dve", cvt, xbr[:, c, 2:258], cw[:, c, 2:3], cvt, ALU.mult, ALU.add)
            P.act(xbc[:, c, :], cvt, AF.Silu, bias=cw[:, c, 3:4])

    def stageB(bj):
        tok0, first, last, which = binfo(bj)
        hb, xbc, rtab = hbs[bj % 2], xbcs[bj % 2], rtabs[bj % 3]
        ch0 = tok0 // 128
        for t in range(2):
            P.copy("act", o_fm[:, t, 0:4, :], xbc[:, 8:12, t * 128:(t + 1) * 128])
        for t in range(2):
            lt = [hb[:, k, 1 + t * 128:1 + (t + 1) * 128] for k in range(8)]

            def proj(c0, n):
                pb = P.bank()
                P.mmgroup(pb[:, 0:n], [(lt[k], w0[:, k, c0:c0 + n]) for k in range(8)])
                return pb
            for j in range(2):
                pb = proj(C_Z + j * 512, 512)
                P.copy("act", o_zr[:, t, j * 512:(j + 1) * 512], pb)
            for j in range(2):
                pb = proj(C_RG + j * 512, 512)
                P.copy("act", o_zr[:, t, 1024 + j * 512:1024 + (j + 1) * 512], pb)
            for j in range(2):
                pb = proj(C_RV + j * 512, 512)
                P.copy("act", o_xv[:, t, 1024 + j * 512:1024 + (j + 1) * 512], pb)
            pb = proj(C_DT, 32)
            P.tt("dve", sp1, pb[:, 0:32], r_dtb, ALU.add)
            P.act(sp2, sp1, AF.Abs)
            P.act(sp2, sp2, AF.Exp, scale=-1.0)
            P.act(sp2, sp2, AF.Ln, bias=1.0)
            P.stt("dve", o_dt[:, t, 0:32], sp1, 0.0, sp2, ALU.max, ALU.add)
            P.tt("dve", sp1, o_dt[:, t, 0:32], ea, ALU.mult)
            P.ts("dve", o_dt[:, t, 32:64], sp1, -1.0, None, op0=ALU.mult)
            for qi, c0 in enumerate((C_RQ, C_RK)):
                pb = proj(c0, 512)
                pv = pb.re("p (h d) -> p h d", h=4)
                cos2 = bc(rtab[:, t, 0:64], [(0, 4), (0, 2), (1, 64)])
                P.tt("dve", rt1.re("p h (a d) -> p h a d", a=2), pv.re("p h (a d) -> p h a d", a=2), cos2, ALU.mult)
                sin1 = bc(rtab[:, t, 64:128], [(0, 4), (1, 64)])
                P.tt("dve", rt2[:, :, 0:64], pv[:, :, 64:128], sin1, ALU.mult)
                P.tt("dve", rt2[:, :, 64:128], pv[:, :, 0:64], sin1, ALU.mult)
                rv = rqk[:, qi, :].re("p (h d) -> p h d", h=4)
                P.tt("pool", rt1[:, :, 0:64], rt1[:, :, 0:64], rt2[:, :, 0:64], ALU.subtract)
                P.tt("pool", rt1[:, :, 64:128], rt1[:, :, 64:128], rt2[:, :, 64:128], ALU.add)
                P.act(rv, rt1, AF.Copy, scale=(1.0 if qi == 0 else 128.0 ** -0.5))
            P.copy("act", o_kt[:, t, 256:768], rqk[:, 1, :])
            pb = P.bank()
            pbb = bank_bf(pb)
            for qi in range(2):
                for h in range(4):
                    P.transpose(pbb[:, (qi * 4 + h) * 128:(qi * 4 + h + 1) * 128], rqk[:, qi, h * 128:(h + 1) * 128], ident_b)
            P.copy("dve", o_fm[:, t, 4:12, :], pbb.re("p (n t) -> p n t", t=128))
            pb = P.bank()
            pbb = bank_bf(pb)
            for c in range(8):
                P.transpose(pbb[:, c * 128:(c + 1) * 128], xbc[:, c, t * 128:(t + 1) * 128], ident_b)
            P.copy("dve", o_xv[:, t, 0:1024], pbb)
            pb = P.bank()
            pbb = bank_bf(pb)
            for c in range(2):
                P.transpose(pbb[:, c * 128:(c + 1) * 128], xbc[:, 8 + c, t * 128:(t + 1) * 128], ident_b)
            P.copy("dve", o_kt[:, t, 0:256], pbb[:, 0:256])
        for t in range(2):
            P.dma("sp", r_zr[ch0 + t], o_zr[:, t, :])
            P.dma("sp", r_xv[ch0 + t], o_xv[:, t, :])
            P.dma("sp", r_kt[ch0 + t], o_kt[:, t, :])
            P.dma("sp", r_fm[ch0 + t], o_fm[:, t])
            P.dma("sp", r_dt[ch0 + t], o_dt[:, t, :])

    nblk = len(blocks)
    load1(0)
    if nblk > 1:
        load1(1)
    stageA(0)
    for bi in range(nblk):
        if bi + 2 < nblk:
            load1(bi + 2)
        if bi + 1 < nblk:
            stageA(bi + 1)
        stageB(bi)
    P.close_scope()
    if stop_after == "P1":
        return _finish(P, nc, [r_zr, r_xv, r_kt, r_fm, r_dt])

    P.open_scope()
    wo0 = P.sb("wo0", [128, 16, D], BF16)
    for k in range(16):
        P.dma("pool", wo0[:, k, :], w_out0[k * 128:(k + 1) * 128, :])
    Er = P.sb("Er", [128, 2, 3, 4], F32)
    Dret = P.sb("Dret", [128, 2, 4, 128], F32)
    lmr = P.sb("lmr", [128, 4, 128], F32)
    for d in range(2):
        la = nla_ret[:, d * 4:(d + 1) * 4]
        pb = P.bank()
        mA, mT = (m_le, m_gt) if d == 0 else (m_ge, m_lt)
        P.mm(pb[:, 0:4], mA, la)
        P.mm(pb[:, 4:8], mT, la)
        P.mm(pb[:, 8:12], ones_f, la)
        P.act(Er[:, d].re("p a h -> p (a h)"), pb[:, 0:12], AF.Exp)
        mS, mR, mM = (m_gt, m_le, m_le) if d == 0 else (m_lt, m_ge, m_ge)
        P.tt("dve", lmr, bc(mS, [(0, 4), (1, 128)]), bc(la, [(1, 4), (0, 128)]), ALU.mult)
        pb = P.bank()
        for h in range(4):
            P.mm(pb[:, h * 128:(h + 1) * 128], lmr[:, h, :], mR)
        P.act(Dret[:, d].re("p h l -> p (h l)"), pb, AF.Exp)
        P.tt("dve", Dret[:, d], Dret[:, d], bc(mM, [(0, 4), (1, 128)]), ALU.mult)

    Hs = P.sb("Hs", [128, 1024], F32)
    Hr = P.sb("Hr", [128, 1024], F32)
    Hsb = P.sb("Hsb", [128, 1024], BF16)
    Hrb = P.sb("Hrb", [128, 1024], BF16)
    i_xv = [P.sb("i_xv%d" % i, [128, 2048], BF16) for i in range(2)]
    i_kt = [P.sb("i_kt%d" % i, [128, 768], BF16) for i in range(2)]
    i_fm = [P.sb("i_fm%d" % i, [128, 12, 128], BF16) for i in range(2)]
    i_dt = [P.sb("i_dt%d" % i, [128, 64], F32) for i in range(2)]
    i_zr = [P.sb("i_zr%d" % i, [128, 2048], BF16) for i in range(2)]
    i_yf = [P.sb("i_yf%d" % i, [128, 2048], F32) for i in range(2)]
    i_x = [P.sb("i_x%d" % i, [128, 8, 128], F32) for i in range(2)]
    E = P.sb("E", [128, 3, 16], F32)
    scm = P.sb("scm", [128, 2, 128], F32)
    Lm = P.sb("Lm", [128, 16, 128], F32)
    expD = P.sb("expD", [128, 16, 128], F32)
    MT = P.sb("MT", [128, 16, 128], BF16)
    MTr = P.sb("MTr", [128, 4, 128], BF16)
    xdt = P.sb("xdt", [128, 1024], BF16)
    xw = P.sb("xw", [128, 1024], BF16)
    rvw = P.sb("rvw", [128, 1024], BF16)
    wv = P.sb("wv", [128, 16], F32)
    ytmp = P.sb("ytmp", [128, 1024], F32)
    yo = [P.sb("yo%d" % i, [128, 2048], F32) for i in range(2)]
    sz = P.sb("sz", [128, 1024], F32)
    junk = P.sb("junk", [128, 1024], F32)
    ss = P.sb("ss", [128, 8], F32)
    ycat = P.sb("ycat", [128, 2048], BF16)
    ycT = P.sb("ycT", [128, 16, 128], BF16)
    xo = P.sb("xo", [128, 8, 128], F32)

    fwd_order = list(range(NCH))
    bwd_order = [1, 0] + list(range(NCH - 1, 1, -1))

    for d in range(2):
        order = fwd_order if d == 0 else bwd_order
        P.memset("dve", Hs, 0.0)
        P.memset("dve", Hr, 0.0)
        P.memset("pool", Hsb, 0.0)
        P.memset("pool", Hrb, 0.0)
        mA, mT = (m_le, m_gt) if d == 0 else (m_ge, m_lt)
        mS, mR, mM = (m_gt, m_le, m_le) if d == 0 else (m_lt, m_ge, m_ge)
        def load_sw(cj):
            ch_ = order[cj]
            b_ = cj % 2
            P.dma("sp", i_xv[b_], r_xv[ch_])
            P.dma("sp", i_kt[b_], r_kt[ch_])
            P.dma("sp", i_fm[b_], r_fm[ch_])
            P.dma("sp", i_dt[b_], r_dt[ch_])

        def load_fin(cj):
            ch_ = order[cj]
            b_ = cj % 2
            P.dma("sp", i_zr[b_], r_zr[ch_])
            P.dma("sp", i_yf[b_], r_yf[ch_])
            if ch_ < 2:
                P.dma("sp", i_x[b_], cT[:, 1 + ch_ * 128:1 + (ch_ + 1) * 128].re("(k p) t -> p k t", p=128))
            else:
                P.dma("sp", i_x[b_], xT[:, 1 + (ch_ - 2) * 128:1 + (ch_ - 1) * 128].re("(k p) t -> p k t", p=128))

        def finish(cj):
            ch = order[cj]
            b = cj % 2
            yout = yo[cj % 2]
            zr = i_zr[b]
            which = 1 if ch < 2 else 0
            P.tt("dve", yout, yout, i_yf[b], ALU.add)
            ys = yout[:, 0:1024]
            yr = yout[:, 1024:2048]
            P.act(sz, zr[:, 0:1024], AF.Silu)
            P.tt("dve", ys, ys, sz, ALU.mult)
            P.act(junk, ys, AF.Square)
            P.reduce("dve", ss[:, 0:1], junk, ALU.add)
            P.ts("dve", ss[:, 1:2], ss[:, 0:1], 1.0 / 1024, EPS, op0=ALU.mult, op1=ALU.add)
            P.rsqrt(ss[:, 1:2])
            P.stt("dve", ycat[:, 0:1024], ys, ss[:, 1:2], r_ssdn, ALU.mult, ALU.mult)
            P.act(junk, yr, AF.Square)
            P.reduce("dve", ss[:, 2:6], junk.re("p (h d) -> p h d", h=4), ALU.add)
            P.ts("dve", ss[:, 2:6], ss[:, 2:6], 1.0 / 256, EPS, op0=ALU.mult, op1=ALU.add)
            P.rsqrt(ss[:, 2:6])
            P.act(sz, zr[:, 1024:2048], AF.Silu)
            P.tt("dve", yr.re("p (h d) -> p h d", h=4), yr.re("p (h d) -> p h d", h=4), bc(ss[:, 2:6], [(1, 4), (0, 256)]), ALU.mult)
            P.tt("dve", ycat[:, 1024:2048], yr, sz, ALU.mult)
            for q in range(2):
                pb = P.bank()
                pbb = bank_bf(pb)
                for j in range(8):
                    P.transpose(pbb[:, j * 128:(j + 1) * 128], ycat[:, (q * 8 + j) * 128:(q * 8 + j + 1) * 128], ident_b)
                P.copy("act", ycT[:, q * 8:(q + 1) * 8, :].re("p n t -> p (n t)"), pbb)
            for q in range(2):
                pb = P.bank()
                for j in range(4):
                    dc = q * 4 + j
                    P.mmgroup(pb[:, j * 128:(j + 1) * 128], [(wo0[:, k, dc * 128:(dc + 1) * 128], ycT[:, k, :]) for k in range(16)])
                for j in range(4):
                    dc = q * 4 + j
                    P.stt("dve", xo[:, dc, :], pb[:, j * 128:(j + 1) * 128], G[0][:, dc, which, 0:1], i_x[b][:, dc, :], ALU.mult, ALU.add)
            P.dma("sp", x_mid[ch], xo)

        load_sw(0)
        for ci, ch in enumerate(order):
            b = ci % 2
            xv, kt, fm, dtt = i_xv[b], i_kt[b], i_fm[b], i_dt[b]
            if ci + 1 < len(order):
                load_sw(ci + 1)
            if d == 1:
                load_fin(ci)
            la = dtt[:, 32 + d * 16:32 + (d + 1) * 16]
            dtd = dtt[:, d * 16:(d + 1) * 16]
            xs = xv[:, 0:1024]
            rvv = xv[:, 1024:2048]
            yout = yo[ci % 2]
            pb = P.bank()
            P.mm(pb[:, 0:16], mA, la)
            P.mm(pb[:, 16:32], mT, la)
            P.mm(pb[:, 32:48], ones_f, la)
            P.act(E.re("p a h -> p (a h)"), pb[:, 0:48], AF.Exp)
            pb = P.bank()
            for g in range(2):
                P.mm(pb[:, g * 128:(g + 1) * 128], fm[:, g, :], fm[:, 2 + g, :])
            P.tt("dve", scm, pb[:, 0:256].re("p (g l) -> p g l", g=2), bc(mM, [(0, 2), (1, 128)]), ALU.mult)
            P.tt("pool", Lm, bc(mS, [(0, 16), (1, 128)]), bc(la, [(1, 16), (0, 128)]), ALU.mult)
            for q in range(4):
                pb = P.bank()
                for j in range(4):
                    P.mm(pb[:, j * 128:(j + 1) * 128], Lm[:, q * 4 + j, :], mR)
                P.act(expD[:, q * 4:(q + 1) * 4, :].re("p h l -> p (h l)"), pb, AF.Exp)
            for g in range(2):
                P.tt("dve", MT[:, g * 8:(g + 1) * 8, :], expD[:, g * 8:(g + 1) * 8, :], bc(scm[:, g, :], [(0, 8), (1, 128)]), ALU.mult)
            P.tt("pool", xdt.re("p (h d) -> p h d", h=16), xs.re("p (h d) -> p h d", h=16), bc(dtd, [(1, 16), (0, 64)]), ALU.mult)
            pd = [P.bank(), P.bank()]
            for h in range(16):
                P.mm(pd[h // 8][:, (h % 8) * 64:(h % 8 + 1) * 64], MT[:, h, :], xdt[:, h * 64:(h + 1) * 64])
            for g in range(2):
                po = P.bank()
                P.mm(po, fm[:, 2 + g, :], Hsb[:, g * 512:(g + 1) * 512])
                P.tt("dve", ytmp[:, g * 512:(g + 1) * 512].re("p (h d) -> p h d", h=8), po.re("p (h d) -> p h d", h=8),
                     bc(E[:, 0, g * 8:(g + 1) * 8], [(1, 8), (0, 64)]), ALU.mult)
                P.tt("dve", yout[:, g * 512:(g + 1) * 512], ytmp[:, g * 512:(g + 1) * 512], pd[g], ALU.add)
            P.tt("dve", wv, dtd, E[:, 1, :], ALU.mult)
            P.tt("pool", xw.re("p (h d) -> p h d", h=16), xs.re("p (h d) -> p h d", h=16), bc(wv, [(1, 16), (0, 64)]), ALU.mult)
            P.tt("dve", Hs.re("p (h d) -> p h d", h=16), Hs.re("p (h d) -> p h d", h=16), bc(E[:, 2, :], [(1, 16), (0, 64)]), ALU.mult)
            for g in range(2):
                pS = P.bank()
                P.mm(pS, kt[:, g * 128:(g + 1) * 128], xw[:, g * 512:(g + 1) * 512])
                P.tt("dve", Hs[:, g * 512:(g + 1) * 512], Hs[:, g * 512:(g + 1) * 512], pS, ALU.add)
            P.copy("act", Hsb, Hs)
            pb = P.bank()
            for h in range(4):
                P.mm(pb[:, h * 128:(h + 1) * 128], fm[:, 8 + h, :], fm[:, 4 + h, :])
            P.tt("dve", MTr, pb.re("p (h l) -> p h l", h=4), Dret[:, d], ALU.mult)
            pd = [P.bank(), P.bank()]
            for h in range(4):
                P.mm(pd[h // 2][:, (h % 2) * 256:(h % 2 + 1) * 256], MTr[:, h, :], rvv[:, h * 256:(h + 1) * 256])
            for g in range(2):
                po = P.bank()
                for hh in range(2):
                    h = g * 2 + hh
                    P.mm(po[:, hh * 256:(hh + 1) * 256], fm[:, 4 + h, :], Hrb[:, h * 256:(h + 1) * 256])
                P.tt("dve", ytmp[:, g * 512:(g + 1) * 512].re("p (h d) -> p h d", h=2), po.re("p (h d) -> p h d", h=2),
                     bc(Er[:, d, 0, g * 2:(g + 1) * 2], [(1, 2), (0, 256)]), ALU.mult)
                P.tt("dve", yout[:, 1024 + g * 512:1024 + (g + 1) * 512], ytmp[:, g * 512:(g + 1) * 512], pd[g], ALU.add)
            P.tt("pool", rvw.re("p (h d) -> p h d", h=4), rvv.re("p (h d) -> p h d", h=4), bc(Er[:, d, 1, :], [(1, 4), (0, 256)]), ALU.mult)
            P.tt("dve", Hr.re("p (h d) -> p h d", h=4), Hr.re("p (h d) -> p h d", h=4), bc(Er[:, d, 2, :], [(1, 4), (0, 256)]), ALU.mult)
            for g in range(2):
                pS = P.bank()
                for hh in range(2):
                    h = g * 2 + hh
                    P.mm(pS[:, hh * 256:(hh + 1) * 256], kt[:, 256 + h * 128:256 + (h + 1) * 128], rvw[:, h * 256:(h + 1) * 256])
                P.tt("dve", Hr[:, g * 512:(g + 1) * 512], Hr[:, g * 512:(g + 1) * 512], pS, ALU.add)
            P.copy("act", Hrb, Hr)
            if d == 0:
                P.dma("sp", r_yf[ch], yout)
                continue
            P.tt("pool", ytmp.re("p (h d) -> p h d", h=16), xs.re("p (h d) -> p h d", h=16), bc(r_dsk, [(1, 16), (0, 64)]), ALU.mult)
            P.tt("dve", yout[:, 0:1024], yout[:, 0:1024], ytmp, ALU.add)
            if ci >= 1:
                finish(ci - 1)
        if d == 1:
            finish(len(order) - 1)
    P.close_scope()
    if stop_after == "P3":
        return _finish(P, nc, [x_mid, r_yf])

    P.open_scope()
    NF = FFN_DENSE // 128
    wg = P.sb("wg", [128, 8, FFN_DENSE], BF16)
    wu = P.sb("wu", [128, 8, FFN_DENSE], BF16)
    wd = P.sb("wd", [128, NF, D], BF16)
    for k in range(8):
        P.dma("pool", wg[:, k, :], ffg[k * 128:(k + 1) * 128, :])
        P.dma("pool", wu[:, k, :], ffu[k * 128:(k + 1) * 128, :])
    for f in range(NF):
        P.dma("pool", wd[:, f, :], ffd[f * 128:(f + 1) * 128, :])
    xb4 = [P.sb("xb4_%d" % i, [128, 8, 256], F32) for i in range(2)]
    tmp4 = {"sq": P.sb("sq4", [128, 8, 256], F32), "rstd": P.sb("rstd4", [128, 256], F32)}
    h4 = P.sb("h4", [128, 8, 256], BF16)
    a4 = P.sb("a4", [128, NF, 256], BF16)
    sg4 = [P.sb("sg4_%d" % i, [128, 256], F32) for i in range(2)]
    xo4 = [P.sb("xo4_%d" % i, [128, 8, 256], F32) for i in range(1)]
    def load4(bj):
        for t in range(2):
            P.dma("sp", xb4[bj % 2][:, :, t * 128:(t + 1) * 128], x_mid[bj * 2 + t])
    load4(0)
    for bi in range(NCH // 2):
        x4 = xb4[bi % 2]
        which = 1 if bi == 0 else 0
        if bi + 1 < NCH // 2:
            load4(bi + 1)
        rms_modulate(x4, 256, AB[0][:, :, which, 1, :], h4, tmp4)
        for f in range(NF):
            pb = P.bank()
            P.mmgroup(pb[:, 0:256], [(wg[:, k, f * 128:(f + 1) * 128], h4[:, k, :]) for k in range(8)])
            P.mmgroup(pb[:, 256:512], [(wu[:, k, f * 128:(f + 1) * 128], h4[:, k, :]) for k in range(8)])
            sg = sg4[f % 2]
            P.act(sg, pb[:, 0:256], AF.Silu)
            P.tt("dve", a4[:, f, :], sg, pb[:, 256:512], ALU.mult)
        xo_ = xo4[0]
        for q in range(4):
            pb = P.bank()
            for j in range(2):
                dc = q * 2 + j
                P.mmgroup(pb[:, j * 256:(j + 1) * 256], [(wd[:, f, dc * 128:(dc + 1) * 128], a4[:, f, :]) for f in range(NF)])
            for j in range(2):
                dc = q * 2 + j
                P.stt("dve", xo_[:, dc, :], pb[:, j * 256:(j + 1) * 256], G[0][:, dc, which, 1:2], x4[:, dc, :], ALU.mult, ALU.add)
        for t in range(2):
            P.dma("sp", x_l1[bi * 2 + t], xo_[:, :, t * 128:(t + 1) * 128])
    P.close_scope()
    if stop_after == "P4":
        return _finish(P, nc, [x_l1])

    P.open_scope()
    w1 = P.sb("w1", [128, 8, 3072], BF16)
    for k in range(8):
        P.dma("pool", w1[:, k, :], w_in1[k * 128:(k + 1) * 128, :])
    xb5 = [P.sb("xb5_%d" % i, [128, 8, 256], F32) for i in range(2)]
    tmp5 = {"sq": P.sb("sq5", [128, 8, 256], F32), "rstd": P.sb("rstd5", [128, 256], F32)}
    h5 = P.sb("h5", [128, 8, 256], BF16)
    atab = P.sb("atab", [128, 2, 128], F32)
    qsq = P.sb("qsq", [128, 1024], F32)
    qn = P.sb("qn", [128, 1024], F32)
    q1 = P.sb("q1", [128, 1024], F32)
    q2 = P.sb("q2", [128, 1024], F32)
    ss5 = P.sb("ss5", [128, 16], F32)
    qkb = P.sb("qkb", [128, 2, 1024], BF16)
    qkT = P.sb("qkT", [128, 2, 8, 256], BF16)
    v5 = [P.sb("v5_%d" % i, [128, 2, 8, 130], BF16) for i in range(2)]
    for i in range(2):
        P.memset("dve", v5[i], 1.0)
    qg = P.sb("qg", [128, 64], F32)
    P.ts("dve", qg, r_qn, 64.0 ** -0.5, None, op0=ALU.mult)
    atabs = [atab, P.sb("atab2", [128, 2, 128], F32), P.sb("atab3", [128, 2, 128], F32)]
    h5s = [h5, P.sb("h5b", [128, 8, 256], BF16)]
    qsqs = [qsq, P.sb("qsq_b", [128, 1024], F32)]
    qns = [qn, P.sb("qn_b", [128, 1024], F32)]
    q1s = [q1, P.sb("q1_b", [128, 1024], F32)]
    q2s = [q2, P.sb("q2_b", [128, 1024], F32)]
    ss5s = [ss5, P.sb("ss5_b", [128, 16], F32)]
    NB5 = NCH // 2

    def load5(bj):
        for t in range(2):
            P.dma("sp", xb5[bj % 2][:, :, t * 128:(t + 1) * 128], x_l1[bj * 2 + t])
        if bj > 0:
            l0 = (bj - 1) * 256
            P.dma("sp", atabs[bj % 3], acs_d[l0:l0 + 256, :].re("(t p) c -> p t c", p=128))

    def stage5A(bj):
        which = 1 if bj == 0 else 0
        rms_modulate(xb5[bj % 2], 256, AB[1][:, :, which, 0, :], h5s[bj % 2], tmp5, eng="act")

    def stage5B(bi):
        h5 = h5s[bi % 2]
        atab = atabs[bi % 3]
        which = 1 if bi == 0 else 0
        vv = v5[bi % 2]
        qis = [1] if which == 1 else [0, 1]
        for t in range(2):
            lt = [h5[:, k, t * 128:(t + 1) * 128] for k in range(8)]
            pbs = {}
            for qi in qis:
                pbs[qi] = []
                for j in range(2):
                    pb = P.bank()
                    c0 = qi * 1024 + j * 512
                    P.mmgroup(pb, [(lt[k], w1[:, k, c0:c0 + 512]) for k in range(8)])
                    pbs[qi].append(pb)
            for qi in qis:
                for j in range(2):
                    P.act(qsqs[qi][:, j * 512:(j + 1) * 512], pbs[qi][j], AF.Square)
            for qi in qis:
                P.reduce("dve", ss5s[qi], qsqs[qi].re("p (g d) -> p g d", d=64), ALU.add)
                P.ts("dve", ss5s[qi], ss5s[qi], 1.0 / 64, EPS, op0=ALU.mult, op1=ALU.add)
            for qi in qis:
                P.rsqrt(ss5s[qi])
            for qi in qis:
                for j in range(2):
                    P.tt("dve", qns[qi][:, j * 512:(j + 1) * 512].re("p (g d) -> p g d", d=64), pbs[qi][j].re("p (g d) -> p g d", d=64),
                         bc(ss5s[qi][:, j * 8:(j + 1) * 8], [(1, 8), (0, 64)]), ALU.mult)
            pvs = []
            for j in range(2):
                pb = P.bank()
                c0 = 2048 + j * 512
                P.mmgroup(pb, [(lt[k], w1[:, k, c0:c0 + 512]) for k in range(8)])
                pvs.append(pb)
            for qi in qis:
                qn = qns[qi]
                gn = qg if qi == 0 else r_kn
                dst = qkb[:, qi, :]
                if which == 1:
                    P.tt("dve", dst.re("p (g d) -> p g d", d=64), qn.re("p (g d) -> p g d", d=64), bc(gn, [(0, 16), (1, 64)]), ALU.mult)
                else:
                    P.tt("dve", qn.re("p (g d) -> p g d", d=64), qn.re("p (g d) -> p g d", d=64), bc(gn, [(0, 16), (1, 64)]), ALU.mult)
            for j in range(2):
                P.copy("act", vv[:, t, j * 4:(j + 1) * 4, 0:128], pvs[j].re("p (h e) -> p h e", h=4))
            if which == 0:
                for qi in qis:
                    qn, q1, q2 = qns[qi], q1s[qi], q2s[qi]
                    P.tt("dve", q1.re("p (g d) -> p g d", d=64), qn.re("p (g d) -> p g d", d=64), bc(atab[:, t, 0:64], [(0, 16), (1, 64)]), ALU.mult)
                    qv = qn.re("p (g a u d) -> p g a u d", a=2, u=2, d=16)
                    q2v = q2.re("p (g a u d) -> p g a u d", a=2, u=2, d=16)
                    for s_ in range(2):
                        sn = bass.AP(atab.ap.tensor, atab[:, t, 64 + s_ * 16:64 + s_ * 16 + 16].ap.offset,
                                     [list(atab.ap.ap[0]), [0, 16], [32, 2], [1, 16]])
                        P.tt("pool", q2v[:, :, :, s_, :], qv[:, :, :, 1 - s_, :], TT(sn, atab.tok), ALU.mult)
                for qi in qis:
                    P.tt("dve", qkb[:, qi, :], q1s[qi], q2s[qi], ALU.add)
            for qi in qis:
                pb = P.bank()
                pbb = bank_bf(pb)
                for h in range(8):
                    P.transpose(pbb[:, h * 128:(h + 1) * 128], qkb[:, qi, h * 128:(h + 1) * 128], ident_b)
                P.copy("act", qkT[:, qi, :, t * 128:(t + 1) * 128], pbb.re("p (h t) -> p h t", h=8))
        tok0 = bi * 256
        P.dma("sp", Kd[:, :, tok0:tok0 + 256].re("h p t -> p h t"), qkT[:, 1])
        if which == 0:
            P.dma("sp", Qd[:, :, tok0 - CTX:tok0 - CTX + 256].re("h p t -> p h t"), qkT[:, 0])
        for t in range(2):
            P.dma("sp", Vd[:, :, bi * 2 + t, :].re("h p e -> p h e"), vv[:, t])

    load5(0)
    if NB5 > 1:
        load5(1)
    stage5A(0)
    for bi in range(NB5):
        if bi + 2 < NB5:
            load5(bi + 2)
        if bi + 1 < NB5:
            stage5A(bi + 1)
        stage5B(bi)
    P.close_scope()
    if stop_after == "P5":
        return _finish(P, nc, [Kd, Vd, Qd])

    P.open_scope()
    NKT = NCH
    NQB = OWN // 512
    lt_ = P.sb("lt_", [128, 128], F32)
    lam2 = P.sb("lam2", [128, 4], F32)
    P.tt("dve", lt_[:, 0:64], r_lam[:, 0:64], r_lam[:, 64:128], ALU.mult)
    P.tt("dve", lt_[:, 64:128], r_lam[:, 128:192], r_lam[:, 192:256], ALU.mult)
    P.reduce("dve", lam2[:, 0:2], lt_.re("p (a d) -> p a d", a=2), ALU.add)
    P.act(lam2[:, 0:2], lam2[:, 0:2], AF.Exp)
    P.tt("dve", lam2[:, 2:3], lam2[:, 1:2], lam2[:, 0:1], ALU.subtract)
    P.ts("dve", lam2[:, 3:4], lam2[:, 2:3], -LAM_INIT, None, op0=ALU.add)
    neglam = lam2[:, 3:4]
    sub_g = P.sb("sub_g", [128, 128], F32)
    P.ts("dve", sub_g, r_subln, 1.0 - LAM_INIT, None, op0=ALU.mult)
    Kh = [P.sb("Kh%d" % i, [128, T], BF16) for i in range(2)]
    Vh = [P.sb("Vh%d" % i, [128, NKT, 130], BF16) for i in range(2)]
    qa = [P.sb("qa%d" % i, [128, 512], BF16) for i in range(2)]
    qb_ = [P.sb("qb%d" % i, [128, 512], BF16) for i in range(2)]
    qs = [P.sb("qs%d" % i, [128, 512], BF16) for i in range(2)]
    pT = [P.sb("pT%d" % i, [128, 512], BF16) for i in range(3)]
    o0 = P.sb("o0", [128, 4, 128], F32)
    o1 = P.sb("o1", [128, 128], F32)
    osq = P.sb("osq", [128, 4, 128], F32)
    rs6 = P.sb("rs6", [128, 4], F32)
    on = P.sb("on", [128, 4, 128], BF16)
    oT = [P.sb("oT%d" % i, [128, 512], BF16) for i in range(2)]
    spb = [P.banks[0], P.banks[1], P.banks[2]]
    ob = [P.banks[3], P.banks[4], P.banks[5], P.banks[6]]
    tb = P.banks[7]
    groups = [(h, qb) for h in range(8) for qb in range(NQB)]
    steps = [(m, kt) for m in range(2) for kt in range(NKT)]

    def load_kv(h):
        P.dma("sp", Kh[h % 2], Kd[h])
        P.dma("sp", Vh[h % 2], Vd[h])

    def load_q(gi):
        h, qb = groups[gi]
        A, B_, Q_ = qa[gi % 2], qb_[gi % 2], qs[gi % 2]
        P.dma("sp", A, Qd[h, :, qb * 512:(qb + 1) * 512])
        if L > OWN:
            P.dma("sp", B_, Qd[h, :, OWN + qb * 512:OWN + (qb + 1) * 512])
            P.ts("pool", Q_, A, sel_sb[:, 0:1], None, op0=ALU.mult)
            P.stt("dve", Q_, B_, sel_sb[:, 1:2], Q_, ALU.mult, ALU.add)
            return Q_
        return A

    load_kv(0)
    Qn = load_q(0)
    pi = 0
    for gi, (h, qb) in enumerate(groups):
        K_, V_ = Kh[h % 2], Vh[h % 2]
        Q_ = Qn
        if qb == 0 and h + 1 < 8:
            load_kv(h + 1)
        if gi + 1 < len(groups):
            Qn = load_q(gi + 1)

        def emit_s(i):
            m, kt = steps[i]
            P.mm(spb[(pi + i) % 3], K_[m * 64:(m + 1) * 64, kt * 128:(kt + 1) * 128], Q_[m * 64:(m + 1) * 64, :])
        emit_s(0)
        emit_s(1)
        for i, (m, kt) in enumerate(steps):
            if i + 2 < len(steps):
                emit_s(i + 2)
            sp_ = spb[(pi + i) % 3]
            p_ = pT[(pi + i) % 3]
            P.act(p_, sp_, AF.Exp, bias=-8.0)
            for s_ in range(4):
                P.mm(ob[s_][:, 0:129], p_[:, s_ * 128:(s_ + 1) * 128], V_[:, kt, 0:129], start=(kt == 0), stop=(kt == NKT - 1))
            if kt == NKT - 1:
                for s_ in range(4):
                    P.recip(rs6[:, s_:s_ + 1], ob[s_][:, 128:129])
                    if m == 0:
                        P.ts("dve", o0[:, s_, :], ob[s_][:, 0:128], rs6[:, s_:s_ + 1], None, op0=ALU.mult)
                    else:
                        P.ts("dve", o1, ob[s_][:, 0:128], rs6[:, s_:s_ + 1], neglam, op0=ALU.mult, op1=ALU.mult)
                        P.tt("dve", o0[:, s_, :], o0[:, s_, :], o1, ALU.add)
        pi += len(steps)
        P.act(osq, o0, AF.Square)
        P.reduce("dve", rs6, osq, ALU.add)
        P.ts("dve", rs6, rs6, 1.0 / 128, EPS, op0=ALU.mult, op1=ALU.add)
        P.rsqrt(rs6)
        tbb = bank_bf(tb)
        for s_ in range(4):
            P.stt("dve", on[:, s_, :], o0[:, s_, :], rs6[:, s_:s_ + 1], sub_g, ALU.mult, ALU.mult)
            P.transpose(tbb[:, s_ * 128:(s_ + 1) * 128], on[:, s_, :], ident_b)
        o_ = oT[gi % 2]
        P.copy("dve", o_, tbb[:, 0:512])
        P.dma("sp", Od[:, h, qb * 512:(qb + 1) * 512], o_)
    P.close_scope()
    if stop_after == "P6":
        return _finish(P, nc, [Od])

    P.open_scope()
    BLK = min(1024, OWN)
    NB = OWN // BLK
    NH = BLK // 512
    NT7 = BLK // 128
    wo1 = P.sb("wo1", [128, 8, D], BF16)
    for k in range(8):
        P.dma("pool", wo1[:, k, :], w_out1[k * 128:(k + 1) * 128, :])
    wr_sb = P.sb("wr_sb", [128, 8, 8], F32)
    P.dma("sp", wr_sb, wr.re("(k p) e -> p k e", p=128))
    selm = P.sb("selm", [8, 1024], F32)
    P.dma("sp", selm, selmat)
    o7 = P.sb("o7", [128, 8, BLK], BF16)
    xa = P.sb("xa", [128, 8, BLK], F32)
    xb7 = P.sb("xb7", [128, 8, 128], F32)
    sq7 = P.sb("sq7", [128, 8, 512], F32)
    rstd7 = P.sb("rstd7", [128, 512], F32)
    h7 = P.sb("h7", [128, 8, BLK], BF16)
    h7f = sq7
    yacc = P.sb("yacc", [128, 8, BLK], F32)
    lg = P.sb("lg", [128, 8], F32)
    lg2 = P.sb("lg2", [128, 8], F32)
    eq1 = P.sb("eq1", [128, 8], F32)
    eq2 = P.sb("eq2", [128, 8], F32)
    mx = P.sb("mx", [128, 8], F32)
    comb = P.sb("comb", [128, 8], F32)
    combT = P.sb("combT", [8, BLK], F32)
    cbc = [P.sb("cbc%d" % i, [128, BLK], BF16) for i in range(2)]
    FG = 2
    NFG = FEXP // (128 * FG)
    FW = 128 * FG
    wge = [P.sb("wge%d" % i, [128, 8, FW], BF16) for i in range(2)]
    wue = [P.sb("wue%d" % i, [128, 8, FW], BF16) for i in range(2)]
    wde = [P.sb("wde%d" % i, [128, FG, D], BF16) for i in range(2)]
    a7 = [P.sb("a7_%d" % i, [128, FG, BLK], BF16) for i in range(2)]
    sg7 = [P.sb("sg7_%d" % i, [128, 512], F32) for i in range(2)]
    t7 = [P.sb("t7_%d" % i, [128, 512], F32) for i in range(2)]
    its = [(nb, e, fg) for nb in range(NB) for e in range(NEXP) for fg in range(NFG)]

    def issue_w(ii):
        nb_, e_, fg_ = its[ii]
        b_ = ii % 2
        f0_ = fg_ * FW
        P.dma("pool", wge[b_], eg[e_, :, f0_:f0_ + FW].re("(k p) f -> p k f", p=128))
        P.dma("pool", wue[b_], eu[e_, :, f0_:f0_ + FW].re("(k p) f -> p k f", p=128))
        P.dma("pool", wde[b_], ed[e_, f0_:f0_ + FW, :].re("(f p) d -> p f d", p=128))
    issue_w(0)
    wi = 0
    for nb in range(NB):
        P.dma("sp", o7, Od[:, :, nb * BLK:(nb + 1) * BLK])
        for t in range(NT7):
            chA = 2 + (nb * BLK) // 128 + t
            P.dma("sp", xa[:, :, t * 128:(t + 1) * 128], x_l1[chA])
            if L > OWN:
                P.dma("sp", xb7, x_l1[chA + OWN // 128])
                P.ts("pool", xa[:, :, t * 128:(t + 1) * 128], xa[:, :, t * 128:(t + 1) * 128], sel_sb[:, 0:1], None, op0=ALU.mult)
                P.stt("pool", xa[:, :, t * 128:(t + 1) * 128], xb7, sel_sb[:, 1:2], xa[:, :, t * 128:(t + 1) * 128], ALU.mult, ALU.add)
        for hf in range(NH):
            cs = slice(hf * 512, (hf + 1) * 512)
            for dc in range(8):
                pb = P.bank()
                P.mmgroup(pb, [(wo1[:, hh, dc * 128:(dc + 1) * 128], o7[:, hh, cs]) for hh in range(8)])
                P.stt("dve", xa[:, dc, cs], pb, G[1][:, dc, 0, 0:1], xa[:, dc, cs], ALU.mult, ALU.add)
            P.act(sq7, xa[:, :, cs], AF.Square)
            pb = P.bank()
            P.mmgroup(pb, [(ones_f, sq7[:, k, :]) for k in range(8)])
            P.ts("dve", rstd7, pb, 1.0 / D, EPS, op0=ALU.mult, op1=ALU.add)
            P.rsqrt(rstd7)
            P.tt("dve", sq7, xa[:, :, cs], bc(rstd7, [(0, 8), (1, 512)]), ALU.mult)
            A2 = AB[1][:, :, 0, 1, :]
            for k in range(8):
                P.ts("pool", h7f[:, k, :], sq7[:, k, :], A2[:, k, 0:1], A2[:, k, 1:2], op0=ALU.mult, op1=ALU.add)
                P.copy("act", h7[:, k, cs], h7f[:, k, :])
            for t4 in range(4):
                pb = P.bank()
                P.mmgroup(pb[:, 0:8], [(h7f[:, k, t4 * 128:(t4 + 1) * 128], wr_sb[:, k, :]) for k in range(8)])
                P.copy("dve", lg, pb[:, 0:8])
                P.reduce("dve", mx[:, 0:1], lg, ALU.max)
                P.ts("dve", eq1, lg, mx[:, 0:1], None, op0=ALU.is_equal)
                P.stt("dve", lg2, eq1, -1e30, lg, ALU.mult, ALU.add)
                P.reduce("dve", mx[:, 1:2], lg2, ALU.max)
                P.ts("dve", eq2, lg2, mx[:, 1:2], None, op0=ALU.is_equal)
                P.tt("dve", mx[:, 2:3], mx[:, 1:2], mx[:, 0:1], ALU.subtract)
                P.act(mx[:, 3:4], mx[:, 2:3], AF.Exp)
                P.ts("dve", mx[:, 4:5], mx[:, 3:4], 1.0, None, op0=ALU.add)
                P.recip(mx[:, 5:6], mx[:, 4:5])
                P.tt("dve", mx[:, 6:7], mx[:, 3:4], mx[:, 5:6], ALU.mult)
                P.ts("dve", comb, eq1, mx[:, 5:6], None, op0=ALU.mult)
                P.stt("dve", comb, eq2, mx[:, 6:7], comb, ALU.mult, ALU.add)
                pb = P.bank()
                P.transpose(pb[0:8, 0:128], comb, ident_f)
                P.copy("dve", combT[:, hf * 512 + t4 * 128:hf * 512 + (t4 + 1) * 128], pb[0:8, 0:128])
        P.memset("pool", yacc, 0.0)
        for e in range(NEXP):
            cb = cbc[e % 2]
            for hf in range(NH):
                cs = slice(hf * 512, (hf + 1) * 512)
                pb = P.bank()
                P.mm(pb, selm[:, e * 128:(e + 1) * 128], combT[:, cs])
                P.copy("act", cb[:, cs], pb)
            for fg in range(NFG):
                b = wi % 2
                wi += 1
                if wi < len(its):
                    issue_w(wi)
                aa = a7[b]
                ii = 0
                for f in range(FG):
                    for hf in range(NH):
                        cs = slice(hf * 512, (hf + 1) * 512)
                        pg = P.bank()
                        pu = P.bank()
                        P.mmgroup(pg, [(wge[b][:, k, f * 128:(f + 1) * 128], h7[:, k, cs]) for k in range(8)])
                        P.mmgroup(pu, [(wue[b][:, k, f * 128:(f + 1) * 128], h7[:, k, cs]) for k in range(8)])
                        sg = sg7[ii % 2]
                        tt_ = t7[ii % 2]
                        ii += 1
                        P.act(sg, pg, AF.Silu)
                        P.tt("dve", tt_, sg, pu, ALU.mult)
                        P.tt("pool", aa[:, f, cs], tt_, cb[:, cs], ALU.mult)
                for hf in range(NH):
                    cs = slice(hf * 512, (hf + 1) * 512)
                    for dc in range(8):
                        pb = P.bank()
                        P.mmgroup(pb, [(wde[b][:, f, dc * 128:(dc + 1) * 128], aa[:, f, cs]) for f in range(FG)])
                        P.tt("dve", yacc[:, dc, cs], yacc[:, dc, cs], pb, ALU.add)
        for dc in range(8):
            P.stt("dve", yacc[:, dc, :], yacc[:, dc, :], G[1][:, dc, 0, 1:2], xa[:, dc, :], ALU.mult, ALU.add)
        P.dma("sp", outT[:, nb * BLK:(nb + 1) * BLK].re("(k p) t -> p k t", p=128), yacc)
    P.close_scope()
    return _finish(P, nc, [outT])


def P_sb_keep(P, name, shape):
    g = P.nc.sbuf_tensor(name, list(shape), F32)
    h = g.__enter__()
    P._ctx.insert(0, g)
    for i in range(len(P._scopes)):
        P._scopes[i] += 1
    return TT(h[:], Tok(name))


def _finish(P, nc, outs):
    P.fence("sp", outs)
    P.emit()
    P.close()
    return nc


def fm_vec(v):
    v = np.asarray(v, np.float32).reshape(-1, 128)
    return np.ascontiguousarray(v.T)


def prep_inputs(inp, L, OWN, ncores_per_batch, nbatch):
    cm, selm = host_consts()
    rcs, acs = rope_tables(L)
    shared = {
        "consts": cm, "selmat": selm, "rcs": rcs, "acs": acs,
        "w_mod0": np.ascontiguousarray(inp["even_w_mod"][0]), "w_mod1": np.ascontiguousarray(inp["odd_w_mod"][0]),
        "b_mod0": fm_vec(inp["even_b_mod"][0]), "b_mod1": fm_vec(inp["odd_b_mod"][0]),
        "norms": np.concatenate([fm_vec(inp["even_norm1"][0]), fm_vec(inp["even_norm2"][0]),
                                 fm_vec(inp["odd_norm1"][0]), fm_vec(inp["odd_norm2"][0])], axis=1),
        "w_in0": np.ascontiguousarray(inp["even_w_in"][0]),
        "w_out0": np.ascontiguousarray(inp["even_w_out"][0]),
        "ffg": np.ascontiguousarray(inp["even_ffn_gate"][0]), "ffu": np.ascontiguousarray(inp["even_ffn_up"][0]),
        "ffd": np.ascontiguousarray(inp["even_ffn_down"][0]),
        "w_in1": np.ascontiguousarray(inp["odd_w_in"][0]), "w_out1": np.ascontiguousarray(inp["odd_w_out"][0]),
        "router": np.ascontiguousarray(inp["odd_router"][0]),
        "eg": np.ascontiguousarray(inp["odd_exp_gate"][0]), "eu": np.ascontiguousarray(inp["odd_exp_up"][0]),
        "ed": np.ascontiguousarray(inp["odd_exp_down"][0]),
    }
    cw = np.concatenate([inp["even_conv_w"][0], inp["even_conv_b"][0][None, :]], axis=0)
    shared["convw"] = np.ascontiguousarray(cw.reshape(4, 12, 128).transpose(2, 1, 0))
    rowp = np.concatenate([
        inp["even_dt_bias"][0].reshape(-1), inp["even_a_log"][0].reshape(-1), inp["even_ret_decay"][0].reshape(-1),
        inp["even_d"][0].reshape(-1), inp["even_ssd_norm"][0].reshape(-1), inp["odd_q_norm"][0].reshape(-1),
        inp["odd_k_norm"][0].reshape(-1), inp["odd_lambda"][0].reshape(-1), inp["odd_subln"][0].reshape(-1)]).astype(np.float32)
    rp = np.zeros((1, 2560), np.float32)
    rp[0, :rowp.size] = rowp
    shared["rowp"] = rp
    maps = []
    for b in range(nbatch):
        xT = np.zeros((D, L + 2), np.float32)
        xT[:, 1:L + 1] = inp["x"][b].T
        cT = np.zeros((D, CTX + 2), np.float32)
        cT[:, 1:CTX + 1] = inp["ctx"][b].T
        cvec = np.concatenate([fm_vec(inp["c"][b]), fm_vec(inp["c_ctx"])], axis=1)
        for hf in range(ncores_per_batch):
            s = np.zeros((128, 2), np.float32)
            s[:, hf] = 1.0
            m = dict(shared)
            m.update({"xT": xT, "ctxT": cT, "cvec": cvec, "sel": s})
            maps.append(m)
    return maps


_NC_CACHE = {}


def kernel(**inputs):
    inp = {k: np.asarray(v) for k, v in inputs.items()}
    B, L, _ = inp["x"].shape
    OWN = L // 2
    key = (L, OWN)
    if key not in _NC_CACHE:
        _NC_CACHE[key] = build(L, OWN)
    nc = _NC_CACHE[key]
    maps = prep_inputs(inp, L, OWN, 2, B)
    res = run_bass_kernel_spmd(nc, maps, core_ids=list(range(len(maps))))
    out = np.empty((B, L, D), np.float32)
    for b in range(B):
        for hf in range(2):
            out[b, hf * OWN:(hf + 1) * OWN, :] = res.results[b * 2 + hf]["outT"].T
    return out
```

```python
import math
import numpy as np
import ml_dtypes
import concourse.bass as bass
import concourse.mybir as mybir
from concourse.bass_utils import run_bass_kernel_spmd

F32 = mybir.dt.float32
BF16 = mybir.dt.bfloat16
AF = mybir.ActivationFunctionType
ALU = mybir.AluOpType
AX = mybir.AxisListType

SAME_ENGINE_SYNC = True
NSLOT = 10


class Tok:
    __slots__ = ("lw", "rd", "name")

    def __init__(self, name=""):
        self.lw = None
        self.rd = []
        self.name = name


class TT:
    __slots__ = ("ap", "tok")

    def __init__(self, ap, tok):
        self.ap = ap
        self.tok = tok

    def __getitem__(self, k):
        return TT(self.ap[k], self.tok)

    def re(self, s, **kw):
        return TT(self.ap.rearrange(s, **kw), self.tok)

    @property
    def shape(self):
        return self.ap.shape


def bc(tt, dims):
    a = tt.ap
    base = list(a.ap)
    return TT(bass.AP(a.tensor, a.offset, [list(base[0])] + [list(d) for d in dims]), tt.tok)


class Op:
    __slots__ = ("stream", "fn", "deps", "dma", "ms", "slot", "val", "didx")

    def __init__(self, stream, fn, dma):
        self.stream = stream
        self.fn = fn
        self.deps = []
        self.dma = dma
        self.ms = False
        self.slot = None
        self.val = None
        self.didx = None


class Prog:
    STREAMS = ("pe", "act", "dve", "pool", "sp")

    def __init__(self, nc):
        self.nc = nc
        self.ops = {s: [] for s in self.STREAMS}
        self.ndma = {s: 0 for s in self.STREAMS}
        self.dmaops = {s: [] for s in self.STREAMS}
        self._ctx = []
        self._scopes = []
        self.banks = []
        self.bank_i = 0

    def sb(self, name, shape, dt=F32):
        g = self.nc.sbuf_tensor(name, list(shape), dt)
        h = g.__enter__()
        self._ctx.append(g)
        return TT(h[:], Tok(name))

    def ps(self, name, shape, dt=F32):
        g = self.nc.psum_tensor(name, list(shape), dt)
        h = g.__enter__()
        self._ctx.append(g)
        return TT(h[:], Tok(name))

    def dram(self, name, shape, dt=F32, kind="Internal"):
        h = self.nc.dram_tensor(name, list(shape), dt, kind=kind)
        return TT(h.ap(), Tok(name))

    def open_scope(self):
        self._scopes.append(len(self._ctx))

    def close_scope(self):
        n = self._scopes.pop()
        self.barrier()
        while len(self._ctx) > n:
            self._ctx.pop().__exit__(None, None, None)

    def close(self):
        while self._ctx:
            self._ctx.pop().__exit__(None, None, None)

    def bank(self):
        b = self.banks[self.bank_i % len(self.banks)]
        self.bank_i += 1
        return b

    def _rec(self, stream, fn, reads, writes, dma=False, extra=()):
        op = Op(stream, fn, dma)
        deps = list(extra)
        for t in reads:
            if t.tok.lw is not None:
                deps.append(t.tok.lw)
        for t in writes:
            if t.tok.lw is not None:
                deps.append(t.tok.lw)
            deps.extend(t.tok.rd)
        if dma:
            op.didx = self.ndma[stream]
            self.ndma[stream] += 1
            self.dmaops[stream].append(op)
            if op.didx >= NSLOT:
                deps.append(self.dmaops[stream][op.didx - NSLOT])
        seen = set()
        for d in deps:
            if d is op or id(d) in seen:
                continue
            if (not d.dma) and d.stream == stream and (stream == "pe" or not SAME_ENGINE_SYNC):
                continue
            seen.add(id(d))
            op.deps.append(d)
            d.ms = True
        for t in reads:
            t.tok.rd.append(op)
        for t in writes:
            t.tok.lw = op
            t.tok.rd = []
        self.ops[stream].append(op)
        return op

    def barrier(self):
        last = []
        for s in self.STREAMS:
            if self.ops[s]:
                for o in reversed(self.ops[s]):
                    if not o.dma and o.fn is not None:
                        last.append(o)
                        break
            last.extend(self.dmaops[s][-NSLOT:])
        for s in self.STREAMS:
            self._rec(s, None, [], [], extra=last)

    def mm(self, out, lhsT, rhs, start=True, stop=True):
        self._rec("pe", lambda e: e.matmul(out.ap, lhsT.ap, rhs.ap, start=start, stop=stop), [lhsT, rhs], [out])

    def mmgroup(self, out, pairs):
        rd = []
        for l, r in pairs:
            rd += [l, r]
        n = len(pairs)

        def fn(e):
            ins = None
            for i, (l, r) in enumerate(pairs):
                ins = e.matmul(out.ap, l.ap, r.ap, start=(i == 0), stop=(i == n - 1))
            return ins
        self._rec("pe", fn, rd, [out])

    def transpose(self, out, in_, ident):
        self._rec("pe", lambda e: e.transpose(out.ap, in_.ap, ident.ap), [in_, ident], [out])

    def act(self, out, in_, func, bias=None, scale=1.0, accum=None):
        rd = [in_]
        kw = {}
        if bias is not None:
            if isinstance(bias, TT):
                rd.append(bias)
                kw["bias"] = bias.ap
            else:
                kw["bias"] = bias
        if isinstance(scale, TT):
            rd.append(scale)
            kw["scale"] = scale.ap
        else:
            kw["scale"] = scale
        wr = [out]
        if accum is not None:
            wr.append(accum)
            kw["accum_out"] = accum.ap
        self._rec("act", lambda e: e.activation(out.ap, in_.ap, func, **kw), rd, wr)

    def tt(self, eng, out, a, b, op):
        self._rec(eng, lambda e: e.tensor_tensor(out.ap, a.ap, b.ap, op), [a, b], [out])

    def ts(self, eng, out, a, s1, s2=None, op0=ALU.mult, op1=None):
        rd = [a]
        s1a = s1.ap if isinstance(s1, TT) else s1
        s2a = s2.ap if isinstance(s2, TT) else s2
        if isinstance(s1, TT):
            rd.append(s1)
        if isinstance(s2, TT):
            rd.append(s2)
        kw = {}
        if op1 is not None:
            kw["op1"] = op1
        self._rec(eng, lambda e: e.tensor_scalar(out.ap, a.ap, s1a, s2a, op0, **kw), rd, [out])

    def stt(self, eng, out, a, s, b, op0, op1):
        rd = [a, b]
        sa = s.ap if isinstance(s, TT) else s
        if isinstance(s, TT):
            rd.append(s)
        self._rec("dve", lambda e: e.scalar_tensor_tensor(out.ap, a.ap, sa, b.ap, op0, op1), rd, [out])

    def copy(self, eng, out, a):
        if eng == "act":
            self._rec(eng, lambda e: e.copy(out.ap, a.ap), [a], [out])
        else:
            self._rec(eng, lambda e: e.tensor_copy(out.ap, a.ap), [a], [out])

    def memset(self, eng, out, val):
        self._rec(eng, lambda e: e.memset(out.ap, val), [], [out])

    def reduce(self, eng, out, a, op, axis=AX.X):
        self._rec(eng, lambda e: e.tensor_reduce(out.ap, a.ap, axis, op), [a], [out])

    def rsqrt(self, x):
        self._rec("act", lambda e: e.activation(x.ap, x.ap, AF.Sqrt), [x], [x])
        self._rec("dve", lambda e: e.reciprocal(x.ap, x.ap), [x], [x])

    def recip(self, out, a):
        self._rec("dve", lambda e: e.reciprocal(out.ap, a.ap), [a], [out])

    def dma(self, q, out, in_):
        self._rec(q, lambda e: e.dma_start(out.ap, in_.ap), [in_], [out], dma=True)

    def fence(self, stream, tts):
        self._rec(stream, None, list(tts), [])

    def emit(self):
        nc = self.nc
        for s in self.STREAMS:
            k = 0
            for op in self.ops[s]:
                if op.dma:
                    op.slot = op.didx % NSLOT
                    op.val = 16 * (op.didx // NSLOT + 1)
                elif op.ms:
                    k += 1
                    op.val = k
        sem_ctx = []
        csem = {}
        dsem = {}
        for s in self.STREAMS:
            g = nc.semaphore("c_" + s)
            csem[s] = g.__enter__()
            sem_ctx.append(g)
            if self.ndma[s] > 0:
                for i in range(NSLOT):
                    g = nc.semaphore("d_%s_%d" % (s, i))
                    dsem[(s, i)] = g.__enter__()
                    sem_ctx.append(g)
        ops = self.ops

        def run(stream, e):
            waited = {}
            for op in ops[stream]:
                for d in op.deps:
                    if d.dma:
                        key = ("d", d.stream, d.slot)
                        sem = dsem[(d.stream, d.slot)]
                    else:
                        key = ("c", d.stream)
                        sem = csem[d.stream]
                    if waited.get(key, 0) < d.val:
                        e.wait_ge(sem, d.val)
                        waited[key] = d.val
                if op.fn is None:
                    continue
                ins = op.fn(e)
                if op.dma:
                    ins.then_inc(dsem[(stream, op.slot)], 16)
                elif op.ms:
                    ins.then_inc(csem[stream], 1)

        with nc.Block() as block:
            @block.sync
            def _(e):
                run("sp", e)

            @block.tensor
            def _(e):
                run("pe", e)

            @block.scalar
            def _(e):
                run("act", e)

            @block.vector
            def _(e):
                run("dve", e)

            @block.gpsimd
            def _(e):
                run("pool", e)
        for g in reversed(sem_ctx):
            g.__exit__(None, None, None)


D = 1024
KC = 8
EPS = 1e-6
EVEN_IN = 5664
FFN_DENSE = 2816
NEXP = 8
FEXP = 3584
CTX = 256
GRID_W = 64
LAM_INIT = 0.8 - 0.6 * math.exp(-0.3 * 1)
C_Z, C_XBC, C_DT, C_RQ, C_RK, C_RV, C_RG = 0, 1024, 2560, 2592, 3104, 3616, 4640


def host_consts():
    k = np.arange(128)[:, None]
    l = np.arange(128)[None, :]
    c = {}
    c["ident"] = (k == l).astype(np.float32)
    c["le"] = (k <= l).astype(np.float32)
    c["gt"] = (k > l).astype(np.float32)
    c["ge"] = (k >= l).astype(np.float32)
    c["lt"] = (k < l).astype(np.float32)
    c["ones"] = np.ones((128, 128), np.float32)
    cm = np.concatenate([c[n] for n in ("ident", "le", "gt", "ge", "lt", "ones")], axis=1)
    sel = np.zeros((8, 8, 128), np.float32)
    for e in range(8):
        sel[e, e, :] = 1.0
    return cm, sel.reshape(8, 1024)


def rope_tables(L):
    f32 = np.float32
    inv = (np.float32(10000.0) ** (-np.arange(64, dtype=f32) / f32(64))).astype(f32)
    pos = np.arange(CTX + L, dtype=f32)
    ang = (pos[:, None] * inv[None, :]).astype(f32)
    rcs = np.concatenate([np.cos(ang), np.sin(ang)], axis=1).astype(f32)
    inv16 = (np.float32(10000.0) ** (-np.arange(16, dtype=f32) / f32(16))).astype(f32)
    t = np.arange(L)
    row = (t // GRID_W).astype(f32)
    col = (t % GRID_W).astype(f32)
    ar = (row[:, None] * inv16[None, :]).astype(f32)
    ac = (col[:, None] * inv16[None, :]).astype(f32)
    cos = np.concatenate([np.cos(ar), np.cos(ar), np.cos(ac), np.cos(ac)], axis=1)
    sins = np.concatenate([-np.sin(ar), np.sin(ar), -np.sin(ac), np.sin(ac)], axis=1)
    acs = np.concatenate([cos, sins], axis=1).astype(f32)
    return rcs, acs


def build(L=8192, OWN=4096, stop_after=None, dbg=()):
    nc = bass.Bass("TRN2", target_bir_lowering=False)
    P = Prog(nc)
    T = CTX + L
    NCH = T // 128
    NLB = L // 256
    dbg = set(dbg)

    def din(name, shape, dt=F32):
        return P.dram(name, shape, dt, kind="ExternalInput")

    xT = din("xT", [D, L + 2])
    cT = din("ctxT", [D, CTX + 2])
    cvec = din("cvec", [128, 16])
    sel = din("sel", [128, 2])
    consts = din("consts", [128, 768])
    selmat = din("selmat", [8, 1024])
    rcs_d = din("rcs", [T, 128])
    acs_d = din("acs", [L, 128])
    w_mod = [din("w_mod0", [D, 6 * D]), din("w_mod1", [D, 6 * D])]
    b_mod = [din("b_mod0", [128, 48]), din("b_mod1", [128, 48])]
    norms = din("norms", [128, 32])
    w_in0 = din("w_in0", [D, EVEN_IN])
    convw = din("convw", [128, 12, 4])
    rowp = din("rowp", [1, 2560])
    w_out0 = din("w_out0", [2048, D])
    ffg = din("ffg", [D, FFN_DENSE])
    ffu = din("ffu", [D, FFN_DENSE])
    ffd = din("ffd", [FFN_DENSE, D])
    w_in1 = din("w_in1", [D, 3072])
    w_out1 = din("w_out1", [D, D])
    wr = din("router", [D, NEXP])
    eg = din("eg", [NEXP, D, FEXP])
    eu = din("eu", [NEXP, D, FEXP])
    ed = din("ed", [NEXP, FEXP, D])
    outT = P.dram("outT", [D, OWN], F32, kind="ExternalOutput")

    def scratch(name, shape, dt=F32):
        return P.dram(name, shape, dt, kind=("ExternalOutput" if name in dbg else "Internal"))

    r_zr = scratch("r_zr", [NCH, 128, 2048], BF16)
    r_xv = scratch("r_xv", [NCH, 128, 2048], BF16)
    r_kt = scratch("r_kt", [NCH, 128, 768], BF16)
    r_fm = scratch("r_fm", [NCH, 128, 12, 128], BF16)
    r_dt = scratch("r_dt", [NCH, 128, 64], F32)
    r_yf = scratch("r_yf", [NCH, 128, 2048], F32)
    x_mid = scratch("x_mid", [NCH, 128, 8, 128], F32)
    x_l1 = scratch("x_l1", [NCH, 128, 8, 128], F32)
    Kd = scratch("Kd", [8, 128, T], BF16)
    Vd = scratch("Vd", [8, 128, NCH, 130], BF16)
    Qd = scratch("Qd", [8, 128, L], BF16)
    Od = scratch("Od", [128, 8, OWN], BF16)

    cst = P.sb("cst", [128, 768], F32)
    P.dma("sp", cst, consts)
    ident_f = cst[:, 0:128]
    m_le, m_gt, m_ge, m_lt, ones_f = (cst[:, 128 * i:128 * (i + 1)] for i in range(1, 6))
    cstb = P.sb("cstb", [128, 768], BF16)
    P.copy("dve", cstb, cst)
    ident_b = cstb[:, 0:128]
    sel_sb = P.sb("sel_sb", [128, 2], F32)
    P.dma("sp", sel_sb, sel)
    rows = P.sb("rows", [128, 2560], F32)
    P.dma("sp", rows, TT(rowp.ap.rearrange("a b -> (a b)").partition_broadcast(128), rowp.tok))
    norm_sb = P.sb("norm_sb", [128, 32], F32)
    P.dma("sp", norm_sb, norms)
    modfm = [P.sb("modfm0", [128, 48, 2], F32), P.sb("modfm1", [128, 48, 2], F32)]
    P.banks = [P.ps("bank%d" % i, [128, 512], F32) for i in range(8)]

    def bank_bf(b):
        a = b.ap
        return TT(a.bitcast(BF16), b.tok)

    AB = [P.sb("AB%d" % i, [128, 8, 2, 2, 2]) for i in range(2)]
    G = [P.sb("G%d" % i, [128, 8, 2, 2]) for i in range(2)]
    P.open_scope()
    cv = P.sb("cv", [128, 16], F32)
    P.dma("sp", cv, cvec)
    scv = P.sb("scv", [128, 8, 2], F32)
    P.act(scv[:, :, 0], cv[:, 0:8], AF.Silu)
    P.act(scv[:, :, 1], cv[:, 8:16], AF.Silu)
    wm = [P.sb("wm%d" % i, [128, 8, 512], F32) for i in range(2)]
    bm_sb = P.sb("bm_sb", [128, 2, 48], F32)
    P.dma("sp", bm_sb[:, 0, :], b_mod[0])
    P.dma("sp", bm_sb[:, 1, :], b_mod[1])
    it = 0
    for lyr in range(2):
        for cg in range(12):
            w = wm[it % 2]
            it += 1
            P.dma("sp", w, w_mod[lyr][:, cg * 512:(cg + 1) * 512].re("(k p) f -> p k f", p=128))
            pb = P.bank()
            for j in range(4):
                P.mmgroup(pb[:, 2 * j:2 * j + 2], [(w[:, k, j * 128:(j + 1) * 128], scv[:, k, :]) for k in range(8)])
            for j in range(4):
                ch = cg * 4 + j
                P.ts("dve", modfm[lyr][:, ch, :], pb[:, 2 * j:2 * j + 2], bm_sb[:, lyr, ch:ch + 1], None, op0=ALU.add)
    for lyr in range(2):
        for w in range(2):
            for n in range(2):
                shift = modfm[lyr][:, (3 * n) * 8:(3 * n) * 8 + 8, w]
                scale = modfm[lyr][:, (3 * n + 1) * 8:(3 * n + 1) * 8 + 8, w]
                gate = modfm[lyr][:, (3 * n + 2) * 8:(3 * n + 2) * 8 + 8, w]
                gain = norm_sb[:, (2 * lyr + n) * 8:(2 * lyr + n) * 8 + 8]
                P.stt("dve", AB[lyr][:, :, w, n, 0], scale, 1.0, gain, ALU.add, ALU.mult)
                P.copy("dve", AB[lyr][:, :, w, n, 1], shift)
                P.copy("dve", G[lyr][:, :, w, n], gate)
    P.close_scope()

    def rms_modulate(xin, ncol, Atab, out_bf, tmp, out_f32=None, eng="pool"):
        sq = tmp["sq"]
        P.act(sq[:, :, 0:ncol], xin, AF.Square)
        pb = P.bank()
        P.mmgroup(pb[:, 0:ncol], [(ones_f, sq[:, k, 0:ncol]) for k in range(8)])
        rstd = tmp["rstd"]
        P.ts("dve", rstd[:, 0:ncol], pb[:, 0:ncol], 1.0 / D, EPS, op0=ALU.mult, op1=ALU.add)
        P.rsqrt(rstd[:, 0:ncol])
        P.tt("dve", sq[:, :, 0:ncol], xin, bc(rstd[:, 0:ncol], [(0, 8), (1, ncol)]), ALU.mult)
        for k in range(8):
            if eng == "act" or (eng == "mix" and k % 2 == 0):
                P.act(out_bf[:, k, :], sq[:, k, 0:ncol], AF.Identity, bias=Atab[:, k, 1:2], scale=Atab[:, k, 0:1])
            else:
                P.ts("pool", out_bf[:, k, :], sq[:, k, 0:ncol], Atab[:, k, 0:1], Atab[:, k, 1:2], op0=ALU.mult, op1=ALU.add)
            if out_f32 is not None:
                P.ts("pool", out_f32[:, k, :], sq[:, k, 0:ncol], Atab[:, k, 0:1], Atab[:, k, 1:2], op0=ALU.mult, op1=ALU.add)

    r_dtb = rows[:, 0:32]
    r_alog = rows[:, 32:64]
    r_retd = rows[:, 64:72]
    r_dsk = rows[:, 72:88]
    r_ssdn = rows[:, 88:1112]
    r_qn = rows[:, 1112:1176]
    r_kn = rows[:, 1176:1240]
    r_lam = rows[:, 1240:1496]
    r_subln = rows[:, 1496:1624]
    ea = P.sb("ea", [128, 32], F32)
    P.act(ea, r_alog, AF.Exp)
    nla_ret = P.sb("nla_ret", [128, 8], F32)
    P.act(nla_ret, r_retd, AF.Exp)
    P.ts("dve", nla_ret, nla_ret, -1.0, None, op0=ALU.mult)

    P.open_scope()
    w0 = P.sb("w0", [128, 8, EVEN_IN], BF16)
    for k in range(8):
        for c0 in range(0, EVEN_IN, 1888):
            P.dma("pool", w0[:, k, c0:c0 + 1888], w_in0[k * 128:(k + 1) * 128, c0:c0 + 1888])
    cw = P.sb("cw", [128, 12, 4], F32)
    P.dma("sp", cw, convw)
    xin = [P.sb("xin%d" % i, [128, 8, 258], F32) for i in range(2)]
    xbr = P.sb("xbr", [128, 12, 258], F32)
    tmp1 = {"sq": xbr[:, 0:8, :], "rstd": P.sb("rstd1", [128, 258], F32)}
    hbs = [P.sb("hb%d" % i, [128, 8, 258], BF16) for i in range(2)]
    xbcs = [P.sb("xbc%d" % i, [128, 12, 256], BF16) for i in range(2)]
    cvts = [P.sb("cvt%d" % i, [128, 256], F32) for i in range(2)]
    o_zr = P.sb("o_zr", [128, 2, 2048], BF16)
    o_xv = P.sb("o_xv", [128, 2, 2048], BF16)
    o_kt = P.sb("o_kt", [128, 2, 768], BF16)
    o_fm = P.sb("o_fm", [128, 2, 12, 128], BF16)
    o_dt = P.sb("o_dt", [128, 2, 64], F32)
    rtabs = [P.sb("rtab%d" % i, [128, 2, 128], F32) for i in range(3)]
    rt1 = P.sb("rt1", [128, 4, 128], F32)
    rt2 = P.sb("rt2", [128, 4, 128], F32)
    rqk = P.sb("rqk", [128, 2, 512], BF16)
    sp1 = P.sb("sp1", [128, 32], F32)
    sp2 = P.sb("sp2", [128, 32], F32)

    blocks = [("c", 0)] + [("l", i) for i in range(NLB)]

    def binfo(bj):
        kind_, i_ = blocks[bj]
        if kind_ == "c":
            return 0, True, True, 1
        return CTX + i_ * 256, (i_ == 0), (i_ == NLB - 1), 0

    def load1(bj):
        kind_, i_ = blocks[bj]
        if kind_ == "c":
            P.dma("sp", xin[bj % 2], cT.re("(k p) t -> p k t", p=128))
        else:
            P.dma("sp", xin[bj % 2], xT[:, i_ * 256:i_ * 256 + 258].re("(k p) t -> p k t", p=128))
        t0_ = binfo(bj)[0]
        P.dma("sp", rtabs[bj % 3], rcs_d[t0_:t0_ + 256, :].re("(t p) c -> p t c", p=128))

    def stageA(bj):
        tok0, first, last, which = binfo(bj)
        xi, hb, xbc = xin[bj % 2], hbs[bj % 2], xbcs[bj % 2]
        rms_modulate(xi, 258, AB[0][:, :, which, 0, :], hb, tmp1, eng="act")
        for c in range(12):
            pb = P.bank()
            P.mmgroup(pb[:, 0:258], [(w0[:, k, C_XBC + c * 128:C_XBC + (c + 1) * 128], hb[:, k, :]) for k in range(8)])
            P.copy("act", xbr[:, c, :], pb[:, 0:258])
        if first:
            P.memset("pool", xbr[:, :, 0:1], 0.0)
        if last:
            P.memset("pool", xbr[:, :, 257:258], 0.0)
        for c in range(12):
            cvt = cvts[c % 2]
            P.act(cvt, xbr[:, c, 0:256], AF.Identity, scale=cw[:, c, 0:1])
            P.stt("dve", cvt, xbr[:, c, 1:257], cw[:, c, 1:2], cvt, ALU.mult, ALU.add)
            P.stt("dve", cvt, xbr[:, c, 2:258], cw[:, c, 2:3], cvt, ALU.mult, ALU.add)
            P.act(xbc[:, c, :], cvt, AF.Silu, bias=cw[:, c, 3:4])

    def stageB(bj):
        tok0, first, last, which = binfo(bj)
        hb, xbc, rtab = hbs[bj % 2], xbcs[bj % 2], rtabs[bj % 3]
        ch0 = tok0 // 128
        for t in range(2):
            P.copy("act", o_fm[:, t, 0:4, :], xbc[:, 8:12, t * 128:(t + 1) * 128])
        for t in range(2):
            lt = [hb[:, k, 1 + t * 128:1 + (t + 1) * 128] for k in range(8)]

            def proj(c0, n):
                pb = P.bank()
                P.mmgroup(pb[:, 0:n], [(lt[k], w0[:, k, c0:c0 + n]) for k in range(8)])
                return pb
            for j in range(2):
                pb = proj(C_Z + j * 512, 512)
                P.copy("act", o_zr[:, t, j * 512:(j + 1) * 512], pb)
            for j in range(2):
                pb = proj(C_RG + j * 512, 512)
                P.copy("act", o_zr[:, t, 1024 + j * 512:1024 + (j + 1) * 512], pb)
            for j in range(2):
                pb = proj(C_RV + j * 512, 512)
                P.copy("act", o_xv[:, t, 1024 + j * 512:1024 + (j + 1) * 512], pb)
            pb = proj(C_DT, 32)
            P.tt("dve", sp1, pb[:, 0:32], r_dtb, ALU.add)
            P.act(sp2, sp1, AF.Abs)
            P.act(sp2, sp2, AF.Exp, scale=-1.0)
            P.act(sp2, sp2, AF.Ln, bias=1.0)
            P.stt("dve", o_dt[:, t, 0:32], sp1, 0.0, sp2, ALU.max, ALU.add)
            P.tt("dve", sp1, o_dt[:, t, 0:32], ea, ALU.mult)
            P.ts("dve", o_dt[:, t, 32:64], sp1, -1.0, None, op0=ALU.mult)
            for qi, c0 in enumerate((C_RQ, C_RK)):
                pb = proj(c0, 512)
                pv = pb.re("p (h d) -> p h d", h=4)
                cos2 = bc(rtab[:, t, 0:64], [(0, 4), (0, 2), (1, 64)])
                P.tt("dve", rt1.re("p h (a d) -> p h a d", a=2), pv.re("p h (a d) -> p h a d", a=2), cos2, ALU.mult)
                sin1 = bc(rtab[:, t, 64:128], [(0, 4), (1, 64)])
                P.tt("dve", rt2[:, :, 0:64], pv[:, :, 64:128], sin1, ALU.mult)
                P.tt("dve", rt2[:, :, 64:128], pv[:, :, 0:64], sin1, ALU.mult)
                rv = rqk[:, qi, :].re("p (h d) -> p h d", h=4)
                P.tt("pool", rt1[:, :, 0:64], rt1[:, :, 0:64], rt2[:, :, 0:64], ALU.subtract)
                P.tt("pool", rt1[:, :, 64:128], rt1[:, :, 64:128], rt2[:, :, 64:128], ALU.add)
                P.act(rv, rt1, AF.Copy, scale=(1.0 if qi == 0 else 128.0 ** -0.5))
            P.copy("act", o_kt[:, t, 256:768], rqk[:, 1, :])
            pb = P.bank()
            pbb = bank_bf(pb)
            for qi in range(2):
                for h in range(4):
                    P.transpose(pbb[:, (qi * 4 + h) * 128:(qi * 4 + h + 1) * 128], rqk[:, qi, h * 128:(h + 1) * 128], ident_b)
            P.copy("dve", o_fm[:, t, 4:12, :], pbb.re("p (n t) -> p n t", t=128))
            pb = P.bank()
            pbb = bank_bf(pb)
            for c in range(8):
                P.transpose(pbb[:, c * 128:(c + 1) * 128], xbc[:, c, t * 128:(t + 1) * 128], ident_b)
            P.copy("dve", o_xv[:, t, 0:1024], pbb)
            pb = P.bank()
            pbb = bank_bf(pb)
            for c in range(2):
                P.transpose(pbb[:, c * 128:(c + 1) * 128], xbc[:, 8 + c, t * 128:(t + 1) * 128], ident_b)
            P.copy("dve", o_kt[:, t, 0:256], pbb[:, 0:256])
        for t in range(2):
            P.dma("sp", r_zr[ch0 + t], o_zr[:, t, :])
            P.dma("sp", r_xv[ch0 + t], o_xv[:, t, :])
            P.dma("sp", r_kt[ch0 + t], o_kt[:, t, :])
            P.dma("sp", r_fm[ch0 + t], o_fm[:, t])
            P.dma("sp", r_dt[ch0 + t], o_dt[:, t, :])

    nblk = len(blocks)
    load1(0)
    if nblk > 1:
        load1(1)
    stageA(0)
    for bi in range(nblk):
        if bi + 2 < nblk:
            load1(bi + 2)
        if bi + 1 < nblk:
            stageA(bi + 1)
        stageB(bi)
    P.close_scope()
    if stop_after == "P1":
        return _finish(P, nc, [r_zr, r_xv, r_kt, r_fm, r_dt])

    P.open_scope()
    wo0 = P.sb("wo0", [128, 16, D], BF16)
    for k in range(16):
        P.dma("pool", wo0[:, k, :], w_out0[k * 128:(k + 1) * 128, :])
    Er = P.sb("Er", [128, 2, 3, 4], F32)
    Dret = P.sb("Dret", [128, 2, 4, 128], F32)
    lmr = P.sb("lmr", [128, 4, 128], F32)
    for d in range(2):
        la = nla_ret[:, d * 4:(d + 1) * 4]
        pb = P.bank()
        mA, mT = (m_le, m_gt) if d == 0 else (m_ge, m_lt)
        P.mm(pb[:, 0:4], mA, la)
        P.mm(pb[:, 4:8], mT, la)
        P.mm(pb[:, 8:12], ones_f, la)
        P.act(Er[:, d].re("p a h -> p (a h)"), pb[:, 0:12], AF.Exp)
        mS, mR, mM = (m_gt, m_le, m_le) if d == 0 else (m_lt, m_ge, m_ge)
        P.tt("dve", lmr, bc(mS, [(0, 4), (1, 128)]), bc(la, [(1, 4), (0, 128)]), ALU.mult)
        pb = P.bank()
        for h in range(4):
            P.mm(pb[:, h * 128:(h + 1) * 128], lmr[:, h, :], mR)
        P.act(Dret[:, d].re("p h l -> p (h l)"), pb, AF.Exp)
        P.tt("dve", Dret[:, d], Dret[:, d], bc(mM, [(0, 4), (1, 128)]), ALU.mult)

    Hs = P.sb("Hs", [128, 1024], F32)
    Hr = P.sb("Hr", [128, 1024], F32)
    Hsb = P.sb("Hsb", [128, 1024], BF16)
    Hrb = P.sb("Hrb", [128, 1024], BF16)
    i_xv = [P.sb("i_xv%d" % i, [128, 2048], BF16) for i in range(2)]
    i_kt = [P.sb("i_kt%d" % i, [128, 768], BF16) for i in range(2)]
    i_fm = [P.sb("i_fm%d" % i, [128, 12, 128], BF16) for i in range(2)]
    i_dt = [P.sb("i_dt%d" % i, [128, 64], F32) for i in range(2)]
    i_zr = [P.sb("i_zr%d" % i, [128, 2048], BF16) for i in range(2)]
    i_yf = [P.sb("i_yf%d" % i, [128, 2048], F32) for i in range(2)]
    i_x = [P.sb("i_x%d" % i, [128, 8, 128], F32) for i in range(2)]
    E = P.sb("E", [128, 3, 16], F32)
    scm = P.sb("scm", [128, 2, 128], F32)
    Lm = P.sb("Lm", [128, 16, 128], F32)
    expD = P.sb("expD", [128, 16, 128], F32)
    MT = P.sb("MT", [128, 16, 128], BF16)
    MTr = P.sb("MTr", [128, 4, 128], BF16)
    xdt = P.sb("xdt", [128, 1024], BF16)
    xw = P.sb("xw", [128, 1024], BF16)
    rvw = P.sb("rvw", [128, 1024], BF16)
    wv = P.sb("wv", [128, 16], F32)
    ytmp = P.sb("ytmp", [128, 1024], F32)
    yo = [P.sb("yo%d" % i, [128, 2048], F32) for i in range(2)]
    sz = P.sb("sz", [128, 1024], F32)
    junk = P.sb("junk", [128, 1024], F32)
    ss = P.sb("ss", [128, 8], F32)
    ycat = P.sb("ycat", [128, 2048], BF16)
    ycT = P.sb("ycT", [128, 16, 128], BF16)
    xo = P.sb("xo", [128, 8, 128], F32)

    fwd_order = list(range(NCH))
    bwd_order = [1, 0] + list(range(NCH - 1, 1, -1))

    for d in range(2):
        order = fwd_order if d == 0 else bwd_order
        P.memset("dve", Hs, 0.0)
        P.memset("dve", Hr, 0.0)
        P.memset("pool", Hsb, 0.0)
        P.memset("pool", Hrb, 0.0)
        mA, mT = (m_le, m_gt) if d == 0 else (m_ge, m_lt)
        mS, mR, mM = (m_gt, m_le, m_le) if d == 0 else (m_lt, m_ge, m_ge)
        def load_sw(cj):
            ch_ = order[cj]
            b_ = cj % 2
            P.dma("sp", i_xv[b_], r_xv[ch_])
            P.dma("sp", i_kt[b_], r_kt[ch_])
            P.dma("sp", i_fm[b_], r_fm[ch_])
            P.dma("sp", i_dt[b_], r_dt[ch_])

        def load_fin(cj):
            ch_ = order[cj]
            b_ = cj % 2
            P.dma("sp", i_zr[b_], r_zr[ch_])
            P.dma("sp", i_yf[b_], r_yf[ch_])
            if ch_ < 2:
                P.dma("sp", i_x[b_], cT[:, 1 + ch_ * 128:1 + (ch_ + 1) * 128].re("(k p) t -> p k t", p=128))
            else:
                P.dma("sp", i_x[b_], xT[:, 1 + (ch_ - 2) * 128:1 + (ch_ - 1) * 128].re("(k p) t -> p k t", p=128))

        def finish(cj):
            ch = order[cj]
            b = cj % 2
            yout = yo[cj % 2]
            zr = i_zr[b]
            which = 1 if ch < 2 else 0
            P.tt("dve", yout, yout, i_yf[b], ALU.add)
            ys = yout[:, 0:1024]
            yr = yout[:, 1024:2048]
            P.act(sz, zr[:, 0:1024], AF.Silu)
            P.tt("dve", ys, ys, sz, ALU.mult)
            P.act(junk, ys, AF.Square)
            P.reduce("dve", ss[:, 0:1], junk, ALU.add)
            P.ts("dve", ss[:, 1:2], ss[:, 0:1], 1.0 / 1024, EPS, op0=ALU.mult, op1=ALU.add)
            P.rsqrt(ss[:, 1:2])
            P.stt("dve", ycat[:, 0:1024], ys, ss[:, 1:2], r_ssdn, ALU.mult, ALU.mult)
            P.act(junk, yr, AF.Square)
            P.reduce("dve", ss[:, 2:6], junk.re("p (h d) -> p h d", h=4), ALU.add)
            P.ts("dve", ss[:, 2:6], ss[:, 2:6], 1.0 / 256, EPS, op0=ALU.mult, op1=ALU.add)
            P.rsqrt(ss[:, 2:6])
            P.act(sz, zr[:, 1024:2048], AF.Silu)
            P.tt("dve", yr.re("p (h d) -> p h d", h=4), yr.re("p (h d) -> p h d", h=4), bc(ss[:, 2:6], [(1, 4), (0, 256)]), ALU.mult)
            P.tt("dve", ycat[:, 1024:2048], yr, sz, ALU.mult)
            for q in range(2):
                pb = P.bank()
                pbb = bank_bf(pb)
                for j in range(8):
                    P.transpose(pbb[:, j * 128:(j + 1) * 128], ycat[:, (q * 8 + j) * 128:(q * 8 + j + 1) * 128], ident_b)
                P.copy("act", ycT[:, q * 8:(q + 1) * 8, :].re("p n t -> p (n t)"), pbb)
            for q in range(2):
                pb = P.bank()
                for j in range(4):
                    dc = q * 4 + j
                    P.mmgroup(pb[:, j * 128:(j + 1) * 128], [(wo0[:, k, dc * 128:(dc + 1) * 128], ycT[:, k, :]) for k in range(16)])
                for j in range(4):
                    dc = q * 4 + j
                    P.stt("dve", xo[:, dc, :], pb[:, j * 128:(j + 1) * 128], G[0][:, dc, which, 0:1], i_x[b][:, dc, :], ALU.mult, ALU.add)
            P.dma("sp", x_mid[ch], xo)

        load_sw(0)
        for ci, ch in enumerate(order):
            b = ci % 2
            xv, kt, fm, dtt = i_xv[b], i_kt[b], i_fm[b], i_dt[b]
            if ci + 1 < len(order):
                load_sw(ci + 1)
            if d == 1:
                load_fin(ci)
            la = dtt[:, 32 + d * 16:32 + (d + 1) * 16]
            dtd = dtt[:, d * 16:(d + 1) * 16]
            xs = xv[:, 0:1024]
            rvv = xv[:, 1024:2048]
            yout = yo[ci % 2]
            pb = P.bank()
            P.mm(pb[:, 0:16], mA, la)
            P.mm(pb[:, 16:32], mT, la)
            P.mm(pb[:, 32:48], ones_f, la)
            P.act(E.re("p a h -> p (a h)"), pb[:, 0:48], AF.Exp)
            pb = P.bank()
            for g in range(2):
                P.mm(pb[:, g * 128:(g + 1) * 128], fm[:, g, :], fm[:, 2 + g, :])
            P.tt("dve", scm, pb[:, 0:256].re("p (g l) -> p g l", g=2), bc(mM, [(0, 2), (1, 128)]), ALU.mult)
            P.tt("pool", Lm, bc(mS, [(0, 16), (1, 128)]), bc(la, [(1, 16), (0, 128)]), ALU.mult)
            for q in range(4):
                pb = P.bank()
                for j in range(4):
                    P.mm(pb[:, j * 128:(j + 1) * 128], Lm[:, q * 4 + j, :], mR)
                P.act(expD[:, q * 4:(q + 1) * 4, :].re("p h l -> p (h l)"), pb, AF.Exp)
            for g in range(2):
                P.tt("dve", MT[:, g * 8:(g + 1) * 8, :], expD[:, g * 8:(g + 1) * 8, :], bc(scm[:, g, :], [(0, 8), (1, 128)]), ALU.mult)
            P.tt("dve", xdt.re("p (h d) -> p h d", h=16), xs.re("p (h d) -> p h d", h=16), bc(dtd, [(1, 16), (0, 64)]), ALU.mult)
            pd = [P.bank(), P.bank()]
            for h in range(16):
                P.mm(pd[h // 8][:, (h % 8) * 64:(h % 8 + 1) * 64], MT[:, h, :], xdt[:, h * 64:(h + 1) * 64])
            for g in range(2):
                po = P.bank()
                P.mm(po, fm[:, 2 + g, :], Hsb[:, g * 512:(g + 1) * 512])
                P.tt("dve", ytmp[:, g * 512:(g + 1) * 512].re("p (h d) -> p h d", h=8), po.re("p (h d) -> p h d", h=8),
                     bc(E[:, 0, g * 8:(g + 1) * 8], [(1, 8), (0, 64)]), ALU.mult)
                P.tt("dve", yout[:, g * 512:(g + 1) * 512], ytmp[:, g * 512:(g + 1) * 512], pd[g], ALU.add)
            P.tt("dve", wv, dtd, E[:, 1, :], ALU.mult)
            P.tt("dve", xw.re("p (h d) -> p h d", h=16), xs.re("p (h d) -> p h d", h=16), bc(wv, [(1, 16), (0, 64)]), ALU.mult)
            P.tt("dve", Hs.re("p (h d) -> p h d", h=16), Hs.re("p (h d) -> p h d", h=16), bc(E[:, 2, :], [(1, 16), (0, 64)]), ALU.mult)
            for g in range(2):
                pS = P.bank()
                P.mm(pS, kt[:, g * 128:(g + 1) * 128], xw[:, g * 512:(g + 1) * 512])
                P.tt("dve", Hs[:, g * 512:(g + 1) * 512], Hs[:, g * 512:(g + 1) * 512], pS, ALU.add)
            P.copy("act", Hsb, Hs)
            pb = P.bank()
            for h in range(4):
                P.mm(pb[:, h * 128:(h + 1) * 128], fm[:, 8 + h, :], fm[:, 4 + h, :])
            P.tt("dve", MTr, pb.re("p (h l) -> p h l", h=4), Dret[:, d], ALU.mult)
            pd = [P.bank(), P.bank()]
            for h in range(4):
                P.mm(pd[h // 2][:, (h % 2) * 256:(h % 2 + 1) * 256], MTr[:, h, :], rvv[:, h * 256:(h + 1) * 256])
            for g in range(2):
                po = P.bank()
                for hh in range(2):
                    h = g * 2 + hh
                    P.mm(po[:, hh * 256:(hh + 1) * 256], fm[:, 4 + h, :], Hrb[:, h * 256:(h + 1) * 256])
                P.tt("dve", ytmp[:, g * 512:(g + 1) * 512].re("p (h d) -> p h d", h=2), po.re("p (h d) -> p h d", h=2),
                     bc(Er[:, d, 0, g * 2:(g + 1) * 2], [(1, 2), (0, 256)]), ALU.mult)
                P.tt("dve", yout[:, 1024 + g * 512:1024 + (g + 1) * 512], ytmp[:, g * 512:(g + 1) * 512], pd[g], ALU.add)
            P.tt("dve", rvw.re("p (h d) -> p h d", h=4), rvv.re("p (h d) -> p h d", h=4), bc(Er[:, d, 1, :], [(1, 4), (0, 256)]), ALU.mult)
            P.tt("dve", Hr.re("p (h d) -> p h d", h=4), Hr.re("p (h d) -> p h d", h=4), bc(Er[:, d, 2, :], [(1, 4), (0, 256)]), ALU.mult)
            for g in range(2):
                pS = P.bank()
                for hh in range(2):
                    h = g * 2 + hh
                    P.mm(pS[:, hh * 256:(hh + 1) * 256], kt[:, 256 + h * 128:256 + (h + 1) * 128], rvw[:, h * 256:(h + 1) * 256])
                P.tt("dve", Hr[:, g * 512:(g + 1) * 512], Hr[:, g * 512:(g + 1) * 512], pS, ALU.add)
            P.copy("act", Hrb, Hr)
            if d == 0:
                P.dma("sp", r_yf[ch], yout)
                continue
            P.tt("pool", ytmp.re("p (h d) -> p h d", h=16), xs.re("p (h d) -> p h d", h=16), bc(r_dsk, [(1, 16), (0, 64)]), ALU.mult)
            P.tt("dve", yout[:, 0:1024], yout[:, 0:1024], ytmp, ALU.add)
            if ci >= 1:
                finish(ci - 1)
        if d == 1:
            finish(len(order) - 1)
    P.close_scope()
    if stop_after == "P3":
        return _finish(P, nc, [x_mid, r_yf])

    P.open_scope()
    NF = FFN_DENSE // 128
    wg = P.sb("wg", [128, 8, FFN_DENSE], BF16)
    wu = P.sb("wu", [128, 8, FFN_DENSE], BF16)
    wd = P.sb("wd", [128, NF, D], BF16)
    for k in range(8):
        P.dma("pool", wg[:, k, :], ffg[k * 128:(k + 1) * 128, :])
        P.dma("pool", wu[:, k, :], ffu[k * 128:(k + 1) * 128, :])
    for f in range(NF):
        P.dma("pool", wd[:, f, :], ffd[f * 128:(f + 1) * 128, :])
    xb4 = [P.sb("xb4_%d" % i, [128, 8, 256], F32) for i in range(2)]
    tmp4 = {"sq": P.sb("sq4", [128, 8, 256], F32), "rstd": P.sb("rstd4", [128, 256], F32)}
    h4 = P.sb("h4", [128, 8, 256], BF16)
    a4 = P.sb("a4", [128, NF, 256], BF16)
    sg4 = [P.sb("sg4_%d" % i, [128, 256], F32) for i in range(2)]
    xo4 = [P.sb("xo4_%d" % i, [128, 8, 256], F32) for i in range(1)]
    def load4(bj):
        for t in range(2):
            P.dma("sp", xb4[bj % 2][:, :, t * 128:(t + 1) * 128], x_mid[bj * 2 + t])
    load4(0)
    for bi in range(NCH // 2):
        x4 = xb4[bi % 2]
        which = 1 if bi == 0 else 0
        if bi + 1 < NCH // 2:
            load4(bi + 1)
        rms_modulate(x4, 256, AB[0][:, :, which, 1, :], h4, tmp4, eng="act")
        for f in range(NF):
            pb = P.bank()
            P.mmgroup(pb[:, 0:256], [(wg[:, k, f * 128:(f + 1) * 128], h4[:, k, :]) for k in range(8)])
            P.mmgroup(pb[:, 256:512], [(wu[:, k, f * 128:(f + 1) * 128], h4[:, k, :]) for k in range(8)])
            sg = sg4[f % 2]
            P.act(sg, pb[:, 0:256], AF.Silu)
            P.tt("dve", a4[:, f, :], sg, pb[:, 256:512], ALU.mult)
        xo_ = xo4[0]
        for q in range(4):
            pb = P.bank()
            for j in range(2):
                dc = q * 2 + j
                P.mmgroup(pb[:, j * 256:(j + 1) * 256], [(wd[:, f, dc * 128:(dc + 1) * 128], a4[:, f, :]) for f in range(NF)])
            for j in range(2):
                dc = q * 2 + j
                P.stt("dve", xo_[:, dc, :], pb[:, j * 256:(j + 1) * 256], G[0][:, dc, which, 1:2], x4[:, dc, :], ALU.mult, ALU.add)
        for t in range(2):
            P.dma("sp", x_l1[bi * 2 + t], xo_[:, :, t * 128:(t + 1) * 128])
    P.close_scope()
    if stop_after == "P4":
        return _finish(P, nc, [x_l1])

    P.open_scope()
    w1 = P.sb("w1", [128, 8, 3072], BF16)
    for k in range(8):
        P.dma("pool", w1[:, k, :], w_in1[k * 128:(k + 1) * 128, :])
    xb5 = [P.sb("xb5_%d" % i, [128, 8, 256], F32) for i in range(2)]
    tmp5 = {"sq": P.sb("sq5", [128, 8, 256], F32), "rstd": P.sb("rstd5", [128, 256], F32)}
    h5 = P.sb("h5", [128, 8, 256], BF16)
    atab = P.sb("atab", [128, 2, 128], F32)
    qsq = P.sb("qsq", [128, 1024], F32)
    qn = P.sb("qn", [128, 1024], F32)
    q1 = P.sb("q1", [128, 1024], F32)
    q2 = P.sb("q2", [128, 1024], F32)
    ss5 = P.sb("ss5", [128, 16], F32)
    qkb = P.sb("qkb", [128, 2, 1024], BF16)
    qkT = P.sb("qkT", [128, 2, 8, 256], BF16)
    v5 = [P.sb("v5_%d" % i, [128, 2, 8, 130], BF16) for i in range(2)]
    for i in range(2):
        P.memset("dve", v5[i], 1.0)
    qg = P.sb("qg", [128, 64], F32)
    P.ts("dve", qg, r_qn, 64.0 ** -0.5, None, op0=ALU.mult)
    atabs = [atab, P.sb("atab2", [128, 2, 128], F32), P.sb("atab3", [128, 2, 128], F32)]
    h5s = [h5, P.sb("h5b", [128, 8, 256], BF16)]
    qsqs = [qsq, P.sb("qsq_b", [128, 1024], F32)]
    qns = [qn, P.sb("qn_b", [128, 1024], F32)]
    q1s = [q1, P.sb("q1_b", [128, 1024], F32)]
    q2s = [q2, P.sb("q2_b", [128, 1024], F32)]
    ss5s = [ss5, P.sb("ss5_b", [128, 16], F32)]
    NB5 = NCH // 2

    def load5(bj):
        for t in range(2):
            P.dma("sp", xb5[bj % 2][:, :, t * 128:(t + 1) * 128], x_l1[bj * 2 + t])
        if bj > 0:
            l0 = (bj - 1) * 256
            P.dma("sp", atabs[bj % 3], acs_d[l0:l0 + 256, :].re("(t p) c -> p t c", p=128))

    def stage5A(bj):
        which = 1 if bj == 0 else 0
        rms_modulate(xb5[bj % 2], 256, AB[1][:, :, which, 0, :], h5s[bj % 2], tmp5, eng="act")

    def stage5B(bi):
        h5 = h5s[bi % 2]
        atab = atabs[bi % 3]
        which = 1 if bi == 0 else 0
        vv = v5[bi % 2]
        qis = [1] if which == 1 else [0, 1]
        for t in range(2):
            lt = [h5[:, k, t * 128:(t + 1) * 128] for k in range(8)]
            pbs = {}
            for qi in qis:
                pbs[qi] = []
                for j in range(2):
                    pb = P.bank()
                    c0 = qi * 1024 + j * 512
                    P.mmgroup(pb, [(lt[k], w1[:, k, c0:c0 + 512]) for k in range(8)])
                    pbs[qi].append(pb)
            for qi in qis:
                for j in range(2):
                    P.act(qsqs[qi][:, j * 512:(j + 1) * 512], pbs[qi][j], AF.Square)
            for qi in qis:
                P.reduce("dve", ss5s[qi], qsqs[qi].re("p (g d) -> p g d", d=64), ALU.add)
                P.ts("dve", ss5s[qi], ss5s[qi], 1.0 / 64, EPS, op0=ALU.mult, op1=ALU.add)
            for qi in qis:
                P.rsqrt(ss5s[qi])
            for qi in qis:
                for j in range(2):
                    P.tt("dve", qns[qi][:, j * 512:(j + 1) * 512].re("p (g d) -> p g d", d=64), pbs[qi][j].re("p (g d) -> p g d", d=64),
                         bc(ss5s[qi][:, j * 8:(j + 1) * 8], [(1, 8), (0, 64)]), ALU.mult)
            pvs = []
            for j in range(2):
                pb = P.bank()
                c0 = 2048 + j * 512
                P.mmgroup(pb, [(lt[k], w1[:, k, c0:c0 + 512]) for k in range(8)])
                pvs.append(pb)
            for qi in qis:
                qn = qns[qi]
                gn = qg if qi == 0 else r_kn
                dst = qkb[:, qi, :]
                if which == 1:
                    P.tt("dve", dst.re("p (g d) -> p g d", d=64), qn.re("p (g d) -> p g d", d=64), bc(gn, [(0, 16), (1, 64)]), ALU.mult)
                else:
                    P.tt("dve", qn.re("p (g d) -> p g d", d=64), qn.re("p (g d) -> p g d", d=64), bc(gn, [(0, 16), (1, 64)]), ALU.mult)
            for j in range(2):
                P.copy("act", vv[:, t, j * 4:(j + 1) * 4, 0:128], pvs[j].re("p (h e) -> p h e", h=4))
            if which == 0:
                for qi in qis:
                    qn, q1, q2 = qns[qi], q1s[qi], q2s[qi]
                    P.tt("dve", q1.re("p (g d) -> p g d", d=64), qn.re("p (g d) -> p g d", d=64), bc(atab[:, t, 0:64], [(0, 16), (1, 64)]), ALU.mult)
                    qv = qn.re("p (g a u d) -> p g a u d", a=2, u=2, d=16)
                    q2v = q2.re("p (g a u d) -> p g a u d", a=2, u=2, d=16)
                    for s_ in range(2):
                        sn = bass.AP(atab.ap.tensor, atab[:, t, 64 + s_ * 16:64 + s_ * 16 + 16].ap.offset,
                                     [list(atab.ap.ap[0]), [0, 16], [32, 2], [1, 16]])
                        P.tt("dve", q2v[:, :, :, s_, :], qv[:, :, :, 1 - s_, :], TT(sn, atab.tok), ALU.mult)
                for qi in qis:
                    P.tt("dve", qkb[:, qi, :], q1s[qi], q2s[qi], ALU.add)
            for qi in qis:
                pb = P.bank()
                pbb = bank_bf(pb)
                for h in range(8):
                    P.transpose(pbb[:, h * 128:(h + 1) * 128], qkb[:, qi, h * 128:(h + 1) * 128], ident_b)
                P.copy("act", qkT[:, qi, :, t * 128:(t + 1) * 128], pbb.re("p (h t) -> p h t", h=8))
        tok0 = bi * 256
        P.dma("sp", Kd[:, :, tok0:tok0 + 256].re("h p t -> p h t"), qkT[:, 1])
        if which == 0:
            P.dma("sp", Qd[:, :, tok0 - CTX:tok0 - CTX + 256].re("h p t -> p h t"), qkT[:, 0])
        for t in range(2):
            P.dma("sp", Vd[:, :, bi * 2 + t, :].re("h p e -> p h e"), vv[:, t])

    load5(0)
    if NB5 > 1:
        load5(1)
    stage5A(0)
    for bi in range(NB5):
        if bi + 2 < NB5:
            load5(bi + 2)
        if bi + 1 < NB5:
            stage5A(bi + 1)
        stage5B(bi)
    P.close_scope()
    if stop_after == "P5":
        return _finish(P, nc, [Kd, Vd, Qd])

    P.open_scope()
    NKT = NCH
    NQB = OWN // 512
    lt_ = P.sb("lt_", [128, 128], F32)
    lam2 = P.sb("lam2", [128, 4], F32)
    P.tt("dve", lt_[:, 0:64], r_lam[:, 0:64], r_lam[:, 64:128], ALU.mult)
    P.tt("dve", lt_[:, 64:128], r_lam[:, 128:192], r_lam[:, 192:256], ALU.mult)
    P.reduce("dve", lam2[:, 0:2], lt_.re("p (a d) -> p a d", a=2), ALU.add)
    P.act(lam2[:, 0:2], lam2[:, 0:2], AF.Exp)
    P.tt("dve", lam2[:, 2:3], lam2[:, 1:2], lam2[:, 0:1], ALU.subtract)
    P.ts("dve", lam2[:, 3:4], lam2[:, 2:3], -LAM_INIT, None, op0=ALU.add)
    neglam = lam2[:, 3:4]
    sub_g = P.sb("sub_g", [128, 128], F32)
    P.ts("dve", sub_g, r_subln, 1.0 - LAM_INIT, None, op0=ALU.mult)
    Kh = [P.sb("Kh%d" % i, [128, T], BF16) for i in range(2)]
    Vh = [P.sb("Vh%d" % i, [128, NKT, 130], BF16) for i in range(2)]
    qa = [P.sb("qa%d" % i, [128, 512], BF16) for i in range(2)]
    qb_ = [P.sb("qb%d" % i, [128, 512], BF16) for i in range(2)]
    qs = [P.sb("qs%d" % i, [128, 512], BF16) for i in range(2)]
    pT = [P.sb("pT%d" % i, [128, 512], BF16) for i in range(3)]
    o0 = P.sb("o0", [128, 4, 128], F32)
    o1 = P.sb("o1", [128, 128], F32)
    osq = P.sb("osq", [128, 4, 128], F32)
    rs6 = P.sb("rs6", [128, 4], F32)
    on = P.sb("on", [128, 4, 128], BF16)
    oT = [P.sb("oT%d" % i, [128, 512], BF16) for i in range(2)]
    spb = [P.banks[0], P.banks[1], P.banks[2]]
    ob = [P.banks[3], P.banks[4], P.banks[5], P.banks[6]]
    tb = P.banks[7]
    groups = [(h, qb) for h in range(8) for qb in range(NQB)]
    steps = [(m, kt) for m in range(2) for kt in range(NKT)]

    def load_kv(h):
        P.dma("sp", Kh[h % 2], Kd[h])
        P.dma("sp", Vh[h % 2], Vd[h])

    def load_q(gi):
        h, qb = groups[gi]
        A, B_, Q_ = qa[gi % 2], qb_[gi % 2], qs[gi % 2]
        P.dma("sp", A, Qd[h, :, qb * 512:(qb + 1) * 512])
        if L > OWN:
            P.dma("sp", B_, Qd[h, :, OWN + qb * 512:OWN + (qb + 1) * 512])
            P.ts("pool", Q_, A, sel_sb[:, 0:1], None, op0=ALU.mult)
            P.stt("dve", Q_, B_, sel_sb[:, 1:2], Q_, ALU.mult, ALU.add)
            return Q_
        return A

    load_kv(0)
    Qn = load_q(0)
    pi = 0
    for gi, (h, qb) in enumerate(groups):
        K_, V_ = Kh[h % 2], Vh[h % 2]
        Q_ = Qn
        if qb == 0 and h + 1 < 8:
            load_kv(h + 1)
        if gi + 1 < len(groups):
            Qn = load_q(gi + 1)

        def emit_s(i):
            m, kt = steps[i]
            P.mm(spb[(pi + i) % 3], K_[m * 64:(m + 1) * 64, kt * 128:(kt + 1) * 128], Q_[m * 64:(m + 1) * 64, :])
        emit_s(0)
        emit_s(1)
        for i, (m, kt) in enumerate(steps):
            if i + 2 < len(steps):
                emit_s(i + 2)
            sp_ = spb[(pi + i) % 3]
            p_ = pT[(pi + i) % 3]
            P.act(p_, sp_, AF.Exp, bias=-8.0)
            for s_ in range(4):
                P.mm(ob[s_][:, 0:129], p_[:, s_ * 128:(s_ + 1) * 128], V_[:, kt, 0:129], start=(kt == 0), stop=(kt == NKT - 1))
            if kt == NKT - 1:
                for s_ in range(4):
                    P.recip(rs6[:, s_:s_ + 1], ob[s_][:, 128:129])
                    if m == 0:
                        P.ts("dve", o0[:, s_, :], ob[s_][:, 0:128], rs6[:, s_:s_ + 1], None, op0=ALU.mult)
                    else:
                        P.ts("dve", o1, ob[s_][:, 0:128], rs6[:, s_:s_ + 1], neglam, op0=ALU.mult, op1=ALU.mult)
                        P.tt("dve", o0[:, s_, :], o0[:, s_, :], o1, ALU.add)
        pi += len(steps)
        P.act(osq, o0, AF.Square)
        P.reduce("dve", rs6, osq, ALU.add)
        P.ts("dve", rs6, rs6, 1.0 / 128, EPS, op0=ALU.mult, op1=ALU.add)
        P.rsqrt(rs6)
        tbb = bank_bf(tb)
        for s_ in range(4):
            P.stt("dve", on[:, s_, :], o0[:, s_, :], rs6[:, s_:s_ + 1], sub_g, ALU.mult, ALU.mult)
            P.transpose(tbb[:, s_ * 128:(s_ + 1) * 128], on[:, s_, :], ident_b)
        o_ = oT[gi % 2]
        P.copy("dve", o_, tbb[:, 0:512])
        P.dma("sp", Od[:, h, qb * 512:(qb + 1) * 512], o_)
    P.close_scope()
    if stop_after == "P6":
        return _finish(P, nc, [Od])

    P.open_scope()
    BLK = min(1024, OWN)
    NB = OWN // BLK
    NH = BLK // 512
    NT7 = BLK // 128
    wo1 = P.sb("wo1", [128, 8, D], BF16)
    for k in range(8):
        P.dma("pool", wo1[:, k, :], w_out1[k * 128:(k + 1) * 128, :])
    wr_sb = P.sb("wr_sb", [128, 8, 8], F32)
    P.dma("sp", wr_sb, wr.re("(k p) e -> p k e", p=128))
    selm = P.sb("selm", [8, 1024], F32)
    P.dma("sp", selm, selmat)
    o7 = P.sb("o7", [128, 8, BLK], BF16)
    xa = P.sb("xa", [128, 8, BLK], F32)
    xb7 = P.sb("xb7", [128, 8, 128], F32)
    sq7 = P.sb("sq7", [128, 8, 512], F32)
    rstd7 = P.sb("rstd7", [128, 512], F32)
    h7 = P.sb("h7", [128, 8, BLK], BF16)
    h7f = sq7
    yacc = P.sb("yacc", [128, 8, BLK], F32)
    lg = P.sb("lg", [128, 8], F32)
    lg2 = P.sb("lg2", [128, 8], F32)
    eq1 = P.sb("eq1", [128, 8], F32)
    eq2 = P.sb("eq2", [128, 8], F32)
    mx = P.sb("mx", [128, 8], F32)
    comb = P.sb("comb", [128, 8], F32)
    combT = P.sb("combT", [8, BLK], F32)
    cbc = [P.sb("cbc%d" % i, [128, BLK], BF16) for i in range(2)]
    FG = 2
    NFG = FEXP // (128 * FG)
    FW = 128 * FG
    wge = [P.sb("wge%d" % i, [128, 8, FW], BF16) for i in range(2)]
    wue = [P.sb("wue%d" % i, [128, 8, FW], BF16) for i in range(2)]
    wde = [P.sb("wde%d" % i, [128, FG, D], BF16) for i in range(2)]
    a7 = [P.sb("a7_%d" % i, [128, FG, BLK], BF16) for i in range(2)]
    sg7 = [P.sb("sg7_%d" % i, [128, 512], F32) for i in range(2)]
    t7 = [P.sb("t7_%d" % i, [128, 512], F32) for i in range(2)]
    its = [(nb, e, fg) for nb in range(NB) for e in range(NEXP) for fg in range(NFG)]

    def issue_w(ii):
        nb_, e_, fg_ = its[ii]
        b_ = ii % 2
        f0_ = fg_ * FW
        P.dma("pool", wge[b_], eg[e_, :, f0_:f0_ + FW].re("(k p) f -> p k f", p=128))
        P.dma("pool", wue[b_], eu[e_, :, f0_:f0_ + FW].re("(k p) f -> p k f", p=128))
        P.dma("pool", wde[b_], ed[e_, f0_:f0_ + FW, :].re("(f p) d -> p f d", p=128))
    issue_w(0)
    wi = 0
    for nb in range(NB):
        P.dma("sp", o7, Od[:, :, nb * BLK:(nb + 1) * BLK])
        for t in range(NT7):
            chA = 2 + (nb * BLK) // 128 + t
            P.dma("sp", xa[:, :, t * 128:(t + 1) * 128], x_l1[chA])
            if L > OWN:
                P.dma("sp", xb7, x_l1[chA + OWN // 128])
                P.ts("pool", xa[:, :, t * 128:(t + 1) * 128], xa[:, :, t * 128:(t + 1) * 128], sel_sb[:, 0:1], None, op0=ALU.mult)
                P.stt("pool", xa[:, :, t * 128:(t + 1) * 128], xb7, sel_sb[:, 1:2], xa[:, :, t * 128:(t + 1) * 128], ALU.mult, ALU.add)
        for hf in range(NH):
            cs = slice(hf * 512, (hf + 1) * 512)
            for dc in range(8):
                pb = P.bank()
                P.mmgroup(pb, [(wo1[:, hh, dc * 128:(dc + 1) * 128], o7[:, hh, cs]) for hh in range(8)])
                P.stt("dve", xa[:, dc, cs], pb, G[1][:, dc, 0, 0:1], xa[:, dc, cs], ALU.mult, ALU.add)
            P.act(sq7, xa[:, :, cs], AF.Square)
            pb = P.bank()
            P.mmgroup(pb, [(ones_f, sq7[:, k, :]) for k in range(8)])
            P.ts("dve", rstd7, pb, 1.0 / D, EPS, op0=ALU.mult, op1=ALU.add)
            P.rsqrt(rstd7)
            P.tt("dve", sq7, xa[:, :, cs], bc(rstd7, [(0, 8), (1, 512)]), ALU.mult)
            A2 = AB[1][:, :, 0, 1, :]
            for k in range(8):
                P.act(h7f[:, k, :], sq7[:, k, :], AF.Identity, bias=A2[:, k, 1:2], scale=A2[:, k, 0:1])
                P.copy("act", h7[:, k, cs], h7f[:, k, :])
            for t4 in range(4):
                pb = P.bank()
                P.mmgroup(pb[:, 0:8], [(h7f[:, k, t4 * 128:(t4 + 1) * 128], wr_sb[:, k, :]) for k in range(8)])
                P.copy("dve", lg, pb[:, 0:8])
                P.reduce("dve", mx[:, 0:1], lg, ALU.max)
                P.ts("dve", eq1, lg, mx[:, 0:1], None, op0=ALU.is_equal)
                P.stt("dve", lg2, eq1, -1e30, lg, ALU.mult, ALU.add)
                P.reduce("dve", mx[:, 1:2], lg2, ALU.max)
                P.ts("dve", eq2, lg2, mx[:, 1:2], None, op0=ALU.is_equal)
                P.tt("dve", mx[:, 2:3], mx[:, 1:2], mx[:, 0:1], ALU.subtract)
                P.act(mx[:, 3:4], mx[:, 2:3], AF.Exp)
                P.ts("dve", mx[:, 4:5], mx[:, 3:4], 1.0, None, op0=ALU.add)
                P.recip(mx[:, 5:6], mx[:, 4:5])
                P.tt("dve", mx[:, 6:7], mx[:, 3:4], mx[:, 5:6], ALU.mult)
                P.ts("dve", comb, eq1, mx[:, 5:6], None, op0=ALU.mult)
                P.stt("dve", comb, eq2, mx[:, 6:7], comb, ALU.mult, ALU.add)
                pb = P.bank()
                P.transpose(pb[0:8, 0:128], comb, ident_f)
                P.copy("dve", combT[:, hf * 512 + t4 * 128:hf * 512 + (t4 + 1) * 128], pb[0:8, 0:128])
        P.memset("pool", yacc, 0.0)
        for e in range(NEXP):
            cb = cbc[e % 2]
            for hf in range(NH):
                cs = slice(hf * 512, (hf + 1) * 512)
                pb = P.bank()
                P.mm(pb, selm[:, e * 128:(e + 1) * 128], combT[:, cs])
                P.copy("act", cb[:, cs], pb)
            for k in range(8):
                P.tt("dve", o7[:, k, :], h7[:, k, :], cb, ALU.mult)
            for fg in range(NFG):
                b = wi % 2
                wi += 1
                if wi < len(its):
                    issue_w(wi)
                aa = a7[b]
                ii = 0
                for f in range(FG):
                    for hf in range(NH):
                        cs = slice(hf * 512, (hf + 1) * 512)
                        pg = P.bank()
                        pu = P.bank()
                        P.mmgroup(pg, [(wge[b][:, k, f * 128:(f + 1) * 128], h7[:, k, cs]) for k in range(8)])
                        P.mmgroup(pu, [(wue[b][:, k, f * 128:(f + 1) * 128], o7[:, k, cs]) for k in range(8)])
                        sg = sg7[ii % 2]
                        tt_ = t7[ii % 2]
                        ii += 1
                        P.act(sg, pg, AF.Silu)
                        P.tt("dve", aa[:, f, cs], sg, pu, ALU.mult)
                for hf in range(NH):
                    cs = slice(hf * 512, (hf + 1) * 512)
                    for dc in range(8):
                        pb = P.bank()
                        P.mmgroup(pb, [(wde[b][:, f, dc * 128:(dc + 1) * 128], aa[:, f, cs]) for f in range(FG)])
                        P.tt("dve", yacc[:, dc, cs], yacc[:, dc, cs], pb, ALU.add)
        for dc in range(8):
            P.stt("dve", yacc[:, dc, :], yacc[:, dc, :], G[1][:, dc, 0, 1:2], xa[:, dc, :], ALU.mult, ALU.add)
        P.dma("sp", outT[:, nb * BLK:(nb + 1) * BLK].re("(k p) t -> p k t", p=128), yacc)
    P.close_scope()
    return _finish(P, nc, [outT])


def P_sb_keep(P, name, shape):
    g = P.nc.sbuf_tensor(name, list(shape), F32)
    h = g.__enter__()
    P._ctx.insert(0, g)
    for i in range(len(P._scopes)):
        P._scopes[i] += 1
    return TT(h[:], Tok(name))


def _finish(P, nc, outs):
    P.fence("sp", outs)
    P.emit()
    P.close()
    return nc


def fm_vec(v):
    v = np.asarray(v, np.float32).reshape(-1, 128)
    return np.ascontiguousarray(v.T)


def prep_inputs(inp, L, OWN, ncores_per_batch, nbatch):
    cm, selm = host_consts()
    rcs, acs = rope_tables(L)
    shared = {
        "consts": cm, "selmat": selm, "rcs": rcs, "acs": acs,
        "w_mod0": np.ascontiguousarray(inp["even_w_mod"][0]), "w_mod1": np.ascontiguousarray(inp["odd_w_mod"][0]),
        "b_mod0": fm_vec(inp["even_b_mod"][0]), "b_mod1": fm_vec(inp["odd_b_mod"][0]),
        "norms": np.concatenate([fm_vec(inp["even_norm1"][0]), fm_vec(inp["even_norm2"][0]),
                                 fm_vec(inp["odd_norm1"][0]), fm_vec(inp["odd_norm2"][0])], axis=1),
        "w_in0": np.ascontiguousarray(inp["even_w_in"][0]),
        "w_out0": np.ascontiguousarray(inp["even_w_out"][0]),
        "ffg": np.ascontiguousarray(inp["even_ffn_gate"][0]), "ffu": np.ascontiguousarray(inp["even_ffn_up"][0]),
        "ffd": np.ascontiguousarray(inp["even_ffn_down"][0]),
        "w_in1": np.ascontiguousarray(inp["odd_w_in"][0]), "w_out1": np.ascontiguousarray(inp["odd_w_out"][0]),
        "router": np.ascontiguousarray(inp["odd_router"][0]),
        "eg": np.ascontiguousarray(inp["odd_exp_gate"][0]), "eu": np.ascontiguousarray(inp["odd_exp_up"][0]),
        "ed": np.ascontiguousarray(inp["odd_exp_down"][0]),
    }
    cw = np.concatenate([inp["even_conv_w"][0], inp["even_conv_b"][0][None, :]], axis=0)
    shared["convw"] = np.ascontiguousarray(cw.reshape(4, 12, 128).transpose(2, 1, 0))
    rowp = np.concatenate([
        inp["even_dt_bias"][0].reshape(-1), inp["even_a_log"][0].reshape(-1), inp["even_ret_decay"][0].reshape(-1),
        inp["even_d"][0].reshape(-1), inp["even_ssd_norm"][0].reshape(-1), inp["odd_q_norm"][0].reshape(-1),
        inp["odd_k_norm"][0].reshape(-1), inp["odd_lambda"][0].reshape(-1), inp["odd_subln"][0].reshape(-1)]).astype(np.float32)
    rp = np.zeros((1, 2560), np.float32)
    rp[0, :rowp.size] = rowp
    shared["rowp"] = rp
    maps = []
    for b in range(nbatch):
        xT = np.zeros((D, L + 2), np.float32)
        xT[:, 1:L + 1] = inp["x"][b].T
        cT = np.zeros((D, CTX + 2), np.float32)
        cT[:, 1:CTX + 1] = inp["ctx"][b].T
        cvec = np.concatenate([fm_vec(inp["c"][b]), fm_vec(inp["c_ctx"])], axis=1)
        for hf in range(ncores_per_batch):
            s = np.zeros((128, 2), np.float32)
            s[:, hf] = 1.0
            m = dict(shared)
            m.update({"xT": xT, "ctxT": cT, "cvec": cvec, "sel": s})
            maps.append(m)
    return maps


_NC_CACHE = {}


def kernel(**inputs):
    inp = {k: np.asarray(v) for k, v in inputs.items()}
    B, L, _ = inp["x"].shape
    OWN = L // 2
    key = (L, OWN)
    if key not in _NC_CACHE:
        _NC_CACHE[key] = build(L, OWN)
    nc = _NC_CACHE[key]
    maps = prep_inputs(inp, L, OWN, 2, B)
    res = run_bass_kernel_spmd(nc, maps, core_ids=list(range(len(maps))))
    out = np.empty((B, L, D), np.float32)
    for b in range(B):
        for hf in range(2):
            out[b, hf * OWN:(hf + 1) * OWN, :] = res.results[b * 2 + hf]["outT"].T
    return out
```

```python
import math
import numpy as np
import ml_dtypes
import concourse.bass as bass
import concourse.mybir as mybir
from concourse.bass_utils import run_bass_kernel_spmd

F32 = mybir.dt.float32
BF16 = mybir.dt.bfloat16
AF = mybir.ActivationFunctionType
ALU = mybir.AluOpType
AX = mybir.AxisListType

SAME_ENGINE_SYNC = True
NSLOT = 10


class Tok:
    __slots__ = ("lw", "rd", "name")

    def __init__(self, name=""):
        self.lw = None
        self.rd = []
        self.name = name


class TT:
    __slots__ = ("ap", "tok")

    def __init__(self, ap, tok):
        self.ap = ap
        self.tok = tok

    def __getitem__(self, k):
        return TT(self.ap[k], self.tok)

    def re(self, s, **kw):
        return TT(self.ap.rearrange(s, **kw), self.tok)

    @property
    def shape(self):
        return self.ap.shape


def bc(tt, dims):
    a = tt.ap
    base = list(a.ap)
    return TT(bass.AP(a.tensor, a.offset, [list(base[0])] + [list(d) for d in dims]), tt.tok)


class Op:
    __slots__ = ("stream", "fn", "deps", "dma", "ms", "slot", "val", "didx")

    def __init__(self, stream, fn, dma):
        self.stream = stream
        self.fn = fn
        self.deps = []
        self.dma = dma
        self.ms = False
        self.slot = None
        self.val = None
        self.didx = None


class Prog:
    STREAMS = ("pe", "act", "dve", "pool", "sp")

    def __init__(self, nc):
        self.nc = nc
        self.ops = {s: [] for s in self.STREAMS}
        self.ndma = {s: 0 for s in self.STREAMS}
        self.dmaops = {s: [] for s in self.STREAMS}
        self._ctx = []
        self._scopes = []
        self.banks = []
        self.bank_i = 0

    def sb(self, name, shape, dt=F32):
        g = self.nc.sbuf_tensor(name, list(shape), dt)
        h = g.__enter__()
        self._ctx.append(g)
        return TT(h[:], Tok(name))

    def ps(self, name, shape, dt=F32):
        g = self.nc.psum_tensor(name, list(shape), dt)
        h = g.__enter__()
        self._ctx.append(g)
        return TT(h[:], Tok(name))

    def dram(self, name, shape, dt=F32, kind="Internal"):
        h = self.nc.dram_tensor(name, list(shape), dt, kind=kind)
        return TT(h.ap(), Tok(name))

    def open_scope(self):
        self._scopes.append(len(self._ctx))

    def close_scope(self):
        n = self._scopes.pop()
        self.barrier()
        while len(self._ctx) > n:
            self._ctx.pop().__exit__(None, None, None)

    def close(self):
        while self._ctx:
            self._ctx.pop().__exit__(None, None, None)

    def bank(self):
        b = self.banks[self.bank_i % len(self.banks)]
        self.bank_i += 1
        return b

    def _rec(self, stream, fn, reads, writes, dma=False, extra=()):
        op = Op(stream, fn, dma)
        deps = list(extra)
        for t in reads:
            if t.tok.lw is not None:
                deps.append(t.tok.lw)
        for t in writes:
            if t.tok.lw is not None:
                deps.append(t.tok.lw)
            deps.extend(t.tok.rd)
        if dma:
            op.didx = self.ndma[stream]
            self.ndma[stream] += 1
            self.dmaops[stream].append(op)
            if op.didx >= NSLOT:
                deps.append(self.dmaops[stream][op.didx - NSLOT])
        seen = set()
        for d in deps:
            if d is op or id(d) in seen:
                continue
            if (not d.dma) and d.stream == stream and (stream == "pe" or not SAME_ENGINE_SYNC):
                continue
            seen.add(id(d))
            op.deps.append(d)
            d.ms = True
        for t in reads:
            t.tok.rd.append(op)
        for t in writes:
            t.tok.lw = op
            t.tok.rd = []
        self.ops[stream].append(op)
        return op

    def barrier(self):
        last = []
        for s in self.STREAMS:
            if self.ops[s]:
                for o in reversed(self.ops[s]):
                    if not o.dma and o.fn is not None:
                        last.append(o)
                        break
            last.extend(self.dmaops[s][-NSLOT:])
        for s in self.STREAMS:
            self._rec(s, None, [], [], extra=last)

    def mm(self, out, lhsT, rhs, start=True, stop=True):
        self._rec("pe", lambda e: e.matmul(out.ap, lhsT.ap, rhs.ap, start=start, stop=stop), [lhsT, rhs], [out])

    def mmgroup(self, out, pairs):
        rd = []
        for l, r in pairs:
            rd += [l, r]
        n = len(pairs)

        def fn(e):
            ins = None
            for i, (l, r) in enumerate(pairs):
                ins = e.matmul(out.ap, l.ap, r.ap, start=(i == 0), stop=(i == n - 1))
            return ins
        self._rec("pe", fn, rd, [out])

    def transpose(self, out, in_, ident):
        self._rec("pe", lambda e: e.transpose(out.ap, in_.ap, ident.ap), [in_, ident], [out])

    def act(self, out, in_, func, bias=None, scale=1.0, accum=None):
        rd = [in_]
        kw = {}
        if bias is not None:
            if isinstance(bias, TT):
                rd.append(bias)
                kw["bias"] = bias.ap
            else:
                kw["bias"] = bias
        if isinstance(scale, TT):
            rd.append(scale)
            kw["scale"] = scale.ap
        else:
            kw["scale"] = scale
        wr = [out]
        if accum is not None:
            wr.append(accum)
            kw["accum_out"] = accum.ap
        self._rec("act", lambda e: e.activation(out.ap, in_.ap, func, **kw), rd, wr)

    def tt(self, eng, out, a, b, op):
        self._rec(eng, lambda e: e.tensor_tensor(out.ap, a.ap, b.ap, op), [a, b], [out])

    def ts(self, eng, out, a, s1, s2=None, op0=ALU.mult, op1=None):
        rd = [a]
        s1a = s1.ap if isinstance(s1, TT) else s1
        s2a = s2.ap if isinstance(s2, TT) else s2
        if isinstance(s1, TT):
            rd.append(s1)
        if isinstance(s2, TT):
            rd.append(s2)
        kw = {}
        if op1 is not None:
            kw["op1"] = op1
        self._rec(eng, lambda e: e.tensor_scalar(out.ap, a.ap, s1a, s2a, op0, **kw), rd, [out])

    def stt(self, eng, out, a, s, b, op0, op1):
        rd = [a, b]
        sa = s.ap if isinstance(s, TT) else s
        if isinstance(s, TT):
            rd.append(s)
        self._rec("dve", lambda e: e.scalar_tensor_tensor(out.ap, a.ap, sa, b.ap, op0, op1), rd, [out])

    def copy(self, eng, out, a):
        if eng == "act":
            self._rec(eng, lambda e: e.copy(out.ap, a.ap), [a], [out])
        else:
            self._rec(eng, lambda e: e.tensor_copy(out.ap, a.ap), [a], [out])

    def memset(self, eng, out, val):
        self._rec(eng, lambda e: e.memset(out.ap, val), [], [out])

    def reduce(self, eng, out, a, op, axis=AX.X):
        self._rec(eng, lambda e: e.tensor_reduce(out.ap, a.ap, axis, op), [a], [out])

    def rsqrt(self, x):
        self._rec("act", lambda e: e.activation(x.ap, x.ap, AF.Sqrt), [x], [x])
        self._rec("dve", lambda e: e.reciprocal(x.ap, x.ap), [x], [x])

    def recip(self, out, a):
        self._rec("dve", lambda e: e.reciprocal(out.ap, a.ap), [a], [out])

    def dma(self, q, out, in_):
        self._rec(q, lambda e: e.dma_start(out.ap, in_.ap), [in_], [out], dma=True)

    def fence(self, stream, tts):
        self._rec(stream, None, list(tts), [])

    def emit(self):
        nc = self.nc
        for s in self.STREAMS:
            k = 0
            for op in self.ops[s]:
                if op.dma:
                    op.slot = op.didx % NSLOT
                    op.val = 16 * (op.didx // NSLOT + 1)
                elif op.ms:
                    k += 1
                    op.val = k
        sem_ctx = []
        csem = {}
        dsem = {}
        for s in self.STREAMS:
            g = nc.semaphore("c_" + s)
            csem[s] = g.__enter__()
            sem_ctx.append(g)
            if self.ndma[s] > 0:
                for i in range(NSLOT):
                    g = nc.semaphore("d_%s_%d" % (s, i))
                    dsem[(s, i)] = g.__enter__()
                    sem_ctx.append(g)
        ops = self.ops

        def run(stream, e):
            waited = {}
            for op in ops[stream]:
                for d in op.deps:
                    if d.dma:
                        key = ("d", d.stream, d.slot)
                        sem = dsem[(d.stream, d.slot)]
                    else:
                        key = ("c", d.stream)
                        sem = csem[d.stream]
                    if waited.get(key, 0) < d.val:
                        e.wait_ge(sem, d.val)
                        waited[key] = d.val
                if op.fn is None:
                    continue
                ins = op.fn(e)
                if op.dma:
                    ins.then_inc(dsem[(stream, op.slot)], 16)
                elif op.ms:
                    ins.then_inc(csem[stream], 1)

        with nc.Block() as block:
            @block.sync
            def _(e):
                run("sp", e)

            @block.tensor
            def _(e):
                run("pe", e)

            @block.scalar
            def _(e):
                run("act", e)

            @block.vector
            def _(e):
                run("dve", e)

            @block.gpsimd
            def _(e):
                run("pool", e)
        for g in reversed(sem_ctx):
            g.__exit__(None, None, None)


D = 1024
KC = 8
EPS = 1e-6
EVEN_IN = 5664
FFN_DENSE = 2816
NEXP = 8
FEXP = 3584
CTX = 256
GRID_W = 64
LAM_INIT = 0.8 - 0.6 * math.exp(-0.3 * 1)
C_Z, C_XBC, C_DT, C_RQ, C_RK, C_RV, C_RG = 0, 1024, 2560, 2592, 3104, 3616, 4640


def host_consts():
    k = np.arange(128)[:, None]
    l = np.arange(128)[None, :]
    c = {}
    c["ident"] = (k == l).astype(np.float32)
    c["le"] = (k <= l).astype(np.float32)
    c["gt"] = (k > l).astype(np.float32)
    c["ge"] = (k >= l).astype(np.float32)
    c["lt"] = (k < l).astype(np.float32)
    c["ones"] = np.ones((128, 128), np.float32)
    cm = np.concatenate([c[n] for n in ("ident", "le", "gt", "ge", "lt", "ones")], axis=1)
    sel = np.zeros((8, 8, 128), np.float32)
    for e in range(8):
        sel[e, e, :] = 1.0
    return cm, sel.reshape(8, 1024)


def rope_tables(L):
    f32 = np.float32
    inv = (np.float32(10000.0) ** (-np.arange(64, dtype=f32) / f32(64))).astype(f32)
    pos = np.arange(CTX + L, dtype=f32)
    ang = (pos[:, None] * inv[None, :]).astype(f32)
    rcs = np.concatenate([np.cos(ang), np.sin(ang)], axis=1).astype(f32)
    inv16 = (np.float32(10000.0) ** (-np.arange(16, dtype=f32) / f32(16))).astype(f32)
    t = np.arange(L)
    row = (t // GRID_W).astype(f32)
    col = (t % GRID_W).astype(f32)
    ar = (row[:, None] * inv16[None, :]).astype(f32)
    ac = (col[:, None] * inv16[None, :]).astype(f32)
    cos = np.concatenate([np.cos(ar), np.cos(ar), np.cos(ac), np.cos(ac)], axis=1)
    sins = np.concatenate([-np.sin(ar), np.sin(ar), -np.sin(ac), np.sin(ac)], axis=1)
    acs = np.concatenate([cos, sins], axis=1).astype(f32)
    return rcs, acs


def build(L=8192, OWN=4096, stop_after=None, dbg=()):
    nc = bass.Bass("TRN2", target_bir_lowering=False)
    P = Prog(nc)
    T = CTX + L
    NCH = T // 128
    NLB = L // 256
    dbg = set(dbg)

    def din(name, shape, dt=F32):
        return P.dram(name, shape, dt, kind="ExternalInput")

    xT = din("xT", [D, L + 2])
    cT = din("ctxT", [D, CTX + 2])
    cvec = din("cvec", [128, 16])
    sel = din("sel", [128, 2])
    consts = din("consts", [128, 768])
    selmat = din("selmat", [8, 1024])
    rcs_d = din("rcs", [T, 128])
    acs_d = din("acs", [L, 128])
    w_mod = [din("w_mod0", [D, 6 * D]), din("w_mod1", [D, 6 * D])]
    b_mod = [din("b_mod0", [128, 48]), din("b_mod1", [128, 48])]
    norms = din("norms", [128, 32])
    w_in0 = din("w_in0", [D, EVEN_IN])
    convw = din("convw", [128, 12, 4])
    rowp = din("rowp", [1, 2560])
    w_out0 = din("w_out0", [2048, D])
    ffg = din("ffg", [D, FFN_DENSE])
    ffu = din("ffu", [D, FFN_DENSE])
    ffd = din("ffd", [FFN_DENSE, D])
    w_in1 = din("w_in1", [D, 3072])
    w_out1 = din("w_out1", [D, D])
    wr = din("router", [D, NEXP])
    eg = din("eg", [NEXP, D, FEXP])
    eu = din("eu", [NEXP, D, FEXP])
    ed = din("ed", [NEXP, FEXP, D])
    outT = P.dram("outT", [D, OWN], F32, kind="ExternalOutput")

    def scratch(name, shape, dt=F32):
        return P.dram(name, shape, dt, kind=("ExternalOutput" if name in dbg else "Internal"))

    r_zr = scratch("r_zr", [NCH, 128, 2048], BF16)
    r_xv = scratch("r_xv", [NCH, 128, 2048], BF16)
    r_kt = scratch("r_kt", [NCH, 128, 768], BF16)
    r_fm = scratch("r_fm", [NCH, 128, 12, 128], BF16)
    r_dt = scratch("r_dt", [NCH, 128, 64], F32)
    r_yf = scratch("r_yf", [NCH, 128, 2048], F32)
    x_mid = scratch("x_mid", [NCH, 128, 8, 128], F32)
    x_l1 = scratch("x_l1", [NCH, 128, 8, 128], F32)
    Kd = scratch("Kd", [8, 128, T], BF16)
    Vd = scratch("Vd", [8, 128, NCH, 130], BF16)
    Qd = scratch("Qd", [8, 128, L], BF16)
    Od = scratch("Od", [128, 8, OWN], BF16)

    cst = P.sb("cst", [128, 768], F32)
    P.dma("sp", cst, consts)
    ident_f = cst[:, 0:128]
    m_le, m_gt, m_ge, m_lt, ones_f = (cst[:, 128 * i:128 * (i + 1)] for i in range(1, 6))
    cstb = P.sb("cstb", [128, 768], BF16)
    P.copy("dve", cstb, cst)
    ident_b = cstb[:, 0:128]
    sel_sb = P.sb("sel_sb", [128, 2], F32)
    P.dma("sp", sel_sb, sel)
    rows = P.sb("rows", [128, 2560], F32)
    P.dma("sp", rows, TT(rowp.ap.rearrange("a b -> (a b)").partition_broadcast(128), rowp.tok))
    norm_sb = P.sb("norm_sb", [128, 32], F32)
    P.dma("sp", norm_sb, norms)
    modfm = [P.sb("modfm0", [128, 48, 2], F32), P.sb("modfm1", [128, 48, 2], F32)]
    P.banks = [P.ps("bank%d" % i, [128, 512], F32) for i in range(8)]

    def bank_bf(b):
        a = b.ap
        return TT(a.bitcast(BF16), b.tok)

    AB = [P.sb("AB%d" % i, [128, 8, 2, 2, 2]) for i in range(2)]
    G = [P.sb("G%d" % i, [128, 8, 2, 2]) for i in range(2)]
    P.open_scope()
    cv = P.sb("cv", [128, 16], F32)
    P.dma("sp", cv, cvec)
    scv = P.sb("scv", [128, 8, 2], F32)
    P.act(scv[:, :, 0], cv[:, 0:8], AF.Silu)
    P.act(scv[:, :, 1], cv[:, 8:16], AF.Silu)
    wm = [P.sb("wm%d" % i, [128, 8, 512], F32) for i in range(2)]
    bm_sb = P.sb("bm_sb", [128, 2, 48], F32)
    P.dma("sp", bm_sb[:, 0, :], b_mod[0])
    P.dma("sp", bm_sb[:, 1, :], b_mod[1])
    it = 0
    for lyr in range(2):
        for cg in range(12):
            w = wm[it % 2]
            it += 1
            P.dma("sp", w, w_mod[lyr][:, cg * 512:(cg + 1) * 512].re("(k p) f -> p k f", p=128))
            pb = P.bank()
            for j in range(4):
                P.mmgroup(pb[:, 2 * j:2 * j + 2], [(w[:, k, j * 128:(j + 1) * 128], scv[:, k, :]) for k in range(8)])
            for j in range(4):
                ch = cg * 4 + j
                P.ts("dve", modfm[lyr][:, ch, :], pb[:, 2 * j:2 * j + 2], bm_sb[:, lyr, ch:ch + 1], None, op0=ALU.add)
    for lyr in range(2):
        for w in range(2):
            for n in range(2):
                shift = modfm[lyr][:, (3 * n) * 8:(3 * n) * 8 + 8, w]
                scale = modfm[lyr][:, (3 * n + 1) * 8:(3 * n + 1) * 8 + 8, w]
                gate = modfm[lyr][:, (3 * n + 2) * 8:(3 * n + 2) * 8 + 8, w]
                gain = norm_sb[:, (2 * lyr + n) * 8:(2 * lyr + n) * 8 + 8]
                P.stt("dve", AB[lyr][:, :, w, n, 0], scale, 1.0, gain, ALU.add, ALU.mult)
                P.copy("dve", AB[lyr][:, :, w, n, 1], shift)
                P.copy("dve", G[lyr][:, :, w, n], gate)
    P.close_scope()

    def rms_modulate(xin, ncol, Atab, out_bf, tmp, out_f32=None, eng="pool"):
        sq = tmp["sq"]
        P.act(sq[:, :, 0:ncol], xin, AF.Square)
        pb = P.bank()
        P.mmgroup(pb[:, 0:ncol], [(ones_f, sq[:, k, 0:ncol]) for k in range(8)])
        rstd = tmp["rstd"]
        P.ts("dve", rstd[:, 0:ncol], pb[:, 0:ncol], 1.0 / D, EPS, op0=ALU.mult, op1=ALU.add)
        P.rsqrt(rstd[:, 0:ncol])
        P.tt("dve", sq[:, :, 0:ncol], xin, bc(rstd[:, 0:ncol], [(0, 8), (1, ncol)]), ALU.mult)
        for k in range(8):
            if eng == "act" or (eng == "mix" and k % 2 == 0):
                P.act(out_bf[:, k, :], sq[:, k, 0:ncol], AF.Identity, bias=Atab[:, k, 1:2], scale=Atab[:, k, 0:1])
            else:
                P.ts("pool", out_bf[:, k, :], sq[:, k, 0:ncol], Atab[:, k, 0:1], Atab[:, k, 1:2], op0=ALU.mult, op1=ALU.add)
            if out_f32 is not None:
                P.ts("pool", out_f32[:, k, :], sq[:, k, 0:ncol], Atab[:, k, 0:1], Atab[:, k, 1:2], op0=ALU.mult, op1=ALU.add)

    r_dtb = rows[:, 0:32]
    r_alog = rows[:, 32:64]
    r_retd = rows[:, 64:72]
    r_dsk = rows[:, 72:88]
    r_ssdn = rows[:, 88:1112]
    r_qn = rows[:, 1112:1176]
    r_kn = rows[:, 1176:1240]
    r_lam = rows[:, 1240:1496]
    r_subln = rows[:, 1496:1624]
    ea = P.sb("ea", [128, 32], F32)
    P.act(ea, r_alog, AF.Exp)
    nla_ret = P.sb("nla_ret", [128, 8], F32)
    P.act(nla_ret, r_retd, AF.Exp)
    P.ts("dve", nla_ret, nla_ret, -1.0, None, op0=ALU.mult)

    P.open_scope()
    w0 = P.sb("w0", [128, 8, EVEN_IN], BF16)
    for k in range(8):
        for c0 in range(0, EVEN_IN, 1888):
            P.dma("pool", w0[:, k, c0:c0 + 1888], w_in0[k * 128:(k + 1) * 128, c0:c0 + 1888])
    cw = P.sb("cw", [128, 12, 4], F32)
    P.dma("sp", cw, convw)
    xin = [P.sb("xin%d" % i, [128, 8, 258], F32) for i in range(2)]
    xbr = P.sb("xbr", [128, 12, 258], F32)
    tmp1 = {"sq": xbr[:, 0:8, :], "rstd": P.sb("rstd1", [128, 258], F32)}
    hbs = [P.sb("hb%d" % i, [128, 8, 258], BF16) for i in range(2)]
    xbcs = [P.sb("xbc%d" % i, [128, 12, 256], BF16) for i in range(2)]
    cvts = [P.sb("cvt%d" % i, [128, 256], F32) for i in range(2)]
    o_zr = P.sb("o_zr", [128, 2, 2048], BF16)
    o_xv = P.sb("o_xv", [128, 2, 2048], BF16)
    o_kt = P.sb("o_kt", [128, 2, 768], BF16)
    o_fm = P.sb("o_fm", [128, 2, 12, 128], BF16)
    o_dt = P.sb("o_dt", [128, 2, 64], F32)
    rtabs = [P.sb("rtab%d" % i, [128, 2, 128], F32) for i in range(3)]
    rt1 = P.sb("rt1", [128, 4, 128], F32)
    rt2 = P.sb("rt2", [128, 4, 128], F32)
    rqk = P.sb("rqk", [128, 2, 512], BF16)
    sp1 = P.sb("sp1", [128, 32], F32)
    sp2 = P.sb("sp2", [128, 32], F32)

    blocks = [("c", 0)] + [("l", i) for i in range(NLB)]

    def binfo(bj):
        kind_, i_ = blocks[bj]
        if kind_ == "c":
            return 0, True, True, 1
        return CTX + i_ * 256, (i_ == 0), (i_ == NLB - 1), 0

    def load1(bj):
        kind_, i_ = blocks[bj]
        if kind_ == "c":
            P.dma("sp", xin[bj % 2], cT.re("(k p) t -> p k t", p=128))
        else:
            P.dma("sp", xin[bj % 2], xT[:, i_ * 256:i_ * 256 + 258].re("(k p) t -> p k t", p=128))
        t0_ = binfo(bj)[0]
        P.dma("sp", rtabs[bj % 3], rcs_d[t0_:t0_ + 256, :].re("(t p) c -> p t c", p=128))

    def stageA(bj):
        tok0, first, last, which = binfo(bj)
        xi, hb, xbc = xin[bj % 2], hbs[bj % 2], xbcs[bj % 2]
        rms_modulate(xi, 258, AB[0][:, :, which, 0, :], hb, tmp1, eng="act")
        for c in range(12):
            pb = P.bank()
            P.mmgroup(pb[:, 0:258], [(w0[:, k, C_XBC + c * 128:C_XBC + (c + 1) * 128], hb[:, k, :]) for k in range(8)])
            P.copy("act", xbr[:, c, :], pb[:, 0:258])
        if first:
            P.memset("pool", xbr[:, :, 0:1], 0.0)
        if last:
            P.memset("pool", xbr[:, :, 257:258], 0.0)
        for c in range(12):
            cvt = cvts[c % 2]
            P.act(cvt, xbr[:, c, 0:256], AF.Identity, scale=cw[:, c, 0:1])
            P.stt("dve", cvt, xbr[:, c, 1:257], cw[:, c, 1:2], cvt, ALU.mult, ALU.add)
            P.stt("dve", cvt, xbr[:, c, 2:258], cw[:, c, 2:3], cvt, ALU.mult, ALU.add)
            P.act(xbc[:, c, :], cvt, AF.Silu, bias=cw[:, c, 3:4])

    def stageB(bj):
        tok0, first, last, which = binfo(bj)
        hb, xbc, rtab = hbs[bj % 2], xbcs[bj % 2], rtabs[bj % 3]
        ch0 = tok0 // 128
        for t in range(2):
            P.copy("act", o_fm[:, t, 0:4, :], xbc[:, 8:12, t * 128:(t + 1) * 128])
        for t in range(2):
            lt = [hb[:, k, 1 + t * 128:1 + (t + 1) * 128] for k in range(8)]

            def proj(c0, n):
                pb = P.bank()
                P.mmgroup(pb[:, 0:n], [(lt[k], w0[:, k, c0:c0 + n]) for k in range(8)])
                return pb
            for j in range(2):
                pb = proj(C_Z + j * 512, 512)
                P.copy("act", o_zr[:, t, j * 512:(j + 1) * 512], pb)
            for j in range(2):
                pb = proj(C_RG + j * 512, 512)
                P.copy("act", o_zr[:, t, 1024 + j * 512:1024 + (j + 1) * 512], pb)
            for j in range(2):
                pb = proj(C_RV + j * 512, 512)
                P.copy("act", o_xv[:, t, 1024 + j * 512:1024 + (j + 1) * 512], pb)
            pb = proj(C_DT, 32)
            P.tt("dve", sp1, pb[:, 0:32], r_dtb, ALU.add)
            P.act(sp2, sp1, AF.Abs)
            P.act(sp2, sp2, AF.Exp, scale=-1.0)
            P.act(sp2, sp2, AF.Ln, bias=1.0)
            P.stt("dve", o_dt[:, t, 0:32], sp1, 0.0, sp2, ALU.max, ALU.add)
            P.tt("dve", sp1, o_dt[:, t, 0:32], ea, ALU.mult)
            P.ts("dve", o_dt[:, t, 32:64], sp1, -1.0, None, op0=ALU.mult)
            for qi, c0 in enumerate((C_RQ, C_RK)):
                pb = proj(c0, 512)
                pv = pb.re("p (h d) -> p h d", h=4)
                cos2 = bc(rtab[:, t, 0:64], [(0, 4), (0, 2), (1, 64)])
                P.tt("dve", rt1.re("p h (a d) -> p h a d", a=2), pv.re("p h (a d) -> p h a d", a=2), cos2, ALU.mult)
                sin1 = bc(rtab[:, t, 64:128], [(0, 4), (1, 64)])
                P.tt("dve", rt2[:, :, 0:64], pv[:, :, 64:128], sin1, ALU.mult)
                P.tt("dve", rt2[:, :, 64:128], pv[:, :, 0:64], sin1, ALU.mult)
                rv = rqk[:, qi, :].re("p (h d) -> p h d", h=4)
                P.tt("dve", rt1[:, :, 0:64], rt1[:, :, 0:64], rt2[:, :, 0:64], ALU.subtract)
                P.tt("dve", rt1[:, :, 64:128], rt1[:, :, 64:128], rt2[:, :, 64:128], ALU.add)
                P.act(rv, rt1, AF.Copy, scale=(1.0 if qi == 0 else 128.0 ** -0.5))
            P.copy("act", o_kt[:, t, 256:768], rqk[:, 1, :])
            pb = P.bank()
            pbb = bank_bf(pb)
            for qi in range(2):
                for h in range(4):
                    P.transpose(pbb[:, (qi * 4 + h) * 128:(qi * 4 + h + 1) * 128], rqk[:, qi, h * 128:(h + 1) * 128], ident_b)
            P.copy("dve", o_fm[:, t, 4:12, :], pbb.re("p (n t) -> p n t", t=128))
            pb = P.bank()
            pbb = bank_bf(pb)
            for c in range(8):
                P.transpose(pbb[:, c * 128:(c + 1) * 128], xbc[:, c, t * 128:(t + 1) * 128], ident_b)
            P.copy("dve", o_xv[:, t, 0:1024], pbb)
            pb = P.bank()
            pbb = bank_bf(pb)
            for c in range(2):
                P.transpose(pbb[:, c * 128:(c + 1) * 128], xbc[:, 8 + c, t * 128:(t + 1) * 128], ident_b)
            P.copy("dve", o_kt[:, t, 0:256], pbb[:, 0:256])
        for t in range(2):
            P.dma("sp", r_zr[ch0 + t], o_zr[:, t, :])
            P.dma("sp", r_xv[ch0 + t], o_xv[:, t, :])
            P.dma("sp", r_kt[ch0 + t], o_kt[:, t, :])
            P.dma("sp", r_fm[ch0 + t], o_fm[:, t])
            P.dma("sp", r_dt[ch0 + t], o_dt[:, t, :])

    nblk = len(blocks)
    load1(0)
    if nblk > 1:
        load1(1)
    stageA(0)
    for bi in range(nblk):
        if bi + 2 < nblk:
            load1(bi + 2)
        if bi + 1 < nblk:
            stageA(bi + 1)
        stageB(bi)
    P.close_scope()
    if stop_after == "P1":
        return _finish(P, nc, [r_zr, r_xv, r_kt, r_fm, r_dt])

    P.open_scope()
    wo0 = P.sb("wo0", [128, 16, D], BF16)
    for k in range(16):
        P.dma("pool", wo0[:, k, :], w_out0[k * 128:(k + 1) * 128, :])
    Er = P.sb("Er", [128, 2, 3, 4], F32)
    Dret = P.sb("Dret", [128, 2, 4, 128], F32)
    lmr = P.sb("lmr", [128, 4, 128], F32)
    for d in range(2):
        la = nla_ret[:, d * 4:(d + 1) * 4]
        pb = P.bank()
        mA, mT = (m_le, m_gt) if d == 0 else (m_ge, m_lt)
        P.mm(pb[:, 0:4], mA, la)
        P.mm(pb[:, 4:8], mT, la)
        P.mm(pb[:, 8:12], ones_f, la)
        P.act(Er[:, d].re("p a h -> p (a h)"), pb[:, 0:12], AF.Exp)
        mS, mR, mM = (m_gt, m_le, m_le) if d == 0 else (m_lt, m_ge, m_ge)
        P.tt("dve", lmr, bc(mS, [(0, 4), (1, 128)]), bc(la, [(1, 4), (0, 128)]), ALU.mult)
        pb = P.bank()
        for h in range(4):
            P.mm(pb[:, h * 128:(h + 1) * 128], lmr[:, h, :], mR)
        P.act(Dret[:, d].re("p h l -> p (h l)"), pb, AF.Exp)
        P.tt("dve", Dret[:, d], Dret[:, d], bc(mM, [(0, 4), (1, 128)]), ALU.mult)

    Hs = P.sb("Hs", [128, 1024], F32)
    Hr = P.sb("Hr", [128, 1024], F32)
    Hsb = P.sb("Hsb", [128, 1024], BF16)
    Hrb = P.sb("Hrb", [128, 1024], BF16)
    i_xv = [P.sb("i_xv%d" % i, [128, 2048], BF16) for i in range(2)]
    i_kt = [P.sb("i_kt%d" % i, [128, 768], BF16) for i in range(2)]
    i_fm = [P.sb("i_fm%d" % i, [128, 12, 128], BF16) for i in range(2)]
    i_dt = [P.sb("i_dt%d" % i, [128, 64], F32) for i in range(2)]
    i_zr = [P.sb("i_zr%d" % i, [128, 2048], BF16) for i in range(2)]
    i_yf = [P.sb("i_yf%d" % i, [128, 2048], F32) for i in range(2)]
    i_x = [P.sb("i_x%d" % i, [128, 8, 128], F32) for i in range(2)]
    E = P.sb("E", [128, 3, 16], F32)
    scm = P.sb("scm", [128, 2, 128], F32)
    Lm = P.sb("Lm", [128, 16, 128], F32)
    expD = P.sb("expD", [128, 16, 128], F32)
    MT = P.sb("MT", [128, 16, 128], BF16)
    MTr = P.sb("MTr", [128, 4, 128], BF16)
    xdt = P.sb("xdt", [128, 1024], BF16)
    xw = P.sb("xw", [128, 1024], BF16)
    rvw = P.sb("rvw", [128, 1024], BF16)
    wv = P.sb("wv", [128, 16], F32)
    ytmp = P.sb("ytmp", [128, 1024], F32)
    yo = [P.sb("yo%d" % i, [128, 2048], F32) for i in range(2)]
    sz = P.sb("sz", [128, 1024], F32)
    junk = P.sb("junk", [128, 1024], F32)
    ss = P.sb("ss", [128, 8], F32)
    ycat = P.sb("ycat", [128, 2048], BF16)
    ycT = P.sb("ycT", [128, 16, 128], BF16)
    xo = P.sb("xo", [128, 8, 128], F32)

    fwd_order = list(range(NCH))
    bwd_order = [1, 0] + list(range(NCH - 1, 1, -1))

    for d in range(2):
        order = fwd_order if d == 0 else bwd_order
        P.memset("dve", Hs, 0.0)
        P.memset("dve", Hr, 0.0)
        P.memset("pool", Hsb, 0.0)
        P.memset("pool", Hrb, 0.0)
        mA, mT = (m_le, m_gt) if d == 0 else (m_ge, m_lt)
        mS, mR, mM = (m_gt, m_le, m_le) if d == 0 else (m_lt, m_ge, m_ge)
        def load_sw(cj):
            ch_ = order[cj]
            b_ = cj % 2
            P.dma("sp", i_xv[b_], r_xv[ch_])
            P.dma("sp", i_kt[b_], r_kt[ch_])
            P.dma("sp", i_fm[b_], r_fm[ch_])
            P.dma("sp", i_dt[b_], r_dt[ch_])

        def load_fin(cj):
            ch_ = order[cj]
            b_ = cj % 2
            P.dma("sp", i_zr[b_], r_zr[ch_])
            P.dma("sp", i_yf[b_], r_yf[ch_])
            if ch_ < 2:
                P.dma("sp", i_x[b_], cT[:, 1 + ch_ * 128:1 + (ch_ + 1) * 128].re("(k p) t -> p k t", p=128))
            else:
                P.dma("sp", i_x[b_], xT[:, 1 + (ch_ - 2) * 128:1 + (ch_ - 1) * 128].re("(k p) t -> p k t", p=128))

        def finish(cj):
            ch = order[cj]
            b = cj % 2
            yout = yo[cj % 2]
            zr = i_zr[b]
            which = 1 if ch < 2 else 0
            P.tt("dve", yout, yout, i_yf[b], ALU.add)
            ys = yout[:, 0:1024]
            yr = yout[:, 1024:2048]
            P.act(sz, zr[:, 0:1024], AF.Silu)
            P.tt("dve", ys, ys, sz, ALU.mult)
            P.act(junk, ys, AF.Square)
            P.reduce("dve", ss[:, 0:1], junk, ALU.add)
            P.ts("dve", ss[:, 1:2], ss[:, 0:1], 1.0 / 1024, EPS, op0=ALU.mult, op1=ALU.add)
            P.rsqrt(ss[:, 1:2])
            P.stt("dve", ycat[:, 0:1024], ys, ss[:, 1:2], r_ssdn, ALU.mult, ALU.mult)
            P.act(junk, yr, AF.Square)
            P.reduce("dve", ss[:, 2:6], junk.re("p (h d) -> p h d", h=4), ALU.add)
            P.ts("dve", ss[:, 2:6], ss[:, 2:6], 1.0 / 256, EPS, op0=ALU.mult, op1=ALU.add)
            P.rsqrt(ss[:, 2:6])
            P.act(sz, zr[:, 1024:2048], AF.Silu)
            P.tt("dve", yr.re("p (h d) -> p h d", h=4), yr.re("p (h d) -> p h d", h=4), bc(ss[:, 2:6], [(1, 4), (0, 256)]), ALU.mult)
            P.tt("dve", ycat[:, 1024:2048], yr, sz, ALU.mult)
            for q in range(2):
                pb = P.bank()
                pbb = bank_bf(pb)
                for j in range(8):
                    P.transpose(pbb[:, j * 128:(j + 1) * 128], ycat[:, (q * 8 + j) * 128:(q * 8 + j + 1) * 128], ident_b)
                P.copy("act", ycT[:, q * 8:(q + 1) * 8, :].re("p n t -> p (n t)"), pbb)
            for q in range(2):
                pb = P.bank()
                for j in range(4):
                    dc = q * 4 + j
                    P.mmgroup(pb[:, j * 128:(j + 1) * 128], [(wo0[:, k, dc * 128:(dc + 1) * 128], ycT[:, k, :]) for k in range(16)])
                for j in range(4):
                    dc = q * 4 + j
                    P.stt("dve", xo[:, dc, :], pb[:, j * 128:(j + 1) * 128], G[0][:, dc, which, 0:1], i_x[b][:, dc, :], ALU.mult, ALU.add)
            P.dma("sp", x_mid[ch], xo)

        load_sw(0)
        for ci, ch in enumerate(order):
            b = ci % 2
            xv, kt, fm, dtt = i_xv[b], i_kt[b], i_fm[b], i_dt[b]
            if ci + 1 < len(order):
                load_sw(ci + 1)
            if d == 1:
                load_fin(ci)
            la = dtt[:, 32 + d * 16:32 + (d + 1) * 16]
            dtd = dtt[:, d * 16:(d + 1) * 16]
            xs = xv[:, 0:1024]
            rvv = xv[:, 1024:2048]
            yout = yo[ci % 2]
            pb = P.bank()
            P.mm(pb[:, 0:16], mA, la)
            P.mm(pb[:, 16:32], mT, la)
            P.mm(pb[:, 32:48], ones_f, la)
            P.act(E.re("p a h -> p (a h)"), pb[:, 0:48], AF.Exp)
            pb = P.bank()
            for g in range(2):
                P.mm(pb[:, g * 128:(g + 1) * 128], fm[:, g, :], fm[:, 2 + g, :])
            P.tt("dve", scm, pb[:, 0:256].re("p (g l) -> p g l", g=2), bc(mM, [(0, 2), (1, 128)]), ALU.mult)
            P.tt("pool", Lm, bc(mS, [(0, 16), (1, 128)]), bc(la, [(1, 16), (0, 128)]), ALU.mult)
            for q in range(4):
                pb = P.bank()
                for j in range(4):
                    P.mm(pb[:, j * 128:(j + 1) * 128], Lm[:, q * 4 + j, :], mR)
                P.act(expD[:, q * 4:(q + 1) * 4, :].re("p h l -> p (h l)"), pb, AF.Exp)
            for g in range(2):
                P.tt("dve", MT[:, g * 8:(g + 1) * 8, :], expD[:, g * 8:(g + 1) * 8, :], bc(scm[:, g, :], [(0, 8), (1, 128)]), ALU.mult)
            P.tt("pool", xdt.re("p (h d) -> p h d", h=16), xs.re("p (h d) -> p h d", h=16), bc(dtd, [(1, 16), (0, 64)]), ALU.mult)
            pd = [P.bank(), P.bank()]
            for h in range(16):
                P.mm(pd[h // 8][:, (h % 8) * 64:(h % 8 + 1) * 64], MT[:, h, :], xdt[:, h * 64:(h + 1) * 64])
            for g in range(2):
                po = P.bank()
                P.mm(po, fm[:, 2 + g, :], Hsb[:, g * 512:(g + 1) * 512])
                P.tt("dve", ytmp[:, g * 512:(g + 1) * 512].re("p (h d) -> p h d", h=8), po.re("p (h d) -> p h d", h=8),
                     bc(E[:, 0, g * 8:(g + 1) * 8], [(1, 8), (0, 64)]), ALU.mult)
                P.tt("dve", yout[:, g * 512:(g + 1) * 512], ytmp[:, g * 512:(g + 1) * 512], pd[g], ALU.add)
            P.tt("dve", wv, dtd, E[:, 1, :], ALU.mult)
            P.tt("pool", xw.re("p (h d) -> p h d", h=16), xs.re("p (h d) -> p h d", h=16), bc(wv, [(1, 16), (0, 64)]), ALU.mult)
            P.tt("dve", Hs.re("p (h d) -> p h d", h=16), Hs.re("p (h d) -> p h d", h=16), bc(E[:, 2, :], [(1, 16), (0, 64)]), ALU.mult)
            for g in range(2):
                pS = P.bank()
                P.mm(pS, kt[:, g * 128:(g + 1) * 128], xw[:, g * 512:(g + 1) * 512])
                P.tt("dve", Hs[:, g * 512:(g + 1) * 512], Hs[:, g * 512:(g + 1) * 512], pS, ALU.add)
            P.copy("act", Hsb, Hs)
            pb = P.bank()
            for h in range(4):
                P.mm(pb[:, h * 128:(h + 1) * 128], fm[:, 8 + h, :], fm[:, 4 + h, :])
            P.tt("dve", MTr, pb.re("p (h l) -> p h l", h=4), Dret[:, d], ALU.mult)
            pd = [P.bank(), P.bank()]
            for h in range(4):
                P.mm(pd[h // 2][:, (h % 2) * 256:(h % 2 + 1) * 256], MTr[:, h, :], rvv[:, h * 256:(h + 1) * 256])
            for g in range(2):
                po = P.bank()
                for hh in range(2):
                    h = g * 2 + hh
                    P.mm(po[:, hh * 256:(hh + 1) * 256], fm[:, 4 + h, :], Hrb[:, h * 256:(h + 1) * 256])
                P.tt("dve", ytmp[:, g * 512:(g + 1) * 512].re("p (h d) -> p h d", h=2), po.re("p (h d) -> p h d", h=2),
                     bc(Er[:, d, 0, g * 2:(g + 1) * 2], [(1, 2), (0, 256)]), ALU.mult)
                P.tt("dve", yout[:, 1024 + g * 512:1024 + (g + 1) * 512], ytmp[:, g * 512:(g + 1) * 512], pd[g], ALU.add)
            P.tt("pool", rvw.re("p (h d) -> p h d", h=4), rvv.re("p (h d) -> p h d", h=4), bc(Er[:, d, 1, :], [(1, 4), (0, 256)]), ALU.mult)
            P.tt("dve", Hr.re("p (h d) -> p h d", h=4), Hr.re("p (h d) -> p h d", h=4), bc(Er[:, d, 2, :], [(1, 4), (0, 256)]), ALU.mult)
            for g in range(2):
                pS = P.bank()
                for hh in range(2):
                    h = g * 2 + hh
                    P.mm(pS[:, hh * 256:(hh + 1) * 256], kt[:, 256 + h * 128:256 + (h + 1) * 128], rvw[:, h * 256:(h + 1) * 256])
                P.tt("dve", Hr[:, g * 512:(g + 1) * 512], Hr[:, g * 512:(g + 1) * 512], pS, ALU.add)
            P.copy("act", Hrb, Hr)
            if d == 0:
                P.dma("sp", r_yf[ch], yout)
                continue
            P.tt("pool", ytmp.re("p (h d) -> p h d", h=16), xs.re("p (h d) -> p h d", h=16), bc(r_dsk, [(1, 16), (0, 64)]), ALU.mult)
            P.tt("dve", yout[:, 0:1024], yout[:, 0:1024], ytmp, ALU.add)
            if ci >= 1:
                finish(ci - 1)
        if d == 1:
            finish(len(order) - 1)
    P.close_scope()
    if stop_after == "P3":
        return _finish(P, nc, [x_mid, r_yf])

    P.open_scope()
    NF = FFN_DENSE // 128
    wg = P.sb("wg", [128, 8, FFN_DENSE], BF16)
    wu = P.sb("wu", [128, 8, FFN_DENSE], BF16)
    wd = P.sb("wd", [128, NF, D], BF16)
    for k in range(8):
        P.dma("pool", wg[:, k, :], ffg[k * 128:(k + 1) * 128, :])
        P.dma("pool", wu[:, k, :], ffu[k * 128:(k + 1) * 128, :])
    for f in range(NF):
        P.dma("pool", wd[:, f, :], ffd[f * 128:(f + 1) * 128, :])
    xb4 = [P.sb("xb4_%d" % i, [128, 8, 256], F32) for i in range(2)]
    tmp4 = {"sq": P.sb("sq4", [128, 8, 256], F32), "rstd": P.sb("rstd4", [128, 256], F32)}
    h4 = P.sb("h4", [128, 8, 256], BF16)
    a4 = P.sb("a4", [128, NF, 256], BF16)
    sg4 = [P.sb("sg4_%d" % i, [128, 256], F32) for i in range(2)]
    xo4 = [P.sb("xo4_%d" % i, [128, 8, 256], F32) for i in range(1)]
    def load4(bj):
        for t in range(2):
            P.dma("sp", xb4[bj % 2][:, :, t * 128:(t + 1) * 128], x_mid[bj * 2 + t])
    load4(0)
    for bi in range(NCH // 2):
        x4 = xb4[bi % 2]
        which = 1 if bi == 0 else 0
        if bi + 1 < NCH // 2:
            load4(bi + 1)
        rms_modulate(x4, 256, AB[0][:, :, which, 1, :], h4, tmp4, eng="act")
        for f in range(NF):
            pb = P.bank()
            P.mmgroup(pb[:, 0:256], [(wg[:, k, f * 128:(f + 1) * 128], h4[:, k, :]) for k in range(8)])
            P.mmgroup(pb[:, 256:512], [(wu[:, k, f * 128:(f + 1) * 128], h4[:, k, :]) for k in range(8)])
            sg = sg4[f % 2]
            P.act(sg, pb[:, 0:256], AF.Silu)
            P.tt("dve", a4[:, f, :], sg, pb[:, 256:512], ALU.mult)
        xo_ = xo4[0]
        for q in range(4):
            pb = P.bank()
            for j in range(2):
                dc = q * 2 + j
                P.mmgroup(pb[:, j * 256:(j + 1) * 256], [(wd[:, f, dc * 128:(dc + 1) * 128], a4[:, f, :]) for f in range(NF)])
            for j in range(2):
                dc = q * 2 + j
                P.stt("dve", xo_[:, dc, :], pb[:, j * 256:(j + 1) * 256], G[0][:, dc, which, 1:2], x4[:, dc, :], ALU.mult, ALU.add)
        for t in range(2):
            P.dma("sp", x_l1[bi * 2 + t], xo_[:, :, t * 128:(t + 1) * 128])
    P.close_scope()
    if stop_after == "P4":
        return _finish(P, nc, [x_l1])

    P.open_scope()
    w1 = P.sb("w1", [128, 8, 3072], BF16)
    for k in range(8):
        P.dma("pool", w1[:, k, :], w_in1[k * 128:(k + 1) * 128, :])
    xb5 = [P.sb("xb5_%d" % i, [128, 8, 256], F32) for i in range(2)]
    tmp5 = {"sq": P.sb("sq5", [128, 8, 256], F32), "rstd": P.sb("rstd5", [128, 256], F32)}
    h5 = P.sb("h5", [128, 8, 256], BF16)
    atab = P.sb("atab", [128, 2, 128], F32)
    qsq = P.sb("qsq", [128, 1024], F32)
    qn = P.sb("qn", [128, 1024], F32)
    q1 = P.sb("q1", [128, 1024], F32)
    q2 = P.sb("q2", [128, 1024], F32)
    ss5 = P.sb("ss5", [128, 16], F32)
    qkb = P.sb("qkb", [128, 2, 1024], BF16)
    qkT = P.sb("qkT", [128, 2, 8, 256], BF16)
    v5 = [P.sb("v5_%d" % i, [128, 2, 8, 130], BF16) for i in range(2)]
    for i in range(2):
        P.memset("dve", v5[i], 1.0)
    qg = P.sb("qg", [128, 64], F32)
    P.ts("dve", qg, r_qn, 64.0 ** -0.5, None, op0=ALU.mult)
    atabs = [atab, P.sb("atab2", [128, 2, 128], F32), P.sb("atab3", [128, 2, 128], F32)]
    h5s = [h5, P.sb("h5b", [128, 8, 256], BF16)]
    qsqs = [qsq, P.sb("qsq_b", [128, 1024], F32)]
    qns = [qn, P.sb("qn_b", [128, 1024], F32)]
    q1s = [q1, P.sb("q1_b", [128, 1024], F32)]
    q2s = [q2, P.sb("q2_b", [128, 1024], F32)]
    ss5s = [ss5, P.sb("ss5_b", [128, 16], F32)]
    NB5 = NCH // 2

    def load5(bj):
        for t in range(2):
            P.dma("sp", xb5[bj % 2][:, :, t * 128:(t + 1) * 128], x_l1[bj * 2 + t])
        if bj > 0:
            l0 = (bj - 1) * 256
            P.dma("sp", atabs[bj % 3], acs_d[l0:l0 + 256, :].re("(t p) c -> p t c", p=128))

    def stage5A(bj):
        which = 1 if bj == 0 else 0
        rms_modulate(xb5[bj % 2], 256, AB[1][:, :, which, 0, :], h5s[bj % 2], tmp5, eng="act")

    def stage5B(bi):
        h5 = h5s[bi % 2]
        atab = atabs[bi % 3]
        which = 1 if bi == 0 else 0
        vv = v5[bi % 2]
        qis = [1] if which == 1 else [0, 1]
        for t in range(2):
            lt = [h5[:, k, t * 128:(t + 1) * 128] for k in range(8)]
            pbs = {}
            for qi in qis:
                pbs[qi] = []
                for j in range(2):
                    pb = P.bank()
                    c0 = qi * 1024 + j * 512
                    P.mmgroup(pb, [(lt[k], w1[:, k, c0:c0 + 512]) for k in range(8)])
                    pbs[qi].append(pb)
            for qi in qis:
                for j in range(2):
                    P.act(qsqs[qi][:, j * 512:(j + 1) * 512], pbs[qi][j], AF.Square)
            for qi in qis:
                P.reduce("dve", ss5s[qi], qsqs[qi].re("p (g d) -> p g d", d=64), ALU.add)
                P.ts("dve", ss5s[qi], ss5s[qi], 1.0 / 64, EPS, op0=ALU.mult, op1=ALU.add)
            for qi in qis:
                P.rsqrt(ss5s[qi])
            for qi in qis:
                for j in range(2):
                    P.tt("dve", qns[qi][:, j * 512:(j + 1) * 512].re("p (g d) -> p g d", d=64), pbs[qi][j].re("p (g d) -> p g d", d=64),
                         bc(ss5s[qi][:, j * 8:(j + 1) * 8], [(1, 8), (0, 64)]), ALU.mult)
            pvs = []
            for j in range(2):
                pb = P.bank()
                c0 = 2048 + j * 512
                P.mmgroup(pb, [(lt[k], w1[:, k, c0:c0 + 512]) for k in range(8)])
                pvs.append(pb)
            for qi in qis:
                qn = qns[qi]
                gn = qg if qi == 0 else r_kn
                dst = qkb[:, qi, :]
                if which == 1:
                    P.tt("dve", dst.re("p (g d) -> p g d", d=64), qn.re("p (g d) -> p g d", d=64), bc(gn, [(0, 16), (1, 64)]), ALU.mult)
                else:
                    P.tt("dve", qn.re("p (g d) -> p g d", d=64), qn.re("p (g d) -> p g d", d=64), bc(gn, [(0, 16), (1, 64)]), ALU.mult)
            for j in range(2):
                P.copy("act", vv[:, t, j * 4:(j + 1) * 4, 0:128], pvs[j].re("p (h e) -> p h e", h=4))
            if which == 0:
                for qi in qis:
                    qn, q1, q2 = qns[qi], q1s[qi], q2s[qi]
                    P.tt("dve", q1.re("p (g d) -> p g d", d=64), qn.re("p (g d) -> p g d", d=64), bc(atab[:, t, 0:64], [(0, 16), (1, 64)]), ALU.mult)
                    qv = qn.re("p (g a u d) -> p g a u d", a=2, u=2, d=16)
                    q2v = q2.re("p (g a u d) -> p g a u d", a=2, u=2, d=16)
                    for s_ in range(2):
                        sn = bass.AP(atab.ap.tensor, atab[:, t, 64 + s_ * 16:64 + s_ * 16 + 16].ap.offset,
                                     [list(atab.ap.ap[0]), [0, 16], [32, 2], [1, 16]])
                        P.tt("dve", q2v[:, :, :, s_, :], qv[:, :, :, 1 - s_, :], TT(sn, atab.tok), ALU.mult)
                for qi in qis:
                    P.tt("dve", qkb[:, qi, :], q1s[qi], q2s[qi], ALU.add)
            for qi in qis:
                pb = P.bank()
                pbb = bank_bf(pb)
                for h in range(8):
                    P.transpose(pbb[:, h * 128:(h + 1) * 128], qkb[:, qi, h * 128:(h + 1) * 128], ident_b)
                P.copy("act", qkT[:, qi, :, t * 128:(t + 1) * 128], pbb.re("p (h t) -> p h t", h=8))
        tok0 = bi * 256
        P.dma("sp", Kd[:, :, tok0:tok0 + 256].re("h p t -> p h t"), qkT[:, 1])
        if which == 0:
            P.dma("sp", Qd[:, :, tok0 - CTX:tok0 - CTX + 256].re("h p t -> p h t"), qkT[:, 0])
        for t in range(2):
            P.dma("sp", Vd[:, :, bi * 2 + t, :].re("h p e -> p h e"), vv[:, t])

    load5(0)
    if NB5 > 1:
        load5(1)
    stage5A(0)
    for bi in range(NB5):
        if bi + 2 < NB5:
            load5(bi + 2)
        if bi + 1 < NB5:
            stage5A(bi + 1)
        stage5B(bi)
    P.close_scope()
    if stop_after == "P5":
        return _finish(P, nc, [Kd, Vd, Qd])

    P.open_scope()
    NKT = NCH
    NQB = OWN // 512
    lt_ = P.sb("lt_", [128, 128], F32)
    lam2 = P.sb("lam2", [128, 4], F32)
    P.tt("dve", lt_[:, 0:64], r_lam[:, 0:64], r_lam[:, 64:128], ALU.mult)
    P.tt("dve", lt_[:, 64:128], r_lam[:, 128:192], r_lam[:, 192:256], ALU.mult)
    P.reduce("dve", lam2[:, 0:2], lt_.re("p (a d) -> p a d", a=2), ALU.add)
    P.act(lam2[:, 0:2], lam2[:, 0:2], AF.Exp)
    P.tt("dve", lam2[:, 2:3], lam2[:, 1:2], lam2[:, 0:1], ALU.subtract)
    P.ts("dve", lam2[:, 3:4], lam2[:, 2:3], -LAM_INIT, None, op0=ALU.add)
    neglam = lam2[:, 3:4]
    sub_g = P.sb("sub_g", [128, 128], F32)
    P.ts("dve", sub_g, r_subln, 1.0 - LAM_INIT, None, op0=ALU.mult)
    Kh = [P.sb("Kh%d" % i, [128, T], BF16) for i in range(2)]
    Vh = [P.sb("Vh%d" % i, [128, NKT, 130], BF16) for i in range(2)]
    qa = [P.sb("qa%d" % i, [128, 512], BF16) for i in range(2)]
    qb_ = [P.sb("qb%d" % i, [128, 512], BF16) for i in range(2)]
    qs = [P.sb("qs%d" % i, [128, 512], BF16) for i in range(2)]
    pT = [P.sb("pT%d" % i, [128, 512], BF16) for i in range(3)]
    o0 = P.sb("o0", [128, 4, 128], F32)
    o1 = P.sb("o1", [128, 128], F32)
    osq = P.sb("osq", [128, 4, 128], F32)
    rs6 = P.sb("rs6", [128, 4], F32)
    on = P.sb("on", [128, 4, 128], BF16)
    oT = [P.sb("oT%d" % i, [128, 512], BF16) for i in range(2)]
    spb = [P.banks[0], P.banks[1], P.banks[2]]
    ob = [P.banks[3], P.banks[4], P.banks[5], P.banks[6]]
    tb = P.banks[7]
    groups = [(h, qb) for h in range(8) for qb in range(NQB)]
    steps = [(m, kt) for m in range(2) for kt in range(NKT)]

    def load_kv(h):
        P.dma("sp", Kh[h % 2], Kd[h])
        P.dma("sp", Vh[h % 2], Vd[h])

    def load_q(gi):
        h, qb = groups[gi]
        A, B_, Q_ = qa[gi % 2], qb_[gi % 2], qs[gi % 2]
        P.dma("sp", A, Qd[h, :, qb * 512:(qb + 1) * 512])
        if L > OWN:
            P.dma("sp", B_, Qd[h, :, OWN + qb * 512:OWN + (qb + 1) * 512])
            P.ts("pool", Q_, A, sel_sb[:, 0:1], None, op0=ALU.mult)
            P.stt("dve", Q_, B_, sel_sb[:, 1:2], Q_, ALU.mult, ALU.add)
            return Q_
        return A

    load_kv(0)
    Qn = load_q(0)
    pi = 0
    for gi, (h, qb) in enumerate(groups):
        K_, V_ = Kh[h % 2], Vh[h % 2]
        Q_ = Qn
        if qb == 0 and h + 1 < 8:
            load_kv(h + 1)
        if gi + 1 < len(groups):
            Qn = load_q(gi + 1)

        def emit_s(i):
            m, kt = steps[i]
            P.mm(spb[(pi + i) % 3], K_[m * 64:(m + 1) * 64, kt * 128:(kt + 1) * 128], Q_[m * 64:(m + 1) * 64, :])
        emit_s(0)
        emit_s(1)
        for i, (m, kt) in enumerate(steps):
            if i + 2 < len(steps):
                emit_s(i + 2)
            sp_ = spb[(pi + i) % 3]
            p_ = pT[(pi + i) % 3]
            P.act(p_, sp_, AF.Exp, bias=-8.0)
            for s_ in range(4):
                P.mm(ob[s_][:, 0:129], p_[:, s_ * 128:(s_ + 1) * 128], V_[:, kt, 0:129], start=(kt == 0), stop=(kt == NKT - 1))
            if kt == NKT - 1:
                for s_ in range(4):
                    P.recip(rs6[:, s_:s_ + 1], ob[s_][:, 128:129])
                    if m == 0:
                        P.ts("dve", o0[:, s_, :], ob[s_][:, 0:128], rs6[:, s_:s_ + 1], None, op0=ALU.mult)
                    else:
                        P.ts("dve", o1, ob[s_][:, 0:128], rs6[:, s_:s_ + 1], neglam, op0=ALU.mult, op1=ALU.mult)
                        P.tt("dve", o0[:, s_, :], o0[:, s_, :], o1, ALU.add)
        pi += len(steps)
        P.act(osq, o0, AF.Square)
        P.reduce("dve", rs6, osq, ALU.add)
        P.ts("dve", rs6, rs6, 1.0 / 128, EPS, op0=ALU.mult, op1=ALU.add)
        P.rsqrt(rs6)
        tbb = bank_bf(tb)
        for s_ in range(4):
            P.stt("dve", on[:, s_, :], o0[:, s_, :], rs6[:, s_:s_ + 1], sub_g, ALU.mult, ALU.mult)
            P.transpose(tbb[:, s_ * 128:(s_ + 1) * 128], on[:, s_, :], ident_b)
        o_ = oT[gi % 2]
        P.copy("dve", o_, tbb[:, 0:512])
        P.dma("sp", Od[:, h, qb * 512:(qb + 1) * 512], o_)
    P.close_scope()
    if stop_after == "P6":
        return _finish(P, nc, [Od])

    P.open_scope()
    BLK = min(1024, OWN)
    NB = OWN // BLK
    NH = BLK // 512
    NT7 = BLK // 128
    wo1 = P.sb("wo1", [128, 8, D], BF16)
    for k in range(8):
        P.dma("pool", wo1[:, k, :], w_out1[k * 128:(k + 1) * 128, :])
    wr_sb = P.sb("wr_sb", [128, 8, 8], F32)
    P.dma("sp", wr_sb, wr.re("(k p) e -> p k e", p=128))
    selm = P.sb("selm", [8, 1024], F32)
    P.dma("sp", selm, selmat)
    o7 = P.sb("o7", [128, 8, BLK], BF16)
    xa = P.sb("xa", [128, 8, BLK], F32)
    xb7 = P.sb("xb7", [128, 8, 128], F32)
    sq7 = P.sb("sq7", [128, 8, 512], F32)
    rstd7 = P.sb("rstd7", [128, 512], F32)
    h7 = P.sb("h7", [128, 8, BLK], BF16)
    h7f = sq7
    yacc = P.sb("yacc", [128, 8, BLK], F32)
    lg = P.sb("lg", [128, 8], F32)
    lg2 = P.sb("lg2", [128, 8], F32)
    eq1 = P.sb("eq1", [128, 8], F32)
    eq2 = P.sb("eq2", [128, 8], F32)
    mx = P.sb("mx", [128, 8], F32)
    comb = P.sb("comb", [128, 8], F32)
    combT = P.sb("combT", [8, BLK], F32)
    cbc = [P.sb("cbc%d" % i, [128, BLK], BF16) for i in range(2)]
    FG = 2
    NFG = FEXP // (128 * FG)
    FW = 128 * FG
    wge = [P.sb("wge%d" % i, [128, 8, FW], BF16) for i in range(2)]
    wue = [P.sb("wue%d" % i, [128, 8, FW], BF16) for i in range(2)]
    wde = [P.sb("wde%d" % i, [128, FG, D], BF16) for i in range(2)]
    a7 = [P.sb("a7_%d" % i, [128, FG, BLK], BF16) for i in range(2)]
    sg7 = [P.sb("sg7_%d" % i, [128, 512], F32) for i in range(2)]
    t7 = [P.sb("t7_%d" % i, [128, 512], F32) for i in range(2)]
    its = [(nb, e, fg) for nb in range(NB) for e in range(NEXP) for fg in range(NFG)]

    def issue_w(ii):
        nb_, e_, fg_ = its[ii]
        b_ = ii % 2
        f0_ = fg_ * FW
        P.dma("pool", wge[b_], eg[e_, :, f0_:f0_ + FW].re("(k p) f -> p k f", p=128))
        P.dma("pool", wue[b_], eu[e_, :, f0_:f0_ + FW].re("(k p) f -> p k f", p=128))
        P.dma("pool", wde[b_], ed[e_, f0_:f0_ + FW, :].re("(f p) d -> p f d", p=128))
    issue_w(0)
    wi = 0
    for nb in range(NB):
        P.dma("sp", o7, Od[:, :, nb * BLK:(nb + 1) * BLK])
        for t in range(NT7):
            chA = 2 + (nb * BLK) // 128 + t
            P.dma("sp", xa[:, :, t * 128:(t + 1) * 128], x_l1[chA])
            if L > OWN:
                P.dma("sp", xb7, x_l1[chA + OWN // 128])
                P.ts("pool", xa[:, :, t * 128:(t + 1) * 128], xa[:, :, t * 128:(t + 1) * 128], sel_sb[:, 0:1], None, op0=ALU.mult)
                P.stt("pool", xa[:, :, t * 128:(t + 1) * 128], xb7, sel_sb[:, 1:2], xa[:, :, t * 128:(t + 1) * 128], ALU.mult, ALU.add)
        for hf in range(NH):
            cs = slice(hf * 512, (hf + 1) * 512)
            for dc in range(8):
                pb = P.bank()
                P.mmgroup(pb, [(wo1[:, hh, dc * 128:(dc + 1) * 128], o7[:, hh, cs]) for hh in range(8)])
                P.stt("dve", xa[:, dc, cs], pb, G[1][:, dc, 0, 0:1], xa[:, dc, cs], ALU.mult, ALU.add)
            P.act(sq7, xa[:, :, cs], AF.Square)
            pb = P.bank()
            P.mmgroup(pb, [(ones_f, sq7[:, k, :]) for k in range(8)])
            P.ts("dve", rstd7, pb, 1.0 / D, EPS, op0=ALU.mult, op1=ALU.add)
            P.rsqrt(rstd7)
            P.tt("dve", sq7, xa[:, :, cs], bc(rstd7, [(0, 8), (1, 512)]), ALU.mult)
            A2 = AB[1][:, :, 0, 1, :]
            for k in range(8):
                P.act(h7f[:, k, :], sq7[:, k, :], AF.Identity, bias=A2[:, k, 1:2], scale=A2[:, k, 0:1])
                P.copy("act", h7[:, k, cs], h7f[:, k, :])
            for t4 in range(4):
                pb = P.bank()
                P.mmgroup(pb[:, 0:8], [(h7f[:, k, t4 * 128:(t4 + 1) * 128], wr_sb[:, k, :]) for k in range(8)])
                P.copy("dve", lg, pb[:, 0:8])
                P.reduce("dve", mx[:, 0:1], lg, ALU.max)
                P.ts("dve", eq1, lg, mx[:, 0:1], None, op0=ALU.is_equal)
                P.stt("dve", lg2, eq1, -1e30, lg, ALU.mult, ALU.add)
                P.reduce("dve", mx[:, 1:2], lg2, ALU.max)
                P.ts("dve", eq2, lg2, mx[:, 1:2], None, op0=ALU.is_equal)
                P.tt("dve", mx[:, 2:3], mx[:, 1:2], mx[:, 0:1], ALU.subtract)
                P.act(mx[:, 3:4], mx[:, 2:3], AF.Exp)
                P.ts("dve", mx[:, 4:5], mx[:, 3:4], 1.0, None, op0=ALU.add)
                P.recip(mx[:, 5:6], mx[:, 4:5])
                P.tt("dve", mx[:, 6:7], mx[:, 3:4], mx[:, 5:6], ALU.mult)
                P.ts("dve", comb, eq1, mx[:, 5:6], None, op0=ALU.mult)
                P.stt("dve", comb, eq2, mx[:, 6:7], comb, ALU.mult, ALU.add)
                pb = P.bank()
                P.transpose(pb[0:8, 0:128], comb, ident_f)
                P.copy("dve", combT[:, hf * 512 + t4 * 128:hf * 512 + (t4 + 1) * 128], pb[0:8, 0:128])
        P.memset("pool", yacc, 0.0)
        for e in range(NEXP):
            cb = cbc[e % 2]
            for hf in range(NH):
                cs = slice(hf * 512, (hf + 1) * 512)
                pb = P.bank()
                P.mm(pb, selm[:, e * 128:(e + 1) * 128], combT[:, cs])
                P.copy("act", cb[:, cs], pb)
            for k in range(8):
                P.tt("dve", o7[:, k, :], h7[:, k, :], cb, ALU.mult)
            for fg in range(NFG):
                b = wi % 2
                wi += 1
                if wi < len(its):
                    issue_w(wi)
                aa = a7[b]
                ii = 0
                for f in range(FG):
                    for hf in range(NH):
                        cs = slice(hf * 512, (hf + 1) * 512)
                        pg = P.bank()
                        pu = P.bank()
                        P.mmgroup(pg, [(wge[b][:, k, f * 128:(f + 1) * 128], h7[:, k, cs]) for k in range(8)])
                        P.mmgroup(pu, [(wue[b][:, k, f * 128:(f + 1) * 128], o7[:, k, cs]) for k in range(8)])
                        sg = sg7[ii % 2]
                        tt_ = t7[ii % 2]
                        ii += 1
                        P.act(sg, pg, AF.Silu)
                        P.tt("dve", aa[:, f, cs], sg, pu, ALU.mult)
                for hf in range(NH):
                    cs = slice(hf * 512, (hf + 1) * 512)
                    for dc in range(8):
                        pb = P.bank()
                        P.mmgroup(pb, [(wde[b][:, f, dc * 128:(dc + 1) * 128], aa[:, f, cs]) for f in range(FG)])
                        P.tt("dve", yacc[:, dc, cs], yacc[:, dc, cs], pb, ALU.add)
        for dc in range(8):
            P.stt("dve", yacc[:, dc, :], yacc[:, dc, :], G[1][:, dc, 0, 1:2], xa[:, dc, :], ALU.mult, ALU.add)
        P.dma("sp", outT[:, nb * BLK:(nb + 1) * BLK].re("(k p) t -> p k t", p=128), yacc)
    P.close_scope()
    return _finish(P, nc, [outT])


def P_sb_keep(P, name, shape):
    g = P.nc.sbuf_tensor(name, list(shape), F32)
    h = g.__enter__()
    P._ctx.insert(0, g)
    for i in range(len(P._scopes)):
        P._scopes[i] += 1
    return TT(h[:], Tok(name))


def _finish(P, nc, outs):
    P.fence("sp", outs)
    P.emit()
    P.close()
    return nc


def fm_vec(v):
    v = np.asarray(v, np.float32).reshape(-1, 128)
    return np.ascontiguousarray(v.T)


def prep_inputs(inp, L, OWN, ncores_per_batch, nbatch):
    cm, selm = host_consts()
    rcs, acs = rope_tables(L)
    shared = {
        "consts": cm, "selmat": selm, "rcs": rcs, "acs": acs,
        "w_mod0": np.ascontiguousarray(inp["even_w_mod"][0]), "w_mod1": np.ascontiguousarray(inp["odd_w_mod"][0]),
        "b_mod0": fm_vec(inp["even_b_mod"][0]), "b_mod1": fm_vec(inp["odd_b_mod"][0]),
        "norms": np.concatenate([fm_vec(inp["even_norm1"][0]), fm_vec(inp["even_norm2"][0]),
                                 fm_vec(inp["odd_norm1"][0]), fm_vec(inp["odd_norm2"][0])], axis=1),
        "w_in0": np.ascontiguousarray(inp["even_w_in"][0]),
        "w_out0": np.ascontiguousarray(inp["even_w_out"][0]),
        "ffg": np.ascontiguousarray(inp["even_ffn_gate"][0]), "ffu": np.ascontiguousarray(inp["even_ffn_up"][0]),
        "ffd": np.ascontiguousarray(inp["even_ffn_down"][0]),
        "w_in1": np.ascontiguousarray(inp["odd_w_in"][0]), "w_out1": np.ascontiguousarray(inp["odd_w_out"][0]),
        "router": np.ascontiguousarray(inp["odd_router"][0]),
        "eg": np.ascontiguousarray(inp["odd_exp_gate"][0]), "eu": np.ascontiguousarray(inp["odd_exp_up"][0]),
        "ed": np.ascontiguousarray(inp["odd_exp_down"][0]),
    }
    cw = np.concatenate([inp["even_conv_w"][0], inp["even_conv_b"][0][None, :]], axis=0)
    shared["convw"] = np.ascontiguousarray(cw.reshape(4, 12, 128).transpose(2, 1, 0))
    rowp = np.concatenate([
        inp["even_dt_bias"][0].reshape(-1), inp["even_a_log"][0].reshape(-1), inp["even_ret_decay"][0].reshape(-1),
        inp["even_d"][0].reshape(-1), inp["even_ssd_norm"][0].reshape(-1), inp["odd_q_norm"][0].reshape(-1),
        inp["odd_k_norm"][0].reshape(-1), inp["odd_lambda"][0].reshape(-1), inp["odd_subln"][0].reshape(-1)]).astype(np.float32)
    rp = np.zeros((1, 2560), np.float32)
    rp[0, :rowp.size] = rowp
    shared["rowp"] = rp
    maps = []
    for b in range(nbatch):
        xT = np.zeros((D, L + 2), np.float32)
        xT[:, 1:L + 1] = inp["x"][b].T
        cT = np.zeros((D, CTX + 2), np.float32)
        cT[:, 1:CTX + 1] = inp["ctx"][b].T
        cvec = np.concatenate([fm_vec(inp["c"][b]), fm_vec(inp["c_ctx"])], axis=1)
        for hf in range(ncores_per_batch):
            s = np.zeros((128, 2), np.float32)
            s[:, hf] = 1.0
            m = dict(shared)
            m.update({"xT": xT, "ctxT": cT, "cvec": cvec, "sel": s})
            maps.append(m)
    return maps


_NC_CACHE = {}


def kernel(**inputs):
    inp = {k: np.asarray(v) for k, v in inputs.items()}
    B, L, _ = inp["x"].shape
    OWN = L // 2
    key = (L, OWN)
    if key not in _NC_CACHE:
        _NC_CACHE[key] = build(L, OWN)
    nc = _NC_CACHE[key]
    maps = prep_inputs(inp, L, OWN, 2, B)
    res = run_bass_kernel_spmd(nc, maps, core_ids=list(range(len(maps))))
    out = np.empty((B, L, D), np.float32)
    for b in range(B):
        for hf in range(2):
            out[b, hf * OWN:(hf + 1) * OWN, :] = res.results[b * 2 + hf]["outT"].T
    return out
```

```python
import math
import numpy as np
import ml_dtypes
import concourse.bass as bass
import concourse.mybir as mybir
from concourse.bass_utils import run_bass_kernel_spmd

F32 = mybir.dt.float32
BF16 = mybir.dt.bfloat16
AF = mybir.ActivationFunctionType
ALU = mybir.AluOpType
AX = mybir.AxisListType

SAME_ENGINE_SYNC = True
NSLOT = 10


class Tok:
    __slots__ = ("lw", "rd", "name", "dw")

    def __init__(self, name=""):
        self.lw = None
        self.rd = []
        self.name = name
        self.dw = []


class TT:
    __slots__ = ("ap", "tok")

    def __init__(self, ap, tok):
        self.ap = ap
        self.tok = tok

    def __getitem__(self, k):
        return TT(self.ap[k], self.tok)

    def re(self, s, **kw):
        return TT(self.ap.rearrange(s, **kw), self.tok)

    @property
    def shape(self):
        return self.ap.shape


def bc(tt, dims):
    a = tt.ap
    base = list(a.ap)
    return TT(bass.AP(a.tensor, a.offset, [list(base[0])] + [list(d) for d in dims]), tt.tok)


class Op:
    __slots__ = ("stream", "fn", "deps", "dma", "ms", "slot", "val", "didx")

    def __init__(self, stream, fn, dma):
        self.stream = stream
        self.fn = fn
        self.deps = []
        self.dma = dma
        self.ms = False
        self.slot = None
        self.val = None
        self.didx = None


class Prog:
    STREAMS = ("pe", "act", "dve", "pool", "sp")

    def __init__(self, nc):
        self.nc = nc
        self.ops = {s: [] for s in self.STREAMS}
        self.ndma = {s: 0 for s in self.STREAMS}
        self.dmaops = {s: [] for s in self.STREAMS}
        self._ctx = []
        self._scopes = []
        self.banks = []
        self.bank_i = 0

    def sb(self, name, shape, dt=F32):
        g = self.nc.sbuf_tensor(name, list(shape), dt)
        h = g.__enter__()
        self._ctx.append(g)
        return TT(h[:], Tok(name))

    def ps(self, name, shape, dt=F32):
        g = self.nc.psum_tensor(name, list(shape), dt)
        h = g.__enter__()
        self._ctx.append(g)
        return TT(h[:], Tok(name))

    def dram(self, name, shape, dt=F32, kind="Internal"):
        h = self.nc.dram_tensor(name, list(shape), dt, kind=kind)
        return TT(h.ap(), Tok(name))

    def open_scope(self):
        self._scopes.append(len(self._ctx))

    def close_scope(self):
        n = self._scopes.pop()
        self.barrier()
        while len(self._ctx) > n:
            self._ctx.pop().__exit__(None, None, None)

    def close(self):
        while self._ctx:
            self._ctx.pop().__exit__(None, None, None)

    def bank(self):
        b = self.banks[self.bank_i % len(self.banks)]
        self.bank_i += 1
        return b

    def _rec(self, stream, fn, reads, writes, dma=False, extra=()):
        op = Op(stream, fn, dma)
        deps = list(extra)
        for t in reads:
            if t.tok.lw is not None:
                deps.append(t.tok.lw)
            deps.extend(t.tok.dw)
        for t in writes:
            if dma:
                if t.tok.lw is not None and not t.tok.lw.dma:
                    deps.append(t.tok.lw)
            else:
                if t.tok.lw is not None:
                    deps.append(t.tok.lw)
                deps.extend(t.tok.dw)
            deps.extend(t.tok.rd)
        if dma:
            op.didx = self.ndma[stream]
            self.ndma[stream] += 1
            self.dmaops[stream].append(op)
            if op.didx >= NSLOT:
                deps.append(self.dmaops[stream][op.didx - NSLOT])
        seen = set()
        for d in deps:
            if d is op or id(d) in seen:
                continue
            if (not d.dma) and d.stream == stream and (stream == "pe" or not SAME_ENGINE_SYNC):
                continue
            seen.add(id(d))
            op.deps.append(d)
            d.ms = True
        for t in reads:
            t.tok.rd.append(op)
        for t in writes:
            t.tok.lw = op
            t.tok.rd = []
            if dma:
                t.tok.dw = (t.tok.dw + [op])[-NSLOT:]
            else:
                t.tok.dw = []
        self.ops[stream].append(op)
        return op

    def barrier(self):
        last = []
        for s in self.STREAMS:
            if self.ops[s]:
                for o in reversed(self.ops[s]):
                    if not o.dma and o.fn is not None:
                        last.append(o)
                        break
            last.extend(self.dmaops[s][-NSLOT:])
        for s in self.STREAMS:
            self._rec(s, None, [], [], extra=last)

    def mm(self, out, lhsT, rhs, start=True, stop=True):
        self._rec("pe", lambda e: e.matmul(out.ap, lhsT.ap, rhs.ap, start=start, stop=stop), [lhsT, rhs], [out])

    def mmgroup(self, out, pairs):
        rd = []
        for l, r in pairs:
            rd += [l, r]
        n = len(pairs)

        def fn(e):
            ins = None
            for i, (l, r) in enumerate(pairs):
                ins = e.matmul(out.ap, l.ap, r.ap, start=(i == 0), stop=(i == n - 1))
            return ins
        self._rec("pe", fn, rd, [out])

    def transpose(self, out, in_, ident):
        self._rec("pe", lambda e: e.transpose(out.ap, in_.ap, ident.ap), [in_, ident], [out])

    def act(self, out, in_, func, bias=None, scale=1.0, accum=None):
        rd = [in_]
        kw = {}
        if bias is not None:
            if isinstance(bias, TT):
                rd.append(bias)
                kw["bias"] = bias.ap
            else:
                kw["bias"] = bias
        if isinstance(scale, TT):
            rd.append(scale)
            kw["scale"] = scale.ap
        else:
            kw["scale"] = scale
        wr = [out]
        if accum is not None:
            wr.append(accum)
            kw["accum_out"] = accum.ap
        self._rec("act", lambda e: e.activation(out.ap, in_.ap, func, **kw), rd, wr)

    def tt(self, eng, out, a, b, op):
        self._rec(eng, lambda e: e.tensor_tensor(out.ap, a.ap, b.ap, op), [a, b], [out])

    def ts(self, eng, out, a, s1, s2=None, op0=ALU.mult, op1=None):
        rd = [a]
        s1a = s1.ap if isinstance(s1, TT) else s1
        s2a = s2.ap if isinstance(s2, TT) else s2
        if isinstance(s1, TT):
            rd.append(s1)
        if isinstance(s2, TT):
            rd.append(s2)
        kw = {}
        if op1 is not None:
            kw["op1"] = op1
        self._rec(eng, lambda e: e.tensor_scalar(out.ap, a.ap, s1a, s2a, op0, **kw), rd, [out])

    def stt(self, eng, out, a, s, b, op0, op1):
        rd = [a, b]
        sa = s.ap if isinstance(s, TT) else s
        if isinstance(s, TT):
            rd.append(s)
        self._rec("dve", lambda e: e.scalar_tensor_tensor(out.ap, a.ap, sa, b.ap, op0, op1), rd, [out])

    def copy(self, eng, out, a):
        if eng == "act":
            self._rec(eng, lambda e: e.copy(out.ap, a.ap), [a], [out])
        else:
            self._rec(eng, lambda e: e.tensor_copy(out.ap, a.ap), [a], [out])

    def memset(self, eng, out, val):
        self._rec(eng, lambda e: e.memset(out.ap, val), [], [out])

    def reduce(self, eng, out, a, op, axis=AX.X):
        self._rec(eng, lambda e: e.tensor_reduce(out.ap, a.ap, axis, op), [a], [out])

    def rsqrt(self, x):
        self._rec("act", lambda e: e.activation(x.ap, x.ap, AF.Sqrt), [x], [x])
        self._rec("dve", lambda e: e.reciprocal(x.ap, x.ap), [x], [x])

    def recip(self, out, a):
        self._rec("dve", lambda e: e.reciprocal(out.ap, a.ap), [a], [out])

    def dma(self, q, out, in_):
        self._rec(q, lambda e: e.dma_start(out.ap, in_.ap), [in_], [out], dma=True)

    def fence(self, stream, tts):
        self._rec(stream, None, list(tts), [])

    def emit(self):
        nc = self.nc
        for s in self.STREAMS:
            k = 0
            for op in self.ops[s]:
                if op.dma:
                    op.slot = op.didx % NSLOT
                    op.val = 16 * (op.didx // NSLOT + 1)
                elif op.ms:
                    k += 1
                    op.val = k
        sem_ctx = []
        csem = {}
        dsem = {}
        for s in self.STREAMS:
            g = nc.semaphore("c_" + s)
            csem[s] = g.__enter__()
            sem_ctx.append(g)
            if self.ndma[s] > 0:
                for i in range(NSLOT):
                    g = nc.semaphore("d_%s_%d" % (s, i))
                    dsem[(s, i)] = g.__enter__()
                    sem_ctx.append(g)
        ops = self.ops

        def run(stream, e):
            waited = {}
            for op in ops[stream]:
                for d in op.deps:
                    if d.dma:
                        key = ("d", d.stream, d.slot)
                        sem = dsem[(d.stream, d.slot)]
                    else:
                        key = ("c", d.stream)
                        sem = csem[d.stream]
                    if waited.get(key, 0) < d.val:
                        e.wait_ge(sem, d.val)
                        waited[key] = d.val
                if op.fn is None:
                    continue
                ins = op.fn(e)
                if op.dma:
                    ins.then_inc(dsem[(stream, op.slot)], 16)
                elif op.ms:
                    ins.then_inc(csem[stream], 1)

        with nc.Block() as block:
            @block.sync
            def _(e):
                run("sp", e)

            @block.tensor
            def _(e):
                run("pe", e)

            @block.scalar
            def _(e):
                run("act", e)

            @block.vector
            def _(e):
                run("dve", e)

            @block.gpsimd
            def _(e):
                run("pool", e)
        for g in reversed(sem_ctx):
            g.__exit__(None, None, None)


D = 1024
KC = 8
EPS = 1e-6
EVEN_IN = 5664
FFN_DENSE = 2816
NEXP = 8
FEXP = 3584
CTX = 256
GRID_W = 64
LAM_INIT = 0.8 - 0.6 * math.exp(-0.3 * 1)
C_Z, C_XBC, C_DT, C_RQ, C_RK, C_RV, C_RG = 0, 1024, 2560, 2592, 3104, 3616, 4640


def host_consts():
    k = np.arange(128)[:, None]
    l = np.arange(128)[None, :]
    c = {}
    c["ident"] = (k == l).astype(np.float32)
    c["le"] = (k <= l).astype(np.float32)
    c["gt"] = (k > l).astype(np.float32)
    c["ge"] = (k >= l).astype(np.float32)
    c["lt"] = (k < l).astype(np.float32)
    c["ones"] = np.ones((128, 128), np.float32)
    cm = np.concatenate([c[n] for n in ("ident", "le", "gt", "ge", "lt", "ones")], axis=1)
    sel = np.zeros((8, 8, 128), np.float32)
    for e in range(8):
        sel[e, e, :] = 1.0
    return cm, sel.reshape(8, 1024)


def rope_tables(L):
    f32 = np.float32
    inv = (np.float32(10000.0) ** (-np.arange(64, dtype=f32) / f32(64))).astype(f32)
    pos = np.arange(CTX + L, dtype=f32)
    ang = (pos[:, None] * inv[None, :]).astype(f32)
    rcs = np.concatenate([np.cos(ang), np.sin(ang)], axis=1).astype(f32)
    inv16 = (np.float32(10000.0) ** (-np.arange(16, dtype=f32) / f32(16))).astype(f32)
    t = np.arange(L)
    row = (t // GRID_W).astype(f32)
    col = (t % GRID_W).astype(f32)
    ar = (row[:, None] * inv16[None, :]).astype(f32)
    ac = (col[:, None] * inv16[None, :]).astype(f32)
    cos = np.concatenate([np.cos(ar), np.cos(ar), np.cos(ac), np.cos(ac)], axis=1)
    sins = np.concatenate([-np.sin(ar), np.sin(ar), -np.sin(ac), np.sin(ac)], axis=1)
    acs = np.concatenate([cos, sins], axis=1).astype(f32)
    return rcs, acs


def build(L=8192, OWN=4096, stop_after=None, dbg=()):
    nc = bass.Bass("TRN2", target_bir_lowering=False)
    P = Prog(nc)
    T = CTX + L
    NCH = T // 128
    NLB = L // 256
    dbg = set(dbg)

    def din(name, shape, dt=F32):
        return P.dram(name, shape, dt, kind="ExternalInput")

    xT = din("xT", [D, L + 2])
    cT = din("ctxT", [D, CTX + 2])
    cvec = din("cvec", [128, 16])
    sel = din("sel", [128, 2])
    consts = din("consts", [128, 768])
    selmat = din("selmat", [8, 1024])
    rcs_d = din("rcs", [T, 128])
    acs_d = din("acs", [L, 128])
    w_mod = [din("w_mod0", [D, 6 * D]), din("w_mod1", [D, 6 * D])]
    b_mod = [din("b_mod0", [128, 48]), din("b_mod1", [128, 48])]
    norms = din("norms", [128, 32])
    w_in0 = din("w_in0", [D, EVEN_IN])
    convw = din("convw", [128, 12, 4])
    rowp = din("rowp", [1, 2560])
    w_out0 = din("w_out0", [2048, D])
    ffg = din("ffg", [D, FFN_DENSE])
    ffu = din("ffu", [D, FFN_DENSE])
    ffd = din("ffd", [FFN_DENSE, D])
    w_in1 = din("w_in1", [D, 3072])
    w_out1 = din("w_out1", [D, D])
    wr = din("router", [D, NEXP])
    eg = din("eg", [NEXP, D, FEXP])
    eu = din("eu", [NEXP, D, FEXP])
    ed = din("ed", [NEXP, FEXP, D])
    outT = P.dram("outT", [D, OWN], F32, kind="ExternalOutput")

    def scratch(name, shape, dt=F32):
        return P.dram(name, shape, dt, kind=("ExternalOutput" if name in dbg else "Internal"))

    r_zr = scratch("r_zr", [NCH, 128, 2048], BF16)
    r_xv = scratch("r_xv", [NCH, 128, 2048], BF16)
    r_kt = scratch("r_kt", [NCH, 128, 768], BF16)
    r_fm = scratch("r_fm", [NCH, 128, 12, 128], BF16)
    r_dt = scratch("r_dt", [NCH, 128, 64], F32)
    r_yf = scratch("r_yf", [NCH, 128, 2048], F32)
    x_mid = scratch("x_mid", [NCH, 128, 8, 128], F32)
    x_l1 = scratch("x_l1", [NCH, 128, 8, 128], F32)
    Kd = scratch("Kd", [8, 128, T], BF16)
    Vd = scratch("Vd", [8, 128, NCH, 130], BF16)
    Qd = scratch("Qd", [8, 128, L], BF16)
    Od = scratch("Od", [128, 8, OWN], BF16)

    cst = P.sb("cst", [128, 768], F32)
    P.dma("sp", cst, consts)
    ident_f = cst[:, 0:128]
    m_le, m_gt, m_ge, m_lt, ones_f = (cst[:, 128 * i:128 * (i + 1)] for i in range(1, 6))
    cstb = P.sb("cstb", [128, 768], BF16)
    P.copy("dve", cstb, cst)
    ident_b = cstb[:, 0:128]
    sel_sb = P.sb("sel_sb", [128, 2], F32)
    P.dma("sp", sel_sb, sel)
    rows = P.sb("rows", [128, 2560], F32)
    P.dma("sp", rows, TT(rowp.ap.rearrange("a b -> (a b)").partition_broadcast(128), rowp.tok))
    norm_sb = P.sb("norm_sb", [128, 32], F32)
    P.dma("sp", norm_sb, norms)
    modfm = [P.sb("modfm0", [128, 48, 2], F32), P.sb("modfm1", [128, 48, 2], F32)]
    P.banks = [P.ps("bank%d" % i, [128, 512], F32) for i in range(8)]

    def bank_bf(b):
        a = b.ap
        return TT(a.bitcast(BF16), b.tok)

    AB = [P.sb("AB%d" % i, [128, 8, 2, 2, 2]) for i in range(2)]
    G = [P.sb("G%d" % i, [128, 8, 2, 2]) for i in range(2)]
    P.open_scope()
    cv = P.sb("cv", [128, 16], F32)
    P.dma("sp", cv, cvec)
    scv = P.sb("scv", [128, 8, 2], F32)
    P.act(scv[:, :, 0], cv[:, 0:8], AF.Silu)
    P.act(scv[:, :, 1], cv[:, 8:16], AF.Silu)
    wm = [P.sb("wm%d" % i, [128, 8, 512], F32) for i in range(2)]
    bm_sb = P.sb("bm_sb", [128, 2, 48], F32)
    P.dma("sp", bm_sb[:, 0, :], b_mod[0])
    P.dma("sp", bm_sb[:, 1, :], b_mod[1])
    it = 0
    for lyr in range(2):
        for cg in range(12):
            w = wm[it % 2]
            it += 1
            P.dma("sp", w, w_mod[lyr][:, cg * 512:(cg + 1) * 512].re("(k p) f -> p k f", p=128))
            pb = P.bank()
            for j in range(4):
                P.mmgroup(pb[:, 2 * j:2 * j + 2], [(w[:, k, j * 128:(j + 1) * 128], scv[:, k, :]) for k in range(8)])
            for j in range(4):
                ch = cg * 4 + j
                P.ts("dve", modfm[lyr][:, ch, :], pb[:, 2 * j:2 * j + 2], bm_sb[:, lyr, ch:ch + 1], None, op0=ALU.add)
    for lyr in range(2):
        for w in range(2):
            for n in range(2):
                shift = modfm[lyr][:, (3 * n) * 8:(3 * n) * 8 + 8, w]
                scale = modfm[lyr][:, (3 * n + 1) * 8:(3 * n + 1) * 8 + 8, w]
                gate = modfm[lyr][:, (3 * n + 2) * 8:(3 * n + 2) * 8 + 8, w]
                gain = norm_sb[:, (2 * lyr + n) * 8:(2 * lyr + n) * 8 + 8]
                P.stt("dve", AB[lyr][:, :, w, n, 0], scale, 1.0, gain, ALU.add, ALU.mult)
                P.copy("dve", AB[lyr][:, :, w, n, 1], shift)
                P.copy("dve", G[lyr][:, :, w, n], gate)
    P.close_scope()

    def rms_modulate(xin, ncol, Atab, out_bf, tmp, out_f32=None, eng="pool"):
        sq = tmp["sq"]
        P.act(sq[:, :, 0:ncol], xin, AF.Square)
        pb = P.bank()
        P.mmgroup(pb[:, 0:ncol], [(ones_f, sq[:, k, 0:ncol]) for k in range(8)])
        rstd = tmp["rstd"]
        P.ts("dve", rstd[:, 0:ncol], pb[:, 0:ncol], 1.0 / D, EPS, op0=ALU.mult, op1=ALU.add)
        P.rsqrt(rstd[:, 0:ncol])
        P.tt("dve", sq[:, :, 0:ncol], xin, bc(rstd[:, 0:ncol], [(0, 8), (1, ncol)]), ALU.mult)
        for k in range(8):
            if eng == "act" or (eng == "mix" and k % 2 == 0):
                P.act(out_bf[:, k, :], sq[:, k, 0:ncol], AF.Identity, bias=Atab[:, k, 1:2], scale=Atab[:, k, 0:1])
            else:
                P.ts("pool", out_bf[:, k, :], sq[:, k, 0:ncol], Atab[:, k, 0:1], Atab[:, k, 1:2], op0=ALU.mult, op1=ALU.add)
            if out_f32 is not None:
                P.ts("pool", out_f32[:, k, :], sq[:, k, 0:ncol], Atab[:, k, 0:1], Atab[:, k, 1:2], op0=ALU.mult, op1=ALU.add)

    r_dtb = rows[:, 0:32]
    r_alog = rows[:, 32:64]
    r_retd = rows[:, 64:72]
    r_dsk = rows[:, 72:88]
    r_ssdn = rows[:, 88:1112]
    r_qn = rows[:, 1112:1176]
    r_kn = rows[:, 1176:1240]
    r_lam = rows[:, 1240:1496]
    r_subln = rows[:, 1496:1624]
    ea = P.sb("ea", [128, 32], F32)
    P.act(ea, r_alog, AF.Exp)
    nla_ret = P.sb("nla_ret", [128, 8], F32)
    P.act(nla_ret, r_retd, AF.Exp)
    P.ts("dve", nla_ret, nla_ret, -1.0, None, op0=ALU.mult)

    P.open_scope()
    w0 = P.sb("w0", [128, 8, EVEN_IN], BF16)
    for k in range(8):
        for c0 in range(0, EVEN_IN, 1888):
            P.dma("pool", w0[:, k, c0:c0 + 1888], w_in0[k * 128:(k + 1) * 128, c0:c0 + 1888])
    cw = P.sb("cw", [128, 12, 4], F32)
    P.dma("sp", cw, convw)
    xin = [P.sb("xin%d" % i, [128, 8, 258], F32) for i in range(2)]
    xbr = P.sb("xbr", [128, 12, 258], F32)
    tmp1 = {"sq": xbr[:, 0:8, :], "rstd": P.sb("rstd1", [128, 258], F32)}
    hbs = [P.sb("hb%d" % i, [128, 8, 258], BF16) for i in range(2)]
    xbcs = [P.sb("xbc%d" % i, [128, 12, 256], BF16) for i in range(2)]
    cvts = [P.sb("cvt%d" % i, [128, 256], F32) for i in range(2)]
    o_zr = P.sb("o_zr", [128, 2, 2048], BF16)
    o_xv = P.sb("o_xv", [128, 2, 2048], BF16)
    o_kt = P.sb("o_kt", [128, 2, 768], BF16)
    o_fm = P.sb("o_fm", [128, 2, 12, 128], BF16)
    o_dt = P.sb("o_dt", [128, 2, 64], F32)
    rtabs = [P.sb("rtab%d" % i, [128, 2, 128], F32) for i in range(3)]
    rt1 = P.sb("rt1", [128, 4, 128], F32)
    rt2 = P.sb("rt2", [128, 4, 128], F32)
    rqk = P.sb("rqk", [128, 2, 512], BF16)
    sp1 = P.sb("sp1", [128, 32], F32)
    sp2 = P.sb("sp2", [128, 32], F32)

    blocks = [("c", 0)] + [("l", i) for i in range(NLB)]

    def binfo(bj):
        kind_, i_ = blocks[bj]
        if kind_ == "c":
            return 0, True, True, 1
        return CTX + i_ * 256, (i_ == 0), (i_ == NLB - 1), 0

    def load1(bj):
        kind_, i_ = blocks[bj]
        if kind_ == "c":
            P.dma("sp", xin[bj % 2], cT.re("(k p) t -> p k t", p=128))
        else:
            P.dma("sp", xin[bj % 2], xT[:, i_ * 256:i_ * 256 + 258].re("(k p) t -> p k t", p=128))
        t0_ = binfo(bj)[0]
        P.dma("sp", rtabs[bj % 3], rcs_d[t0_:t0_ + 256, :].re("(t p) c -> p t c", p=128))

    def stageA(bj):
        tok0, first, last, which = binfo(bj)
        xi, hb, xbc = xin[bj % 2], hbs[bj % 2], xbcs[bj % 2]
        rms_modulate(xi, 258, AB[0][:, :, which, 0, :], hb, tmp1, eng="act")
        for c in range(12):
            pb = P.bank()
            P.mmgroup(pb[:, 0:258], [(w0[:, k, C_XBC + c * 128:C_XBC + (c + 1) * 128], hb[:, k, :]) for k in range(8)])
            P.copy("act", xbr[:, c, :], pb[:, 0:258])
        if first:
            P.memset("pool", xbr[:, :, 0:1], 0.0)
        if last:
            P.memset("pool", xbr[:, :, 257:258], 0.0)
        for c in range(12):
            cvt = cvts[c % 2]
            P.act(cvt, xbr[:, c, 0:256], AF.Identity, scale=cw[:, c, 0:1])
            P.stt("dve", cvt, xbr[:, c, 1:257], cw[:, c, 1:2], cvt, ALU.mult, ALU.add)
            P.stt("dve", cvt, xbr[:, c, 2:258], cw[:, c, 2:3], cvt, ALU.mult, ALU.add)
            P.act(xbc[:, c, :], cvt, AF.Silu, bias=cw[:, c, 3:4])

    def stageB(bj):
        tok0, first, last, which = binfo(bj)
        hb, xbc, rtab = hbs[bj % 2], xbcs[bj % 2], rtabs[bj % 3]
        ch0 = tok0 // 128
        for t in range(2):
            P.copy("act", o_fm[:, t, 0:4, :], xbc[:, 8:12, t * 128:(t + 1) * 128])
        for t in range(2):
            lt = [hb[:, k, 1 + t * 128:1 + (t + 1) * 128] for k in range(8)]

            def proj(c0, n):
                pb = P.bank()
                P.mmgroup(pb[:, 0:n], [(lt[k], w0[:, k, c0:c0 + n]) for k in range(8)])
                return pb
            for j in range(2):
                pb = proj(C_Z + j * 512, 512)
                P.copy("act", o_zr[:, t, j * 512:(j + 1) * 512], pb)
            for j in range(2):
                pb = proj(C_RG + j * 512, 512)
                P.copy("act", o_zr[:, t, 1024 + j * 512:1024 + (j + 1) * 512], pb)
            for j in range(2):
                pb = proj(C_RV + j * 512, 512)
                P.copy("act", o_xv[:, t, 1024 + j * 512:1024 + (j + 1) * 512], pb)
            pb = proj(C_DT, 32)
            P.tt("dve", sp1, pb[:, 0:32], r_dtb, ALU.add)
            P.act(sp2, sp1, AF.Abs)
            P.act(sp2, sp2, AF.Exp, scale=-1.0)
            P.act(sp2, sp2, AF.Ln, bias=1.0)
            P.stt("dve", o_dt[:, t, 0:32], sp1, 0.0, sp2, ALU.max, ALU.add)
            P.tt("dve", sp1, o_dt[:, t, 0:32], ea, ALU.mult)
            P.ts("dve", o_dt[:, t, 32:64], sp1, -1.0, None, op0=ALU.mult)
            for qi, c0 in enumerate((C_RQ, C_RK)):
                pb = proj(c0, 512)
                pv = pb.re("p (h d) -> p h d", h=4)
                cos2 = bc(rtab[:, t, 0:64], [(0, 4), (0, 2), (1, 64)])
                P.tt("dve", rt1.re("p h (a d) -> p h a d", a=2), pv.re("p h (a d) -> p h a d", a=2), cos2, ALU.mult)
                sin1 = bc(rtab[:, t, 64:128], [(0, 4), (1, 64)])
                P.tt("dve", rt2[:, :, 0:64], pv[:, :, 64:128], sin1, ALU.mult)
                P.tt("dve", rt2[:, :, 64:128], pv[:, :, 0:64], sin1, ALU.mult)
                rv = rqk[:, qi, :].re("p (h d) -> p h d", h=4)
                P.tt("dve", rt1[:, :, 0:64], rt1[:, :, 0:64], rt2[:, :, 0:64], ALU.subtract)
                P.tt("dve", rt1[:, :, 64:128], rt1[:, :, 64:128], rt2[:, :, 64:128], ALU.add)
                P.act(rv, rt1, AF.Copy, scale=(1.0 if qi == 0 else 128.0 ** -0.5))
            P.copy("act", o_kt[:, t, 256:768], rqk[:, 1, :])
            pb = P.bank()
            pbb = bank_bf(pb)
            for qi in range(2):
                for h in range(4):
                    P.transpose(pbb[:, (qi * 4 + h) * 128:(qi * 4 + h + 1) * 128], rqk[:, qi, h * 128:(h + 1) * 128], ident_b)
            P.copy("dve", o_fm[:, t, 4:12, :], pbb.re("p (n t) -> p n t", t=128))
            pb = P.bank()
            pbb = bank_bf(pb)
            for c in range(8):
                P.transpose(pbb[:, c * 128:(c + 1) * 128], xbc[:, c, t * 128:(t + 1) * 128], ident_b)
            P.copy("dve", o_xv[:, t, 0:1024], pbb)
            pb = P.bank()
            pbb = bank_bf(pb)
            for c in range(2):
                P.transpose(pbb[:, c * 128:(c + 1) * 128], xbc[:, 8 + c, t * 128:(t + 1) * 128], ident_b)
            P.copy("dve", o_kt[:, t, 0:256], pbb[:, 0:256])
        for t in range(2):
            P.dma("sp", r_zr[ch0 + t], o_zr[:, t, :])
            P.dma("sp", r_xv[ch0 + t], o_xv[:, t, :])
            P.dma("sp", r_kt[ch0 + t], o_kt[:, t, :])
            P.dma("sp", r_fm[ch0 + t], o_fm[:, t])
            P.dma("sp", r_dt[ch0 + t], o_dt[:, t, :])

    nblk = len(blocks)
    load1(0)
    if nblk > 1:
        load1(1)
    stageA(0)
    for bi in range(nblk):
        if bi + 2 < nblk:
            load1(bi + 2)
        if bi + 1 < nblk:
            stageA(bi + 1)
        stageB(bi)
    P.close_scope()
    if stop_after == "P1":
        return _finish(P, nc, [r_zr, r_xv, r_kt, r_fm, r_dt])

    P.open_scope()
    wo0 = P.sb("wo0", [128, 16, D], BF16)
    for k in range(16):
        P.dma("pool", wo0[:, k, :], w_out0[k * 128:(k + 1) * 128, :])
    Er = P.sb("Er", [128, 2, 3, 4], F32)
    Dret = P.sb("Dret", [128, 2, 4, 128], F32)
    lmr = P.sb("lmr", [128, 4, 128], F32)
    for d in range(2):
        la = nla_ret[:, d * 4:(d + 1) * 4]
        pb = P.bank()
        mA, mT = (m_le, m_gt) if d == 0 else (m_ge, m_lt)
        P.mm(pb[:, 0:4], mA, la)
        P.mm(pb[:, 4:8], mT, la)
        P.mm(pb[:, 8:12], ones_f, la)
        P.act(Er[:, d].re("p a h -> p (a h)"), pb[:, 0:12], AF.Exp)
        mS, mR, mM = (m_gt, m_le, m_le) if d == 0 else (m_lt, m_ge, m_ge)
        P.tt("dve", lmr, bc(mS, [(0, 4), (1, 128)]), bc(la, [(1, 4), (0, 128)]), ALU.mult)
        pb = P.bank()
        for h in range(4):
            P.mm(pb[:, h * 128:(h + 1) * 128], lmr[:, h, :], mR)
        P.act(Dret[:, d].re("p h l -> p (h l)"), pb, AF.Exp)
        P.tt("dve", Dret[:, d], Dret[:, d], bc(mM, [(0, 4), (1, 128)]), ALU.mult)

    Hs = P.sb("Hs", [128, 1024], F32)
    Hr = P.sb("Hr", [128, 1024], F32)
    Hsb = P.sb("Hsb", [128, 1024], BF16)
    Hrb = P.sb("Hrb", [128, 1024], BF16)
    i_xv = [P.sb("i_xv%d" % i, [128, 2048], BF16) for i in range(2)]
    i_kt = [P.sb("i_kt%d" % i, [128, 768], BF16) for i in range(2)]
    i_fm = [P.sb("i_fm%d" % i, [128, 12, 128], BF16) for i in range(2)]
    i_dt = [P.sb("i_dt%d" % i, [128, 64], F32) for i in range(2)]
    i_zr = [P.sb("i_zr%d" % i, [128, 2048], BF16) for i in range(2)]
    i_yf = [P.sb("i_yf%d" % i, [128, 2048], F32) for i in range(2)]
    i_x = [P.sb("i_x%d" % i, [128, 8, 128], F32) for i in range(2)]
    E = P.sb("E", [128, 3, 16], F32)
    scm = P.sb("scm", [128, 2, 128], F32)
    Lm = P.sb("Lm", [128, 16, 128], F32)
    expD = P.sb("expD", [128, 16, 128], F32)
    MT = P.sb("MT", [128, 16, 128], BF16)
    MTr = P.sb("MTr", [128, 4, 128], BF16)
    xdt = P.sb("xdt", [128, 1024], BF16)
    xw = P.sb("xw", [128, 1024], BF16)
    rvw = P.sb("rvw", [128, 1024], BF16)
    wv = P.sb("wv", [128, 16], F32)
    ytmp = P.sb("ytmp", [128, 1024], F32)
    yo = [P.sb("yo%d" % i, [128, 2048], F32) for i in range(2)]
    sz = P.sb("sz", [128, 1024], F32)
    junk = P.sb("junk", [128, 1024], F32)
    ss = P.sb("ss", [128, 8], F32)
    ycat = P.sb("ycat", [128, 2048], BF16)
    ycT = P.sb("ycT", [128, 16, 128], BF16)
    xo = P.sb("xo", [128, 8, 128], F32)

    fwd_order = list(range(NCH))
    bwd_order = [1, 0] + list(range(NCH - 1, 1, -1))

    for d in range(2):
        order = fwd_order if d == 0 else bwd_order
        P.memset("dve", Hs, 0.0)
        P.memset("dve", Hr, 0.0)
        P.memset("pool", Hsb, 0.0)
        P.memset("pool", Hrb, 0.0)
        mA, mT = (m_le, m_gt) if d == 0 else (m_ge, m_lt)
        mS, mR, mM = (m_gt, m_le, m_le) if d == 0 else (m_lt, m_ge, m_ge)
        def load_sw(cj):
            ch_ = order[cj]
            b_ = cj % 2
            P.dma("sp", i_xv[b_], r_xv[ch_])
            P.dma("sp", i_kt[b_], r_kt[ch_])
            P.dma("sp", i_fm[b_], r_fm[ch_])
            P.dma("sp", i_dt[b_], r_dt[ch_])

        def load_fin(cj):
            ch_ = order[cj]
            b_ = cj % 2
            P.dma("sp", i_zr[b_], r_zr[ch_])
            P.dma("sp", i_yf[b_], r_yf[ch_])
            if ch_ < 2:
                P.dma("sp", i_x[b_], cT[:, 1 + ch_ * 128:1 + (ch_ + 1) * 128].re("(k p) t -> p k t", p=128))
            else:
                P.dma("sp", i_x[b_], xT[:, 1 + (ch_ - 2) * 128:1 + (ch_ - 1) * 128].re("(k p) t -> p k t", p=128))

        def finish(cj):
            ch = order[cj]
            b = cj % 2
            yout = yo[cj % 2]
            zr = i_zr[b]
            which = 1 if ch < 2 else 0
            P.tt("dve", yout, yout, i_yf[b], ALU.add)
            ys = yout[:, 0:1024]
            yr = yout[:, 1024:2048]
            P.act(sz, zr[:, 0:1024], AF.Silu)
            P.tt("dve", ys, ys, sz, ALU.mult)
            P.act(junk, ys, AF.Square)
            P.reduce("dve", ss[:, 0:1], junk, ALU.add)
            P.ts("dve", ss[:, 1:2], ss[:, 0:1], 1.0 / 1024, EPS, op0=ALU.mult, op1=ALU.add)
            P.rsqrt(ss[:, 1:2])
            P.stt("dve", ycat[:, 0:1024], ys, ss[:, 1:2], r_ssdn, ALU.mult, ALU.mult)
            P.act(junk, yr, AF.Square)
            P.reduce("dve", ss[:, 2:6], junk.re("p (h d) -> p h d", h=4), ALU.add)
            P.ts("dve", ss[:, 2:6], ss[:, 2:6], 1.0 / 256, EPS, op0=ALU.mult, op1=ALU.add)
            P.rsqrt(ss[:, 2:6])
            P.act(sz, zr[:, 1024:2048], AF.Silu)
            P.tt("dve", yr.re("p (h d) -> p h d", h=4), yr.re("p (h d) -> p h d", h=4), bc(ss[:, 2:6], [(1, 4), (0, 256)]), ALU.mult)
            P.tt("dve", ycat[:, 1024:2048], yr, sz, ALU.mult)
            for q in range(2):
                pb = P.bank()
                pbb = bank_bf(pb)
                for j in range(8):
                    P.transpose(pbb[:, j * 128:(j + 1) * 128], ycat[:, (q * 8 + j) * 128:(q * 8 + j + 1) * 128], ident_b)
                P.copy("act", ycT[:, q * 8:(q + 1) * 8, :].re("p n t -> p (n t)"), pbb)
            for q in range(2):
                pb = P.bank()
                for j in range(4):
                    dc = q * 4 + j
                    P.mmgroup(pb[:, j * 128:(j + 1) * 128], [(wo0[:, k, dc * 128:(dc + 1) * 128], ycT[:, k, :]) for k in range(16)])
                for j in range(4):
                    dc = q * 4 + j
                    P.stt("dve", xo[:, dc, :], pb[:, j * 128:(j + 1) * 128], G[0][:, dc, which, 0:1], i_x[b][:, dc, :], ALU.mult, ALU.add)
            P.dma("sp", x_mid[ch], xo)

        load_sw(0)
        for ci, ch in enumerate(order):
            b = ci % 2
            xv, kt, fm, dtt = i_xv[b], i_kt[b], i_fm[b], i_dt[b]
            if ci + 1 < len(order):
                load_sw(ci + 1)
            if d == 1:
                load_fin(ci)
            la = dtt[:, 32 + d * 16:32 + (d + 1) * 16]
            dtd = dtt[:, d * 16:(d + 1) * 16]
            xs = xv[:, 0:1024]
            rvv = xv[:, 1024:2048]
            yout = yo[ci % 2]
            pb = P.bank()
            P.mm(pb[:, 0:16], mA, la)
            P.mm(pb[:, 16:32], mT, la)
            P.mm(pb[:, 32:48], ones_f, la)
            P.act(E.re("p a h -> p (a h)"), pb[:, 0:48], AF.Exp)
            pb = P.bank()
            for g in range(2):
                P.mm(pb[:, g * 128:(g + 1) * 128], fm[:, g, :], fm[:, 2 + g, :])
            P.tt("dve", scm, pb[:, 0:256].re("p (g l) -> p g l", g=2), bc(mM, [(0, 2), (1, 128)]), ALU.mult)
            P.tt("pool", Lm, bc(mS, [(0, 16), (1, 128)]), bc(la, [(1, 16), (0, 128)]), ALU.mult)
            for q in range(4):
                pb = P.bank()
                for j in range(4):
                    P.mm(pb[:, j * 128:(j + 1) * 128], Lm[:, q * 4 + j, :], mR)
                P.act(expD[:, q * 4:(q + 1) * 4, :].re("p h l -> p (h l)"), pb, AF.Exp)
            for g in range(2):
                P.tt("dve", MT[:, g * 8:(g + 1) * 8, :], expD[:, g * 8:(g + 1) * 8, :], bc(scm[:, g, :], [(0, 8), (1, 128)]), ALU.mult)
            P.tt("pool", xdt.re("p (h d) -> p h d", h=16), xs.re("p (h d) -> p h d", h=16), bc(dtd, [(1, 16), (0, 64)]), ALU.mult)
            pd = [P.bank(), P.bank()]
            for h in range(16):
                P.mm(pd[h // 8][:, (h % 8) * 64:(h % 8 + 1) * 64], MT[:, h, :], xdt[:, h * 64:(h + 1) * 64])
            for g in range(2):
                po = P.bank()
                P.mm(po, fm[:, 2 + g, :], Hsb[:, g * 512:(g + 1) * 512])
                P.tt("dve", ytmp[:, g * 512:(g + 1) * 512].re("p (h d) -> p h d", h=8), po.re("p (h d) -> p h d", h=8),
                     bc(E[:, 0, g * 8:(g + 1) * 8], [(1, 8), (0, 64)]), ALU.mult)
                P.tt("dve", yout[:, g * 512:(g + 1) * 512], ytmp[:, g * 512:(g + 1) * 512], pd[g], ALU.add)
            P.tt("dve", wv, dtd, E[:, 1, :], ALU.mult)
            P.tt("pool", xw.re("p (h d) -> p h d", h=16), xs.re("p (h d) -> p h d", h=16), bc(wv, [(1, 16), (0, 64)]), ALU.mult)
            P.tt("dve", Hs.re("p (h d) -> p h d", h=16), Hs.re("p (h d) -> p h d", h=16), bc(E[:, 2, :], [(1, 16), (0, 64)]), ALU.mult)
            for g in range(2):
                pS = P.bank()
                P.mm(pS, kt[:, g * 128:(g + 1) * 128], xw[:, g * 512:(g + 1) * 512])
                P.tt("dve", Hs[:, g * 512:(g + 1) * 512], Hs[:, g * 512:(g + 1) * 512], pS, ALU.add)
            P.copy("act", Hsb, Hs)
            pb = P.bank()
            for h in range(4):
                P.mm(pb[:, h * 128:(h + 1) * 128], fm[:, 8 + h, :], fm[:, 4 + h, :])
            P.tt("dve", MTr, pb.re("p (h l) -> p h l", h=4), Dret[:, d], ALU.mult)
            pd = [P.bank(), P.bank()]
            for h in range(4):
                P.mm(pd[h // 2][:, (h % 2) * 256:(h % 2 + 1) * 256], MTr[:, h, :], rvv[:, h * 256:(h + 1) * 256])
            for g in range(2):
                po = P.bank()
                for hh in range(2):
                    h = g * 2 + hh
                    P.mm(po[:, hh * 256:(hh + 1) * 256], fm[:, 4 + h, :], Hrb[:, h * 256:(h + 1) * 256])
                P.tt("dve", ytmp[:, g * 512:(g + 1) * 512].re("p (h d) -> p h d", h=2), po.re("p (h d) -> p h d", h=2),
                     bc(Er[:, d, 0, g * 2:(g + 1) * 2], [(1, 2), (0, 256)]), ALU.mult)
                P.tt("dve", yout[:, 1024 + g * 512:1024 + (g + 1) * 512], ytmp[:, g * 512:(g + 1) * 512], pd[g], ALU.add)
            P.tt("pool", rvw.re("p (h d) -> p h d", h=4), rvv.re("p (h d) -> p h d", h=4), bc(Er[:, d, 1, :], [(1, 4), (0, 256)]), ALU.mult)
            P.tt("dve", Hr.re("p (h d) -> p h d", h=4), Hr.re("p (h d) -> p h d", h=4), bc(Er[:, d, 2, :], [(1, 4), (0, 256)]), ALU.mult)
            for g in range(2):
                pS = P.bank()
                for hh in range(2):
                    h = g * 2 + hh
                    P.mm(pS[:, hh * 256:(hh + 1) * 256], kt[:, 256 + h * 128:256 + (h + 1) * 128], rvw[:, h * 256:(h + 1) * 256])
                P.tt("dve", Hr[:, g * 512:(g + 1) * 512], Hr[:, g * 512:(g + 1) * 512], pS, ALU.add)
            P.copy("act", Hrb, Hr)
            if d == 0:
                P.dma("sp", r_yf[ch], yout)
                continue
            P.tt("pool", ytmp.re("p (h d) -> p h d", h=16), xs.re("p (h d) -> p h d", h=16), bc(r_dsk, [(1, 16), (0, 64)]), ALU.mult)
            P.tt("dve", yout[:, 0:1024], yout[:, 0:1024], ytmp, ALU.add)
            if ci >= 1:
                finish(ci - 1)
        if d == 1:
            finish(len(order) - 1)
    P.close_scope()
    if stop_after == "P3":
        return _finish(P, nc, [x_mid, r_yf])

    P.open_scope()
    NF = FFN_DENSE // 128
    wg = P.sb("wg", [128, 8, FFN_DENSE], BF16)
    wu = P.sb("wu", [128, 8, FFN_DENSE], BF16)
    wd = P.sb("wd", [128, NF, D], BF16)
    for k in range(8):
        P.dma("pool", wg[:, k, :], ffg[k * 128:(k + 1) * 128, :])
        P.dma("pool", wu[:, k, :], ffu[k * 128:(k + 1) * 128, :])
    for f in range(NF):
        P.dma("pool", wd[:, f, :], ffd[f * 128:(f + 1) * 128, :])
    xb4 = [P.sb("xb4_%d" % i, [128, 8, 256], F32) for i in range(2)]
    tmp4 = {"sq": P.sb("sq4", [128, 8, 256], F32), "rstd": P.sb("rstd4", [128, 256], F32)}
    h4 = P.sb("h4", [128, 8, 256], BF16)
    a4 = P.sb("a4", [128, NF, 256], BF16)
    sg4 = [P.sb("sg4_%d" % i, [128, 256], F32) for i in range(2)]
    xo4 = [P.sb("xo4_%d" % i, [128, 8, 256], F32) for i in range(1)]
    def load4(bj):
        for t in range(2):
            P.dma("sp", xb4[bj % 2][:, :, t * 128:(t + 1) * 128], x_mid[bj * 2 + t])
    load4(0)
    for bi in range(NCH // 2):
        x4 = xb4[bi % 2]
        which = 1 if bi == 0 else 0
        if bi + 1 < NCH // 2:
            load4(bi + 1)
        rms_modulate(x4, 256, AB[0][:, :, which, 1, :], h4, tmp4, eng="act")
        for f in range(NF):
            pb = P.bank()
            P.mmgroup(pb[:, 0:256], [(wg[:, k, f * 128:(f + 1) * 128], h4[:, k, :]) for k in range(8)])
            P.mmgroup(pb[:, 256:512], [(wu[:, k, f * 128:(f + 1) * 128], h4[:, k, :]) for k in range(8)])
            sg = sg4[f % 2]
            P.act(sg, pb[:, 0:256], AF.Silu)
            P.tt("dve", a4[:, f, :], sg, pb[:, 256:512], ALU.mult)
        xo_ = xo4[0]
        for q in range(4):
            pb = P.bank()
            for j in range(2):
                dc = q * 2 + j
                P.mmgroup(pb[:, j * 256:(j + 1) * 256], [(wd[:, f, dc * 128:(dc + 1) * 128], a4[:, f, :]) for f in range(NF)])
            for j in range(2):
                dc = q * 2 + j
                P.stt("dve", xo_[:, dc, :], pb[:, j * 256:(j + 1) * 256], G[0][:, dc, which, 1:2], x4[:, dc, :], ALU.mult, ALU.add)
        for t in range(2):
            P.dma("sp", x_l1[bi * 2 + t], xo_[:, :, t * 128:(t + 1) * 128])
    P.close_scope()
    if stop_after == "P4":
        return _finish(P, nc, [x_l1])

    P.open_scope()
    w1 = P.sb("w1", [128, 8, 3072], BF16)
    for k in range(8):
        P.dma("pool", w1[:, k, :], w_in1[k * 128:(k + 1) * 128, :])
    xb5 = [P.sb("xb5_%d" % i, [128, 8, 256], F32) for i in range(2)]
    tmp5 = {"sq": P.sb("sq5", [128, 8, 256], F32), "rstd": P.sb("rstd5", [128, 256], F32)}
    h5 = P.sb("h5", [128, 8, 256], BF16)
    atab = P.sb("atab", [128, 2, 128], F32)
    qsq = P.sb("qsq", [128, 1024], F32)
    qn = P.sb("qn", [128, 1024], F32)
    q1 = P.sb("q1", [128, 1024], F32)
    q2 = P.sb("q2", [128, 1024], F32)
    ss5 = P.sb("ss5", [128, 16], F32)
    qkb = P.sb("qkb", [128, 2, 1024], BF16)
    qkT = P.sb("qkT", [128, 2, 8, 256], BF16)
    v5 = [P.sb("v5_%d" % i, [128, 2, 8, 130], BF16) for i in range(2)]
    for i in range(2):
        P.memset("dve", v5[i], 1.0)
    qg = P.sb("qg", [128, 64], F32)
    P.ts("dve", qg, r_qn, 64.0 ** -0.5, None, op0=ALU.mult)
    atabs = [atab, P.sb("atab2", [128, 2, 128], F32), P.sb("atab3", [128, 2, 128], F32)]
    h5s = [h5, P.sb("h5b", [128, 8, 256], BF16)]
    qsqs = [qsq, P.sb("qsq_b", [128, 1024], F32)]
    qns = [qn, P.sb("qn_b", [128, 1024], F32)]
    q1s = [q1, P.sb("q1_b", [128, 1024], F32)]
    q2s = [q2, P.sb("q2_b", [128, 1024], F32)]
    ss5s = [ss5, P.sb("ss5_b", [128, 16], F32)]
    NB5 = NCH // 2

    def load5(bj):
        for t in range(2):
            P.dma("sp", xb5[bj % 2][:, :, t * 128:(t + 1) * 128], x_l1[bj * 2 + t])
        if bj > 0:
            l0 = (bj - 1) * 256
            P.dma("sp", atabs[bj % 3], acs_d[l0:l0 + 256, :].re("(t p) c -> p t c", p=128))

    def stage5A(bj):
        which = 1 if bj == 0 else 0
        rms_modulate(xb5[bj % 2], 256, AB[1][:, :, which, 0, :], h5s[bj % 2], tmp5, eng="act")

    def stage5B(bi):
        h5 = h5s[bi % 2]
        atab = atabs[bi % 3]
        which = 1 if bi == 0 else 0
        vv = v5[bi % 2]
        qis = [1] if which == 1 else [0, 1]
        for t in range(2):
            lt = [h5[:, k, t * 128:(t + 1) * 128] for k in range(8)]
            pbs = {}
            for qi in qis:
                pbs[qi] = []
                for j in range(2):
                    pb = P.bank()
                    c0 = qi * 1024 + j * 512
                    P.mmgroup(pb, [(lt[k], w1[:, k, c0:c0 + 512]) for k in range(8)])
                    pbs[qi].append(pb)
            for qi in qis:
                for j in range(2):
                    P.act(qsqs[qi][:, j * 512:(j + 1) * 512], pbs[qi][j], AF.Square)
            for qi in qis:
                P.reduce("dve", ss5s[qi], qsqs[qi].re("p (g d) -> p g d", d=64), ALU.add)
                P.ts("dve", ss5s[qi], ss5s[qi], 1.0 / 64, EPS, op0=ALU.mult, op1=ALU.add)
            for qi in qis:
                P.rsqrt(ss5s[qi])
            for qi in qis:
                for j in range(2):
                    P.tt("dve", qns[qi][:, j * 512:(j + 1) * 512].re("p (g d) -> p g d", d=64), pbs[qi][j].re("p (g d) -> p g d", d=64),
                         bc(ss5s[qi][:, j * 8:(j + 1) * 8], [(1, 8), (0, 64)]), ALU.mult)
            pvs = []
            for j in range(2):
                pb = P.bank()
                c0 = 2048 + j * 512
                P.mmgroup(pb, [(lt[k], w1[:, k, c0:c0 + 512]) for k in range(8)])
                pvs.append(pb)
            for qi in qis:
                qn = qns[qi]
                gn = qg if qi == 0 else r_kn
                dst = qkb[:, qi, :]
                if which == 1:
                    P.tt("dve", dst.re("p (g d) -> p g d", d=64), qn.re("p (g d) -> p g d", d=64), bc(gn, [(0, 16), (1, 64)]), ALU.mult)
                else:
                    P.tt("dve", qn.re("p (g d) -> p g d", d=64), qn.re("p (g d) -> p g d", d=64), bc(gn, [(0, 16), (1, 64)]), ALU.mult)
            for j in range(2):
                P.copy("act", vv[:, t, j * 4:(j + 1) * 4, 0:128], pvs[j].re("p (h e) -> p h e", h=4))
            if which == 0:
                for qi in qis:
                    qn, q1, q2 = qns[qi], q1s[qi], q2s[qi]
                    P.tt("dve", q1.re("p (g d) -> p g d", d=64), qn.re("p (g d) -> p g d", d=64), bc(atab[:, t, 0:64], [(0, 16), (1, 64)]), ALU.mult)
                    qv = qn.re("p (g a u d) -> p g a u d", a=2, u=2, d=16)
                    q2v = q2.re("p (g a u d) -> p g a u d", a=2, u=2, d=16)
                    for s_ in range(2):
                        sn = bass.AP(atab.ap.tensor, atab[:, t, 64 + s_ * 16:64 + s_ * 16 + 16].ap.offset,
                                     [list(atab.ap.ap[0]), [0, 16], [32, 2], [1, 16]])
                        P.tt("dve", q2v[:, :, :, s_, :], qv[:, :, :, 1 - s_, :], TT(sn, atab.tok), ALU.mult)
                for qi in qis:
                    P.tt("dve", qkb[:, qi, :], q1s[qi], q2s[qi], ALU.add)
            for qi in qis:
                pb = P.bank()
                pbb = bank_bf(pb)
                for h in range(8):
                    P.transpose(pbb[:, h * 128:(h + 1) * 128], qkb[:, qi, h * 128:(h + 1) * 128], ident_b)
                P.copy("act", qkT[:, qi, :, t * 128:(t + 1) * 128], pbb.re("p (h t) -> p h t", h=8))
        tok0 = bi * 256
        P.dma("sp", Kd[:, :, tok0:tok0 + 256].re("h p t -> p h t"), qkT[:, 1])
        if which == 0:
            P.dma("sp", Qd[:, :, tok0 - CTX:tok0 - CTX + 256].re("h p t -> p h t"), qkT[:, 0])
        for t in range(2):
            P.dma("sp", Vd[:, :, bi * 2 + t, :].re("h p e -> p h e"), vv[:, t])

    load5(0)
    if NB5 > 1:
        load5(1)
    stage5A(0)
    for bi in range(NB5):
        if bi + 2 < NB5:
            load5(bi + 2)
        if bi + 1 < NB5:
            stage5A(bi + 1)
        stage5B(bi)
    P.close_scope()
    if stop_after == "P5":
        return _finish(P, nc, [Kd, Vd, Qd])

    P.open_scope()
    NKT = NCH
    NQB = OWN // 512
    lt_ = P.sb("lt_", [128, 128], F32)
    lam2 = P.sb("lam2", [128, 4], F32)
    P.tt("dve", lt_[:, 0:64], r_lam[:, 0:64], r_lam[:, 64:128], ALU.mult)
    P.tt("dve", lt_[:, 64:128], r_lam[:, 128:192], r_lam[:, 192:256], ALU.mult)
    P.reduce("dve", lam2[:, 0:2], lt_.re("p (a d) -> p a d", a=2), ALU.add)
    P.act(lam2[:, 0:2], lam2[:, 0:2], AF.Exp)
    P.tt("dve", lam2[:, 2:3], lam2[:, 1:2], lam2[:, 0:1], ALU.subtract)
    P.ts("dve", lam2[:, 3:4], lam2[:, 2:3], -LAM_INIT, None, op0=ALU.add)
    neglam = lam2[:, 3:4]
    sub_g = P.sb("sub_g", [128, 128], F32)
    P.ts("dve", sub_g, r_subln, 1.0 - LAM_INIT, None, op0=ALU.mult)
    Kh = [P.sb("Kh%d" % i, [128, T], BF16) for i in range(2)]
    Vh = [P.sb("Vh%d" % i, [128, NKT, 130], BF16) for i in range(2)]
    qa = [P.sb("qa%d" % i, [128, 512], BF16) for i in range(2)]
    qb_ = [P.sb("qb%d" % i, [128, 512], BF16) for i in range(2)]
    qs = [P.sb("qs%d" % i, [128, 512], BF16) for i in range(2)]
    pT = [P.sb("pT%d" % i, [128, 512], BF16) for i in range(3)]
    o0 = P.sb("o0", [128, 4, 128], F32)
    o1 = P.sb("o1", [128, 128], F32)
    osq = P.sb("osq", [128, 4, 128], F32)
    rs6 = P.sb("rs6", [128, 4], F32)
    on = P.sb("on", [128, 4, 128], BF16)
    oT = [P.sb("oT%d" % i, [128, 512], BF16) for i in range(2)]
    spb = [P.banks[0], P.banks[1], P.banks[2]]
    ob = [P.banks[3], P.banks[4], P.banks[5], P.banks[6]]
    tb = P.banks[7]
    groups = [(h, qb) for h in range(8) for qb in range(NQB)]
    steps = [(m, kt) for m in range(2) for kt in range(NKT)]

    def load_kv(h):
        P.dma("sp", Kh[h % 2], Kd[h])
        P.dma("sp", Vh[h % 2], Vd[h])

    def load_q(gi):
        h, qb = groups[gi]
        A, B_, Q_ = qa[gi % 2], qb_[gi % 2], qs[gi % 2]
        P.dma("sp", A, Qd[h, :, qb * 512:(qb + 1) * 512])
        if L > OWN:
            P.dma("sp", B_, Qd[h, :, OWN + qb * 512:OWN + (qb + 1) * 512])
            P.ts("pool", Q_, A, sel_sb[:, 0:1], None, op0=ALU.mult)
            P.stt("dve", Q_, B_, sel_sb[:, 1:2], Q_, ALU.mult, ALU.add)
            return Q_
        return A

    load_kv(0)
    Qn = load_q(0)
    pi = 0
    for gi, (h, qb) in enumerate(groups):
        K_, V_ = Kh[h % 2], Vh[h % 2]
        Q_ = Qn
        if qb == 0 and h + 1 < 8:
            load_kv(h + 1)
        if gi + 1 < len(groups):
            Qn = load_q(gi + 1)

        def emit_s(i):
            m, kt = steps[i]
            P.mm(spb[(pi + i) % 3], K_[m * 64:(m + 1) * 64, kt * 128:(kt + 1) * 128], Q_[m * 64:(m + 1) * 64, :])
        emit_s(0)
        emit_s(1)
        for i, (m, kt) in enumerate(steps):
            if i + 2 < len(steps):
                emit_s(i + 2)
            sp_ = spb[(pi + i) % 3]
            p_ = pT[(pi + i) % 3]
            P.act(p_, sp_, AF.Exp, bias=-8.0)
            for s_ in range(4):
                P.mm(ob[s_][:, 0:129], p_[:, s_ * 128:(s_ + 1) * 128], V_[:, kt, 0:129], start=(kt == 0), stop=(kt == NKT - 1))
            if kt == NKT - 1:
                for s_ in range(4):
                    P.recip(rs6[:, s_:s_ + 1], ob[s_][:, 128:129])
                    if m == 0:
                        P.ts("dve", o0[:, s_, :], ob[s_][:, 0:128], rs6[:, s_:s_ + 1], None, op0=ALU.mult)
                    else:
                        P.ts("dve", o1, ob[s_][:, 0:128], rs6[:, s_:s_ + 1], neglam, op0=ALU.mult, op1=ALU.mult)
                        P.tt("dve", o0[:, s_, :], o0[:, s_, :], o1, ALU.add)
        pi += len(steps)
        P.act(osq, o0, AF.Square)
        P.reduce("dve", rs6, osq, ALU.add)
        P.ts("dve", rs6, rs6, 1.0 / 128, EPS, op0=ALU.mult, op1=ALU.add)
        P.rsqrt(rs6)
        tbb = bank_bf(tb)
        for s_ in range(4):
            P.stt("dve", on[:, s_, :], o0[:, s_, :], rs6[:, s_:s_ + 1], sub_g, ALU.mult, ALU.mult)
            P.transpose(tbb[:, s_ * 128:(s_ + 1) * 128], on[:, s_, :], ident_b)
        o_ = oT[gi % 2]
        P.copy("dve", o_, tbb[:, 0:512])
        P.dma("sp", Od[:, h, qb * 512:(qb + 1) * 512], o_)
    P.close_scope()
    if stop_after == "P6":
        return _finish(P, nc, [Od])

    P.open_scope()
    BLK = min(1024, OWN)
    NB = OWN // BLK
    NH = BLK // 512
    NT7 = BLK // 128
    wo1 = P.sb("wo1", [128, 8, D], BF16)
    for k in range(8):
        P.dma("pool", wo1[:, k, :], w_out1[k * 128:(k + 1) * 128, :])
    wr_sb = P.sb("wr_sb", [128, 8, 8], F32)
    P.dma("sp", wr_sb, wr.re("(k p) e -> p k e", p=128))
    selm = P.sb("selm", [8, 1024], F32)
    P.dma("sp", selm, selmat)
    o7 = P.sb("o7", [128, 8, BLK], BF16)
    xa = P.sb("xa", [128, 8, BLK], F32)
    xb7 = P.sb("xb7", [128, 8, 128], F32)
    sq7 = P.sb("sq7", [128, 8, 512], F32)
    rstd7 = P.sb("rstd7", [128, 512], F32)
    h7 = P.sb("h7", [128, 8, BLK], BF16)
    h7f = sq7
    yacc = P.sb("yacc", [128, 8, BLK], F32)
    lg = P.sb("lg", [128, 8], F32)
    lg2 = P.sb("lg2", [128, 8], F32)
    eq1 = P.sb("eq1", [128, 8], F32)
    eq2 = P.sb("eq2", [128, 8], F32)
    mx = P.sb("mx", [128, 8], F32)
    comb = P.sb("comb", [128, 8], F32)
    combT = P.sb("combT", [8, BLK], F32)
    cbc = [P.sb("cbc%d" % i, [128, BLK], BF16) for i in range(2)]
    FG = 2
    NFG = FEXP // (128 * FG)
    FW = 128 * FG
    wge = [P.sb("wge%d" % i, [128, 8, FW], BF16) for i in range(2)]
    wue = [P.sb("wue%d" % i, [128, 8, FW], BF16) for i in range(2)]
    wde = [P.sb("wde%d" % i, [128, FG, D], BF16) for i in range(2)]
    a7 = [P.sb("a7_%d" % i, [128, FG, BLK], BF16) for i in range(2)]
    sg7 = [P.sb("sg7_%d" % i, [128, 512], F32) for i in range(2)]
    t7 = [P.sb("t7_%d" % i, [128, 512], F32) for i in range(2)]
    its = [(nb, e, fg) for nb in range(NB) for e in range(NEXP) for fg in range(NFG)]

    def issue_w(ii):
        nb_, e_, fg_ = its[ii]
        b_ = ii % 2
        f0_ = fg_ * FW
        P.dma("pool", wge[b_], eg[e_, :, f0_:f0_ + FW].re("(k p) f -> p k f", p=128))
        P.dma("pool", wue[b_], eu[e_, :, f0_:f0_ + FW].re("(k p) f -> p k f", p=128))
        P.dma("pool", wde[b_], ed[e_, f0_:f0_ + FW, :].re("(f p) d -> p f d", p=128))
    issue_w(0)
    wi = 0
    for nb in range(NB):
        P.dma("sp", o7, Od[:, :, nb * BLK:(nb + 1) * BLK])
        for t in range(NT7):
            chA = 2 + (nb * BLK) // 128 + t
            P.dma("sp", xa[:, :, t * 128:(t + 1) * 128], x_l1[chA])
            if L > OWN:
                P.dma("sp", xb7, x_l1[chA + OWN // 128])
                P.ts("pool", xa[:, :, t * 128:(t + 1) * 128], xa[:, :, t * 128:(t + 1) * 128], sel_sb[:, 0:1], None, op0=ALU.mult)
                P.stt("pool", xa[:, :, t * 128:(t + 1) * 128], xb7, sel_sb[:, 1:2], xa[:, :, t * 128:(t + 1) * 128], ALU.mult, ALU.add)
        for hf in range(NH):
            cs = slice(hf * 512, (hf + 1) * 512)
            for dc in range(8):
                pb = P.bank()
                P.mmgroup(pb, [(wo1[:, hh, dc * 128:(dc + 1) * 128], o7[:, hh, cs]) for hh in range(8)])
                P.stt("dve", xa[:, dc, cs], pb, G[1][:, dc, 0, 0:1], xa[:, dc, cs], ALU.mult, ALU.add)
            P.act(sq7, xa[:, :, cs], AF.Square)
            pb = P.bank()
            P.mmgroup(pb, [(ones_f, sq7[:, k, :]) for k in range(8)])
            P.ts("dve", rstd7, pb, 1.0 / D, EPS, op0=ALU.mult, op1=ALU.add)
            P.rsqrt(rstd7)
            P.tt("dve", sq7, xa[:, :, cs], bc(rstd7, [(0, 8), (1, 512)]), ALU.mult)
            A2 = AB[1][:, :, 0, 1, :]
            for k in range(8):
                P.act(h7f[:, k, :], sq7[:, k, :], AF.Identity, bias=A2[:, k, 1:2], scale=A2[:, k, 0:1])
                P.copy("act", h7[:, k, cs], h7f[:, k, :])
            for t4 in range(4):
                pb = P.bank()
                P.mmgroup(pb[:, 0:8], [(h7f[:, k, t4 * 128:(t4 + 1) * 128], wr_sb[:, k, :]) for k in range(8)])
                P.copy("dve", lg, pb[:, 0:8])
                P.reduce("dve", mx[:, 0:1], lg, ALU.max)
                P.ts("dve", eq1, lg, mx[:, 0:1], None, op0=ALU.is_equal)
                P.stt("dve", lg2, eq1, -1e30, lg, ALU.mult, ALU.add)
                P.reduce("dve", mx[:, 1:2], lg2, ALU.max)
                P.ts("dve", eq2, lg2, mx[:, 1:2], None, op0=ALU.is_equal)
                P.tt("dve", mx[:, 2:3], mx[:, 1:2], mx[:, 0:1], ALU.subtract)
                P.act(mx[:, 3:4], mx[:, 2:3], AF.Exp)
                P.ts("dve", mx[:, 4:5], mx[:, 3:4], 1.0, None, op0=ALU.add)
                P.recip(mx[:, 5:6], mx[:, 4:5])
                P.tt("dve", mx[:, 6:7], mx[:, 3:4], mx[:, 5:6], ALU.mult)
                P.ts("dve", comb, eq1, mx[:, 5:6], None, op0=ALU.mult)
                P.stt("dve", comb, eq2, mx[:, 6:7], comb, ALU.mult, ALU.add)
                pb = P.bank()
                P.transpose(pb[0:8, 0:128], comb, ident_f)
                P.copy("dve", combT[:, hf * 512 + t4 * 128:hf * 512 + (t4 + 1) * 128], pb[0:8, 0:128])
        P.memset("pool", yacc, 0.0)
        for e in range(NEXP):
            cb = cbc[e % 2]
            for hf in range(NH):
                cs = slice(hf * 512, (hf + 1) * 512)
                pb = P.bank()
                P.mm(pb, selm[:, e * 128:(e + 1) * 128], combT[:, cs])
                P.copy("act", cb[:, cs], pb)
            for k in range(8):
                P.tt("dve", o7[:, k, :], h7[:, k, :], cb, ALU.mult)
            for fg in range(NFG):
                b = wi % 2
                wi += 1
                if wi < len(its):
                    issue_w(wi)
                aa = a7[b]
                ii = 0
                for f in range(FG):
                    for hf in range(NH):
                        cs = slice(hf * 512, (hf + 1) * 512)
                        pg = P.bank()
                        pu = P.bank()
                        P.mmgroup(pg, [(wge[b][:, k, f * 128:(f + 1) * 128], h7[:, k, cs]) for k in range(8)])
                        P.mmgroup(pu, [(wue[b][:, k, f * 128:(f + 1) * 128], o7[:, k, cs]) for k in range(8)])
                        sg = sg7[ii % 2]
                        tt_ = t7[ii % 2]
                        ii += 1
                        P.act(sg, pg, AF.Silu)
                        P.tt("dve", aa[:, f, cs], sg, pu, ALU.mult)
                for hf in range(NH):
                    cs = slice(hf * 512, (hf + 1) * 512)
                    for dc in range(8):
                        pb = P.bank()
                        P.mmgroup(pb, [(wde[b][:, f, dc * 128:(dc + 1) * 128], aa[:, f, cs]) for f in range(FG)])
                        P.tt("dve", yacc[:, dc, cs], yacc[:, dc, cs], pb, ALU.add)
        for dc in range(8):
            P.stt("dve", yacc[:, dc, :], yacc[:, dc, :], G[1][:, dc, 0, 1:2], xa[:, dc, :], ALU.mult, ALU.add)
        P.dma("sp", outT[:, nb * BLK:(nb + 1) * BLK].re("(k p) t -> p k t", p=128), yacc)
    P.close_scope()
    return _finish(P, nc, [outT])


def P_sb_keep(P, name, shape):
    g = P.nc.sbuf_tensor(name, list(shape), F32)
    h = g.__enter__()
    P._ctx.insert(0, g)
    for i in range(len(P._scopes)):
        P._scopes[i] += 1
    return TT(h[:], Tok(name))


def _finish(P, nc, outs):
    P.fence("sp", outs)
    P.emit()
    P.close()
    return nc


def fm_vec(v):
    v = np.asarray(v, np.float32).reshape(-1, 128)
    return np.ascontiguousarray(v.T)


def prep_inputs(inp, L, OWN, ncores_per_batch, nbatch):
    cm, selm = host_consts()
    rcs, acs = rope_tables(L)
    shared = {
        "consts": cm, "selmat": selm, "rcs": rcs, "acs": acs,
        "w_mod0": np.ascontiguousarray(inp["even_w_mod"][0]), "w_mod1": np.ascontiguousarray(inp["odd_w_mod"][0]),
        "b_mod0": fm_vec(inp["even_b_mod"][0]), "b_mod1": fm_vec(inp["odd_b_mod"][0]),
        "norms": np.concatenate([fm_vec(inp["even_norm1"][0]), fm_vec(inp["even_norm2"][0]),
                                 fm_vec(inp["odd_norm1"][0]), fm_vec(inp["odd_norm2"][0])], axis=1),
        "w_in0": np.ascontiguousarray(inp["even_w_in"][0]),
        "w_out0": np.ascontiguousarray(inp["even_w_out"][0]),
        "ffg": np.ascontiguousarray(inp["even_ffn_gate"][0]), "ffu": np.ascontiguousarray(inp["even_ffn_up"][0]),
        "ffd": np.ascontiguousarray(inp["even_ffn_down"][0]),
        "w_in1": np.ascontiguousarray(inp["odd_w_in"][0]), "w_out1": np.ascontiguousarray(inp["odd_w_out"][0]),
        "router": np.ascontiguousarray(inp["odd_router"][0]),
        "eg": np.ascontiguousarray(inp["odd_exp_gate"][0]), "eu": np.ascontiguousarray(inp["odd_exp_up"][0]),
        "ed": np.ascontiguousarray(inp["odd_exp_down"][0]),
    }
    cw = np.concatenate([inp["even_conv_w"][0], inp["even_conv_b"][0][None, :]], axis=0)
    shared["convw"] = np.ascontiguousarray(cw.reshape(4, 12, 128).transpose(2, 1, 0))
    rowp = np.concatenate([
        inp["even_dt_bias"][0].reshape(-1), inp["even_a_log"][0].reshape(-1), inp["even_ret_decay"][0].reshape(-1),
        inp["even_d"][0].reshape(-1), inp["even_ssd_norm"][0].reshape(-1), inp["odd_q_norm"][0].reshape(-1),
        inp["odd_k_norm"][0].reshape(-1), inp["odd_lambda"][0].reshape(-1), inp["odd_subln"][0].reshape(-1)]).astype(np.float32)
    rp = np.zeros((1, 2560), np.float32)
    rp[0, :rowp.size] = rowp
    shared["rowp"] = rp
    maps = []
    for b in range(nbatch):
        xT = np.zeros((D, L + 2), np.float32)
        xT[:, 1:L + 1] = inp["x"][b].T
        cT = np.zeros((D, CTX + 2), np.float32)
        cT[:, 1:CTX + 1] = inp["ctx"][b].T
        cvec = np.concatenate([fm_vec(inp["c"][b]), fm_vec(inp["c_ctx"])], axis=1)
        for hf in range(ncores_per_batch):
            s = np.zeros((128, 2), np.float32)
            s[:, hf] = 1.0
            m = dict(shared)
            m.update({"xT": xT, "ctxT": cT, "cvec": cvec, "sel": s})
            maps.append(m)
    return maps


_NC_CACHE = {}


def kernel(**inputs):
    inp = {k: np.asarray(v) for k, v in inputs.items()}
    B, L, _ = inp["x"].shape
    OWN = L // 2
    key = (L, OWN)
    if key not in _NC_CACHE:
        _NC_CACHE[key] = build(L, OWN)
    nc = _NC_CACHE[key]
    maps = prep_inputs(inp, L, OWN, 2, B)
    res = run_bass_kernel_spmd(nc, maps, core_ids=list(range(len(maps))))
    out = np.empty((B, L, D), np.float32)
    for b in range(B):
        for hf in range(2):
            out[b, hf * OWN:(hf + 1) * OWN, :] = res.results[b * 2 + hf]["outT"].T
    return out
```
